# Optimizing a Trainium2 kernel written in Bass

```python
import math
import jax, jax.numpy as jnp
from jax import lax
import numpy as np


D_MODEL = 1024
BATCH = 32
SEQ = 2048
DEPTH = 2

PLE_DIM = 256
N_BRANCH = 3
SSD_HEADS = 16
SSD_HEAD_DIM = 64
SSD_INNER = SSD_HEADS * SSD_HEAD_DIM
SSD_GROUPS = 4
SSD_STATE = 128
SSD_CONV = 4
SSD_CHUNK = 128
SSD_XBC = SSD_INNER + 2 * SSD_GROUPS * SSD_STATE
CONF_CH = 1024
CONF_KERNEL = 31
ATT_Q_HEADS = 16
ATT_KV_HEADS = 4
ATT_HEAD_DIM = 64
ATT_WINDOW = 128
ATT_BLOCK = 128
ROPE_THETA = 10000.0
MOE_GROUPS = 4
MOE_EXPERTS_PER_GROUP = 8
MOE_EXPERTS = MOE_GROUPS * MOE_EXPERTS_PER_GROUP
MOE_TOP_K = 2
MOE_FF = 512
MOE_BLOCK = 256
DEEPNORM_ALPHA = (2 * DEPTH) ** 0.25
DEEPNORM_BETA = (8 * DEPTH) ** -0.25
LN_EPS = 1e-5
IN_WIDTHS = (N_BRANCH * D_MODEL, SSD_INNER, SSD_XBC, SSD_HEADS, 2 * CONF_CH,
             ATT_Q_HEADS * ATT_HEAD_DIM, ATT_KV_HEADS * ATT_HEAD_DIM, ATT_KV_HEADS * ATT_HEAD_DIM)
IN_WIDTH = sum(IN_WIDTHS)

kernel_name = 'hybrid_ssd_conformer_swa_hmoe_deepnorm'


def _split_points(widths):
    pts, acc = [], 0
    for w in widths[:-1]:
        acc += w
        pts.append(acc)
    return pts


def layer_norm(x, g, b):
    xf = x.astype(jnp.float32)
    mu = jnp.mean(xf, axis=-1, keepdims=True)
    var = jnp.mean(jnp.square(xf - mu), axis=-1, keepdims=True)
    return ((xf - mu) * lax.rsqrt(var + LN_EPS) * g + b).astype(x.dtype)


def causal_dwconv(x, w, b):
    width = w.shape[0]
    y = lax.conv_general_dilated(x, w[:, None, :].astype(x.dtype), window_strides=(1,),
                                 padding=[(width - 1, 0)],
                                 dimension_numbers=('NWC', 'WIO', 'NWC'),
                                 feature_group_count=x.shape[-1])
    return y + b


def rope(x, positions):
    half = x.shape[-1] // 2
    inv_freq = ROPE_THETA ** (-jnp.arange(half, dtype=jnp.float32) / half)
    ang = positions.astype(jnp.float32)[..., None] * inv_freq
    cos = jnp.cos(ang)[:, :, None, :]
    sin = jnp.sin(ang)[:, :, None, :]
    xf = x.astype(jnp.float32)
    x1, x2 = xf[..., :half], xf[..., half:]
    return jnp.concatenate([x1 * cos - x2 * sin, x2 * cos + x1 * sin], axis=-1).astype(x.dtype)


def ssd_mixer(z, xbc, dt_raw, conv_w, conv_b, dt_bias, a_log, d_skip, norm_w, w_proj):
    bsz, seq, _ = z.shape
    G, R, P, N, L = SSD_GROUPS, SSD_HEADS // SSD_GROUPS, SSD_HEAD_DIM, SSD_STATE, SSD_CHUNK
    nc = seq // L
    f32 = jnp.float32
    xbc = jax.nn.silu(causal_dwconv(xbc, conv_w, conv_b))
    xs, bm, cm = jnp.split(xbc, [SSD_INNER, SSD_INNER + G * N], axis=-1)
    dt = jax.nn.softplus(dt_raw.astype(f32) + dt_bias.astype(f32))
    a = -jnp.exp(a_log.astype(f32)).reshape(G, R)
    xh = xs.astype(f32).reshape(bsz, nc, L, G, R, P)
    dtc = dt.reshape(bsz, nc, L, G, R)
    bc = bm.astype(f32).reshape(bsz, nc, L, G, N)
    cc = cm.astype(f32).reshape(bsz, nc, L, G, N)
    xdt = xh * dtc[..., None]
    cs = jnp.moveaxis(jnp.cumsum(dtc * a, axis=2), 2, -1)
    causal = jnp.tril(jnp.ones((L, L), dtype=bool))
    seg = cs[..., :, None] - cs[..., None, :]
    decay_in = jnp.exp(jnp.where(causal, seg, -jnp.inf))
    cb = jnp.einsum('bclgn,bcsgn->bcgls', cc, bc)
    y_diag = jnp.einsum('bcgrls,bcsgrp->bclgrp', cb[:, :, :, None] * decay_in, xdt)
    decay_to_end = jnp.exp(cs[..., -1:] - cs)
    chunk_states = jnp.einsum('bclgn,bcgrl,bclgrp->bcgrpn', bc, decay_to_end, xdt)
    chunk_decay = jnp.exp(cs[..., -1])

    def step(h, inp):
        st, dec = inp
        return h * dec[..., None, None] + st, h

    h0 = jnp.zeros((bsz, G, R, P, N), f32)
    _, prev = lax.scan(step, h0, (jnp.moveaxis(chunk_states, 1, 0), jnp.moveaxis(chunk_decay, 1, 0)))
    prev = jnp.moveaxis(prev, 0, 1)
    y_off = jnp.einsum('bclgn,bcgrpn,bcgrl->bclgrp', cc, prev, jnp.exp(cs))
    y = y_diag + y_off + xh * d_skip.astype(f32).reshape(G, R)[..., None]
    y = y.reshape(bsz, seq, SSD_INNER) * jax.nn.silu(z.astype(f32))
    yg = y.reshape(bsz, seq, G, SSD_INNER // G)
    yg = yg * lax.rsqrt(jnp.mean(jnp.square(yg), axis=-1, keepdims=True) + LN_EPS)
    y = yg.reshape(bsz, seq, SSD_INNER) * norm_w.astype(f32)
    return y.astype(z.dtype) @ w_proj


def conformer_conv(u, dw_w, dw_b, ln_g, ln_b, w_proj):
    a, gate = jnp.split(u, 2, axis=-1)
    h = a * jax.nn.sigmoid(gate)
    h = causal_dwconv(h, dw_w, dw_b)
    h = jax.nn.silu(layer_norm(h, ln_g, ln_b))
    return h @ w_proj


def swa_sink_attention(q, k, v, positions, sinks, w_proj):
    bsz, seq, _ = q.shape
    R = ATT_Q_HEADS // ATT_KV_HEADS
    nb = seq // ATT_BLOCK
    q = rope(q.reshape(bsz, seq, ATT_Q_HEADS, ATT_HEAD_DIM), positions)
    k = rope(k.reshape(bsz, seq, ATT_KV_HEADS, ATT_HEAD_DIM), positions)
    v = v.reshape(bsz, seq, ATT_KV_HEADS, ATT_HEAD_DIM)
    qb = q.reshape(bsz, nb, ATT_BLOCK, ATT_KV_HEADS, R, ATT_HEAD_DIM)

    def band(t):
        tb = t.reshape(bsz, nb, ATT_BLOCK, ATT_KV_HEADS, ATT_HEAD_DIM)
        prev = jnp.pad(tb[:, :-1], ((0, 0), (1, 0), (0, 0), (0, 0), (0, 0)))
        return jnp.concatenate([prev, tb], axis=2)

    kw, vw = band(k), band(v)
    blk = jnp.arange(nb)[:, None, None]
    qi = jnp.arange(ATT_BLOCK)[None, :, None]
    kj = jnp.arange(2 * ATT_BLOCK)[None, None, :]
    rel = qi + ATT_BLOCK - kj
    mask = (rel >= 0) & (rel < ATT_WINDOW) & (blk * ATT_BLOCK - ATT_BLOCK + kj >= 0)
    scale = ATT_HEAD_DIM ** -0.5
    sink = sinks.astype(jnp.float32).reshape(ATT_KV_HEADS, R)[None, :, :, None, None]

    def block_attend(args):
        qblk, kblk, vblk, m = args
        s = jnp.einsum('bqkrd,bjkd->bkrqj', qblk, kblk).astype(jnp.float32) * scale
        s = jnp.where(m[None, None, None], s, -jnp.inf)
        mx = jnp.maximum(jnp.max(s, axis=-1, keepdims=True), sink)
        e = jnp.exp(s - mx)
        probs = e / (jnp.sum(e, axis=-1, keepdims=True) + jnp.exp(sink - mx))
        return jnp.einsum('bkrqj,bjkd->bqkrd', probs.astype(vblk.dtype), vblk)

    out = lax.map(block_attend, (jnp.moveaxis(qb, 1, 0), jnp.moveaxis(kw, 1, 0),
                                 jnp.moveaxis(vw, 1, 0), mask))
    out = jnp.moveaxis(out, 0, 1).reshape(bsz, seq, ATT_Q_HEADS * ATT_HEAD_DIM)
    return out @ w_proj


def hierarchical_moe(x, w_group, b_group, w_expert, b_expert, w_gate, w_up, w_down):
    bsz, seq, d = x.shape
    T = bsz * seq
    TK = T * MOE_TOP_K
    xt = x.reshape(T, d)
    tok = jnp.arange(T, dtype=jnp.int32)
    g_logits = (xt @ w_group + b_group).astype(jnp.float32)
    g_prob = jax.nn.softmax(g_logits, axis=-1)
    g_idx = jnp.argmax(g_logits, axis=-1).astype(jnp.int32)
    g_w = g_prob[tok, g_idx][:, None]
    e_logits = (xt @ w_expert + b_expert).astype(jnp.float32).reshape(T, MOE_GROUPS, MOE_EXPERTS_PER_GROUP)
    e_in = e_logits[tok, g_idx]
    top_v, top_i = lax.top_k(e_in, MOE_TOP_K)
    e_w = jax.nn.softmax(top_v, axis=-1) * g_w
    e_id = g_idx[:, None] * MOE_EXPERTS_PER_GROUP + top_i.astype(jnp.int32)
    flat_e = e_id.reshape(TK)
    flat_w = e_w.reshape(TK)
    flat_tok = jnp.repeat(tok, MOE_TOP_K)
    order = jnp.argsort(flat_e, stable=True)
    se, stok, sw = flat_e[order], flat_tok[order], flat_w[order]
    counts = jnp.bincount(flat_e, length=MOE_EXPERTS).astype(jnp.int32)
    starts = jnp.cumsum(counts) - counts
    padded = ((counts + MOE_BLOCK - 1) // MOE_BLOCK) * MOE_BLOCK
    pends = jnp.cumsum(padded)
    pstarts = pends - padded
    dest = pstarts[se] + jnp.arange(TK, dtype=jnp.int32) - starts[se]
    n_rows = TK + MOE_EXPERTS * MOE_BLOCK
    n_blocks = n_rows // MOE_BLOCK
    row_tok = jnp.full((n_rows,), T, dtype=jnp.int32).at[dest].set(stok)
    x_rows = jnp.concatenate([xt, jnp.zeros((1, d), xt.dtype)], axis=0)[row_tok]
    x_rows = x_rows.reshape(n_blocks, MOE_BLOCK, d)
    block_e = jnp.searchsorted(pends, jnp.arange(n_blocks, dtype=jnp.int32) * MOE_BLOCK, side='right')
    block_e = jnp.minimum(block_e, MOE_EXPERTS - 1).astype(jnp.int32)

    def expert_block(args):
        xb, e = args
        h = jax.nn.silu(xb @ w_gate[e]) * (xb @ w_up[e])
        return h @ w_down[e]

    y_rows = lax.map(expert_block, (x_rows, block_e)).reshape(n_rows, d)
    y = jax.ops.segment_sum(y_rows[dest] * sw[:, None].astype(x.dtype), stok, num_segments=T)
    return y.reshape(bsz, seq, d)


def decoder_layer(x, p_i, positions, w_in, b_gate, ssd_conv_w, ssd_conv_b, ssd_dt_bias, ssd_a_log,
                  ssd_d, ssd_norm_w, ssd_w_out, conf_dw_w, conf_dw_b, conf_ln_g, conf_ln_b,
                  conf_w_out, attn_sinks, attn_w_out, w_out, ln1_g, ln1_b, moe_w_group,
                  moe_b_group, moe_w_expert, moe_b_expert, moe_w_gate, moe_w_up, moe_w_down,
                  ln2_g, ln2_b, ple_w_gate, ple_w_proj):
    bsz, seq, d = x.shape
    h = x @ w_in
    gates_raw, z, xbc, dt_raw, u_conf, q, k, v = jnp.split(h, _split_points(IN_WIDTHS), axis=-1)
    gates = jax.nn.sigmoid(gates_raw.reshape(bsz, seq, N_BRANCH, d) + b_gate)
    y_ssd = ssd_mixer(z, xbc, dt_raw, ssd_conv_w, ssd_conv_b, ssd_dt_bias, ssd_a_log, ssd_d,
                      ssd_norm_w, ssd_w_out)
    y_conf = conformer_conv(u_conf, conf_dw_w, conf_dw_b, conf_ln_g, conf_ln_b, conf_w_out)
    y_att = swa_sink_attention(q, k, v, positions, attn_sinks, attn_w_out)
    mixed = gates[:, :, 0] * y_ssd + gates[:, :, 1] * y_conf + gates[:, :, 2] * y_att
    x = layer_norm(DEEPNORM_ALPHA * x + mixed @ w_out, ln1_g, ln1_b)
    ffn = hierarchical_moe(x, moe_w_group, moe_b_group, moe_w_expert, moe_b_expert,
                           moe_w_gate, moe_w_up, moe_w_down)
    x = layer_norm(DEEPNORM_ALPHA * x + ffn, ln2_g, ln2_b)
    return x + jax.nn.sigmoid(x @ ple_w_gate) * (p_i @ ple_w_proj)


def setup_inputs(seed: int = 0) -> dict:
    key = jax.random.key(seed)
    ks = jax.random.split(key, 40)
    f32 = jnp.float32
    Lr = DEPTH

    def nrm(k, shape, scale):
        return jax.random.normal(k, shape, f32) * scale

    dt0 = jnp.exp(jax.random.uniform(ks[8], (Lr, SSD_HEADS), f32, math.log(1e-3), math.log(1e-1)))
    return {
        'x': nrm(ks[0], (BATCH, SEQ, D_MODEL), 1.0),
        'p': nrm(ks[1], (DEPTH, BATCH, SEQ, PLE_DIM), 1.0),
        'positions': jnp.arange(SEQ, dtype=jnp.int32)[None, :]
                     + jax.random.randint(ks[2], (BATCH, 1), 0, SEQ, dtype=jnp.int32),
        'w_in': nrm(ks[3], (Lr, D_MODEL, IN_WIDTH), D_MODEL ** -0.5),
        'b_gate': nrm(ks[4], (Lr, N_BRANCH, D_MODEL), 0.01),
        'ssd_conv_w': nrm(ks[5], (Lr, SSD_CONV, SSD_XBC), SSD_CONV ** -0.5),
        'ssd_conv_b': nrm(ks[6], (Lr, SSD_XBC), 0.01),
        'ssd_dt_bias': dt0 + jnp.log(-jnp.expm1(-dt0)),
        'ssd_a_log': jnp.log(jax.random.uniform(ks[9], (Lr, SSD_HEADS), f32, 1.0, 16.0)),
        'ssd_d': 1.0 + nrm(ks[10], (Lr, SSD_HEADS), 0.1),
        'ssd_norm_w': 1.0 + nrm(ks[11], (Lr, SSD_INNER), 0.05),
        'ssd_w_out': nrm(ks[12], (Lr, SSD_INNER, D_MODEL), SSD_INNER ** -0.5),
        'conf_dw_w': nrm(ks[13], (Lr, CONF_KERNEL, CONF_CH), CONF_KERNEL ** -0.5),
        'conf_dw_b': nrm(ks[14], (Lr, CONF_CH), 0.01),
        'conf_ln_g': 1.0 + nrm(ks[15], (Lr, CONF_CH), 0.05),
        'conf_ln_b': nrm(ks[16], (Lr, CONF_CH), 0.01),
        'conf_w_out': nrm(ks[17], (Lr, CONF_CH, D_MODEL), CONF_CH ** -0.5),
        'attn_sinks': nrm(ks[18], (Lr, ATT_Q_HEADS), 0.5),
        'attn_w_out': nrm(ks[19], (Lr, ATT_Q_HEADS * ATT_HEAD_DIM, D_MODEL), (ATT_Q_HEADS * ATT_HEAD_DIM) ** -0.5),
        'w_out': nrm(ks[20], (Lr, D_MODEL, D_MODEL), DEEPNORM_BETA * D_MODEL ** -0.5),
        'ln1_g': 1.0 + nrm(ks[21], (Lr, D_MODEL), 0.05),
        'ln1_b': nrm(ks[22], (Lr, D_MODEL), 0.01),
        'moe_w_group': nrm(ks[23], (Lr, D_MODEL, MOE_GROUPS), D_MODEL ** -0.5),
        'moe_b_group': nrm(ks[24], (Lr, MOE_GROUPS), 0.01),
        'moe_w_expert': nrm(ks[25], (Lr, D_MODEL, MOE_EXPERTS), D_MODEL ** -0.5),
        'moe_b_expert': nrm(ks[26], (Lr, MOE_EXPERTS), 0.01),
        'moe_w_gate': nrm(ks[27], (Lr, MOE_EXPERTS, D_MODEL, MOE_FF), D_MODEL ** -0.5),
        'moe_w_up': nrm(ks[28], (Lr, MOE_EXPERTS, D_MODEL, MOE_FF), D_MODEL ** -0.5),
        'moe_w_down': nrm(ks[29], (Lr, MOE_EXPERTS, MOE_FF, D_MODEL), DEEPNORM_BETA * MOE_FF ** -0.5),
        'ln2_g': 1.0 + nrm(ks[30], (Lr, D_MODEL), 0.05),
        'ln2_b': nrm(ks[31], (Lr, D_MODEL), 0.01),
        'ple_w_gate': nrm(ks[32], (Lr, D_MODEL, D_MODEL), D_MODEL ** -0.5),
        'ple_w_proj': nrm(ks[33], (Lr, PLE_DIM, D_MODEL), PLE_DIM ** -0.5),
    }


def reference(x, p, positions, w_in, b_gate, ssd_conv_w, ssd_conv_b, ssd_dt_bias, ssd_a_log, ssd_d,
              ssd_norm_w, ssd_w_out, conf_dw_w, conf_dw_b, conf_ln_g, conf_ln_b, conf_w_out,
              attn_sinks, attn_w_out, w_out, ln1_g, ln1_b, moe_w_group, moe_b_group, moe_w_expert,
              moe_b_expert, moe_w_gate, moe_w_up, moe_w_down, ln2_g, ln2_b, ple_w_gate, ple_w_proj):
    for i in range(DEPTH):
        x = decoder_layer(x, p[i], positions, w_in[i], b_gate[i], ssd_conv_w[i], ssd_conv_b[i],
                          ssd_dt_bias[i], ssd_a_log[i], ssd_d[i], ssd_norm_w[i], ssd_w_out[i],
                          conf_dw_w[i], conf_dw_b[i], conf_ln_g[i], conf_ln_b[i], conf_w_out[i],
                          attn_sinks[i], attn_w_out[i], w_out[i], ln1_g[i], ln1_b[i],
                          moe_w_group[i], moe_b_group[i], moe_w_expert[i], moe_b_expert[i],
                          moe_w_gate[i], moe_w_up[i], moe_w_down[i], ln2_g[i], ln2_b[i],
                          ple_w_gate[i], ple_w_proj[i])
    return x
```

```python
import contextlib
import math
import numpy as np
import concourse.bass as bass
import concourse.mybir as mybir
from concourse.bass_utils import run_bass_kernel_spmd

F32 = mybir.dt.float32
BF16 = mybir.dt.bfloat16
I32 = mybir.dt.int32
AF = mybir.ActivationFunctionType
ALU = mybir.AluOpType
AX = mybir.AxisListType

ENGS = ("pe", "act", "dve", "pool", "sp")
NDMASEM = 8

D = 1024
SEQ = 2048
DEPTH = 2
NCORES = 8
SEQ_PER_CORE = 4
PLE = 256
NEXP = 32
FF = 512
ALPHA = (2 * DEPTH) ** 0.25
EPS = 1e-5
OFF_GATE, OFF_Z, OFF_XBC, OFF_DT, OFF_CONF, OFF_Q, OFF_K, OFF_V = 0, 3072, 4096, 6144, 6160, 8208, 9232, 9488
FM_GATE, FM_XBC, FM_CONF, FM_Q, FM_K = 0, 24, 40, 56, 64
NFM = 66
C_BGATE = 0
C_SCW = 24
C_SCB = 88
C_CDW = 104
C_CDB = 352
C_CLG = 360
C_CLB = 368
C_NW = 376
C_L1G = 384
C_L1B = 392
C_L2G = 400
C_L2B = 408
NCOLS = 416
R_DTB, R_ALOG, R_D, R_SINK, R_RB = 0, 16, 32, 48, 64
NROWS = 100
K_ID, K_TU, K_TL, K_ONE, K_ROT, K_DUP0, K_DUP1, K_INVF, K_EPS, K_ONEC = 0, 128, 256, 384, 512, 640, 768, 896, 897, 898
NCF = 900


class Op:
    __slots__ = ("eng", "fn", "dma", "idx", "tick", "sem_i", "sem_v", "deps", "prewait")


class Sched:
    def __init__(self, nc):
        self.nc = nc
        self.ops = []
        self.state = {}
        self.cnt = {e: 0 for e in ENGS}
        self.dcnt = {e: 0 for e in ENGS}
        self.dma_hist = {e: [] for e in ENGS}
        self.last = {e: None for e in ENGS}
        self.pending = {e: set() for e in ENGS}
        self.exclusive = set()

    def fence(self):
        F = set()
        for e in ENGS:
            if self.last[e] is not None:
                F.add(self.last[e])
            for op in self.dma_hist[e][-NDMASEM:]:
                F.add(op)
        for e in ENGS:
            self.pending[e] |= F

    @staticmethod
    def _norm(lst):
        out = []
        for r in lst:
            if isinstance(r, tuple):
                out.append((id(r[0]), r[1]))
            else:
                out.append((id(r), None))
        return out

    def _conf(self, res):
        b, k = res
        d = self.state.get(b)
        if d is None:
            return
        if k is None:
            for st in d.values():
                yield st
        else:
            st = d.get(k)
            if st is not None:
                yield st
            st = d.get(None)
            if st is not None:
                yield st

    def add(self, eng, fn, reads=(), writes=(), dma=False):
        reads = self._norm(reads)
        writes = self._norm(writes)
        if self.exclusive:
            ex = [r for r in reads if r[0] in self.exclusive]
            if ex:
                reads = [r for r in reads if r[0] not in self.exclusive]
                writes = writes + [r for r in ex if r not in writes]
        op = Op()
        op.eng, op.fn, op.dma, op.prewait = eng, fn, dma, None
        op.idx = len(self.ops)
        deps = set()
        for r in reads:
            for st in self._conf(r):
                if st[0] is not None:
                    deps.add(st[0])
        for w in writes:
            for st in self._conf(w):
                if st[0] is not None:
                    deps.add(st[0])
                deps.update(st[1])
        if self.pending[eng]:
            deps |= self.pending[eng]
            self.pending[eng] = set()
        op.deps = deps
        for w in writes:
            b, k = w
            d = self.state.setdefault(b, {})
            if k is None:
                d.clear()
            d[k] = [op, []]
        for r in reads:
            b, k = r
            d = self.state.setdefault(b, {})
            st = d.get(k)
            if st is None:
                st = [None, []]
                for s2 in self._conf(r):
                    if s2[0] is not None and (st[0] is None or s2[0].idx > st[0].idx):
                        st[0] = s2[0]
                d[k] = st
            st[1].append(op)
        if dma:
            i = self.dcnt[eng]
            self.dcnt[eng] += 1
            op.sem_i = i % NDMASEM
            op.sem_v = 16 * (i // NDMASEM + 1)
            hist = self.dma_hist[eng]
            if i >= NDMASEM:
                op.prewait = hist[i - NDMASEM]
            hist.append(op)
            op.tick = None
        else:
            self.cnt[eng] += 1
            op.tick = self.cnt[eng]
            self.last[eng] = op
        self.ops.append(op)
        return op

    def pe(self, fn, reads=(), writes=()):
        return self.add("pe", fn, reads, writes)

    def act(self, fn, reads=(), writes=()):
        return self.add("act", fn, reads, writes)

    def dve(self, fn, reads=(), writes=()):
        return self.add("dve", fn, reads, writes)

    def pool(self, fn, reads=(), writes=()):
        return self.add("pool", fn, reads, writes)

    def dma(self, eng, out, in_, reads=(), writes=()):
        return self.add(eng, lambda e: e.dma_start(out=out, in_=in_), reads, writes, dma=True)

    def emit(self):
        nc = self.nc
        with contextlib.ExitStack() as es:
            esem = {e: es.enter_context(nc.semaphore("s_" + e)) for e in ENGS}
            dsem = {e: [es.enter_context(nc.semaphore("d_%s%d" % (e, i))) for i in range(NDMASEM)]
                    for e in ENGS if self.dcnt[e] > 0}
            block = es.enter_context(nc.Block())
            per = {e: [op for op in self.ops if op.eng == e] for e in ENGS}

            def run(engname, eng):
                waited = {}

                def wait_for(dep):
                    if dep.dma:
                        s = dsem[dep.eng][dep.sem_i]
                        v = dep.sem_v
                    else:
                        if dep.eng == "pe" and engname == "pe":
                            return
                        s = esem[dep.eng]
                        v = dep.tick
                    key = id(s)
                    if waited.get(key, 0) >= v:
                        return
                    waited[key] = v
                    eng.wait_ge(s, v)

                for op in per[engname]:
                    for dep in sorted(op.deps, key=lambda d: d.idx):
                        wait_for(dep)
                    if op.prewait is not None:
                        wait_for(op.prewait)
                    inst = op.fn(eng)
                    if op.dma:
                        inst.then_inc(dsem[engname][op.sem_i], 16)
                    else:
                        inst.then_inc(esem[engname], 1)
                for op in self.dma_hist[engname][-NDMASEM:]:
                    wait_for(op)

            @block.sync
            def _(eng):
                run("sp", eng)

            @block.tensor
            def _(eng):
                run("pe", eng)

            @block.scalar
            def _(eng):
                run("act", eng)

            @block.vector
            def _(eng):
                run("dve", eng)

            @block.gpsimd
            def _(eng):
                run("pool", eng)


def pstride(t):
    return int(np.prod(list(t.shape)[1:]))


class View:
    def __init__(self, h, off, shape, res=None):
        self.h, self.off, self.shape = h, off, list(shape)
        self.dtype = h.dtype
        st, s = [], 1
        for n in reversed(self.shape[1:]):
            st.append(s)
            s *= n
        self.strides = list(reversed(st))
        self.ps = pstride(h)
        self.res = self if res is None else res

    def __getitem__(self, idx):
        if not isinstance(idx, tuple):
            idx = (idx,)
        idx = list(idx) + [slice(None)] * (len(self.shape) - len(idx))
        p = idx[0]
        p0, p1, _ = p.indices(self.shape[0])
        off = self.off
        dims = []
        for i, ix in enumerate(idx[1:]):
            stride, n = self.strides[i], self.shape[i + 1]
            if isinstance(ix, int):
                off += ix * stride
            else:
                a, b, st = ix.indices(n)
                cnt = len(range(a, b, st))
                off += a * stride
                dims.append([stride * st, cnt])
        m = [list(d) for d in dims]
        if not m:
            m = [[1, 1]]
        return bass.AP(self.h, p0 * self.ps + off, [[self.ps, p1 - p0]] + m)

    def bc(self, off, dims, npart=128, p0=0):
        return bass.AP(self.h, p0 * self.ps + self.off + off, [[self.ps, npart]] + [list(d) for d in dims])

    def reshape(self, shape):
        return View(self.h, self.off, shape, res=self.res)


class Builder:
    def __init__(self, nseq=SEQ_PER_CORE, nlayer=DEPTH, T=1024, nseg=None, debug=(), nexp=NEXP, stop_after=None,
                 only=None):
        self.nseq, self.nlayer, self.T = nseq, nlayer, T
        self.NT = T // 512
        self.NB = T // 128
        self.nseg = (SEQ // T) if nseg is None else nseg
        self.debug = set(debug)
        self.nexp = nexp
        self.stop_after = stop_after
        self.only = None if only is None else set(only.split(',')) if isinstance(only, str) else set(only)
        self.dbg_outs = {}
        nc = self.nc = bass.Bass("TRN2", target_bir_lowering=False)
        self.S = Sched(nc)
        self.declare_dram()
        self.alloc()
        self.program()
        self.S.emit()

    def din(self, name, shape, dt=F32):
        return self.nc.dram_tensor(name, list(shape), dt, kind="ExternalInput").ap()

    def pers(self, name, shape, dt):
        h = self.nc.alloc_sbuf_tensor(name, list(shape), dt)
        return View(h, 0, shape)

    def areset(self):
        self.S.fence()
        self.aoff = 0

    def aalloc(self, fshape, dt, npart=128):
        size = {F32: 4, BF16: 2, I32: 4}[dt]
        n = int(np.prod(fshape)) * size
        n = (n + 31) // 32 * 32
        off = self.aoff
        self.aoff += n
        assert self.aoff <= self.arena_bytes, ("arena overflow", self.aoff, self.arena_bytes)
        return View(self.arena[dt], off // size, [npart] + list(fshape))

    def dbg(self, name, view, reads=None):
        if name not in self.debug or name in self.dbg_outs:
            return
        o = self.nc.dram_tensor("dbg_" + name, list(view.shape), view.dtype, kind="ExternalOutput").ap()
        self.dbg_outs[name] = o
        self.S.dma("sp", o, view[:], reads=[view.res] if reads is None else reads)

    def declare_dram(self):
        L = self.nlayer
        self.d_x = self.din("x", [self.nseq, SEQ, D])
        self.d_p = self.din("p", [DEPTH, self.nseq, SEQ, PLE])
        self.d_pos = self.din("pos", [self.nseq, SEQ], I32)
        self.d_cst = self.din("cst", [128, NCF])
        self.d_out = self.nc.dram_tensor("out", [self.nseq, SEQ, D], F32, kind="ExternalOutput").ap()
        self.dw = []
        for l in range(L):
            w = {}
            w["win_fm"] = self.din("win_fm%d" % l, [NFM, 128, 8, 128])
            w["win_tm"] = self.din("win_tm%d" % l, [D, 1296])
            for nm in ("wssd", "wconf", "wattn", "wout", "wpg"):
                w[nm] = self.din("%s%d" % (nm, l), [8, 128, 8, 128])
            w["wpp"] = self.din("wpp%d" % l, [8, 128, 2, 128])
            w["wr"] = self.din("wr%d" % l, [D, 36])
            w["wg"] = self.din("wg%d" % l, [NEXP, 128, 4, 8, 128])
            w["wu"] = self.din("wu%d" % l, [NEXP, 128, 4, 8, 128])
            w["wd"] = self.din("wd%d" % l, [NEXP, FF, D])
            w["cols"] = self.din("cols%d" % l, [128, NCOLS])
            w["rows"] = self.din("rows%d" % l, [1, NROWS])
            self.dw.append(w)

    def alloc(self):
        T, nc, L = self.T, self.nc, self.nlayer
        pers = self.pers
        self.X = pers("X", [128, 8, T], F32)
        self.XT = pers("XT", [128, 8, T], BF16)
        self.MT = pers("MT", [128, 8, T], BF16)
        self.CST = pers("CST", [128, NCF], F32)
        self.CSB = pers("CSB", [128, NCF], BF16)
        self.COLS = [pers("COLS%d" % l, [128, NCOLS], F32) for l in range(L)]
        self.COLA = [pers("COLA%d" % l, [128, 16], F32) for l in range(L)]
        self.ROWS = [pers("ROWS%d" % l, [128, NROWS], F32) for l in range(L)]
        self.ROWX = [pers("ROWX%d" % l, [128, 32], F32) for l in range(L)]
        self.H = [pers("H%d" % l, [128, 1024], F32) for l in range(L)]
        self.CTAIL = [pers("CTAIL%d" % l, [128, 8, 32], BF16) for l in range(L)]
        self.STAIL = [pers("STAIL%d" % l, [128, 16, 4], BF16) for l in range(L)]
        self.KCAR = [pers("KCAR%d" % l, [128, 4, 128], BF16) for l in range(L)]
        self.VCAR = [pers("VCAR%d" % l, [128, 4, 65], BF16) for l in range(L)]
        ph = [nc.alloc_psum_tensor("P%d" % i, [128, 512], F32) for i in range(8)]
        self.P = [View(h, 0, [128, 512]) for h in ph]
        self.PB = [View(h.bitcast(BF16), 0, [128, 1024], res=v) for h, v in zip(ph, self.P)]
        self.S.exclusive = {id(v) for v in self.P}
        rem = nc.sbuf_bytes_remaining
        rem = rem() if callable(rem) else rem
        self.arena_bytes = (int(rem) - 8192) // 64 * 64
        ah = nc.alloc_sbuf_tensor("ARENA", [128, self.arena_bytes // 4], F32)
        self.arena = {F32: ah, BF16: ah.bitcast(BF16), I32: ah.bitcast(I32)}
        self.aoff = 0

    def mm(self, out, lhsT, rhs, start, stop, reads, writes):
        self.S.pe(lambda e: e.matmul(out, lhsT, rhs, start=start, stop=stop), reads, writes)

    def tr(self, out, in_, ident, reads, writes):
        self.S.pe(lambda e: e.transpose(out, in_, ident), reads, writes)

    def actf(self, out, in_, func, reads, writes, scale=1.0, bias=None):
        if bias is None:
            self.S.act(lambda e: e.activation(out=out, in_=in_, func=func, scale=scale), reads, writes)
        else:
            self.S.act(lambda e: e.activation(out=out, in_=in_, func=func, scale=scale, bias=bias), reads, writes)

    def tt(self, eng, out, in0, in1, op, reads, writes):
        self.S.add(eng, lambda e: e.tensor_tensor(out=out, in0=in0, in1=in1, op=op), reads, writes)

    def ts(self, eng, out, in0, s1, op0, reads, writes, s2=None, op1=None):
        if op1 is None:
            self.S.add(eng, lambda e: e.tensor_scalar(out=out, in0=in0, scalar1=s1, scalar2=None, op0=op0), reads, writes)
        else:
            self.S.add(eng, lambda e: e.tensor_scalar(out=out, in0=in0, scalar1=s1, scalar2=s2, op0=op0, op1=op1),
                       reads, writes)

    def stt(self, out, in0, scalar, in1, op0, op1, reads, writes):
        self.S.dve(lambda e: e.scalar_tensor_tensor(out=out, in0=in0, scalar=scalar, in1=in1, op0=op0, op1=op1),
                   reads, writes)

    def cp(self, eng, out, in_, reads, writes):
        if eng == "act":
            self.S.act(lambda e: e.copy(out, in_), reads, writes)
        else:
            self.S.add(eng, lambda e: e.tensor_copy(out, in_), reads, writes)

    def memset(self, eng, ap, val, writes):
        self.S.add(eng, lambda e: e.memset(ap, val), (), writes)

    def cF(self, off, n=128, npart=128, p0=0):
        return self.CST[p0:p0 + npart, off:off + n]

    def cB(self, off, n=128, npart=128, p0=0):
        return self.CSB[p0:p0 + npart, off:off + n]

    def col(self, l, c, npart=128):
        return self.COLS[l][0:npart, c:c + 1]

    def proj_fm(self, srcs, nchunk, consumer, wbufs, psums):
        NT = self.NT
        ns = len(srcs)
        cnt = 0
        for i in range(nchunk):
            wbs = []
            for si, (wsrc, c0, KC, rhs_fn) in enumerate(srcs):
                wb = wbufs[si][i % len(wbufs[si])]
                self.S.dma("pool", wb[:, 0:KC, :], wsrc[c0 + i], writes=[wb.res])
                wbs.append(wb)
            for tt in range(NT):
                pss = []
                for si, (wsrc, c0, KC, rhs_fn) in enumerate(srcs):
                    ps = psums[si][cnt % len(psums[si])]
                    for k in range(KC):
                        rap, rres = rhs_fn(k, tt)
                        self.mm(ps[:, :], wbs[si][:, k, :], rap, k == 0, k == KC - 1,
                                reads=[wbs[si].res, rres], writes=[ps.res])
                    pss.append(ps)
                cnt += 1
                consumer(i, tt, pss)

    def xt_rhs(self, k, tt):
        return self.XT[:, k, tt * 512:(tt + 1) * 512], (self.XT.res, tt)

    def gated_out(self, l, branch, YT, wname, first):
        w = self.dw[l]
        wbA = [self.aalloc([8, 128], BF16) for _ in range(3)]
        wbB = [self.aalloc([8, 128], BF16) for _ in range(3)]
        SG = [self.aalloc([512], F32) for _ in range(2)]
        TM = [self.aalloc([512], BF16) for _ in range(2)]
        P = self.P
        st = {"n": 0}

        def yt_rhs(k, tt):
            return YT[:, k, tt * 512:(tt + 1) * 512], (YT.res, tt)

        def consumer(i, tt, pss):
            n = st["n"]
            st["n"] += 1
            sg = SG[n % 2]
            self.actf(sg[:, :], pss[1][:, :], AF.Sigmoid, reads=[pss[1].res], writes=[sg.res],
                      bias=self.col(l, C_BGATE + branch * 8 + i))
            dst = self.MT[:, i, tt * 512:(tt + 1) * 512]
            if first:
                self.tt("dve", dst, pss[0][:, :], sg[:, :], ALU.mult, reads=[pss[0].res, sg.res],
                        writes=[(self.MT.res, tt)])
            else:
                tm = TM[n % 2]
                self.tt("dve", tm[:, :], pss[0][:, :], sg[:, :], ALU.mult, reads=[pss[0].res, sg.res],
                        writes=[tm.res])
                self.tt("pool", dst, dst, tm[:, :], ALU.add, reads=[tm.res, (self.MT.res, tt)],
                        writes=[(self.MT.res, tt)])

        self.proj_fm([(w[wname], 0, 8, yt_rhs), (w["win_fm"], FM_GATE + branch * 8, 8, self.xt_rhs)], 8, consumer,
                     [wbA, wbB], [[P[0], P[1]], [P[2], P[3]]])

    def ln_fm(self, SRC, tt, out_fn, tmp):
        P = self.P
        SQ, MEAN, RSTD, TN = tmp
        sl = slice(tt * 512, (tt + 1) * 512)
        for m in range(8):
            self.mm(P[6][:, :], self.cF(K_ONE), SRC[:, m, sl], m == 0, m == 7,
                    reads=[self.CST.res, (SRC.res, tt)], writes=[P[6].res])
        for m in range(8):
            sq = SQ[m % 2]
            self.actf(sq[:, :], SRC[:, m, sl], AF.Square, reads=[(SRC.res, tt)], writes=[sq.res])
            self.mm(P[7][:, :], self.cF(K_ONE), sq[:, :], m == 0, m == 7, reads=[self.CST.res, sq.res],
                    writes=[P[7].res])
        self.ts("dve", MEAN[:, :], P[6][:, :], 1.0 / 1024, ALU.mult, reads=[P[6].res], writes=[MEAN.res])
        self.tt("dve", RSTD[:, :], MEAN[:, :], MEAN[:, :], ALU.mult, reads=[MEAN.res], writes=[RSTD.res])
        self.stt(RSTD[:, :], P[7][:, :], 1.0 / 1024, RSTD[:, :], ALU.mult, ALU.subtract, reads=[P[7].res, RSTD.res],
                 writes=[RSTD.res])
        self.actf(RSTD[:, :], RSTD[:, :], AF.Sqrt, reads=[RSTD.res], writes=[RSTD.res], bias=self.cF(K_EPS, 1))
        self.S.dve(lambda e: e.reciprocal(RSTD[:, :], RSTD[:, :]), reads=[RSTD.res], writes=[RSTD.res])
        for m in range(8):
            tn = TN[m % 2]
            self.tt("dve", tn[:, :], SRC[:, m, sl], MEAN[:, :], ALU.subtract, reads=[(SRC.res, tt), MEAN.res],
                    writes=[tn.res])
            self.tt("dve", tn[:, :], tn[:, :], RSTD[:, :], ALU.mult, reads=[tn.res, RSTD.res], writes=[tn.res])
            out_fn(m, tn)

    def ln_tmp(self):
        return ([self.aalloc([512], F32) for _ in range(2)], self.aalloc([512], F32), self.aalloc([512], F32),
                [self.aalloc([512], F32) for _ in range(2)])

    def program(self):
        S = self.S
        S.dma("sp", self.CST[:, :], self.d_cst, writes=[self.CST.res])
        self.cp("dve", self.CSB[:, :], self.CST[:, :], reads=[self.CST.res], writes=[self.CSB.res])
        import os
        self.bis = int(os.environ.get("BIS", "0"))
        for l in range(self.nlayer):
            if self.bis & 1:
                break
            S.dma("sp", self.COLS[l][:, :], self.dw[l]["cols"], writes=[self.COLS[l].res])
            S.dma("sp", self.ROWS[l][:, :], bass.AP(self.dw[l]["rows"].tensor, 0, [[0, 128], [1, NROWS]]),
                  writes=[self.ROWS[l].res])
            self.actf(self.ROWX[l][:, 0:16], self.ROWS[l][:, R_ALOG:R_ALOG + 16], AF.Exp, reads=[self.ROWS[l].res],
                      writes=[self.ROWX[l].res])
            self.ts("dve", self.ROWX[l][:, 0:16], self.ROWX[l][:, 0:16], -1.0, ALU.mult, reads=[self.ROWX[l].res],
                    writes=[self.ROWX[l].res])
            self.actf(self.ROWX[l][:, 16:32], self.ROWS[l][:, R_SINK:R_SINK + 16], AF.Exp, reads=[self.ROWS[l].res],
                      writes=[self.ROWX[l].res])
            self.ts("dve", self.COLA[l][:, 0:16], self.COLS[l][:, C_L1G:C_L1G + 16], float(ALPHA), ALU.mult,
                    reads=[self.COLS[l].res], writes=[self.COLA[l].res])
        for seq in range(self.nseq):
            for seg in range(self.nseg):
                self.segment(seq, seg)

    def segment(self, seq, seg):
        T = self.T
        t0 = seg * T
        first = seg == 0
        self.areset()
        if not self.bis & 4:
            self.load_x(seq, t0)
        if first and not self.bis & 2:
            for l in range(self.nlayer):
                self.memset("pool", self.H[l][:, :], 0.0, [self.H[l].res])
                self.memset("pool", self.CTAIL[l][:, :, :], 0.0, [self.CTAIL[l].res])
                self.memset("pool", self.STAIL[l][:, :, :], 0.0, [self.STAIL[l].res])
        for l in range(self.nlayer):
            self.mt_first = True
            stages = [("ssd", lambda: self.ssd(l, first)),
                      ("conf", lambda: self.conformer(l)),
                      ("attn", lambda: self.attention(l, seq, t0, first)),
                      ("ln1", lambda: self.outproj_ln1(l)),
                      ("moe", lambda: self.moe(l)),
                      ("ple", lambda: self.ln2_ple(l, seq, t0))]
            for nm, fn in stages:
                if self.only is not None and nm not in self.only:
                    continue
                self.areset()
                fn()
                self.dbg("MT", self.MT)
                if self.stop_after == (l, nm):
                    break
            if self.stop_after is not None and self.stop_after[0] == l:
                break
        self.areset()
        if not self.bis & 8:
            self.store_x(seq, t0)

    def load_x(self, seq, t0):
        S, P = self.S, self.P
        STG = [self.aalloc([1024], F32) for _ in range(2)]
        for blk in range(self.NB):
            stg = STG[blk % 2]
            tt = blk // 4
            S.dma("sp", stg[:, :], self.d_x[seq, t0 + blk * 128:t0 + (blk + 1) * 128, :], writes=[stg.res])
            for half in range(2):
                ps = P[(blk * 2 + half) % 4].reshape([128, 4, 128])
                for j in range(4):
                    c = half * 4 + j
                    self.tr(ps[:, j, :], stg[:, c * 128:(c + 1) * 128], self.cF(K_ID), reads=[stg.res, self.CST.res],
                            writes=[ps.res])
                self.cp("act", self.X[:, half * 4:half * 4 + 4, blk * 128:(blk + 1) * 128], ps[:, :, :],
                        reads=[ps.res], writes=[(self.X.res, tt)])
                self.cp("dve", self.XT[:, half * 4:half * 4 + 4, blk * 128:(blk + 1) * 128], ps[:, :, :],
                        reads=[ps.res], writes=[(self.XT.res, tt)])

    def store_x(self, seq, t0):
        S, P = self.S, self.P
        STG = [self.aalloc([1024], F32) for _ in range(2)]
        for blk in range(self.NB):
            stg = STG[blk % 2]
            tt = blk // 4
            for half in range(2):
                ps = P[(blk * 2 + half) % 4].reshape([128, 4, 128])
                for j in range(4):
                    c = half * 4 + j
                    self.tr(ps[:, j, :], self.X[:, c, blk * 128:(blk + 1) * 128], self.cF(K_ID),
                            reads=[(self.X.res, tt), self.CST.res], writes=[ps.res])
                self.cp("act" if half == 0 else "dve", stg[:, half * 512:(half + 1) * 512],
                        ps[:, :, :], reads=[ps.res], writes=[stg.res])
            S.dma("sp", self.d_out[seq, t0 + blk * 128:t0 + (blk + 1) * 128, :], stg[:, :], reads=[stg.res])

    def conformer(self, l):
        T, NT, P, S = self.T, self.NT, self.P, self.S
        w = self.dw[l]
        HPAD = [self.aalloc([32 + T], BF16) for _ in range(2)]
        DG = [self.aalloc([31, 128], BF16) for _ in range(2)]
        CO = self.aalloc([8, T], F32)
        HT = self.aalloc([8, T], BF16)
        SG = [self.aalloc([512], F32) for _ in range(2)]
        wbA = [self.aalloc([8, 128], BF16) for _ in range(2)]
        wbG = [self.aalloc([8, 128], BF16) for _ in range(2)]
        lnt = self.ln_tmp()
        for m in range(8):
            hp, dg = HPAD[m % 2], DG[m % 2]
            self.cp("pool", hp[:, 0:32], self.CTAIL[l][:, m, :], reads=[self.CTAIL[l].res], writes=[hp.res])
            self.tt("pool", dg[:, :, :], self.CSB.bc(K_ID, [[0, 31], [1, 128]]),
                    self.COLS[l].bc(C_CDW + m * 31, [[1, 31], [0, 128]]), ALU.mult,
                    reads=[self.CSB.res, self.COLS[l].res], writes=[dg.res])
            wa, wg = wbA[m % 2], wbG[m % 2]
            S.dma("pool", wa[:, :, :], w["win_fm"][FM_CONF + m], writes=[wa.res])
            S.dma("pool", wg[:, :, :], w["win_fm"][FM_CONF + 8 + m], writes=[wg.res])
            for tt in range(NT):
                sl = slice(tt * 512, (tt + 1) * 512)
                a_ps, g_ps, c_ps = P[tt % 2], P[2 + tt % 2], P[4 + tt % 2]
                for k in range(8):
                    self.mm(a_ps[:, :], wa[:, k, :], self.XT[:, k, sl], k == 0, k == 7,
                            reads=[wa.res, (self.XT.res, tt)], writes=[a_ps.res])
                for k in range(8):
                    self.mm(g_ps[:, :], wg[:, k, :], self.XT[:, k, sl], k == 0, k == 7,
                            reads=[wg.res, (self.XT.res, tt)], writes=[g_ps.res])
                sg = SG[tt % 2]
                self.actf(sg[:, :], g_ps[:, :], AF.Sigmoid, reads=[g_ps.res], writes=[sg.res])
                self.tt("dve", hp[:, 32 + tt * 512:32 + (tt + 1) * 512], a_ps[:, :], sg[:, :], ALU.mult,
                        reads=[a_ps.res, sg.res], writes=[hp.res])
                for k in range(31):
                    self.mm(c_ps[:, :], dg[:, k, :], hp[:, 2 + k + tt * 512:2 + k + (tt + 1) * 512], k == 0, k == 30,
                            reads=[dg.res, hp.res], writes=[c_ps.res])
                self.actf(CO[:, m, sl], c_ps[:, :], AF.Identity, reads=[c_ps.res], writes=[(CO.res, tt)],
                          bias=self.col(l, C_CDB + m))
            self.cp("pool", self.CTAIL[l][:, m, :], hp[:, T:T + 32], reads=[hp.res], writes=[self.CTAIL[l].res])
        self.dbg("CO", CO)
        for tt in range(NT):
            sl = slice(tt * 512, (tt + 1) * 512)

            def out_fn(m, tn, tt=tt, sl=sl):
                self.actf(HT[:, m, sl], tn[:, :], AF.Silu, reads=[tn.res], writes=[(HT.res, tt)],
                          scale=self.col(l, C_CLG + m), bias=self.col(l, C_CLB + m))

            self.ln_fm(CO, tt, out_fn, lnt)
        self.dbg("HT", HT)
        self.gated_out(l, 1, HT, "wconf", self.mt_first)
        self.mt_first = False

    def outproj_ln1(self, l):
        T, NT, P, S = self.T, self.NT, self.P, self.S
        w = self.dw[l]
        wb = [self.aalloc([8, 128], BF16) for _ in range(3)]
        lnt = self.ln_tmp()
        self.dbg("MT", self.MT)

        def mt_rhs(k, tt):
            return self.MT[:, k, tt * 512:(tt + 1) * 512], (self.MT.res, tt)

        def consumer(i, tt, pss):
            sl = slice(tt * 512, (tt + 1) * 512)
            self.stt(self.X[:, i, sl], self.X[:, i, sl], float(ALPHA), pss[0][:, :], ALU.mult, ALU.add,
                     reads=[(self.X.res, tt), pss[0].res], writes=[(self.X.res, tt)])

        self.proj_fm([(w["wout"], 0, 8, mt_rhs)], 8, consumer, [wb], [[P[0], P[1]]])
        for tt in range(NT):
            sl = slice(tt * 512, (tt + 1) * 512)

            def out_fn(m, tn, tt=tt, sl=sl):
                self.actf(self.XT[:, m, sl], tn[:, :], AF.Identity, reads=[tn.res], writes=[(self.XT.res, tt)],
                          scale=self.col(l, C_L1G + m), bias=self.col(l, C_L1B + m))
                self.actf(self.X[:, m, sl], tn[:, :], AF.Identity, reads=[tn.res], writes=[(self.X.res, tt)],
                          scale=self.COLA[l][:, m:m + 1], bias=self.COLA[l][:, 8 + m:9 + m])

            self.ln_fm(self.X, tt, out_fn, lnt)
        self.dbg("X1T", self.XT)

    def ln2_ple(self, l, seq, t0):
        T, NT, NB, P, S = self.T, self.NT, self.NB, self.P, self.S
        w = self.dw[l]
        lnt = self.ln_tmp()
        self.dbg("XMOE", self.X)
        for tt in range(NT):
            sl = slice(tt * 512, (tt + 1) * 512)

            def out_fn(m, tn, tt=tt, sl=sl):
                self.actf(self.XT[:, m, sl], tn[:, :], AF.Identity, reads=[tn.res], writes=[(self.XT.res, tt)],
                          scale=self.col(l, C_L2G + m), bias=self.col(l, C_L2B + m))
                self.actf(self.X[:, m, sl], tn[:, :], AF.Identity, reads=[tn.res], writes=[(self.X.res, tt)],
                          scale=self.col(l, C_L2G + m), bias=self.col(l, C_L2B + m))

            self.ln_fm(self.X, tt, out_fn, lnt)
        self.dbg("X2T", self.XT)
        PSTG = [self.aalloc([256], F32) for _ in range(2)]
        PT = self.aalloc([2, T], BF16)
        for blk in range(NB):
            stg = PSTG[blk % 2]
            S.dma("sp", stg[:, :], self.d_p[l, seq, t0 + blk * 128:t0 + (blk + 1) * 128, :], writes=[stg.res])
            ps = P[6 + blk % 2].reshape([128, 4, 128])
            for j in range(2):
                self.tr(ps[:, j, :], stg[:, j * 128:(j + 1) * 128], self.cF(K_ID), reads=[stg.res, self.CST.res],
                        writes=[ps.res])
            self.cp("act", PT[:, :, blk * 128:(blk + 1) * 128], ps[:, 0:2, :], reads=[ps.res],
                    writes=[(PT.res, blk // 4)])
        wbA = [self.aalloc([8, 128], BF16) for _ in range(3)]
        wbB = [self.aalloc([8, 128], BF16) for _ in range(3)]
        SG = [self.aalloc([512], F32) for _ in range(2)]
        TM = [self.aalloc([512], F32) for _ in range(2)]
        st = {"n": 0}

        def pt_rhs(k, tt):
            return PT[:, k, tt * 512:(tt + 1) * 512], (PT.res, tt)

        def consumer(i, tt, pss):
            n = st["n"]
            st["n"] += 1
            sl = slice(tt * 512, (tt + 1) * 512)
            sg, tm = SG[n % 2], TM[n % 2]
            self.actf(sg[:, :], pss[0][:, :], AF.Sigmoid, reads=[pss[0].res], writes=[sg.res])
            self.tt("dve", tm[:, :], pss[1][:, :], sg[:, :], ALU.mult, reads=[pss[1].res, sg.res], writes=[tm.res])
            self.tt("pool", self.X[:, i, sl], self.X[:, i, sl], tm[:, :], ALU.add, reads=[tm.res, (self.X.res, tt)],
                    writes=[(self.X.res, tt)])

        self.proj_fm([(w["wpg"], 0, 8, self.xt_rhs), (w["wpp"], 0, 2, pt_rhs)], 8, consumer, [wbA, wbB],
                     [[P[0], P[1]], [P[2], P[3]]])
        if l < self.nlayer - 1:
            for tt in range(NT):
                sl = slice(tt * 512, (tt + 1) * 512)
                for m in range(8):
                    self.cp("act" if m % 2 else "pool", self.XT[:, m, sl], self.X[:, m, sl], reads=[(self.X.res, tt)],
                            writes=[(self.XT.res, tt)])

    def amark(self):
        return self.aoff

    def arelease(self, mark):
        self.S.fence()
        self.aoff = mark

    def ssd(self, l, first):
        T, NT, NB, P, PB, S = self.T, self.NT, self.NB, self.P, self.PB, self.S
        w = self.dw[l]
        ROWS, ROWX, COLS, CST, CSB = self.ROWS[l], self.ROWX[l], self.COLS[l], self.CST, self.CSB
        YST = self.aalloc([8, T], BF16)
        mark0 = self.amark()
        WZ = self.aalloc([8, 1024], BF16)
        WDT = self.aalloc([8, 16], BF16)
        BFM = self.aalloc([4, T], BF16)
        CFM = self.aalloc([4, T], BF16)
        XST = self.aalloc([NB, 1024], BF16)
        BTK = self.aalloc([NB, 512], BF16)
        DT, DTA, ECS, DTE, CDEC = [self.aalloc([NB, 16], F32) for _ in range(5)]
        mark1 = self.amark()
        XPAD = [self.aalloc([4 + T], BF16) for _ in range(2)]
        DG = [self.aalloc([4, 128], BF16) for _ in range(2)]
        XFM = [self.aalloc([T], BF16) for _ in range(2)]
        wbuf = [self.aalloc([8, 128], BF16) for _ in range(3)]
        S.dma("pool", WZ[:, :, :], w["win_tm"][:, 0:1024].rearrange("(k p) n -> p k n", p=128), writes=[WZ.res])
        S.dma("pool", WDT[:, :, :], w["win_tm"][:, 1024:1040].rearrange("(k p) n -> p k n", p=128), writes=[WDT.res])
        for m in range(16):
            xp, dg = XPAD[m % 2], DG[m % 2]
            if m < 8:
                dst = XFM[m % 2]
                dsl = lambda sl, dst=dst: dst[:, sl]
            elif m < 12:
                dst = BFM
                dsl = lambda sl, g=m - 8: BFM[:, g, sl]
            else:
                dst = CFM
                dsl = lambda sl, g=m - 12: CFM[:, g, sl]
            self.cp("pool", xp[:, 0:4], self.STAIL[l][:, m, :], reads=[self.STAIL[l].res], writes=[xp.res])
            self.tt("pool", dg[:, :, :], CSB.bc(K_ID, [[0, 4], [1, 128]]), COLS.bc(C_SCW + m * 4, [[1, 4], [0, 128]]),
                    ALU.mult, reads=[CSB.res, COLS.res], writes=[dg.res])
            wb = wbuf[m % 3]
            S.dma("pool", wb[:, :, :], w["win_fm"][FM_XBC + m], writes=[wb.res])
            for tt in range(NT):
                sl = slice(tt * 512, (tt + 1) * 512)
                ps, cps = P[tt % 2], P[2 + tt % 2]
                for k in range(8):
                    self.mm(ps[:, :], wb[:, k, :], self.XT[:, k, sl], k == 0, k == 7,
                            reads=[wb.res, (self.XT.res, tt)], writes=[ps.res])
                self.cp("act", xp[:, 4 + tt * 512:4 + (tt + 1) * 512], ps[:, :], reads=[ps.res], writes=[xp.res])
                for k in range(4):
                    self.mm(cps[:, :], dg[:, k, :], xp[:, 1 + k + tt * 512:1 + k + (tt + 1) * 512], k == 0, k == 3,
                            reads=[dg.res, xp.res], writes=[cps.res])
                self.actf(dsl(sl), cps[:, :], AF.Silu, reads=[cps.res], writes=[dst.res],
                          bias=self.col(l, C_SCB + m))
            self.cp("pool", self.STAIL[l][:, m, :], xp[:, T:T + 4], reads=[xp.res], writes=[self.STAIL[l].res])
            if m < 12:
                for b0 in range(0, NB, 8):
                    nb = min(8, NB - b0)
                    pb = PB[4 + (m + b0 // 8) % 2].reshape([128, 8, 128])
                    for j in range(nb):
                        blk = b0 + j
                        src = dst[:, blk * 128:(blk + 1) * 128] if m < 8 else BFM[:, m - 8, blk * 128:(blk + 1) * 128]
                        self.tr(pb[:, j, :], src, self.cB(K_ID), reads=[dst.res, CSB.res], writes=[pb.res])
                    if m < 8:
                        self.cp("dve", XST[:, b0:b0 + nb, m * 128:(m + 1) * 128], pb[:, 0:nb, :], reads=[pb.res],
                                writes=[XST.res])
                    else:
                        self.cp("dve", BTK[:, b0:b0 + nb, (m - 8) * 128:(m - 7) * 128], pb[:, 0:nb, :],
                                reads=[pb.res], writes=[BTK.res])
        PD = P[6].reshape([128, 32, 16])
        for blk in range(NB):
            for k in range(8):
                self.mm(PD[:, blk, :], self.XT[:, k, blk * 128:(blk + 1) * 128], WDT[:, k, :], k == 0, k == 7,
                        reads=[(self.XT.res, blk // 4), WDT.res], writes=[PD.res])
        self.tt("dve", DT[:, :, :], PD[:, 0:NB, :], ROWS.bc(R_DTB, [[0, NB], [1, 16]]), ALU.add,
                reads=[PD.res, ROWS.res], writes=[DT.res])
        self.actf(DT[:, :, :], DT[:, :, :], AF.Exp, reads=[DT.res], writes=[DT.res])
        self.actf(DT[:, :, :], DT[:, :, :], AF.Ln, reads=[DT.res], writes=[DT.res], bias=self.cF(K_ONEC, 1))
        self.tt("dve", DTA[:, :, :], DT[:, :, :], ROWX.bc(0, [[0, NB], [1, 16]]), ALU.mult,
                reads=[DT.res, ROWX.res], writes=[DTA.res])
        dta2 = DTA.reshape([128, NB * 16])
        for (cst, dstv, ps) in ((K_TU, ECS, P[7]), (K_TL, DTE, P[6]), (K_ONE, CDEC, P[7])):
            self.mm(ps[:, 0:NB * 16], self.cF(cst), dta2[:, :], True, True, reads=[CST.res, DTA.res], writes=[ps.res])
            self.actf(dstv.reshape([128, NB * 16])[:, :], ps[:, 0:NB * 16], AF.Exp, reads=[ps.res], writes=[dstv.res])
        self.arelease(mark1)
        RS = [self.aalloc([4, 128], F32) for _ in range(2)]
        DEC = [self.aalloc([4, 128], F32) for _ in range(2)]
        MTG = [self.aalloc([4, 128], BF16) for _ in range(2)]
        CBM = self.aalloc([4, 128], F32)
        XDT = [self.aalloc([16, 64], BF16) for _ in range(2)]
        XDD = [self.aalloc([16, 64], BF16) for _ in range(2)]
        XD = [self.aalloc([16, 64], BF16) for _ in range(2)]
        T1 = self.aalloc([1024], F32)
        SZ = self.aalloc([1024], F32)
        YN = self.aalloc([1024], BF16)
        HB = self.aalloc([1024], BF16)
        SS = self.aalloc([4], F32)
        H = self.H[l]
        self.cp("pool", HB[:, :], H[:, :], reads=[H.res], writes=[HB.res])
        for c in range(NB):
            csl = slice(c * 128, (c + 1) * 128)
            xdt, xdd, xd = XDT[c % 2], XDD[c % 2], XD[c % 2]
            xs3 = XST.reshape([128, NB, 16, 64])
            self.tt("dve", xdt[:, :, :], xs3[:, c, :, :], DT.bc(c * 16, [[1, 16], [0, 64]]), ALU.mult,
                    reads=[XST.res, DT.res], writes=[xdt.res])
            self.tt("pool", xdd[:, :, :], xdt[:, :, :], DTE.bc(c * 16, [[1, 16], [0, 64]]), ALU.mult,
                    reads=[xdt.res, DTE.res], writes=[xdd.res])
            self.tt("pool", xd[:, :, :], xs3[:, c, :, :], ROWS.bc(R_D, [[1, 16], [0, 64]]), ALU.mult,
                    reads=[XST.res, ROWS.res], writes=[xd.res])
            p0 = P[0].reshape([128, 4, 128])
            for g in range(4):
                self.mm(p0[:, g, :], BFM[:, g, csl], CFM[:, g, csl], True, True, reads=[BFM.res, CFM.res],
                        writes=[p0.res])
            self.tt("dve", CBM[:, :, :], p0[:, :, :], CST.bc(K_TU, [[0, 4], [1, 128]]), ALU.mult,
                    reads=[p0.res, CST.res], writes=[CBM.res])
            xd2 = xd.reshape([128, 1024])
            xdt2 = xdt.reshape([128, 1024])
            for half in range(2):
                self.mm(P[3 + half][:, :], self.cB(K_ID), xd2[:, half * 512:(half + 1) * 512], True, False,
                        reads=[CSB.res, xd.res], writes=[P[3 + half].res])
            for g in range(4):
                rs, dec, mtg = RS[g % 2], DEC[g % 2], MTG[g % 2]
                self.tt("dve", rs[:, :, :], DTA.bc(c * 16 + 4 * g, [[1, 4], [0, 128]]),
                        CST.bc(K_TU, [[0, 4], [1, 128]]), ALU.mult, reads=[DTA.res, CST.res], writes=[rs.res])
                self.mm(P[1][:, :], self.cF(K_TL), rs.reshape([128, 512])[:, :], True, True,
                        reads=[CST.res, rs.res], writes=[P[1].res])
                self.actf(dec.reshape([128, 512])[:, :], P[1][:, :], AF.Exp, reads=[P[1].res], writes=[dec.res])
                self.tt("dve", mtg[:, :, :], dec[:, :, :], CBM.bc(g * 128, [[0, 4], [1, 128]]), ALU.mult,
                        reads=[dec.res, CBM.res], writes=[mtg.res])
                for r in range(4):
                    h = 4 * g + r
                    half = h // 8
                    self.mm(P[3 + half][:, (h % 8) * 64:(h % 8 + 1) * 64], mtg[:, r, :],
                            xdt2[:, h * 64:(h + 1) * 64], False, h % 8 == 7,
                            reads=[mtg.res, xdt.res], writes=[P[3 + half].res])
            for g in range(4):
                ps = P[5 + g // 2]
                self.mm(ps[:, (g % 2) * 256:(g % 2 + 1) * 256], CFM[:, g, csl], HB[:, g * 256:(g + 1) * 256],
                        True, True, reads=[CFM.res, HB.res], writes=[ps.res])
            t13 = T1.reshape([128, 16, 64])
            for half in range(2):
                self.tt("dve", t13[:, half * 8:(half + 1) * 8, :], P[5 + half].reshape([128, 8, 64])[:, :, :],
                        ECS.bc(c * 16 + half * 8, [[1, 8], [0, 64]]), ALU.mult,
                        reads=[P[5 + half].res, ECS.res], writes=[T1.res])
            for half in range(2):
                hs = slice(half * 512, (half + 1) * 512)
                self.tt("dve", T1[:, hs], P[3 + half][:, :], T1[:, hs], ALU.add, reads=[P[3 + half].res, T1.res],
                        writes=[T1.res])
            for half in range(2):
                hs = slice(half * 512, (half + 1) * 512)
                pz = P[7] if half == 0 else P[0]
                for k in range(8):
                    self.mm(pz[:, :], self.XT[:, k, csl], WZ[:, k, hs], k == 0, k == 7,
                            reads=[(self.XT.res, c // 4), WZ.res], writes=[pz.res])
                self.actf(SZ[:, hs], pz[:, :], AF.Silu, reads=[pz.res], writes=[SZ.res])
            self.tt("dve", T1[:, :], T1[:, :], SZ[:, :], ALU.mult, reads=[T1.res, SZ.res], writes=[T1.res])
            self.tt("dve", SZ[:, :], T1[:, :], T1[:, :], ALU.mult, reads=[T1.res], writes=[SZ.res])
            self.S.dve(lambda e: e.tensor_reduce(out=SS[:, :], in_=SZ.reshape([128, 4, 256])[:, :, :], op=ALU.add,
                                                 axis=AX.X), reads=[SZ.res], writes=[SS.res])
            self.ts("dve", SS[:, :], SS[:, :], 1.0 / 256, ALU.mult, reads=[SS.res], writes=[SS.res], s2=float(EPS),
                    op1=ALU.add)
            self.actf(SS[:, :], SS[:, :], AF.Sqrt, reads=[SS.res], writes=[SS.res])
            self.S.dve(lambda e: e.reciprocal(SS[:, :], SS[:, :]), reads=[SS.res], writes=[SS.res])
            self.tt("dve", YN.reshape([128, 4, 256])[:, :, :], T1.reshape([128, 4, 256])[:, :, :],
                    SS.bc(0, [[1, 4], [0, 256]]), ALU.mult, reads=[T1.res, SS.res], writes=[YN.res])
            pb = PB[2].reshape([128, 8, 128])
            for m in range(8):
                self.tr(pb[:, m, :], YN[:, m * 128:(m + 1) * 128], self.cB(K_ID), reads=[YN.res, CSB.res],
                        writes=[pb.res])
            self.tt("dve", YST[:, :, csl], pb[:, :, :], COLS.bc(C_NW, [[1, 8], [0, 128]]), ALU.mult,
                    reads=[pb.res, COLS.res], writes=[(YST.res, c // 4)])
            xdd2 = xdd.reshape([128, 1024])
            for g in range(4):
                ps = P[5 + g // 2]
                self.mm(ps[:, (g % 2) * 256:(g % 2 + 1) * 256], BTK[:, c, g * 128:(g + 1) * 128],
                        xdd2[:, g * 256:(g + 1) * 256], True, True, reads=[BTK.res, xdd.res], writes=[ps.res])
            self.tt("dve", H.reshape([128, 16, 64])[:, :, :], H.reshape([128, 16, 64])[:, :, :],
                    CDEC.bc(c * 16, [[1, 16], [0, 64]]), ALU.mult, reads=[H.res, CDEC.res], writes=[H.res])
            for half in range(2):
                hs = slice(half * 512, (half + 1) * 512)
                self.tt("dve", H[:, hs], P[5 + half][:, :], H[:, hs], ALU.add, reads=[P[5 + half].res, H.res],
                        writes=[H.res])
            self.cp("act", HB[:, :], H[:, :], reads=[H.res], writes=[HB.res])
        self.dbg("YST", YST)
        self.arelease(mark0)
        self.gated_out(l, 0, YST, "wssd", self.mt_first)
        self.mt_first = False

    def attention(self, l, seq, t0, first):
        T, NT, NB, P, PB, S = self.T, self.NT, self.NB, self.P, self.PB, self.S
        w = self.dw[l]
        ROWX, CST, CSB = self.ROWX[l], self.CST, self.CSB
        AT = self.aalloc([8, T], BF16)
        mark0 = self.amark()
        COS = self.aalloc([T], F32)
        SIN = self.aalloc([T], F32)
        QR = self.aalloc([8, T], BF16)
        KD = self.aalloc([4, 128 + T], BF16)
        VA = self.aalloc([1 + NB, 4, 65], BF16)
        AO = self.aalloc([NB, 1024], BF16)
        mark1 = self.amark()
        POSI = self.aalloc([T], I32)
        ANG = self.aalloc([T], F32)
        A2 = self.aalloc([T], F32)
        KI = self.aalloc([T], I32)
        KF = self.aalloc([T], F32)
        S.dma("sp", POSI[:, :], bass.AP(self.d_pos.tensor, seq * SEQ + t0, [[0, 128], [1, T]]), writes=[POSI.res])
        self.cp("dve", ANG[:, :], POSI[:, :], reads=[POSI.res], writes=[ANG.res])
        self.ts("dve", ANG[:, :], ANG[:, :], self.cF(K_INVF, 1), ALU.mult, reads=[ANG.res, CST.res], writes=[ANG.res])
        TWO_PI = 2.0 * math.pi
        C1 = 6.28125
        C2 = TWO_PI - C1
        PI_SAFE = 3.1415925
        for dst, shift in ((SIN, 0.0), (COS, 0.5 * math.pi)):
            self.ts("dve", A2[:, :], ANG[:, :], float(shift), ALU.add, reads=[ANG.res], writes=[A2.res])
            self.ts("dve", KI[:, :], A2[:, :], 1.0 / TWO_PI, ALU.mult, reads=[A2.res], writes=[KI.res])
            self.cp("dve", KF[:, :], KI[:, :], reads=[KI.res], writes=[KF.res])
            self.stt(A2[:, :], KF[:, :], -C1, A2[:, :], ALU.mult, ALU.add, reads=[KF.res, A2.res], writes=[A2.res])
            self.stt(A2[:, :], KF[:, :], -C2, A2[:, :], ALU.mult, ALU.add, reads=[KF.res, A2.res], writes=[A2.res])
            self.ts("dve", A2[:, :], A2[:, :], -PI_SAFE, ALU.max, reads=[A2.res], writes=[A2.res], s2=PI_SAFE,
                    op1=ALU.min)
            self.actf(dst[:, :], A2[:, :], AF.Sin, reads=[A2.res], writes=[dst.res])
        self.arelease(mark1)
        Q32 = [self.aalloc([512], F32) for _ in range(2)]
        TA = [self.aalloc([512], F32) for _ in range(2)]
        TB = [self.aalloc([512], F32) for _ in range(2)]
        KR = [self.aalloc([512], BF16) for _ in range(2)]
        wbuf = [self.aalloc([8, 128], BF16) for _ in range(3)]
        WV = self.aalloc([8, 256], BF16)
        EC = [self.aalloc([512], BF16) for _ in range(2)]
        EP = [self.aalloc([512], BF16) for _ in range(2)]
        DEN = [self.aalloc([4], F32) for _ in range(2)]
        S.dma("pool", WV[:, :, :], w["win_tm"][:, 1040:1296].rearrange("(k p) n -> p k n", p=128), writes=[WV.res])
        st = {"n": 0}

        def rope(i, tt, pss, is_k):
            n = st["n"]
            st["n"] += 1
            sl = slice(tt * 512, (tt + 1) * 512)
            q32, ta, tb, pr = Q32[n % 2], TA[n % 2], TB[n % 2], P[2 + n % 2]
            self.cp("act", q32[:, :], pss[0][:, :], reads=[pss[0].res], writes=[q32.res])
            self.mm(pr[:, :], self.cF(K_ROT), q32[:, :], True, True, reads=[CST.res, q32.res], writes=[pr.res])
            self.tt("dve", ta[:, :], q32[:, :], COS[:, sl], ALU.mult, reads=[q32.res, COS.res], writes=[ta.res])
            self.tt("dve", tb[:, :], pr[:, :], SIN[:, sl], ALU.mult, reads=[pr.res, SIN.res], writes=[tb.res])
            if not is_k:
                self.tt("pool", QR[:, i, sl], ta[:, :], tb[:, :], ALU.add, reads=[ta.res, tb.res],
                        writes=[(QR.res, tt)])
            else:
                kr = KR[n % 2]
                self.tt("pool", kr[:, :], ta[:, :], tb[:, :], ALU.add, reads=[ta.res, tb.res], writes=[kr.res])
                for half in range(2):
                    h = 2 * i + half
                    pd = P[4 + half]
                    self.mm(pd[:, :], self.cB(K_DUP0 + half * 128), kr[:, :], True, True, reads=[CSB.res, kr.res],
                            writes=[pd.res])
                    self.cp("act" if half == 0 else "dve", KD[:, h, 128 + tt * 512:128 + (tt + 1) * 512], pd[:, :],
                            reads=[pd.res], writes=[KD.res])

        self.proj_fm([(w["win_fm"], FM_Q, 8, self.xt_rhs)], 8, lambda i, tt, pss: rope(i, tt, pss, False), [wbuf],
                     [[P[0], P[1]]])
        self.proj_fm([(w["win_fm"], FM_K, 8, self.xt_rhs)], 2, lambda i, tt, pss: rope(i, tt, pss, True), [wbuf],
                     [[P[0], P[1]]])
        self.memset("pool", VA[:, :, :, 64:65], 1.0, [VA.res])
        if not first:
            self.cp("pool", KD[:, :, 0:128], self.KCAR[l][:, :, :], reads=[self.KCAR[l].res], writes=[KD.res])
            self.cp("pool", VA[:, 0, :, :], self.VCAR[l][:, :, :], reads=[self.VCAR[l].res], writes=[VA.res])
        for blk in range(NB):
            pv = P[6 + blk % 2]
            for k in range(8):
                self.mm(pv[:, 0:256], self.XT[:, k, blk * 128:(blk + 1) * 128], WV[:, k, :], k == 0, k == 7,
                        reads=[(self.XT.res, blk // 4), WV.res], writes=[pv.res])
            self.cp("act", VA[:, 1 + blk, :, 0:64], pv.reshape([128, 8, 64])[:, 0:4, :], reads=[pv.res],
                    writes=[VA.res])
        self.dbg("QR", QR)
        self.dbg("KD", KD)
        n = 0
        for qb in range(NB):
            gfirst = first and qb == 0
            for h in range(4):
                po = P[4 + n % 2]
                ec, ep, den = EC[n % 2], EP[n % 2], DEN[n % 2]
                n += 1
                qsl = slice(qb * 128, (qb + 1) * 128)
                ec3, ep3 = ec.reshape([128, 4, 128]), ep.reshape([128, 4, 128])
                for r in range(4):
                    hq = 4 * h + r
                    ch, hf = hq // 2, hq % 2
                    ps_ = slice(hf * 64, hf * 64 + 64)
                    sc, sp = P[hf], P[2 + hf]
                    cs_ = slice((r // 2) * 128, (r // 2 + 1) * 128)
                    self.mm(sc[:, cs_], KD[ps_, h, 128 + qb * 128:128 + (qb + 1) * 128],
                            QR[ps_, ch, qsl], True, True, reads=[KD.res, (QR.res, qb // 4)], writes=[sc.res])
                    if not gfirst:
                        self.mm(sp[:, cs_], KD[ps_, h, qb * 128:(qb + 1) * 128],
                                QR[ps_, ch, qsl], True, True, reads=[KD.res, (QR.res, qb // 4)], writes=[sp.res])
                for hf in range(2):
                    self.actf(ec3[:, hf::2, :], P[hf].reshape([128, 4, 128])[:, 0:2, :], AF.Exp, reads=[P[hf].res],
                              writes=[ec.res], scale=0.125)
                self.tt("pool", ec3[:, :, :], ec3[:, :, :],
                        CSB.bc(K_TU, [[0, 4], [1, 128]]), ALU.mult, reads=[ec.res, CSB.res], writes=[ec.res])
                if not gfirst:
                    for hf in range(2):
                        self.actf(ep3[:, hf::2, :], P[2 + hf].reshape([128, 4, 128])[:, 0:2, :], AF.Exp,
                                  reads=[P[2 + hf].res], writes=[ep.res], scale=0.125)
                    self.tt("pool", ep3[:, :, :], ep3[:, :, :],
                            CSB.bc(K_TL, [[0, 4], [1, 128]]), ALU.mult, reads=[ep.res, CSB.res], writes=[ep.res])
                po3 = po.reshape([128, 4, 128])
                for r in range(4):
                    if not gfirst:
                        self.mm(po3[:, r, 0:65], ep[:, r * 128:(r + 1) * 128], VA[:, qb, h, :], True, False,
                                reads=[ep.res, VA.res], writes=[po.res])
                    self.mm(po3[:, r, 0:65], ec[:, r * 128:(r + 1) * 128], VA[:, qb + 1, h, :], gfirst, True,
                            reads=[ec.res, VA.res], writes=[po.res])
                self.tt("dve", den[:, :], po3[:, :, 64], ROWX.bc(16 + 4 * h, [[1, 4]]), ALU.add,
                        reads=[po.res, ROWX.res], writes=[den.res])
                self.S.dve(lambda e, den=den: e.reciprocal(den[:, :], den[:, :]), reads=[den.res], writes=[den.res])
                self.tt("dve", AO.reshape([128, NB, 16, 64])[:, qb, 4 * h:4 * h + 4, :], po3[:, :, 0:64],
                        den.bc(0, [[1, 4], [0, 64]]), ALU.mult, reads=[po.res, den.res], writes=[(AO.res, qb)])
        for blk in range(NB):
            pb = PB[6 + blk % 2].reshape([128, 8, 128])
            for m in range(8):
                self.tr(pb[:, m, :], AO[:, blk, m * 128:(m + 1) * 128], self.cB(K_ID), reads=[(AO.res, blk), CSB.res],
                        writes=[pb.res])
            self.cp("act" if blk % 2 else "dve", AT[:, :, blk * 128:(blk + 1) * 128], pb[:, :, :], reads=[pb.res],
                    writes=[(AT.res, blk // 4)])
        self.cp("pool", self.KCAR[l][:, :, :], KD[:, :, T:T + 128], reads=[KD.res], writes=[self.KCAR[l].res])
        self.cp("pool", self.VCAR[l][:, :, :], VA[:, NB, :, :], reads=[VA.res], writes=[self.VCAR[l].res])
        self.dbg("AT", AT)
        self.arelease(mark0)
        self.gated_out(l, 2, AT, "wattn", self.mt_first)
        self.mt_first = False

    def moe(self, l):
        T, NT, NB, P, S = self.T, self.NT, self.NB, self.P, self.S
        w = self.dw[l]
        ROWS, CST = self.ROWS[l], self.CST
        WR = self.aalloc([8, 36], BF16)
        LOG = self.aalloc([NB, 36], F32)
        WTOK = self.aalloc([NB, 4, 8], F32)
        WT = self.aalloc([T], F32)
        S.dma("pool", WR[:, :, :], w["wr"].rearrange("(k p) n -> p k n", p=128), writes=[WR.res])
        for blk in range(NB):
            pl = P[6 + blk % 2]
            for k in range(8):
                self.mm(pl[:, 0:36], self.XT[:, k, blk * 128:(blk + 1) * 128], WR[:, k, :], k == 0, k == 7,
                        reads=[(self.XT.res, blk // 4), WR.res], writes=[pl.res])
            self.tt("dve", LOG[:, blk, :], pl[:, 0:36], ROWS.bc(R_RB, [[1, 36]]), ALU.add, reads=[pl.res, ROWS.res],
                    writes=[LOG.res])
        mk = self.amark()
        f = lambda shp: self.aalloc(shp, F32)
        GMAX, GS, GE, GMASK = f([NB]), f([NB]), f([NB, 4]), f([NB, 4])
        V1, V2, P1, P2 = f([NB, 4]), f([NB, 4]), f([NB, 4]), f([NB, 4])
        M1, M2, E2 = f([NB, 4, 8]), f([NB, 4, 8]), f([NB, 4, 8])
        GL = lambda: LOG[:, :, 0:4]
        EL = lambda: LOG.bc(4, [[36, NB], [8, 4], [1, 8]])
        red = lambda out, in_, op, rd, wr: self.S.dve(
            lambda e: e.tensor_reduce(out=out, in_=in_, op=op, axis=AX.X), reads=rd, writes=wr)
        red(GMAX[:, :], GL(), ALU.max, [LOG.res], [GMAX.res])
        self.tt("dve", GE[:, :, :], GL(), GMAX.bc(0, [[1, NB], [0, 4]]), ALU.subtract, reads=[LOG.res, GMAX.res],
                writes=[GE.res])
        self.tt("dve", GMASK[:, :, :], GL(), GMAX.bc(0, [[1, NB], [0, 4]]), ALU.is_equal, reads=[LOG.res, GMAX.res],
                writes=[GMASK.res])
        self.actf(GE[:, :, :], GE[:, :, :], AF.Exp, reads=[GE.res], writes=[GE.res])
        red(GS[:, :], GE[:, :, :], ALU.add, [GE.res], [GS.res])
        self.S.dve(lambda e: e.reciprocal(GS[:, :], GS[:, :]), reads=[GS.res], writes=[GS.res])
        self.tt("dve", GMASK[:, :, :], GMASK[:, :, :], GS.bc(0, [[1, NB], [0, 4]]), ALU.mult,
                reads=[GMASK.res, GS.res], writes=[GMASK.res])
        red(V1[:, :, :], EL(), ALU.max, [LOG.res], [V1.res])
        self.tt("dve", M1[:, :, :, :], EL(), V1.bc(0, [[4, NB], [1, 4], [0, 8]]), ALU.is_equal,
                reads=[LOG.res, V1.res], writes=[M1.res])
        self.stt(E2[:, :, :, :], M1[:, :, :, :], -1.0e30, EL(), ALU.mult, ALU.add, reads=[M1.res, LOG.res],
                 writes=[E2.res])
        red(V2[:, :, :], E2[:, :, :, :], ALU.max, [E2.res], [V2.res])
        self.tt("dve", M2[:, :, :, :], E2[:, :, :, :], V2.bc(0, [[4, NB], [1, 4], [0, 8]]), ALU.is_equal,
                reads=[E2.res, V2.res], writes=[M2.res])
        self.tt("dve", P1[:, :, :], V1[:, :, :], V2[:, :, :], ALU.subtract, reads=[V1.res, V2.res], writes=[P1.res])
        self.actf(P1[:, :, :], P1[:, :, :], AF.Sigmoid, reads=[P1.res], writes=[P1.res])
        self.ts("dve", P2[:, :, :], P1[:, :, :], -1.0, ALU.mult, reads=[P1.res], writes=[P2.res], s2=1.0, op1=ALU.add)
        self.tt("dve", P1[:, :, :], P1[:, :, :], GMASK[:, :, :], ALU.mult, reads=[P1.res, GMASK.res], writes=[P1.res])
        self.tt("dve", P2[:, :, :], P2[:, :, :], GMASK[:, :, :], ALU.mult, reads=[P2.res, GMASK.res], writes=[P2.res])
        self.tt("dve", M1[:, :, :, :], M1[:, :, :, :], P1.bc(0, [[4, NB], [1, 4], [0, 8]]), ALU.mult,
                reads=[M1.res, P1.res], writes=[M1.res])
        self.tt("dve", M2[:, :, :, :], M2[:, :, :, :], P2.bc(0, [[4, NB], [1, 4], [0, 8]]), ALU.mult,
                reads=[M2.res, P2.res], writes=[M2.res])
        self.tt("dve", WTOK[:, :, :, :], M1[:, :, :, :], M2[:, :, :, :], ALU.add, reads=[M1.res, M2.res],
                writes=[WTOK.res])
        self.dbg("WTOK", WTOK)
        wt2 = WTOK.reshape([128, NB, 32])
        for b0 in range(0, NB, 4):
            ps = P[7]
            for j in range(4):
                self.tr(ps[0:32, j * 128:(j + 1) * 128], wt2[:, b0 + j, :], self.cF(K_ID), reads=[WTOK.res, CST.res],
                        writes=[ps.res])
            self.cp("act", WT[0:32, b0 * 128:(b0 + 4) * 128], ps[0:32, :], reads=[ps.res], writes=[WT.res])
        self.arelease(mk)
        WG = [self.aalloc([4, 8, 128], BF16) for _ in range(2)]
        WU = [self.aalloc([4, 8, 128], BF16) for _ in range(2)]
        WD = [self.aalloc([4, 1024], BF16) for _ in range(2)]
        HT = [self.aalloc([4, 512], BF16) for _ in range(2)]
        SIL = [self.aalloc([512], F32) for _ in range(2)]
        S2 = [self.aalloc([512], F32) for _ in range(2)]
        WBC = [self.aalloc([512], F32) for _ in range(2)]
        WM = [self.aalloc([512], F32) for _ in range(2)]
        n = 0
        for e in range(self.nexp):
            wg, wu, wd = WG[e % 2], WU[e % 2], WD[e % 2]
            for m in range(4):
                S.dma("pool", wg[:, m, :, :], w["wg"][e, :, m, :, :], writes=[wg.res])
                S.dma("pool", wu[:, m, :, :], w["wu"][e, :, m, :, :], writes=[wu.res])
            for kk in range(4):
                S.dma("pool", wd[:, kk, :], w["wd"][e, kk * 128:(kk + 1) * 128, :], writes=[wd.res])
            for tt in range(NT):
                sl = slice(tt * 512, (tt + 1) * 512)
                ht, wbc, wm = HT[n % 2], WBC[n % 2], WM[n % 2]
                n += 1
                self.ts("pool", wm[0:32, :], WT[0:32, sl], self.cF(K_ID + e, 1, npart=32), ALU.mult,
                        reads=[WT.res, CST.res], writes=[wm.res])
                self.mm(P[7][:, :], self.cF(K_ONE, 128, npart=32), wm[0:32, :], True, True, reads=[CST.res, wm.res],
                        writes=[P[7].res])
                self.cp("act", wbc[:, :], P[7][:, :], reads=[P[7].res], writes=[wbc.res])
                for m in range(4):
                    pg, pu = P[m % 2], P[2 + m % 2]
                    sil, s2 = SIL[m % 2], S2[m % 2]
                    for k in range(8):
                        self.mm(pg[:, :], wg[:, m, k, :], self.XT[:, k, sl], k == 0, k == 7,
                                reads=[wg.res, (self.XT.res, tt)], writes=[pg.res])
                    for k in range(8):
                        self.mm(pu[:, :], wu[:, m, k, :], self.XT[:, k, sl], k == 0, k == 7,
                                reads=[wu.res, (self.XT.res, tt)], writes=[pu.res])
                    self.actf(sil[:, :], pg[:, :], AF.Silu, reads=[pg.res], writes=[sil.res])
                    self.tt("pool", s2[:, :], sil[:, :], wbc[:, :], ALU.mult, reads=[sil.res, wbc.res], writes=[s2.res])
                    self.tt("dve", ht[:, m, :], pu[:, :], s2[:, :], ALU.mult, reads=[pu.res, s2.res], writes=[ht.res])
                for dc in range(8):
                    py = P[4 + dc % 2]
                    for m in range(4):
                        self.mm(py[:, :], wd[:, m, dc * 128:(dc + 1) * 128], ht[:, m, :], m == 0, m == 3,
                                reads=[wd.res, ht.res], writes=[py.res])
                    self.tt("dve", self.X[:, dc, sl], py[:, :], self.X[:, dc, sl], ALU.add,
                            reads=[py.res, (self.X.res, tt)], writes=[(self.X.res, tt)])


def host_consts():
    c = np.zeros((128, NCF), np.float32)
    k = np.arange(128)[:, None]
    m = np.arange(128)[None, :]
    c[:, K_ID:K_ID + 128] = (k == m)
    c[:, K_TU:K_TU + 128] = (k <= m)
    c[:, K_TL:K_TL + 128] = (k > m)
    c[:, K_ONE:K_ONE + 128] = 1.0
    R = np.zeros((128, 128), np.float32)
    D0 = np.zeros((128, 128), np.float32)
    D1 = np.zeros((128, 128), np.float32)
    for mm_ in range(128):
        b, j = mm_ // 64 * 64, mm_ % 64
        if j < 32:
            R[b + j + 32, mm_] = -1.0
        else:
            R[b + j - 32, mm_] = 1.0
        D0[j, mm_] = 1.0
        D1[64 + j, mm_] = 1.0
    c[:, K_ROT:K_ROT + 128] = R
    c[:, K_DUP0:K_DUP0 + 128] = D0
    c[:, K_DUP1:K_DUP1 + 128] = D1
    invf = (np.float32(10000.0) ** (-np.arange(32, dtype=np.float32) / np.float32(32))).astype(np.float32)
    c[:, K_INVF] = invf[np.arange(128) % 32]
    c[:, K_EPS] = EPS
    c[:, K_ONEC] = 1.0
    return c


def fm_layout(w):
    K, N = w.shape
    return np.ascontiguousarray(w.reshape(K // 128, 128, N // 128, 128).transpose(2, 1, 0, 3))


def colsT(v, n):
    return np.asarray(v, np.float32).reshape(n, 128).T


def prep_weights(inp, l):
    f = lambda n: np.asarray(inp[n][l], np.float32)
    w_in = f("w_in")
    o = {}
    o["win_fm%d" % l] = fm_layout(np.concatenate(
        [w_in[:, 0:3072], w_in[:, 4096:6144], w_in[:, 6160:8208], w_in[:, 8208:9232], w_in[:, 9232:9488]], axis=1))
    o["win_tm%d" % l] = np.ascontiguousarray(np.concatenate(
        [w_in[:, 3072:4096], w_in[:, 6144:6160], w_in[:, 9488:9744]], axis=1))
    for nm, src in (("wssd", "ssd_w_out"), ("wconf", "conf_w_out"), ("wattn", "attn_w_out"), ("wout", "w_out"),
                    ("wpg", "ple_w_gate"), ("wpp", "ple_w_proj")):
        o["%s%d" % (nm, l)] = fm_layout(f(src))
    o["wr%d" % l] = np.ascontiguousarray(np.concatenate([f("moe_w_group"), f("moe_w_expert")], axis=1))
    wg, wu = f("moe_w_gate"), f("moe_w_up")
    o["wg%d" % l] = np.ascontiguousarray(wg.reshape(NEXP, 8, 128, 4, 128).transpose(0, 2, 3, 1, 4))
    o["wu%d" % l] = np.ascontiguousarray(wu.reshape(NEXP, 8, 128, 4, 128).transpose(0, 2, 3, 1, 4))
    o["wd%d" % l] = np.ascontiguousarray(f("moe_w_down"))
    cols = np.zeros((128, NCOLS), np.float32)
    cols[:, C_BGATE:C_BGATE + 24] = colsT(f("b_gate"), 24)
    cols[:, C_SCW:C_SCW + 64] = f("ssd_conv_w").reshape(4, 16, 128).transpose(2, 1, 0).reshape(128, 64)
    cols[:, C_SCB:C_SCB + 16] = colsT(f("ssd_conv_b"), 16)
    cols[:, C_CDW:C_CDW + 248] = f("conf_dw_w").reshape(31, 8, 128).transpose(2, 1, 0).reshape(128, 248)
    for c0, nm in ((C_CDB, "conf_dw_b"), (C_CLG, "conf_ln_g"), (C_CLB, "conf_ln_b"), (C_NW, "ssd_norm_w"),
                   (C_L1G, "ln1_g"), (C_L1B, "ln1_b"), (C_L2G, "ln2_g"), (C_L2B, "ln2_b")):
        cols[:, c0:c0 + 8] = colsT(f(nm), 8)
    o["cols%d" % l] = cols
    rows = np.zeros((1, NROWS), np.float32)
    rows[0, R_DTB:R_DTB + 16] = f("ssd_dt_bias")
    rows[0, R_ALOG:R_ALOG + 16] = f("ssd_a_log")
    rows[0, R_D:R_D + 16] = f("ssd_d")
    rows[0, R_SINK:R_SINK + 16] = f("attn_sinks")
    rows[0, R_RB:R_RB + 4] = f("moe_b_group")
    rows[0, R_RB + 4:R_RB + 36] = f("moe_b_expert")
    o["rows%d" % l] = rows
    return o


def make_in_maps(inp, seq_lists, nlayer=DEPTH):
    shared = {"cst": host_consts()}
    for l in range(nlayer):
        shared.update(prep_weights(inp, l))
    maps = []
    for seqs in seq_lists:
        m = dict(shared)
        m["x"] = np.ascontiguousarray(np.asarray(inp["x"], np.float32)[seqs])
        m["p"] = np.ascontiguousarray(np.asarray(inp["p"], np.float32)[:, seqs])
        m["pos"] = np.ascontiguousarray(np.asarray(inp["positions"], np.int32)[seqs])
        maps.append(m)
    return maps


_NC_CACHE = {}


def kernel(**inputs):
    if "nc" not in _NC_CACHE:
        _NC_CACHE["nc"] = Builder().nc
    nc = _NC_CACHE["nc"]
    seq_lists = [list(range(c * SEQ_PER_CORE, (c + 1) * SEQ_PER_CORE)) for c in range(NCORES)]
    maps = make_in_maps(inputs, seq_lists)
    res = run_bass_kernel_spmd(nc, maps, core_ids=list(range(NCORES)))
    out = np.concatenate([np.asarray(r["out"], np.float32) for r in res.results], axis=0)
    return out
```

```python
import contextlib
import math
import numpy as np
import concourse.bass as bass
import concourse.mybir as mybir
from concourse.bass_utils import run_bass_kernel_spmd

F32 = mybir.dt.float32
BF16 = mybir.dt.bfloat16
I32 = mybir.dt.int32
AF = mybir.ActivationFunctionType
ALU = mybir.AluOpType
AX = mybir.AxisListType

ENGS = ("pe", "act", "dve", "pool", "sp")
NDMASEM = 8

D = 1024
SEQ = 2048
DEPTH = 2
NCORES = 8
SEQ_PER_CORE = 4
PLE = 256
NEXP = 32
FF = 512
ALPHA = (2 * DEPTH) ** 0.25
EPS = 1e-5
OFF_GATE, OFF_Z, OFF_XBC, OFF_DT, OFF_CONF, OFF_Q, OFF_K, OFF_V = 0, 3072, 4096, 6144, 6160, 8208, 9232, 9488
FM_GATE, FM_XBC, FM_CONF, FM_Q, FM_K = 0, 24, 40, 56, 64
NFM = 66
C_BGATE = 0
C_SCW = 24
C_SCB = 88
C_CDW = 104
C_CDB = 352
C_CLG = 360
C_CLB = 368
C_NW = 376
C_L1G = 384
C_L1B = 392
C_L2G = 400
C_L2B = 408
NCOLS = 416
R_DTB, R_ALOG, R_D, R_SINK, R_RB = 0, 16, 32, 48, 64
NROWS = 100
K_ID, K_TU, K_TL, K_ONE, K_ROT, K_DUP0, K_DUP1, K_INVF, K_EPS, K_ONEC = 0, 128, 256, 384, 512, 640, 768, 896, 897, 898
NCF = 900


class Op:
    __slots__ = ("eng", "fn", "dma", "idx", "tick", "sem_i", "sem_v", "deps", "prewait")


class Sched:
    def __init__(self, nc):
        self.nc = nc
        self.ops = []
        self.state = {}
        self.cnt = {e: 0 for e in ENGS}
        self.dcnt = {e: 0 for e in ENGS}
        self.dma_hist = {e: [] for e in ENGS}
        self.last = {e: None for e in ENGS}
        self.pending = {e: set() for e in ENGS}
        self.exclusive = set()

    def fence(self):
        F = set()
        for e in ENGS:
            if self.last[e] is not None:
                F.add(self.last[e])
            for op in self.dma_hist[e][-NDMASEM:]:
                F.add(op)
        for e in ENGS:
            self.pending[e] |= F

    @staticmethod
    def _norm(lst):
        out = []
        for r in lst:
            if isinstance(r, tuple):
                out.append((id(r[0]), r[1]))
            else:
                out.append((id(r), None))
        return out

    def _conf(self, res):
        b, k = res
        d = self.state.get(b)
        if d is None:
            return
        if k is None:
            for st in d.values():
                yield st
        else:
            st = d.get(k)
            if st is not None:
                yield st
            st = d.get(None)
            if st is not None:
                yield st

    def add(self, eng, fn, reads=(), writes=(), dma=False):
        reads = self._norm(reads)
        writes = self._norm(writes)
        if self.exclusive:
            ex = [r for r in reads if r[0] in self.exclusive]
            if ex:
                reads = [r for r in reads if r[0] not in self.exclusive]
                writes = writes + [r for r in ex if r not in writes]
        op = Op()
        op.eng, op.fn, op.dma, op.prewait = eng, fn, dma, None
        op.idx = len(self.ops)
        deps = set()
        for r in reads:
            for st in self._conf(r):
                if st[0] is not None:
                    deps.add(st[0])
        for w in writes:
            for st in self._conf(w):
                if st[0] is not None:
                    deps.add(st[0])
                deps.update(st[1])
        if self.pending[eng]:
            deps |= self.pending[eng]
            self.pending[eng] = set()
        op.deps = deps
        for w in writes:
            b, k = w
            d = self.state.setdefault(b, {})
            if k is None:
                d.clear()
            d[k] = [op, []]
        for r in reads:
            b, k = r
            d = self.state.setdefault(b, {})
            st = d.get(k)
            if st is None:
                st = [None, []]
                for s2 in self._conf(r):
                    if s2[0] is not None and (st[0] is None or s2[0].idx > st[0].idx):
                        st[0] = s2[0]
                d[k] = st
            st[1].append(op)
        if dma:
            i = self.dcnt[eng]
            self.dcnt[eng] += 1
            op.sem_i = i % NDMASEM
            op.sem_v = 16 * (i // NDMASEM + 1)
            hist = self.dma_hist[eng]
            if i >= NDMASEM:
                op.prewait = hist[i - NDMASEM]
            hist.append(op)
            op.tick = None
        else:
            self.cnt[eng] += 1
            op.tick = self.cnt[eng]
            self.last[eng] = op
        self.ops.append(op)
        return op

    def pe(self, fn, reads=(), writes=()):
        return self.add("pe", fn, reads, writes)

    def act(self, fn, reads=(), writes=()):
        return self.add("act", fn, reads, writes)

    def dve(self, fn, reads=(), writes=()):
        return self.add("dve", fn, reads, writes)

    def pool(self, fn, reads=(), writes=()):
        return self.add("pool", fn, reads, writes)

    def dma(self, eng, out, in_, reads=(), writes=()):
        return self.add(eng, lambda e: e.dma_start(out=out, in_=in_), reads, writes, dma=True)

    def emit(self):
        nc = self.nc
        with contextlib.ExitStack() as es:
            esem = {e: es.enter_context(nc.semaphore("s_" + e)) for e in ENGS}
            dsem = {e: [es.enter_context(nc.semaphore("d_%s%d" % (e, i))) for i in range(NDMASEM)]
                    for e in ENGS if self.dcnt[e] > 0}
            block = es.enter_context(nc.Block())
            per = {e: [op for op in self.ops if op.eng == e] for e in ENGS}

            def run(engname, eng):
                waited = {}

                def wait_for(dep):
                    if dep.dma:
                        s = dsem[dep.eng][dep.sem_i]
                        v = dep.sem_v
                    else:
                        if dep.eng == "pe" and engname == "pe":
                            return
                        s = esem[dep.eng]
                        v = dep.tick
                    key = id(s)
                    if waited.get(key, 0) >= v:
                        return
                    waited[key] = v
                    eng.wait_ge(s, v)

                for op in per[engname]:
                    for dep in sorted(op.deps, key=lambda d: d.idx):
                        wait_for(dep)
                    if op.prewait is not None:
                        wait_for(op.prewait)
                    inst = op.fn(eng)
                    if op.dma:
                        inst.then_inc(dsem[engname][op.sem_i], 16)
                    else:
                        inst.then_inc(esem[engname], 1)
                for op in self.dma_hist[engname][-NDMASEM:]:
                    wait_for(op)

            @block.sync
            def _(eng):
                run("sp", eng)

            @block.tensor
            def _(eng):
                run("pe", eng)

            @block.scalar
            def _(eng):
                run("act", eng)

            @block.vector
            def _(eng):
                run("dve", eng)

            @block.gpsimd
            def _(eng):
                run("pool", eng)


def pstride(t):
    return int(np.prod(list(t.shape)[1:]))


class View:
    def __init__(self, h, off, shape, res=None):
        self.h, self.off, self.shape = h, off, list(shape)
        self.dtype = h.dtype
        st, s = [], 1
        for n in reversed(self.shape[1:]):
            st.append(s)
            s *= n
        self.strides = list(reversed(st))
        self.ps = pstride(h)
        self.res = self if res is None else res

    def __getitem__(self, idx):
        if not isinstance(idx, tuple):
            idx = (idx,)
        idx = list(idx) + [slice(None)] * (len(self.shape) - len(idx))
        p = idx[0]
        p0, p1, _ = p.indices(self.shape[0])
        off = self.off
        dims = []
        for i, ix in enumerate(idx[1:]):
            stride, n = self.strides[i], self.shape[i + 1]
            if isinstance(ix, int):
                off += ix * stride
            else:
                a, b, st = ix.indices(n)
                cnt = len(range(a, b, st))
                off += a * stride
                dims.append([stride * st, cnt])
        m = [list(d) for d in dims]
        if not m:
            m = [[1, 1]]
        return bass.AP(self.h, p0 * self.ps + off, [[self.ps, p1 - p0]] + m)

    def bc(self, off, dims, npart=128, p0=0):
        return bass.AP(self.h, p0 * self.ps + self.off + off, [[self.ps, npart]] + [list(d) for d in dims])

    def reshape(self, shape):
        return View(self.h, self.off, shape, res=self.res)


class Builder:
    def __init__(self, nseq=SEQ_PER_CORE, nlayer=DEPTH, T=1024, nseg=None, debug=(), nexp=NEXP, stop_after=None,
                 only=None):
        self.nseq, self.nlayer, self.T = nseq, nlayer, T
        self.NT = T // 512
        self.NB = T // 128
        self.nseg = (SEQ // T) if nseg is None else nseg
        self.debug = set(debug)
        self.nexp = nexp
        self.stop_after = stop_after
        self.only = None if only is None else set(only.split(',')) if isinstance(only, str) else set(only)
        self.dbg_outs = {}
        nc = self.nc = bass.Bass("TRN2", target_bir_lowering=False)
        self.S = Sched(nc)
        self.declare_dram()
        self.alloc()
        self.program()
        self.S.emit()

    def din(self, name, shape, dt=F32):
        return self.nc.dram_tensor(name, list(shape), dt, kind="ExternalInput").ap()

    def pers(self, name, shape, dt):
        h = self.nc.alloc_sbuf_tensor(name, list(shape), dt)
        return View(h, 0, shape)

    def areset(self):
        self.S.fence()
        self.aoff = 0

    def aalloc(self, fshape, dt, npart=128):
        size = {F32: 4, BF16: 2, I32: 4}[dt]
        n = int(np.prod(fshape)) * size
        n = (n + 31) // 32 * 32
        off = self.aoff
        self.aoff += n
        assert self.aoff <= self.arena_bytes, ("arena overflow", self.aoff, self.arena_bytes)
        return View(self.arena[dt], off // size, [npart] + list(fshape))

    def dbg(self, name, view, reads=None):
        if name not in self.debug or name in self.dbg_outs:
            return
        o = self.nc.dram_tensor("dbg_" + name, list(view.shape), view.dtype, kind="ExternalOutput").ap()
        self.dbg_outs[name] = o
        self.S.dma("sp", o, view[:], reads=[view.res] if reads is None else reads)

    def declare_dram(self):
        L = self.nlayer
        self.d_x = self.din("x", [self.nseq, SEQ, D])
        self.d_p = self.din("p", [DEPTH, self.nseq, SEQ, PLE])
        self.d_pos = self.din("pos", [self.nseq, SEQ], I32)
        self.d_cst = self.din("cst", [128, NCF])
        self.d_out = self.nc.dram_tensor("out", [self.nseq, SEQ, D], F32, kind="ExternalOutput").ap()
        self.dw = []
        for l in range(L):
            w = {}
            w["win_fm"] = self.din("win_fm%d" % l, [NFM, 128, 8, 128])
            w["win_tm"] = self.din("win_tm%d" % l, [D, 1296])
            for nm in ("wssd", "wconf", "wattn", "wout", "wpg"):
                w[nm] = self.din("%s%d" % (nm, l), [8, 128, 8, 128])
            w["wpp"] = self.din("wpp%d" % l, [8, 128, 2, 128])
            w["wr"] = self.din("wr%d" % l, [D, 36])
            w["wg"] = self.din("wg%d" % l, [NEXP, 128, 4, 8, 128])
            w["wu"] = self.din("wu%d" % l, [NEXP, 128, 4, 8, 128])
            w["wd"] = self.din("wd%d" % l, [NEXP, FF, D])
            w["cols"] = self.din("cols%d" % l, [128, NCOLS])
            w["rows"] = self.din("rows%d" % l, [1, NROWS])
            self.dw.append(w)

    def alloc(self):
        T, nc, L = self.T, self.nc, self.nlayer
        pers = self.pers
        self.X = pers("X", [128, 8, T], F32)
        self.XT = pers("XT", [128, 8, T], BF16)
        self.MT = pers("MT", [128, 8, T], BF16)
        self.CST = pers("CST", [128, NCF], F32)
        self.CSB = pers("CSB", [128, NCF], BF16)
        self.COLS = [pers("COLS%d" % l, [128, NCOLS], F32) for l in range(L)]
        self.COLA = [pers("COLA%d" % l, [128, 16], F32) for l in range(L)]
        self.ROWS = [pers("ROWS%d" % l, [128, NROWS], F32) for l in range(L)]
        self.ROWX = [pers("ROWX%d" % l, [128, 32], F32) for l in range(L)]
        self.H = [pers("H%d" % l, [128, 1024], F32) for l in range(L)]
        self.CTAIL = [pers("CTAIL%d" % l, [128, 8, 32], BF16) for l in range(L)]
        self.STAIL = [pers("STAIL%d" % l, [128, 16, 4], BF16) for l in range(L)]
        self.KCAR = [pers("KCAR%d" % l, [128, 4, 128], BF16) for l in range(L)]
        self.VCAR = [pers("VCAR%d" % l, [128, 4, 65], BF16) for l in range(L)]
        ph = [nc.alloc_psum_tensor("P%d" % i, [128, 512], F32) for i in range(8)]
        self.P = [View(h, 0, [128, 512]) for h in ph]
        self.PB = [View(h.bitcast(BF16), 0, [128, 1024], res=v) for h, v in zip(ph, self.P)]
        self.S.exclusive = {id(v) for v in self.P}
        rem = nc.sbuf_bytes_remaining
        rem = rem() if callable(rem) else rem
        self.arena_bytes = (int(rem) - 8192) // 64 * 64
        ah = nc.alloc_sbuf_tensor("ARENA", [128, self.arena_bytes // 4], F32)
        self.arena = {F32: ah, BF16: ah.bitcast(BF16), I32: ah.bitcast(I32)}
        self.aoff = 0

    def mm(self, out, lhsT, rhs, start, stop, reads, writes):
        self.S.pe(lambda e: e.matmul(out, lhsT, rhs, start=start, stop=stop), reads, writes)

    def tr(self, out, in_, ident, reads, writes):
        self.S.pe(lambda e: e.transpose(out, in_, ident), reads, writes)

    def actf(self, out, in_, func, reads, writes, scale=1.0, bias=None):
        if bias is None:
            self.S.act(lambda e: e.activation(out=out, in_=in_, func=func, scale=scale), reads, writes)
        else:
            self.S.act(lambda e: e.activation(out=out, in_=in_, func=func, scale=scale, bias=bias), reads, writes)

    def tt(self, eng, out, in0, in1, op, reads, writes):
        self.S.add(eng, lambda e: e.tensor_tensor(out=out, in0=in0, in1=in1, op=op), reads, writes)

    def ts(self, eng, out, in0, s1, op0, reads, writes, s2=None, op1=None):
        if op1 is None:
            self.S.add(eng, lambda e: e.tensor_scalar(out=out, in0=in0, scalar1=s1, scalar2=None, op0=op0), reads, writes)
        else:
            self.S.add(eng, lambda e: e.tensor_scalar(out=out, in0=in0, scalar1=s1, scalar2=s2, op0=op0, op1=op1),
                       reads, writes)

    def stt(self, out, in0, scalar, in1, op0, op1, reads, writes):
        self.S.dve(lambda e: e.scalar_tensor_tensor(out=out, in0=in0, scalar=scalar, in1=in1, op0=op0, op1=op1),
                   reads, writes)

    def cp(self, eng, out, in_, reads, writes):
        if eng == "act":
            self.S.act(lambda e: e.copy(out, in_), reads, writes)
        else:
            self.S.add(eng, lambda e: e.tensor_copy(out, in_), reads, writes)

    def memset(self, eng, ap, val, writes):
        self.S.add(eng, lambda e: e.memset(ap, val), (), writes)

    def cF(self, off, n=128, npart=128, p0=0):
        return self.CST[p0:p0 + npart, off:off + n]

    def cB(self, off, n=128, npart=128, p0=0):
        return self.CSB[p0:p0 + npart, off:off + n]

    def col(self, l, c, npart=128):
        return self.COLS[l][0:npart, c:c + 1]

    def proj_fm(self, srcs, nchunk, consumer, wbufs, psums):
        NT = self.NT
        cnt = 0
        depth = min(len(wb) for wb in wbufs) - 1

        def issue(i):
            for si, (wsrc, c0, KC, rhs_fn) in enumerate(srcs):
                wb = wbufs[si][i % len(wbufs[si])]
                self.S.dma("pool", wb[:, 0:KC, :], wsrc[c0 + i], writes=[wb.res])

        for i in range(min(depth, nchunk)):
            issue(i)
        for i in range(nchunk):
            if i + depth < nchunk:
                issue(i + depth)
            wbs = [wbufs[si][i % len(wbufs[si])] for si in range(len(srcs))]
            for tt in range(NT):
                pss = []
                for si, (wsrc, c0, KC, rhs_fn) in enumerate(srcs):
                    ps = psums[si][cnt % len(psums[si])]
                    for k in range(KC):
                        rap, rres = rhs_fn(k, tt)
                        self.mm(ps[:, :], wbs[si][:, k, :], rap, k == 0, k == KC - 1,
                                reads=[wbs[si].res, rres], writes=[ps.res])
                    pss.append(ps)
                cnt += 1
                consumer(i, tt, pss)

    def xt_rhs(self, k, tt):
        return self.XT[:, k, tt * 512:(tt + 1) * 512], (self.XT.res, tt)

    def gated_out(self, l, branch, YT, wname, first):
        w = self.dw[l]
        wbA = [self.aalloc([8, 128], BF16) for _ in range(3)]
        wbB = [self.aalloc([8, 128], BF16) for _ in range(3)]
        SG = [self.aalloc([512], F32) for _ in range(2)]
        TM = [self.aalloc([512], BF16) for _ in range(2)]
        P = self.P
        st = {"n": 0}

        def yt_rhs(k, tt):
            return YT[:, k, tt * 512:(tt + 1) * 512], (YT.res, tt)

        def consumer(i, tt, pss):
            n = st["n"]
            st["n"] += 1
            sg = SG[n % 2]
            self.actf(sg[:, :], pss[1][:, :], AF.Sigmoid, reads=[pss[1].res], writes=[sg.res],
                      bias=self.col(l, C_BGATE + branch * 8 + i))
            dst = self.MT[:, i, tt * 512:(tt + 1) * 512]
            if first:
                self.tt("dve", dst, pss[0][:, :], sg[:, :], ALU.mult, reads=[pss[0].res, sg.res],
                        writes=[(self.MT.res, tt)])
            else:
                tm = TM[n % 2]
                self.tt("dve", tm[:, :], pss[0][:, :], sg[:, :], ALU.mult, reads=[pss[0].res, sg.res],
                        writes=[tm.res])
                self.tt("pool", dst, dst, tm[:, :], ALU.add, reads=[tm.res, (self.MT.res, tt)],
                        writes=[(self.MT.res, tt)])

        self.proj_fm([(w[wname], 0, 8, yt_rhs), (w["win_fm"], FM_GATE + branch * 8, 8, self.xt_rhs)], 8, consumer,
                     [wbA, wbB], [[P[0], P[1]], [P[2], P[3]]])

    def ln_fm(self, SRC, tt, out_fn, tmp):
        P = self.P
        SQ, MEAN, RSTD, TN = tmp
        sl = slice(tt * 512, (tt + 1) * 512)
        for m in range(8):
            self.mm(P[6][:, :], self.cF(K_ONE), SRC[:, m, sl], m == 0, m == 7,
                    reads=[self.CST.res, (SRC.res, tt)], writes=[P[6].res])
        for m in range(8):
            sq = SQ[m % 2]
            self.actf(sq[:, :], SRC[:, m, sl], AF.Square, reads=[(SRC.res, tt)], writes=[sq.res])
            self.mm(P[7][:, :], self.cF(K_ONE), sq[:, :], m == 0, m == 7, reads=[self.CST.res, sq.res],
                    writes=[P[7].res])
        self.ts("dve", MEAN[:, :], P[6][:, :], 1.0 / 1024, ALU.mult, reads=[P[6].res], writes=[MEAN.res])
        self.tt("dve", RSTD[:, :], MEAN[:, :], MEAN[:, :], ALU.mult, reads=[MEAN.res], writes=[RSTD.res])
        self.stt(RSTD[:, :], P[7][:, :], 1.0 / 1024, RSTD[:, :], ALU.mult, ALU.subtract, reads=[P[7].res, RSTD.res],
                 writes=[RSTD.res])
        self.actf(RSTD[:, :], RSTD[:, :], AF.Sqrt, reads=[RSTD.res], writes=[RSTD.res], bias=self.cF(K_EPS, 1))
        self.S.dve(lambda e: e.reciprocal(RSTD[:, :], RSTD[:, :]), reads=[RSTD.res], writes=[RSTD.res])
        for m in range(8):
            tn = TN[m % 2]
            self.tt("dve", tn[:, :], SRC[:, m, sl], MEAN[:, :], ALU.subtract, reads=[(SRC.res, tt), MEAN.res],
                    writes=[tn.res])
            self.tt("dve", tn[:, :], tn[:, :], RSTD[:, :], ALU.mult, reads=[tn.res, RSTD.res], writes=[tn.res])
            out_fn(m, tn)

    def ln_tmp(self):
        return ([self.aalloc([512], F32) for _ in range(2)], self.aalloc([512], F32), self.aalloc([512], F32),
                [self.aalloc([512], F32) for _ in range(2)])

    def program(self):
        S = self.S
        S.dma("sp", self.CST[:, :], self.d_cst, writes=[self.CST.res])
        self.cp("dve", self.CSB[:, :], self.CST[:, :], reads=[self.CST.res], writes=[self.CSB.res])
        import os
        self.bis = int(os.environ.get("BIS", "0"))
        for l in range(self.nlayer):
            if self.bis & 1:
                break
            S.dma("sp", self.COLS[l][:, :], self.dw[l]["cols"], writes=[self.COLS[l].res])
            S.dma("sp", self.ROWS[l][:, :], bass.AP(self.dw[l]["rows"].tensor, 0, [[0, 128], [1, NROWS]]),
                  writes=[self.ROWS[l].res])
            self.actf(self.ROWX[l][:, 0:16], self.ROWS[l][:, R_ALOG:R_ALOG + 16], AF.Exp, reads=[self.ROWS[l].res],
                      writes=[self.ROWX[l].res])
            self.ts("dve", self.ROWX[l][:, 0:16], self.ROWX[l][:, 0:16], -1.0, ALU.mult, reads=[self.ROWX[l].res],
                    writes=[self.ROWX[l].res])
            self.actf(self.ROWX[l][:, 16:32], self.ROWS[l][:, R_SINK:R_SINK + 16], AF.Exp, reads=[self.ROWS[l].res],
                      writes=[self.ROWX[l].res])
            self.ts("dve", self.COLA[l][:, 0:16], self.COLS[l][:, C_L1G:C_L1G + 16], float(ALPHA), ALU.mult,
                    reads=[self.COLS[l].res], writes=[self.COLA[l].res])
        for seq in range(self.nseq):
            for seg in range(self.nseg):
                self.segment(seq, seg)

    def segment(self, seq, seg):
        T = self.T
        t0 = seg * T
        first = seg == 0
        self.areset()
        if not self.bis & 4:
            self.load_x(seq, t0)
        if first and not self.bis & 2:
            for l in range(self.nlayer):
                self.memset("pool", self.H[l][:, :], 0.0, [self.H[l].res])
                self.memset("pool", self.CTAIL[l][:, :, :], 0.0, [self.CTAIL[l].res])
                self.memset("pool", self.STAIL[l][:, :, :], 0.0, [self.STAIL[l].res])
        for l in range(self.nlayer):
            self.mt_first = True
            stages = [("ssd", lambda: self.ssd(l, first)),
                      ("conf", lambda: self.conformer(l)),
                      ("attn", lambda: self.attention(l, seq, t0, first)),
                      ("ln1", lambda: self.outproj_ln1(l)),
                      ("moe", lambda: self.moe(l)),
                      ("ple", lambda: self.ln2_ple(l, seq, t0))]
            for nm, fn in stages:
                if self.only is not None and nm not in self.only:
                    continue
                self.areset()
                fn()
                self.dbg("MT", self.MT)
                if self.stop_after == (l, nm):
                    break
            if self.stop_after is not None and self.stop_after[0] == l:
                break
        self.areset()
        if not self.bis & 8:
            self.store_x(seq, t0)

    def load_x(self, seq, t0):
        S, P = self.S, self.P
        STG = [self.aalloc([1024], F32) for _ in range(2)]
        for blk in range(self.NB):
            stg = STG[blk % 2]
            tt = blk // 4
            S.dma("sp", stg[:, :], self.d_x[seq, t0 + blk * 128:t0 + (blk + 1) * 128, :], writes=[stg.res])
            for half in range(2):
                ps = P[(blk * 2 + half) % 4].reshape([128, 4, 128])
                for j in range(4):
                    c = half * 4 + j
                    self.tr(ps[:, j, :], stg[:, c * 128:(c + 1) * 128], self.cF(K_ID), reads=[stg.res, self.CST.res],
                            writes=[ps.res])
                self.cp("act", self.X[:, half * 4:half * 4 + 4, blk * 128:(blk + 1) * 128], ps[:, :, :],
                        reads=[ps.res], writes=[(self.X.res, tt)])
                self.cp("dve", self.XT[:, half * 4:half * 4 + 4, blk * 128:(blk + 1) * 128], ps[:, :, :],
                        reads=[ps.res], writes=[(self.XT.res, tt)])

    def store_x(self, seq, t0):
        S, P = self.S, self.P
        STG = [self.aalloc([1024], F32) for _ in range(2)]
        for blk in range(self.NB):
            stg = STG[blk % 2]
            tt = blk // 4
            for half in range(2):
                ps = P[(blk * 2 + half) % 4].reshape([128, 4, 128])
                for j in range(4):
                    c = half * 4 + j
                    self.tr(ps[:, j, :], self.X[:, c, blk * 128:(blk + 1) * 128], self.cF(K_ID),
                            reads=[(self.X.res, tt), self.CST.res], writes=[ps.res])
                self.cp("act" if half == 0 else "dve", stg[:, half * 512:(half + 1) * 512],
                        ps[:, :, :], reads=[ps.res], writes=[stg.res])
            S.dma("sp", self.d_out[seq, t0 + blk * 128:t0 + (blk + 1) * 128, :], stg[:, :], reads=[stg.res])

    def conformer(self, l):
        T, NT, P, S = self.T, self.NT, self.P, self.S
        w = self.dw[l]
        HPAD = [self.aalloc([32 + T], BF16) for _ in range(2)]
        DG = [self.aalloc([31, 128], BF16) for _ in range(2)]
        CO = self.aalloc([8, T], F32)
        HT = self.aalloc([8, T], BF16)
        SG = [self.aalloc([512], F32) for _ in range(2)]
        wbA = [self.aalloc([8, 128], BF16) for _ in range(2)]
        wbG = [self.aalloc([8, 128], BF16) for _ in range(2)]
        lnt = self.ln_tmp()

        def cload(m):
            S.dma("pool", wbA[m % 2][:, :, :], w["win_fm"][FM_CONF + m], writes=[wbA[m % 2].res])
            S.dma("pool", wbG[m % 2][:, :, :], w["win_fm"][FM_CONF + 8 + m], writes=[wbG[m % 2].res])

        for m in range(8):
            hp, dg = HPAD[m % 2], DG[m % 2]
            self.cp("pool", hp[:, 0:32], self.CTAIL[l][:, m, :], reads=[self.CTAIL[l].res], writes=[hp.res])
            self.tt("pool", dg[:, :, :], self.CSB.bc(K_ID, [[0, 31], [1, 128]]),
                    self.COLS[l].bc(C_CDW + m * 31, [[1, 31], [0, 128]]), ALU.mult,
                    reads=[self.CSB.res, self.COLS[l].res], writes=[dg.res])
            wa, wg = wbA[m % 2], wbG[m % 2]
            if m == 0:
                cload(0)
            if m + 1 < 8:
                cload(m + 1)
            for tt in range(NT):
                sl = slice(tt * 512, (tt + 1) * 512)
                a_ps, g_ps, c_ps = P[tt % 2], P[2 + tt % 2], P[4 + tt % 2]
                for k in range(8):
                    self.mm(a_ps[:, :], wa[:, k, :], self.XT[:, k, sl], k == 0, k == 7,
                            reads=[wa.res, (self.XT.res, tt)], writes=[a_ps.res])
                for k in range(8):
                    self.mm(g_ps[:, :], wg[:, k, :], self.XT[:, k, sl], k == 0, k == 7,
                            reads=[wg.res, (self.XT.res, tt)], writes=[g_ps.res])
                sg = SG[tt % 2]
                self.actf(sg[:, :], g_ps[:, :], AF.Sigmoid, reads=[g_ps.res], writes=[sg.res])
                self.tt("dve", hp[:, 32 + tt * 512:32 + (tt + 1) * 512], a_ps[:, :], sg[:, :], ALU.mult,
                        reads=[a_ps.res, sg.res], writes=[hp.res])
                for k in range(31):
                    self.mm(c_ps[:, :], dg[:, k, :], hp[:, 2 + k + tt * 512:2 + k + (tt + 1) * 512], k == 0, k == 30,
                            reads=[dg.res, hp.res], writes=[c_ps.res])
                self.actf(CO[:, m, sl], c_ps[:, :], AF.Identity, reads=[c_ps.res], writes=[(CO.res, tt)],
                          bias=self.col(l, C_CDB + m))
            self.cp("pool", self.CTAIL[l][:, m, :], hp[:, T:T + 32], reads=[hp.res], writes=[self.CTAIL[l].res])
        self.dbg("CO", CO)
        for tt in range(NT):
            sl = slice(tt * 512, (tt + 1) * 512)

            def out_fn(m, tn, tt=tt, sl=sl):
                self.actf(HT[:, m, sl], tn[:, :], AF.Silu, reads=[tn.res], writes=[(HT.res, tt)],
                          scale=self.col(l, C_CLG + m), bias=self.col(l, C_CLB + m))

            self.ln_fm(CO, tt, out_fn, lnt)
        self.dbg("HT", HT)
        self.gated_out(l, 1, HT, "wconf", self.mt_first)
        self.mt_first = False

    def outproj_ln1(self, l):
        T, NT, P, S = self.T, self.NT, self.P, self.S
        w = self.dw[l]
        wb = [self.aalloc([8, 128], BF16) for _ in range(3)]
        lnt = self.ln_tmp()
        self.dbg("MT", self.MT)

        def mt_rhs(k, tt):
            return self.MT[:, k, tt * 512:(tt + 1) * 512], (self.MT.res, tt)

        def consumer(i, tt, pss):
            sl = slice(tt * 512, (tt + 1) * 512)
            self.stt(self.X[:, i, sl], self.X[:, i, sl], float(ALPHA), pss[0][:, :], ALU.mult, ALU.add,
                     reads=[(self.X.res, tt), pss[0].res], writes=[(self.X.res, tt)])

        self.proj_fm([(w["wout"], 0, 8, mt_rhs)], 8, consumer, [wb], [[P[0], P[1]]])
        for tt in range(NT):
            sl = slice(tt * 512, (tt + 1) * 512)

            def out_fn(m, tn, tt=tt, sl=sl):
                self.actf(self.XT[:, m, sl], tn[:, :], AF.Identity, reads=[tn.res], writes=[(self.XT.res, tt)],
                          scale=self.col(l, C_L1G + m), bias=self.col(l, C_L1B + m))
                self.actf(self.X[:, m, sl], tn[:, :], AF.Identity, reads=[tn.res], writes=[(self.X.res, tt)],
                          scale=self.COLA[l][:, m:m + 1], bias=self.COLA[l][:, 8 + m:9 + m])

            self.ln_fm(self.X, tt, out_fn, lnt)
        self.dbg("X1T", self.XT)

    def ln2_ple(self, l, seq, t0):
        T, NT, NB, P, S = self.T, self.NT, self.NB, self.P, self.S
        w = self.dw[l]
        lnt = self.ln_tmp()
        self.dbg("XMOE", self.X)
        for tt in range(NT):
            sl = slice(tt * 512, (tt + 1) * 512)

            def out_fn(m, tn, tt=tt, sl=sl):
                self.actf(self.XT[:, m, sl], tn[:, :], AF.Identity, reads=[tn.res], writes=[(self.XT.res, tt)],
                          scale=self.col(l, C_L2G + m), bias=self.col(l, C_L2B + m))
                self.actf(self.X[:, m, sl], tn[:, :], AF.Identity, reads=[tn.res], writes=[(self.X.res, tt)],
                          scale=self.col(l, C_L2G + m), bias=self.col(l, C_L2B + m))

            self.ln_fm(self.X, tt, out_fn, lnt)
        self.dbg("X2T", self.XT)
        PSTG = [self.aalloc([256], F32) for _ in range(2)]
        PT = self.aalloc([2, T], BF16)
        for blk in range(NB):
            stg = PSTG[blk % 2]
            S.dma("sp", stg[:, :], self.d_p[l, seq, t0 + blk * 128:t0 + (blk + 1) * 128, :], writes=[stg.res])
            ps = P[6 + blk % 2].reshape([128, 4, 128])
            for j in range(2):
                self.tr(ps[:, j, :], stg[:, j * 128:(j + 1) * 128], self.cF(K_ID), reads=[stg.res, self.CST.res],
                        writes=[ps.res])
            self.cp("act", PT[:, :, blk * 128:(blk + 1) * 128], ps[:, 0:2, :], reads=[ps.res],
                    writes=[(PT.res, blk // 4)])
        wbA = [self.aalloc([8, 128], BF16) for _ in range(3)]
        wbB = [self.aalloc([8, 128], BF16) for _ in range(3)]
        SG = [self.aalloc([512], F32) for _ in range(2)]
        TM = [self.aalloc([512], F32) for _ in range(2)]
        st = {"n": 0}

        def pt_rhs(k, tt):
            return PT[:, k, tt * 512:(tt + 1) * 512], (PT.res, tt)

        def consumer(i, tt, pss):
            n = st["n"]
            st["n"] += 1
            sl = slice(tt * 512, (tt + 1) * 512)
            sg, tm = SG[n % 2], TM[n % 2]
            self.actf(sg[:, :], pss[0][:, :], AF.Sigmoid, reads=[pss[0].res], writes=[sg.res])
            self.tt("dve", tm[:, :], pss[1][:, :], sg[:, :], ALU.mult, reads=[pss[1].res, sg.res], writes=[tm.res])
            self.tt("pool", self.X[:, i, sl], self.X[:, i, sl], tm[:, :], ALU.add, reads=[tm.res, (self.X.res, tt)],
                    writes=[(self.X.res, tt)])

        self.proj_fm([(w["wpg"], 0, 8, self.xt_rhs), (w["wpp"], 0, 2, pt_rhs)], 8, consumer, [wbA, wbB],
                     [[P[0], P[1]], [P[2], P[3]]])
        if l < self.nlayer - 1:
            for tt in range(NT):
                sl = slice(tt * 512, (tt + 1) * 512)
                for m in range(8):
                    self.cp("act" if m % 2 else "pool", self.XT[:, m, sl], self.X[:, m, sl], reads=[(self.X.res, tt)],
                            writes=[(self.XT.res, tt)])

    def amark(self):
        return self.aoff

    def arelease(self, mark):
        self.S.fence()
        self.aoff = mark

    def ssd(self, l, first):
        T, NT, NB, P, PB, S = self.T, self.NT, self.NB, self.P, self.PB, self.S
        w = self.dw[l]
        ROWS, ROWX, COLS, CST, CSB = self.ROWS[l], self.ROWX[l], self.COLS[l], self.CST, self.CSB
        YST = self.aalloc([8, T], BF16)
        mark0 = self.amark()
        WZ = self.aalloc([8, 1024], BF16)
        WDT = self.aalloc([8, 16], BF16)
        BFM = self.aalloc([4, T], BF16)
        CFM = self.aalloc([4, T], BF16)
        XST = self.aalloc([NB, 1024], BF16)
        BTK = self.aalloc([NB, 512], BF16)
        DT, DTA, ECS, DTE, CDEC = [self.aalloc([NB, 16], F32) for _ in range(5)]
        mark1 = self.amark()
        XPAD = [self.aalloc([4 + T], BF16) for _ in range(2)]
        DG = [self.aalloc([4, 128], BF16) for _ in range(2)]
        XFM = [self.aalloc([T], BF16) for _ in range(2)]
        wbuf = [self.aalloc([8, 128], BF16) for _ in range(3)]
        S.dma("pool", WZ[:, :, :], w["win_tm"][:, 0:1024].rearrange("(k p) n -> p k n", p=128), writes=[WZ.res])
        S.dma("pool", WDT[:, :, :], w["win_tm"][:, 1024:1040].rearrange("(k p) n -> p k n", p=128), writes=[WDT.res])
        for m in range(16):
            xp, dg = XPAD[m % 2], DG[m % 2]
            if m < 8:
                dst = XFM[m % 2]
                dsl = lambda sl, dst=dst: dst[:, sl]
            elif m < 12:
                dst = BFM
                dsl = lambda sl, g=m - 8: BFM[:, g, sl]
            else:
                dst = CFM
                dsl = lambda sl, g=m - 12: CFM[:, g, sl]
            self.cp("pool", xp[:, 0:4], self.STAIL[l][:, m, :], reads=[self.STAIL[l].res], writes=[xp.res])
            self.tt("pool", dg[:, :, :], CSB.bc(K_ID, [[0, 4], [1, 128]]), COLS.bc(C_SCW + m * 4, [[1, 4], [0, 128]]),
                    ALU.mult, reads=[CSB.res, COLS.res], writes=[dg.res])
            wb = wbuf[m % 3]
            if m == 0:
                for mm_ in range(2):
                    S.dma("pool", wbuf[mm_ % 3][:, :, :], w["win_fm"][FM_XBC + mm_], writes=[wbuf[mm_ % 3].res])
            if m + 2 < 16:
                S.dma("pool", wbuf[(m + 2) % 3][:, :, :], w["win_fm"][FM_XBC + m + 2], writes=[wbuf[(m + 2) % 3].res])
            for tt in range(NT):
                sl = slice(tt * 512, (tt + 1) * 512)
                ps, cps = P[tt % 2], P[2 + tt % 2]
                for k in range(8):
                    self.mm(ps[:, :], wb[:, k, :], self.XT[:, k, sl], k == 0, k == 7,
                            reads=[wb.res, (self.XT.res, tt)], writes=[ps.res])
                self.cp("act", xp[:, 4 + tt * 512:4 + (tt + 1) * 512], ps[:, :], reads=[ps.res], writes=[xp.res])
                for k in range(4):
                    self.mm(cps[:, :], dg[:, k, :], xp[:, 1 + k + tt * 512:1 + k + (tt + 1) * 512], k == 0, k == 3,
                            reads=[dg.res, xp.res], writes=[cps.res])
                self.actf(dsl(sl), cps[:, :], AF.Silu, reads=[cps.res], writes=[dst.res],
                          bias=self.col(l, C_SCB + m))
            self.cp("pool", self.STAIL[l][:, m, :], xp[:, T:T + 4], reads=[xp.res], writes=[self.STAIL[l].res])
            if m < 12:
                for b0 in range(0, NB, 8):
                    nb = min(8, NB - b0)
                    pb = PB[4 + (m + b0 // 8) % 2].reshape([128, 8, 128])
                    for j in range(nb):
                        blk = b0 + j
                        src = dst[:, blk * 128:(blk + 1) * 128] if m < 8 else BFM[:, m - 8, blk * 128:(blk + 1) * 128]
                        self.tr(pb[:, j, :], src, self.cB(K_ID), reads=[dst.res, CSB.res], writes=[pb.res])
                    if m < 8:
                        self.cp("dve", XST[:, b0:b0 + nb, m * 128:(m + 1) * 128], pb[:, 0:nb, :], reads=[pb.res],
                                writes=[XST.res])
                    else:
                        self.cp("dve", BTK[:, b0:b0 + nb, (m - 8) * 128:(m - 7) * 128], pb[:, 0:nb, :],
                                reads=[pb.res], writes=[BTK.res])
        PD = P[6].reshape([128, 32, 16])
        for blk in range(NB):
            for k in range(8):
                self.mm(PD[:, blk, :], self.XT[:, k, blk * 128:(blk + 1) * 128], WDT[:, k, :], k == 0, k == 7,
                        reads=[(self.XT.res, blk // 4), WDT.res], writes=[PD.res])
        self.tt("dve", DT[:, :, :], PD[:, 0:NB, :], ROWS.bc(R_DTB, [[0, NB], [1, 16]]), ALU.add,
                reads=[PD.res, ROWS.res], writes=[DT.res])
        self.actf(DT[:, :, :], DT[:, :, :], AF.Exp, reads=[DT.res], writes=[DT.res])
        self.actf(DT[:, :, :], DT[:, :, :], AF.Ln, reads=[DT.res], writes=[DT.res], bias=self.cF(K_ONEC, 1))
        self.tt("dve", DTA[:, :, :], DT[:, :, :], ROWX.bc(0, [[0, NB], [1, 16]]), ALU.mult,
                reads=[DT.res, ROWX.res], writes=[DTA.res])
        dta2 = DTA.reshape([128, NB * 16])
        for (cst, dstv, ps) in ((K_TU, ECS, P[7]), (K_TL, DTE, P[6]), (K_ONE, CDEC, P[7])):
            self.mm(ps[:, 0:NB * 16], self.cF(cst), dta2[:, :], True, True, reads=[CST.res, DTA.res], writes=[ps.res])
            self.actf(dstv.reshape([128, NB * 16])[:, :], ps[:, 0:NB * 16], AF.Exp, reads=[ps.res], writes=[dstv.res])
        self.arelease(mark1)
        RS = [self.aalloc([4, 128], F32) for _ in range(2)]
        DEC = [self.aalloc([4, 128], F32) for _ in range(2)]
        MTG = [self.aalloc([4, 128], BF16) for _ in range(2)]
        CBM = self.aalloc([4, 128], F32)
        XDT = [self.aalloc([16, 64], BF16) for _ in range(2)]
        XDD = [self.aalloc([16, 64], BF16) for _ in range(2)]
        XD = [self.aalloc([16, 64], BF16) for _ in range(2)]
        T1 = self.aalloc([1024], F32)
        SZ = self.aalloc([1024], F32)
        YN = self.aalloc([1024], BF16)
        HB = self.aalloc([1024], BF16)
        SS = self.aalloc([4], F32)
        H = self.H[l]
        self.cp("pool", HB[:, :], H[:, :], reads=[H.res], writes=[HB.res])
        for c in range(NB):
            csl = slice(c * 128, (c + 1) * 128)
            xdt, xdd, xd = XDT[c % 2], XDD[c % 2], XD[c % 2]
            xs3 = XST.reshape([128, NB, 16, 64])
            self.tt("dve", xdt[:, :, :], xs3[:, c, :, :], DT.bc(c * 16, [[1, 16], [0, 64]]), ALU.mult,
                    reads=[XST.res, DT.res], writes=[xdt.res])
            self.tt("pool", xdd[:, :, :], xdt[:, :, :], DTE.bc(c * 16, [[1, 16], [0, 64]]), ALU.mult,
                    reads=[xdt.res, DTE.res], writes=[xdd.res])
            self.tt("pool", xd[:, :, :], xs3[:, c, :, :], ROWS.bc(R_D, [[1, 16], [0, 64]]), ALU.mult,
                    reads=[XST.res, ROWS.res], writes=[xd.res])
            p0 = P[0].reshape([128, 4, 128])
            for g in range(4):
                self.mm(p0[:, g, :], BFM[:, g, csl], CFM[:, g, csl], True, True, reads=[BFM.res, CFM.res],
                        writes=[p0.res])
            self.tt("dve", CBM[:, :, :], p0[:, :, :], CST.bc(K_TU, [[0, 4], [1, 128]]), ALU.mult,
                    reads=[p0.res, CST.res], writes=[CBM.res])
            xd2 = xd.reshape([128, 1024])
            xdt2 = xdt.reshape([128, 1024])
            for half in range(2):
                self.mm(P[3 + half][:, :], self.cB(K_ID), xd2[:, half * 512:(half + 1) * 512], True, False,
                        reads=[CSB.res, xd.res], writes=[P[3 + half].res])
            for g in range(4):
                rs, dec, mtg = RS[g % 2], DEC[g % 2], MTG[g % 2]
                self.tt("dve", rs[:, :, :], DTA.bc(c * 16 + 4 * g, [[1, 4], [0, 128]]),
                        CST.bc(K_TU, [[0, 4], [1, 128]]), ALU.mult, reads=[DTA.res, CST.res], writes=[rs.res])
                self.mm(P[1][:, :], self.cF(K_TL), rs.reshape([128, 512])[:, :], True, True,
                        reads=[CST.res, rs.res], writes=[P[1].res])
                self.actf(dec.reshape([128, 512])[:, :], P[1][:, :], AF.Exp, reads=[P[1].res], writes=[dec.res])
                self.tt("dve", mtg[:, :, :], dec[:, :, :], CBM.bc(g * 128, [[0, 4], [1, 128]]), ALU.mult,
                        reads=[dec.res, CBM.res], writes=[mtg.res])
                for r in range(4):
                    h = 4 * g + r
                    half = h // 8
                    self.mm(P[3 + half][:, (h % 8) * 64:(h % 8 + 1) * 64], mtg[:, r, :],
                            xdt2[:, h * 64:(h + 1) * 64], False, h % 8 == 7,
                            reads=[mtg.res, xdt.res], writes=[P[3 + half].res])
            for g in range(4):
                ps = P[5 + g // 2]
                self.mm(ps[:, (g % 2) * 256:(g % 2 + 1) * 256], CFM[:, g, csl], HB[:, g * 256:(g + 1) * 256],
                        True, True, reads=[CFM.res, HB.res], writes=[ps.res])
            t13 = T1.reshape([128, 16, 64])
            for half in range(2):
                self.tt("dve", t13[:, half * 8:(half + 1) * 8, :], P[5 + half].reshape([128, 8, 64])[:, :, :],
                        ECS.bc(c * 16 + half * 8, [[1, 8], [0, 64]]), ALU.mult,
                        reads=[P[5 + half].res, ECS.res], writes=[T1.res])
            for half in range(2):
                hs = slice(half * 512, (half + 1) * 512)
                self.tt("dve", T1[:, hs], P[3 + half][:, :], T1[:, hs], ALU.add, reads=[P[3 + half].res, T1.res],
                        writes=[T1.res])
            for half in range(2):
                hs = slice(half * 512, (half + 1) * 512)
                pz = P[7] if half == 0 else P[0]
                for k in range(8):
                    self.mm(pz[:, :], self.XT[:, k, csl], WZ[:, k, hs], k == 0, k == 7,
                            reads=[(self.XT.res, c // 4), WZ.res], writes=[pz.res])
                self.actf(SZ[:, hs], pz[:, :], AF.Silu, reads=[pz.res], writes=[SZ.res])
            self.tt("dve", T1[:, :], T1[:, :], SZ[:, :], ALU.mult, reads=[T1.res, SZ.res], writes=[T1.res])
            self.tt("dve", SZ[:, :], T1[:, :], T1[:, :], ALU.mult, reads=[T1.res], writes=[SZ.res])
            self.S.dve(lambda e: e.tensor_reduce(out=SS[:, :], in_=SZ.reshape([128, 4, 256])[:, :, :], op=ALU.add,
                                                 axis=AX.X), reads=[SZ.res], writes=[SS.res])
            self.ts("dve", SS[:, :], SS[:, :], 1.0 / 256, ALU.mult, reads=[SS.res], writes=[SS.res], s2=float(EPS),
                    op1=ALU.add)
            self.actf(SS[:, :], SS[:, :], AF.Sqrt, reads=[SS.res], writes=[SS.res])
            self.S.dve(lambda e: e.reciprocal(SS[:, :], SS[:, :]), reads=[SS.res], writes=[SS.res])
            self.tt("dve", YN.reshape([128, 4, 256])[:, :, :], T1.reshape([128, 4, 256])[:, :, :],
                    SS.bc(0, [[1, 4], [0, 256]]), ALU.mult, reads=[T1.res, SS.res], writes=[YN.res])
            pb = PB[2].reshape([128, 8, 128])
            for m in range(8):
                self.tr(pb[:, m, :], YN[:, m * 128:(m + 1) * 128], self.cB(K_ID), reads=[YN.res, CSB.res],
                        writes=[pb.res])
            self.tt("dve", YST[:, :, csl], pb[:, :, :], COLS.bc(C_NW, [[1, 8], [0, 128]]), ALU.mult,
                    reads=[pb.res, COLS.res], writes=[(YST.res, c // 4)])
            xdd2 = xdd.reshape([128, 1024])
            for g in range(4):
                ps = P[5 + g // 2]
                self.mm(ps[:, (g % 2) * 256:(g % 2 + 1) * 256], BTK[:, c, g * 128:(g + 1) * 128],
                        xdd2[:, g * 256:(g + 1) * 256], True, True, reads=[BTK.res, xdd.res], writes=[ps.res])
            self.tt("dve", H.reshape([128, 16, 64])[:, :, :], H.reshape([128, 16, 64])[:, :, :],
                    CDEC.bc(c * 16, [[1, 16], [0, 64]]), ALU.mult, reads=[H.res, CDEC.res], writes=[H.res])
            for half in range(2):
                hs = slice(half * 512, (half + 1) * 512)
                self.tt("dve", H[:, hs], P[5 + half][:, :], H[:, hs], ALU.add, reads=[P[5 + half].res, H.res],
                        writes=[H.res])
            self.cp("act", HB[:, :], H[:, :], reads=[H.res], writes=[HB.res])
        self.dbg("YST", YST)
        self.arelease(mark0)
        self.gated_out(l, 0, YST, "wssd", self.mt_first)
        self.mt_first = False

    def attention(self, l, seq, t0, first):
        T, NT, NB, P, PB, S = self.T, self.NT, self.NB, self.P, self.PB, self.S
        w = self.dw[l]
        ROWX, CST, CSB = self.ROWX[l], self.CST, self.CSB
        AT = self.aalloc([8, T], BF16)
        mark0 = self.amark()
        COS = self.aalloc([T], F32)
        SIN = self.aalloc([T], F32)
        QR = self.aalloc([8, T], BF16)
        KD = self.aalloc([4, 128 + T], BF16)
        VA = self.aalloc([1 + NB, 4, 65], BF16)
        AO = self.aalloc([NB, 1024], BF16)
        mark1 = self.amark()
        POSI = self.aalloc([T], I32)
        ANG = self.aalloc([T], F32)
        A2 = self.aalloc([T], F32)
        KI = self.aalloc([T], I32)
        KF = self.aalloc([T], F32)
        S.dma("sp", POSI[:, :], bass.AP(self.d_pos.tensor, seq * SEQ + t0, [[0, 128], [1, T]]), writes=[POSI.res])
        self.cp("dve", ANG[:, :], POSI[:, :], reads=[POSI.res], writes=[ANG.res])
        self.ts("dve", ANG[:, :], ANG[:, :], self.cF(K_INVF, 1), ALU.mult, reads=[ANG.res, CST.res], writes=[ANG.res])
        TWO_PI = 2.0 * math.pi
        C1 = 6.28125
        C2 = TWO_PI - C1
        PI_SAFE = 3.1415925
        for dst, shift in ((SIN, 0.0), (COS, 0.5 * math.pi)):
            self.ts("dve", A2[:, :], ANG[:, :], float(shift), ALU.add, reads=[ANG.res], writes=[A2.res])
            self.ts("dve", KI[:, :], A2[:, :], 1.0 / TWO_PI, ALU.mult, reads=[A2.res], writes=[KI.res])
            self.cp("dve", KF[:, :], KI[:, :], reads=[KI.res], writes=[KF.res])
            self.stt(A2[:, :], KF[:, :], -C1, A2[:, :], ALU.mult, ALU.add, reads=[KF.res, A2.res], writes=[A2.res])
            self.stt(A2[:, :], KF[:, :], -C2, A2[:, :], ALU.mult, ALU.add, reads=[KF.res, A2.res], writes=[A2.res])
            self.ts("dve", A2[:, :], A2[:, :], -PI_SAFE, ALU.max, reads=[A2.res], writes=[A2.res], s2=PI_SAFE,
                    op1=ALU.min)
            self.actf(dst[:, :], A2[:, :], AF.Sin, reads=[A2.res], writes=[dst.res])
        self.arelease(mark1)
        Q32 = [self.aalloc([512], F32) for _ in range(2)]
        TA = [self.aalloc([512], F32) for _ in range(2)]
        TB = [self.aalloc([512], F32) for _ in range(2)]
        KR = [self.aalloc([512], BF16) for _ in range(2)]
        wbuf = [self.aalloc([8, 128], BF16) for _ in range(3)]
        WV = self.aalloc([8, 256], BF16)
        EC = [self.aalloc([512], BF16) for _ in range(2)]
        EP = [self.aalloc([512], BF16) for _ in range(2)]
        DEN = [self.aalloc([4], F32) for _ in range(2)]
        S.dma("pool", WV[:, :, :], w["win_tm"][:, 1040:1296].rearrange("(k p) n -> p k n", p=128), writes=[WV.res])
        st = {"n": 0}

        def rope(i, tt, pss, is_k):
            n = st["n"]
            st["n"] += 1
            sl = slice(tt * 512, (tt + 1) * 512)
            q32, ta, tb, pr = Q32[n % 2], TA[n % 2], TB[n % 2], P[2 + n % 2]
            self.cp("act", q32[:, :], pss[0][:, :], reads=[pss[0].res], writes=[q32.res])
            self.mm(pr[:, :], self.cF(K_ROT), q32[:, :], True, True, reads=[CST.res, q32.res], writes=[pr.res])
            self.tt("dve", ta[:, :], q32[:, :], COS[:, sl], ALU.mult, reads=[q32.res, COS.res], writes=[ta.res])
            self.tt("dve", tb[:, :], pr[:, :], SIN[:, sl], ALU.mult, reads=[pr.res, SIN.res], writes=[tb.res])
            if not is_k:
                self.tt("pool", QR[:, i, sl], ta[:, :], tb[:, :], ALU.add, reads=[ta.res, tb.res],
                        writes=[(QR.res, tt)])
            else:
                kr = KR[n % 2]
                self.tt("pool", kr[:, :], ta[:, :], tb[:, :], ALU.add, reads=[ta.res, tb.res], writes=[kr.res])
                for half in range(2):
                    h = 2 * i + half
                    pd = P[4 + half]
                    self.mm(pd[:, :], self.cB(K_DUP0 + half * 128), kr[:, :], True, True, reads=[CSB.res, kr.res],
                            writes=[pd.res])
                    self.cp("act" if half == 0 else "dve", KD[:, h, 128 + tt * 512:128 + (tt + 1) * 512], pd[:, :],
                            reads=[pd.res], writes=[KD.res])

        self.proj_fm([(w["win_fm"], FM_Q, 8, self.xt_rhs)], 8, lambda i, tt, pss: rope(i, tt, pss, False), [wbuf],
                     [[P[0], P[1]]])
        self.proj_fm([(w["win_fm"], FM_K, 8, self.xt_rhs)], 2, lambda i, tt, pss: rope(i, tt, pss, True), [wbuf],
                     [[P[0], P[1]]])
        self.memset("pool", VA[:, :, :, 64:65], 1.0, [VA.res])
        if not first:
            self.cp("pool", KD[:, :, 0:128], self.KCAR[l][:, :, :], reads=[self.KCAR[l].res], writes=[KD.res])
            self.cp("pool", VA[:, 0, :, :], self.VCAR[l][:, :, :], reads=[self.VCAR[l].res], writes=[VA.res])
        for blk in range(NB):
            pv = P[6 + blk % 2]
            for k in range(8):
                self.mm(pv[:, 0:256], self.XT[:, k, blk * 128:(blk + 1) * 128], WV[:, k, :], k == 0, k == 7,
                        reads=[(self.XT.res, blk // 4), WV.res], writes=[pv.res])
            self.cp("act", VA[:, 1 + blk, :, 0:64], pv.reshape([128, 8, 64])[:, 0:4, :], reads=[pv.res],
                    writes=[VA.res])
        self.dbg("QR", QR)
        self.dbg("KD", KD)
        n = 0
        for qb in range(NB):
            gfirst = first and qb == 0
            for h in range(4):
                po = P[4 + n % 2]
                ec, ep, den = EC[n % 2], EP[n % 2], DEN[n % 2]
                n += 1
                qsl = slice(qb * 128, (qb + 1) * 128)
                ec3, ep3 = ec.reshape([128, 4, 128]), ep.reshape([128, 4, 128])
                for r in range(4):
                    hq = 4 * h + r
                    ch, hf = hq // 2, hq % 2
                    ps_ = slice(hf * 64, hf * 64 + 64)
                    sc, sp = P[hf], P[2 + hf]
                    cs_ = slice((r // 2) * 128, (r // 2 + 1) * 128)
                    self.mm(sc[:, cs_], KD[ps_, h, 128 + qb * 128:128 + (qb + 1) * 128],
                            QR[ps_, ch, qsl], True, True, reads=[KD.res, (QR.res, qb // 4)], writes=[sc.res])
                    if not gfirst:
                        self.mm(sp[:, cs_], KD[ps_, h, qb * 128:(qb + 1) * 128],
                                QR[ps_, ch, qsl], True, True, reads=[KD.res, (QR.res, qb // 4)], writes=[sp.res])
                for hf in range(2):
                    self.actf(ec3[:, hf::2, :], P[hf].reshape([128, 4, 128])[:, 0:2, :], AF.Exp, reads=[P[hf].res],
                              writes=[ec.res], scale=0.125)
                self.tt("pool", ec3[:, :, :], ec3[:, :, :],
                        CSB.bc(K_TU, [[0, 4], [1, 128]]), ALU.mult, reads=[ec.res, CSB.res], writes=[ec.res])
                if not gfirst:
                    for hf in range(2):
                        self.actf(ep3[:, hf::2, :], P[2 + hf].reshape([128, 4, 128])[:, 0:2, :], AF.Exp,
                                  reads=[P[2 + hf].res], writes=[ep.res], scale=0.125)
                    self.tt("pool", ep3[:, :, :], ep3[:, :, :],
                            CSB.bc(K_TL, [[0, 4], [1, 128]]), ALU.mult, reads=[ep.res, CSB.res], writes=[ep.res])
                po3 = po.reshape([128, 4, 128])
                for r in range(4):
                    if not gfirst:
                        self.mm(po3[:, r, 0:65], ep[:, r * 128:(r + 1) * 128], VA[:, qb, h, :], True, False,
                                reads=[ep.res, VA.res], writes=[po.res])
                    self.mm(po3[:, r, 0:65], ec[:, r * 128:(r + 1) * 128], VA[:, qb + 1, h, :], gfirst, True,
                            reads=[ec.res, VA.res], writes=[po.res])
                self.tt("dve", den[:, :], po3[:, :, 64], ROWX.bc(16 + 4 * h, [[1, 4]]), ALU.add,
                        reads=[po.res, ROWX.res], writes=[den.res])
                self.S.dve(lambda e, den=den: e.reciprocal(den[:, :], den[:, :]), reads=[den.res], writes=[den.res])
                self.tt("dve", AO.reshape([128, NB, 16, 64])[:, qb, 4 * h:4 * h + 4, :], po3[:, :, 0:64],
                        den.bc(0, [[1, 4], [0, 64]]), ALU.mult, reads=[po.res, den.res], writes=[(AO.res, qb)])
        for blk in range(NB):
            pb = PB[6 + blk % 2].reshape([128, 8, 128])
            for m in range(8):
                self.tr(pb[:, m, :], AO[:, blk, m * 128:(m + 1) * 128], self.cB(K_ID), reads=[(AO.res, blk), CSB.res],
                        writes=[pb.res])
            self.cp("act" if blk % 2 else "dve", AT[:, :, blk * 128:(blk + 1) * 128], pb[:, :, :], reads=[pb.res],
                    writes=[(AT.res, blk // 4)])
        self.cp("pool", self.KCAR[l][:, :, :], KD[:, :, T:T + 128], reads=[KD.res], writes=[self.KCAR[l].res])
        self.cp("pool", self.VCAR[l][:, :, :], VA[:, NB, :, :], reads=[VA.res], writes=[self.VCAR[l].res])
        self.dbg("AT", AT)
        self.arelease(mark0)
        self.gated_out(l, 2, AT, "wattn", self.mt_first)
        self.mt_first = False

    def moe(self, l):
        T, NT, NB, P, S = self.T, self.NT, self.NB, self.P, self.S
        w = self.dw[l]
        ROWS, CST = self.ROWS[l], self.CST
        WR = self.aalloc([8, 36], BF16)
        LOG = self.aalloc([NB, 36], F32)
        WTOK = self.aalloc([NB, 4, 8], F32)
        WT = self.aalloc([T], F32)
        S.dma("pool", WR[:, :, :], w["wr"].rearrange("(k p) n -> p k n", p=128), writes=[WR.res])
        for blk in range(NB):
            pl = P[6 + blk % 2]
            for k in range(8):
                self.mm(pl[:, 0:36], self.XT[:, k, blk * 128:(blk + 1) * 128], WR[:, k, :], k == 0, k == 7,
                        reads=[(self.XT.res, blk // 4), WR.res], writes=[pl.res])
            self.tt("dve", LOG[:, blk, :], pl[:, 0:36], ROWS.bc(R_RB, [[1, 36]]), ALU.add, reads=[pl.res, ROWS.res],
                    writes=[LOG.res])
        mk = self.amark()
        f = lambda shp: self.aalloc(shp, F32)
        GMAX, GS, GE, GMASK = f([NB]), f([NB]), f([NB, 4]), f([NB, 4])
        V1, V2, P1, P2 = f([NB, 4]), f([NB, 4]), f([NB, 4]), f([NB, 4])
        M1, M2, E2 = f([NB, 4, 8]), f([NB, 4, 8]), f([NB, 4, 8])
        GL = lambda: LOG[:, :, 0:4]
        EL = lambda: LOG.bc(4, [[36, NB], [8, 4], [1, 8]])
        red = lambda out, in_, op, rd, wr: self.S.dve(
            lambda e: e.tensor_reduce(out=out, in_=in_, op=op, axis=AX.X), reads=rd, writes=wr)
        red(GMAX[:, :], GL(), ALU.max, [LOG.res], [GMAX.res])
        self.tt("dve", GE[:, :, :], GL(), GMAX.bc(0, [[1, NB], [0, 4]]), ALU.subtract, reads=[LOG.res, GMAX.res],
                writes=[GE.res])
        self.tt("dve", GMASK[:, :, :], GL(), GMAX.bc(0, [[1, NB], [0, 4]]), ALU.is_equal, reads=[LOG.res, GMAX.res],
                writes=[GMASK.res])
        self.actf(GE[:, :, :], GE[:, :, :], AF.Exp, reads=[GE.res], writes=[GE.res])
        red(GS[:, :], GE[:, :, :], ALU.add, [GE.res], [GS.res])
        self.S.dve(lambda e: e.reciprocal(GS[:, :], GS[:, :]), reads=[GS.res], writes=[GS.res])
        self.tt("dve", GMASK[:, :, :], GMASK[:, :, :], GS.bc(0, [[1, NB], [0, 4]]), ALU.mult,
                reads=[GMASK.res, GS.res], writes=[GMASK.res])
        red(V1[:, :, :], EL(), ALU.max, [LOG.res], [V1.res])
        self.tt("dve", M1[:, :, :, :], EL(), V1.bc(0, [[4, NB], [1, 4], [0, 8]]), ALU.is_equal,
                reads=[LOG.res, V1.res], writes=[M1.res])
        self.stt(E2[:, :, :, :], M1[:, :, :, :], -1.0e30, EL(), ALU.mult, ALU.add, reads=[M1.res, LOG.res],
                 writes=[E2.res])
        red(V2[:, :, :], E2[:, :, :, :], ALU.max, [E2.res], [V2.res])
        self.tt("dve", M2[:, :, :, :], E2[:, :, :, :], V2.bc(0, [[4, NB], [1, 4], [0, 8]]), ALU.is_equal,
                reads=[E2.res, V2.res], writes=[M2.res])
        self.tt("dve", P1[:, :, :], V1[:, :, :], V2[:, :, :], ALU.subtract, reads=[V1.res, V2.res], writes=[P1.res])
        self.actf(P1[:, :, :], P1[:, :, :], AF.Sigmoid, reads=[P1.res], writes=[P1.res])
        self.ts("dve", P2[:, :, :], P1[:, :, :], -1.0, ALU.mult, reads=[P1.res], writes=[P2.res], s2=1.0, op1=ALU.add)
        self.tt("dve", P1[:, :, :], P1[:, :, :], GMASK[:, :, :], ALU.mult, reads=[P1.res, GMASK.res], writes=[P1.res])
        self.tt("dve", P2[:, :, :], P2[:, :, :], GMASK[:, :, :], ALU.mult, reads=[P2.res, GMASK.res], writes=[P2.res])
        self.tt("dve", M1[:, :, :, :], M1[:, :, :, :], P1.bc(0, [[4, NB], [1, 4], [0, 8]]), ALU.mult,
                reads=[M1.res, P1.res], writes=[M1.res])
        self.tt("dve", M2[:, :, :, :], M2[:, :, :, :], P2.bc(0, [[4, NB], [1, 4], [0, 8]]), ALU.mult,
                reads=[M2.res, P2.res], writes=[M2.res])
        self.tt("dve", WTOK[:, :, :, :], M1[:, :, :, :], M2[:, :, :, :], ALU.add, reads=[M1.res, M2.res],
                writes=[WTOK.res])
        self.dbg("WTOK", WTOK)
        wt2 = WTOK.reshape([128, NB, 32])
        for b0 in range(0, NB, 4):
            ps = P[7]
            for j in range(4):
                self.tr(ps[0:32, j * 128:(j + 1) * 128], wt2[:, b0 + j, :], self.cF(K_ID), reads=[WTOK.res, CST.res],
                        writes=[ps.res])
            self.cp("act", WT[0:32, b0 * 128:(b0 + 4) * 128], ps[0:32, :], reads=[ps.res], writes=[WT.res])
        self.arelease(mk)
        NWB = 3
        WG = [self.aalloc([4, 8, 128], BF16) for _ in range(NWB)]
        WU = [self.aalloc([4, 8, 128], BF16) for _ in range(NWB)]
        WD = [self.aalloc([4, 1024], BF16) for _ in range(NWB)]
        HT = [self.aalloc([4, 512], BF16) for _ in range(2)]
        SIL = [self.aalloc([512], F32) for _ in range(2)]
        WBC = [self.aalloc([512], F32) for _ in range(2)]
        WM = [self.aalloc([512], F32) for _ in range(2)]

        def load_expert(e):
            wg, wu, wd = WG[e % NWB], WU[e % NWB], WD[e % NWB]
            S.dma("pool", wg.bc(0, [[1024, 4], [1, 1024]]), w["wg"][e].rearrange("p m k c -> p m (k c)"),
                  writes=[wg.res])
            S.dma("pool", wu.bc(0, [[1024, 4], [1, 1024]]), w["wu"][e].rearrange("p m k c -> p m (k c)"),
                  writes=[wu.res])
            S.dma("pool", wd[:, :, :], w["wd"][e].rearrange("(k p) n -> p k n", p=128), writes=[wd.res])

        PY = [P[4], P[5], P[6]]
        items = [(e, tt) for e in range(self.nexp) for tt in range(NT)]
        st = {"dc": 0}

        def gate_up(n, e, tt):
            wg, wu = WG[e % NWB], WU[e % NWB]
            sl = slice(tt * 512, (tt + 1) * 512)
            ht, wbc, wm = HT[n % 2], WBC[n % 2], WM[n % 2]
            self.ts("pool", wm[0:32, :], WT[0:32, sl], self.cF(K_ID + e, 1, npart=32), ALU.mult,
                    reads=[WT.res, CST.res], writes=[wm.res])
            self.mm(P[7][:, :], self.cF(K_ONE, 128, npart=32), wm[0:32, :], True, True, reads=[CST.res, wm.res],
                    writes=[P[7].res])
            self.cp("act", wbc[:, :], P[7][:, :], reads=[P[7].res], writes=[wbc.res])
            for m in range(4):
                pg, pu = P[m % 2], P[2 + m % 2]
                sil = SIL[m % 2]
                for k in range(8):
                    self.mm(pg[:, :], wg[:, m, k, :], self.XT[:, k, sl], k == 0, k == 7,
                            reads=[wg.res, (self.XT.res, tt)], writes=[pg.res])
                for k in range(8):
                    self.mm(pu[:, :], wu[:, m, k, :], self.XT[:, k, sl], k == 0, k == 7,
                            reads=[wu.res, (self.XT.res, tt)], writes=[pu.res])
                self.actf(sil[:, :], pg[:, :], AF.Silu, reads=[pg.res], writes=[sil.res])
                self.tt("dve", sil[:, :], pu[:, :], sil[:, :], ALU.mult, reads=[pu.res, sil.res], writes=[sil.res])
                self.tt("dve", ht[:, m, :], sil[:, :], wbc[:, :], ALU.mult, reads=[sil.res, wbc.res],
                        writes=[ht.res])

        def down(n, e, tt):
            wd = WD[e % NWB]
            sl = slice(tt * 512, (tt + 1) * 512)
            ht = HT[n % 2]
            for dc in range(8):
                py = PY[st["dc"] % 3]
                st["dc"] += 1
                for m in range(4):
                    self.mm(py[:, :], wd[:, m, dc * 128:(dc + 1) * 128], ht[:, m, :], m == 0, m == 3,
                            reads=[wd.res, ht.res], writes=[py.res])
                self.tt("dve", self.X[:, dc, sl], py[:, :], self.X[:, dc, sl], ALU.add,
                        reads=[py.res, (self.X.res, tt)], writes=[(self.X.res, tt)])

        for e in range(min(NWB - 1, self.nexp)):
            load_expert(e)
        for n, (e, tt) in enumerate(items):
            gate_up(n, e, tt)
            if n >= 1:
                pe_, ptt = items[n - 1]
                down(n - 1, pe_, ptt)
                if ptt == NT - 1 and pe_ + NWB < self.nexp + 0 and pe_ + NWB - 1 < self.nexp:
                    pass
            if tt == 0 and e + NWB - 1 < self.nexp and e >= 1:
                pass
            if n >= 1 and items[n - 1][1] == NT - 1:
                nxt = items[n - 1][0] + NWB
                if nxt < self.nexp:
                    load_expert(nxt)
            if n == 0 and NWB - 1 < self.nexp:
                load_expert(NWB - 1)
        down(len(items) - 1, *items[-1])


def host_consts():
    c = np.zeros((128, NCF), np.float32)
    k = np.arange(128)[:, None]
    m = np.arange(128)[None, :]
    c[:, K_ID:K_ID + 128] = (k == m)
    c[:, K_TU:K_TU + 128] = (k <= m)
    c[:, K_TL:K_TL + 128] = (k > m)
    c[:, K_ONE:K_ONE + 128] = 1.0
    R = np.zeros((128, 128), np.float32)
    D0 = np.zeros((128, 128), np.float32)
    D1 = np.zeros((128, 128), np.float32)
    for mm_ in range(128):
        b, j = mm_ // 64 * 64, mm_ % 64
        if j < 32:
            R[b + j + 32, mm_] = -1.0
        else:
            R[b + j - 32, mm_] = 1.0
        D0[j, mm_] = 1.0
        D1[64 + j, mm_] = 1.0
    c[:, K_ROT:K_ROT + 128] = R
    c[:, K_DUP0:K_DUP0 + 128] = D0
    c[:, K_DUP1:K_DUP1 + 128] = D1
    invf = (np.float32(10000.0) ** (-np.arange(32, dtype=np.float32) / np.float32(32))).astype(np.float32)
    c[:, K_INVF] = invf[np.arange(128) % 32]
    c[:, K_EPS] = EPS
    c[:, K_ONEC] = 1.0
    return c


def fm_layout(w):
    K, N = w.shape
    return np.ascontiguousarray(w.reshape(K // 128, 128, N // 128, 128).transpose(2, 1, 0, 3))


def colsT(v, n):
    return np.asarray(v, np.float32).reshape(n, 128).T


def prep_weights(inp, l):
    f = lambda n: np.asarray(inp[n][l], np.float32)
    w_in = f("w_in")
    o = {}
    o["win_fm%d" % l] = fm_layout(np.concatenate(
        [w_in[:, 0:3072], w_in[:, 4096:6144], w_in[:, 6160:8208], w_in[:, 8208:9232], w_in[:, 9232:9488]], axis=1))
    o["win_tm%d" % l] = np.ascontiguousarray(np.concatenate(
        [w_in[:, 3072:4096], w_in[:, 6144:6160], w_in[:, 9488:9744]], axis=1))
    for nm, src in (("wssd", "ssd_w_out"), ("wconf", "conf_w_out"), ("wattn", "attn_w_out"), ("wout", "w_out"),
                    ("wpg", "ple_w_gate"), ("wpp", "ple_w_proj")):
        o["%s%d" % (nm, l)] = fm_layout(f(src))
    o["wr%d" % l] = np.ascontiguousarray(np.concatenate([f("moe_w_group"), f("moe_w_expert")], axis=1))
    wg, wu = f("moe_w_gate"), f("moe_w_up")
    o["wg%d" % l] = np.ascontiguousarray(wg.reshape(NEXP, 8, 128, 4, 128).transpose(0, 2, 3, 1, 4))
    o["wu%d" % l] = np.ascontiguousarray(wu.reshape(NEXP, 8, 128, 4, 128).transpose(0, 2, 3, 1, 4))
    o["wd%d" % l] = np.ascontiguousarray(f("moe_w_down"))
    cols = np.zeros((128, NCOLS), np.float32)
    cols[:, C_BGATE:C_BGATE + 24] = colsT(f("b_gate"), 24)
    cols[:, C_SCW:C_SCW + 64] = f("ssd_conv_w").reshape(4, 16, 128).transpose(2, 1, 0).reshape(128, 64)
    cols[:, C_SCB:C_SCB + 16] = colsT(f("ssd_conv_b"), 16)
    cols[:, C_CDW:C_CDW + 248] = f("conf_dw_w").reshape(31, 8, 128).transpose(2, 1, 0).reshape(128, 248)
    for c0, nm in ((C_CDB, "conf_dw_b"), (C_CLG, "conf_ln_g"), (C_CLB, "conf_ln_b"), (C_NW, "ssd_norm_w"),
                   (C_L1G, "ln1_g"), (C_L1B, "ln1_b"), (C_L2G, "ln2_g"), (C_L2B, "ln2_b")):
        cols[:, c0:c0 + 8] = colsT(f(nm), 8)
    o["cols%d" % l] = cols
    rows = np.zeros((1, NROWS), np.float32)
    rows[0, R_DTB:R_DTB + 16] = f("ssd_dt_bias")
    rows[0, R_ALOG:R_ALOG + 16] = f("ssd_a_log")
    rows[0, R_D:R_D + 16] = f("ssd_d")
    rows[0, R_SINK:R_SINK + 16] = f("attn_sinks")
    rows[0, R_RB:R_RB + 4] = f("moe_b_group")
    rows[0, R_RB + 4:R_RB + 36] = f("moe_b_expert")
    o["rows%d" % l] = rows
    return o


def make_in_maps(inp, seq_lists, nlayer=DEPTH):
    shared = {"cst": host_consts()}
    for l in range(nlayer):
        shared.update(prep_weights(inp, l))
    maps = []
    for seqs in seq_lists:
        m = dict(shared)
        m["x"] = np.ascontiguousarray(np.asarray(inp["x"], np.float32)[seqs])
        m["p"] = np.ascontiguousarray(np.asarray(inp["p"], np.float32)[:, seqs])
        m["pos"] = np.ascontiguousarray(np.asarray(inp["positions"], np.int32)[seqs])
        maps.append(m)
    return maps


_NC_CACHE = {}


def kernel(**inputs):
    if "nc" not in _NC_CACHE:
        _NC_CACHE["nc"] = Builder().nc
    nc = _NC_CACHE["nc"]
    seq_lists = [list(range(c * SEQ_PER_CORE, (c + 1) * SEQ_PER_CORE)) for c in range(NCORES)]
    maps = make_in_maps(inputs, seq_lists)
    res = run_bass_kernel_spmd(nc, maps, core_ids=list(range(NCORES)))
    out = np.concatenate([np.asarray(r["out"], np.float32) for r in res.results], axis=0)
    return out
```

```python
import contextlib
import math
import numpy as np
import concourse.bass as bass
import concourse.mybir as mybir
from concourse.bass_utils import run_bass_kernel_spmd

F32 = mybir.dt.float32
BF16 = mybir.dt.bfloat16
I32 = mybir.dt.int32
AF = mybir.ActivationFunctionType
ALU = mybir.AluOpType
AX = mybir.AxisListType

ENGS = ("pe", "act", "dve", "pool", "sp")
NDMASEM = 8

D = 1024
SEQ = 2048
DEPTH = 2
NCORES = 8
SEQ_PER_CORE = 4
PLE = 256
NEXP = 32
FF = 512
ALPHA = (2 * DEPTH) ** 0.25
EPS = 1e-5
OFF_GATE, OFF_Z, OFF_XBC, OFF_DT, OFF_CONF, OFF_Q, OFF_K, OFF_V = 0, 3072, 4096, 6144, 6160, 8208, 9232, 9488
FM_GATE, FM_XBC, FM_CONF, FM_Q, FM_K = 0, 24, 40, 56, 64
NFM = 66
C_BGATE = 0
C_SCW = 24
C_SCB = 88
C_CDW = 104
C_CDB = 352
C_CLG = 360
C_CLB = 368
C_NW = 376
C_L1G = 384
C_L1B = 392
C_L2G = 400
C_L2B = 408
NCOLS = 416
R_DTB, R_ALOG, R_D, R_SINK, R_RB = 0, 16, 32, 48, 64
NROWS = 100
K_ID, K_TU, K_TL, K_ONE, K_ROT, K_DUP0, K_DUP1, K_INVF, K_EPS, K_ONEC = 0, 128, 256, 384, 512, 640, 768, 896, 897, 898
NCF = 900


class Op:
    __slots__ = ("eng", "fn", "dma", "idx", "tick", "sem_i", "sem_v", "deps", "prewait")


class Sched:
    def __init__(self, nc):
        self.nc = nc
        self.ops = []
        self.state = {}
        self.cnt = {e: 0 for e in ENGS}
        self.dcnt = {e: 0 for e in ENGS}
        self.dma_hist = {e: [] for e in ENGS}
        self.last = {e: None for e in ENGS}
        self.pending = {e: set() for e in ENGS}
        self.exclusive = set()

    def fence(self):
        F = set()
        for e in ENGS:
            if self.last[e] is not None:
                F.add(self.last[e])
            for op in self.dma_hist[e][-NDMASEM:]:
                F.add(op)
        for e in ENGS:
            self.pending[e] |= F

    @staticmethod
    def _norm(lst):
        out = []
        for r in lst:
            if isinstance(r, tuple):
                out.append((id(r[0]), r[1]))
            else:
                out.append((id(r), None))
        return out

    def _conf(self, res):
        b, k = res
        d = self.state.get(b)
        if d is None:
            return
        if k is None:
            for st in d.values():
                yield st
        else:
            st = d.get(k)
            if st is not None:
                yield st
            st = d.get(None)
            if st is not None:
                yield st

    def add(self, eng, fn, reads=(), writes=(), dma=False):
        reads = self._norm(reads)
        writes = self._norm(writes)
        if self.exclusive:
            ex = [r for r in reads if r[0] in self.exclusive]
            if ex:
                reads = [r for r in reads if r[0] not in self.exclusive]
                writes = writes + [r for r in ex if r not in writes]
        op = Op()
        op.eng, op.fn, op.dma, op.prewait = eng, fn, dma, None
        op.idx = len(self.ops)
        deps = set()
        for r in reads:
            for st in self._conf(r):
                if st[0] is not None:
                    deps.add(st[0])
        for w in writes:
            for st in self._conf(w):
                if st[0] is not None:
                    deps.add(st[0])
                deps.update(st[1])
        if self.pending[eng]:
            deps |= self.pending[eng]
            self.pending[eng] = set()
        op.deps = deps
        for w in writes:
            b, k = w
            d = self.state.setdefault(b, {})
            if k is None:
                d.clear()
            d[k] = [op, []]
        for r in reads:
            b, k = r
            d = self.state.setdefault(b, {})
            st = d.get(k)
            if st is None:
                st = [None, []]
                for s2 in self._conf(r):
                    if s2[0] is not None and (st[0] is None or s2[0].idx > st[0].idx):
                        st[0] = s2[0]
                d[k] = st
            st[1].append(op)
        if dma:
            i = self.dcnt[eng]
            self.dcnt[eng] += 1
            op.sem_i = i % NDMASEM
            op.sem_v = 16 * (i // NDMASEM + 1)
            hist = self.dma_hist[eng]
            if i >= NDMASEM:
                op.prewait = hist[i - NDMASEM]
            hist.append(op)
            op.tick = None
        else:
            self.cnt[eng] += 1
            op.tick = self.cnt[eng]
            self.last[eng] = op
        self.ops.append(op)
        return op

    def pe(self, fn, reads=(), writes=()):
        return self.add("pe", fn, reads, writes)

    def act(self, fn, reads=(), writes=()):
        return self.add("act", fn, reads, writes)

    def dve(self, fn, reads=(), writes=()):
        return self.add("dve", fn, reads, writes)

    def pool(self, fn, reads=(), writes=()):
        return self.add("pool", fn, reads, writes)

    def dma(self, eng, out, in_, reads=(), writes=()):
        return self.add(eng, lambda e: e.dma_start(out=out, in_=in_), reads, writes, dma=True)

    def emit(self):
        nc = self.nc
        with contextlib.ExitStack() as es:
            esem = {e: es.enter_context(nc.semaphore("s_" + e)) for e in ENGS}
            dsem = {e: [es.enter_context(nc.semaphore("d_%s%d" % (e, i))) for i in range(NDMASEM)]
                    for e in ENGS if self.dcnt[e] > 0}
            block = es.enter_context(nc.Block())
            per = {e: [op for op in self.ops if op.eng == e] for e in ENGS}

            def run(engname, eng):
                waited = {}

                def wait_for(dep):
                    if dep.dma:
                        s = dsem[dep.eng][dep.sem_i]
                        v = dep.sem_v
                    else:
                        if dep.eng == "pe" and engname == "pe":
                            return
                        s = esem[dep.eng]
                        v = dep.tick
                    key = id(s)
                    if waited.get(key, 0) >= v:
                        return
                    waited[key] = v
                    eng.wait_ge(s, v)

                for op in per[engname]:
                    for dep in sorted(op.deps, key=lambda d: d.idx):
                        wait_for(dep)
                    if op.prewait is not None:
                        wait_for(op.prewait)
                    inst = op.fn(eng)
                    if op.dma:
                        inst.then_inc(dsem[engname][op.sem_i], 16)
                    else:
                        inst.then_inc(esem[engname], 1)
                for op in self.dma_hist[engname][-NDMASEM:]:
                    wait_for(op)

            @block.sync
            def _(eng):
                run("sp", eng)

            @block.tensor
            def _(eng):
                run("pe", eng)

            @block.scalar
            def _(eng):
                run("act", eng)

            @block.vector
            def _(eng):
                run("dve", eng)

            @block.gpsimd
            def _(eng):
                run("pool", eng)


def pstride(t):
    return int(np.prod(list(t.shape)[1:]))


class View:
    def __init__(self, h, off, shape, res=None):
        self.h, self.off, self.shape = h, off, list(shape)
        self.dtype = h.dtype
        st, s = [], 1
        for n in reversed(self.shape[1:]):
            st.append(s)
            s *= n
        self.strides = list(reversed(st))
        self.ps = pstride(h)
        self.res = self if res is None else res

    def __getitem__(self, idx):
        if not isinstance(idx, tuple):
            idx = (idx,)
        idx = list(idx) + [slice(None)] * (len(self.shape) - len(idx))
        p = idx[0]
        p0, p1, _ = p.indices(self.shape[0])
        off = self.off
        dims = []
        for i, ix in enumerate(idx[1:]):
            stride, n = self.strides[i], self.shape[i + 1]
            if isinstance(ix, int):
                off += ix * stride
            else:
                a, b, st = ix.indices(n)
                cnt = len(range(a, b, st))
                off += a * stride
                dims.append([stride * st, cnt])
        m = [list(d) for d in dims]
        if not m:
            m = [[1, 1]]
        return bass.AP(self.h, p0 * self.ps + off, [[self.ps, p1 - p0]] + m)

    def bc(self, off, dims, npart=128, p0=0):
        return bass.AP(self.h, p0 * self.ps + self.off + off, [[self.ps, npart]] + [list(d) for d in dims])

    def reshape(self, shape):
        return View(self.h, self.off, shape, res=self.res)


class Builder:
    def __init__(self, nseq=SEQ_PER_CORE, nlayer=DEPTH, T=1024, nseg=None, debug=(), nexp=NEXP, stop_after=None,
                 only=None):
        self.nseq, self.nlayer, self.T = nseq, nlayer, T
        self.NT = T // 512
        self.NB = T // 128
        self.nseg = (SEQ // T) if nseg is None else nseg
        self.debug = set(debug)
        self.nexp = nexp
        self.stop_after = stop_after
        self.only = None if only is None else set(only.split(',')) if isinstance(only, str) else set(only)
        self.dbg_outs = {}
        nc = self.nc = bass.Bass("TRN2", target_bir_lowering=False)
        self.S = Sched(nc)
        self.declare_dram()
        self.alloc()
        self.program()
        self.S.emit()

    def din(self, name, shape, dt=F32):
        return self.nc.dram_tensor(name, list(shape), dt, kind="ExternalInput").ap()

    def pers(self, name, shape, dt):
        h = self.nc.alloc_sbuf_tensor(name, list(shape), dt)
        return View(h, 0, shape)

    def areset(self):
        self.S.fence()
        self.aoff = 0

    def aalloc(self, fshape, dt, npart=128):
        size = {F32: 4, BF16: 2, I32: 4}[dt]
        n = int(np.prod(fshape)) * size
        n = (n + 31) // 32 * 32
        off = self.aoff
        self.aoff += n
        assert self.aoff <= self.arena_bytes, ("arena overflow", self.aoff, self.arena_bytes)
        return View(self.arena[dt], off // size, [npart] + list(fshape))

    def dbg(self, name, view, reads=None):
        if name not in self.debug or name in self.dbg_outs:
            return
        o = self.nc.dram_tensor("dbg_" + name, list(view.shape), view.dtype, kind="ExternalOutput").ap()
        self.dbg_outs[name] = o
        self.S.dma("sp", o, view[:], reads=[view.res] if reads is None else reads)

    def declare_dram(self):
        L = self.nlayer
        self.d_x = self.din("x", [self.nseq, SEQ, D])
        self.d_p = self.din("p", [DEPTH, self.nseq, SEQ, PLE])
        self.d_pos = self.din("pos", [self.nseq, SEQ], I32)
        self.d_cst = self.din("cst", [128, NCF])
        self.d_out = self.nc.dram_tensor("out", [self.nseq, SEQ, D], F32, kind="ExternalOutput").ap()
        self.dw = []
        for l in range(L):
            w = {}
            w["win_fm"] = self.din("win_fm%d" % l, [NFM, 128, 8, 128])
            w["win_tm"] = self.din("win_tm%d" % l, [D, 1296])
            for nm in ("wssd", "wconf", "wattn", "wout", "wpg"):
                w[nm] = self.din("%s%d" % (nm, l), [8, 128, 8, 128])
            w["wpp"] = self.din("wpp%d" % l, [8, 128, 2, 128])
            w["wr"] = self.din("wr%d" % l, [D, 36])
            w["wg"] = self.din("wg%d" % l, [NEXP, 128, 4, 8, 128])
            w["wu"] = self.din("wu%d" % l, [NEXP, 128, 4, 8, 128])
            w["wd"] = self.din("wd%d" % l, [NEXP, FF, D])
            w["cols"] = self.din("cols%d" % l, [128, NCOLS])
            w["rows"] = self.din("rows%d" % l, [1, NROWS])
            self.dw.append(w)

    def alloc(self):
        T, nc, L = self.T, self.nc, self.nlayer
        pers = self.pers
        self.X = pers("X", [128, 8, T], F32)
        self.XT = pers("XT", [128, 8, T], BF16)
        self.MT = pers("MT", [128, 8, T], BF16)
        self.CST = pers("CST", [128, NCF], F32)
        self.CSB = pers("CSB", [128, NCF], BF16)
        self.COLS = [pers("COLS%d" % l, [128, NCOLS], F32) for l in range(L)]
        self.COLA = [pers("COLA%d" % l, [128, 16], F32) for l in range(L)]
        self.ROWS = [pers("ROWS%d" % l, [128, NROWS], F32) for l in range(L)]
        self.ROWX = [pers("ROWX%d" % l, [128, 32], F32) for l in range(L)]
        self.H = [pers("H%d" % l, [128, 1024], F32) for l in range(L)]
        self.CTAIL = [pers("CTAIL%d" % l, [128, 8, 32], BF16) for l in range(L)]
        self.STAIL = [pers("STAIL%d" % l, [128, 16, 4], BF16) for l in range(L)]
        self.KCAR = [pers("KCAR%d" % l, [128, 4, 128], BF16) for l in range(L)]
        self.VCAR = [pers("VCAR%d" % l, [128, 4, 65], BF16) for l in range(L)]
        ph = [nc.alloc_psum_tensor("P%d" % i, [128, 512], F32) for i in range(8)]
        self.P = [View(h, 0, [128, 512]) for h in ph]
        self.PB = [View(h.bitcast(BF16), 0, [128, 1024], res=v) for h, v in zip(ph, self.P)]
        self.S.exclusive = {id(v) for v in self.P}
        rem = nc.sbuf_bytes_remaining
        rem = rem() if callable(rem) else rem
        self.arena_bytes = (int(rem) - 8192) // 64 * 64
        ah = nc.alloc_sbuf_tensor("ARENA", [128, self.arena_bytes // 4], F32)
        self.arena = {F32: ah, BF16: ah.bitcast(BF16), I32: ah.bitcast(I32)}
        self.aoff = 0

    def mm(self, out, lhsT, rhs, start, stop, reads, writes):
        self.S.pe(lambda e: e.matmul(out, lhsT, rhs, start=start, stop=stop), reads, writes)

    def tr(self, out, in_, ident, reads, writes):
        self.S.pe(lambda e: e.transpose(out, in_, ident), reads, writes)

    def actf(self, out, in_, func, reads, writes, scale=1.0, bias=None):
        if bias is None:
            self.S.act(lambda e: e.activation(out=out, in_=in_, func=func, scale=scale), reads, writes)
        else:
            self.S.act(lambda e: e.activation(out=out, in_=in_, func=func, scale=scale, bias=bias), reads, writes)

    def tt(self, eng, out, in0, in1, op, reads, writes):
        self.S.add(eng, lambda e: e.tensor_tensor(out=out, in0=in0, in1=in1, op=op), reads, writes)

    def ts(self, eng, out, in0, s1, op0, reads, writes, s2=None, op1=None):
        if op1 is None:
            self.S.add(eng, lambda e: e.tensor_scalar(out=out, in0=in0, scalar1=s1, scalar2=None, op0=op0), reads, writes)
        else:
            self.S.add(eng, lambda e: e.tensor_scalar(out=out, in0=in0, scalar1=s1, scalar2=s2, op0=op0, op1=op1),
                       reads, writes)

    def stt(self, out, in0, scalar, in1, op0, op1, reads, writes):
        self.S.dve(lambda e: e.scalar_tensor_tensor(out=out, in0=in0, scalar=scalar, in1=in1, op0=op0, op1=op1),
                   reads, writes)

    def cp(self, eng, out, in_, reads, writes):
        if eng == "act":
            self.S.act(lambda e: e.copy(out, in_), reads, writes)
        else:
            self.S.add(eng, lambda e: e.tensor_copy(out, in_), reads, writes)

    def memset(self, eng, ap, val, writes):
        self.S.add(eng, lambda e: e.memset(ap, val), (), writes)

    def cF(self, off, n=128, npart=128, p0=0):
        return self.CST[p0:p0 + npart, off:off + n]

    def cB(self, off, n=128, npart=128, p0=0):
        return self.CSB[p0:p0 + npart, off:off + n]

    def col(self, l, c, npart=128):
        return self.COLS[l][0:npart, c:c + 1]

    def proj_fm(self, srcs, nchunk, consumer, wbufs, psums):
        NT = self.NT
        cnt = 0
        depth = min(len(wb) for wb in wbufs) - 1

        def issue(i):
            for si, (wsrc, c0, KC, rhs_fn) in enumerate(srcs):
                wb = wbufs[si][i % len(wbufs[si])]
                self.S.dma("pool", wb[:, 0:KC, :], wsrc[c0 + i], writes=[wb.res])

        for i in range(min(depth, nchunk)):
            issue(i)
        for i in range(nchunk):
            if i + depth < nchunk:
                issue(i + depth)
            wbs = [wbufs[si][i % len(wbufs[si])] for si in range(len(srcs))]
            for tt in range(NT):
                pss = []
                for si, (wsrc, c0, KC, rhs_fn) in enumerate(srcs):
                    ps = psums[si][cnt % len(psums[si])]
                    for k in range(KC):
                        rap, rres = rhs_fn(k, tt)
                        self.mm(ps[:, :], wbs[si][:, k, :], rap, k == 0, k == KC - 1,
                                reads=[wbs[si].res, rres], writes=[ps.res])
                    pss.append(ps)
                cnt += 1
                consumer(i, tt, pss)

    def xt_rhs(self, k, tt):
        return self.XT[:, k, tt * 512:(tt + 1) * 512], (self.XT.res, tt)

    def gated_out(self, l, branch, YT, wname, first):
        w = self.dw[l]
        wbA = [self.aalloc([8, 128], BF16) for _ in range(3)]
        wbB = [self.aalloc([8, 128], BF16) for _ in range(3)]
        SG = [self.aalloc([512], F32) for _ in range(2)]
        TM = [self.aalloc([512], BF16) for _ in range(2)]
        P = self.P
        st = {"n": 0}

        def yt_rhs(k, tt):
            return YT[:, k, tt * 512:(tt + 1) * 512], (YT.res, tt)

        def consumer(i, tt, pss):
            n = st["n"]
            st["n"] += 1
            sg = SG[n % 2]
            self.actf(sg[:, :], pss[1][:, :], AF.Sigmoid, reads=[pss[1].res], writes=[sg.res],
                      bias=self.col(l, C_BGATE + branch * 8 + i))
            dst = self.MT[:, i, tt * 512:(tt + 1) * 512]
            if first:
                self.tt("dve", dst, pss[0][:, :], sg[:, :], ALU.mult, reads=[pss[0].res, sg.res],
                        writes=[(self.MT.res, tt)])
            else:
                tm = TM[n % 2]
                self.tt("dve", tm[:, :], pss[0][:, :], sg[:, :], ALU.mult, reads=[pss[0].res, sg.res],
                        writes=[tm.res])
                self.tt("dve", dst, dst, tm[:, :], ALU.add, reads=[tm.res, (self.MT.res, tt)],
                        writes=[(self.MT.res, tt)])

        self.proj_fm([(w[wname], 0, 8, yt_rhs), (w["win_fm"], FM_GATE + branch * 8, 8, self.xt_rhs)], 8, consumer,
                     [wbA, wbB], [[P[0], P[1]], [P[2], P[3]]])

    def ln_fm(self, SRC, tt, out_fn, tmp):
        P = self.P
        SQ, MEAN, RSTD, TN = tmp
        sl = slice(tt * 512, (tt + 1) * 512)
        for m in range(8):
            self.mm(P[6][:, :], self.cF(K_ONE), SRC[:, m, sl], m == 0, m == 7,
                    reads=[self.CST.res, (SRC.res, tt)], writes=[P[6].res])
        for m in range(8):
            sq = SQ[m % 2]
            self.actf(sq[:, :], SRC[:, m, sl], AF.Square, reads=[(SRC.res, tt)], writes=[sq.res])
            self.mm(P[7][:, :], self.cF(K_ONE), sq[:, :], m == 0, m == 7, reads=[self.CST.res, sq.res],
                    writes=[P[7].res])
        self.ts("dve", MEAN[:, :], P[6][:, :], 1.0 / 1024, ALU.mult, reads=[P[6].res], writes=[MEAN.res])
        self.tt("dve", RSTD[:, :], MEAN[:, :], MEAN[:, :], ALU.mult, reads=[MEAN.res], writes=[RSTD.res])
        self.stt(RSTD[:, :], P[7][:, :], 1.0 / 1024, RSTD[:, :], ALU.mult, ALU.subtract, reads=[P[7].res, RSTD.res],
                 writes=[RSTD.res])
        self.actf(RSTD[:, :], RSTD[:, :], AF.Sqrt, reads=[RSTD.res], writes=[RSTD.res], bias=self.cF(K_EPS, 1))
        self.S.dve(lambda e: e.reciprocal(RSTD[:, :], RSTD[:, :]), reads=[RSTD.res], writes=[RSTD.res])
        for m in range(8):
            tn = TN[m % 2]
            self.tt("dve", tn[:, :], SRC[:, m, sl], MEAN[:, :], ALU.subtract, reads=[(SRC.res, tt), MEAN.res],
                    writes=[tn.res])
            self.tt("dve", tn[:, :], tn[:, :], RSTD[:, :], ALU.mult, reads=[tn.res, RSTD.res], writes=[tn.res])
            out_fn(m, tn)

    def ln_tmp(self):
        return ([self.aalloc([512], F32) for _ in range(2)], self.aalloc([512], F32), self.aalloc([512], F32),
                [self.aalloc([512], F32) for _ in range(2)])

    def program(self):
        S = self.S
        S.dma("sp", self.CST[:, :], self.d_cst, writes=[self.CST.res])
        self.cp("dve", self.CSB[:, :], self.CST[:, :], reads=[self.CST.res], writes=[self.CSB.res])
        import os
        self.bis = int(os.environ.get("BIS", "0"))
        for l in range(self.nlayer):
            if self.bis & 1:
                break
            S.dma("sp", self.COLS[l][:, :], self.dw[l]["cols"], writes=[self.COLS[l].res])
            S.dma("sp", self.ROWS[l][:, :], bass.AP(self.dw[l]["rows"].tensor, 0, [[0, 128], [1, NROWS]]),
                  writes=[self.ROWS[l].res])
            self.actf(self.ROWX[l][:, 0:16], self.ROWS[l][:, R_ALOG:R_ALOG + 16], AF.Exp, reads=[self.ROWS[l].res],
                      writes=[self.ROWX[l].res])
            self.ts("dve", self.ROWX[l][:, 0:16], self.ROWX[l][:, 0:16], -1.0, ALU.mult, reads=[self.ROWX[l].res],
                    writes=[self.ROWX[l].res])
            self.actf(self.ROWX[l][:, 16:32], self.ROWS[l][:, R_SINK:R_SINK + 16], AF.Exp, reads=[self.ROWS[l].res],
                      writes=[self.ROWX[l].res])
            self.ts("dve", self.COLA[l][:, 0:16], self.COLS[l][:, C_L1G:C_L1G + 16], float(ALPHA), ALU.mult,
                    reads=[self.COLS[l].res], writes=[self.COLA[l].res])
        for seq in range(self.nseq):
            for seg in range(self.nseg):
                self.segment(seq, seg)

    def segment(self, seq, seg):
        T = self.T
        t0 = seg * T
        first = seg == 0
        self.areset()
        if not self.bis & 4:
            self.load_x(seq, t0)
        if first and not self.bis & 2:
            for l in range(self.nlayer):
                self.memset("pool", self.H[l][:, :], 0.0, [self.H[l].res])
                self.memset("pool", self.CTAIL[l][:, :, :], 0.0, [self.CTAIL[l].res])
                self.memset("pool", self.STAIL[l][:, :, :], 0.0, [self.STAIL[l].res])
        for l in range(self.nlayer):
            self.mt_first = True
            stages = [("ssd", lambda: self.ssd(l, first)),
                      ("conf", lambda: self.conformer(l)),
                      ("attn", lambda: self.attention(l, seq, t0, first)),
                      ("ln1", lambda: self.outproj_ln1(l)),
                      ("moe", lambda: self.moe(l)),
                      ("ple", lambda: self.ln2_ple(l, seq, t0))]
            for nm, fn in stages:
                if self.only is not None and nm not in self.only:
                    continue
                self.areset()
                fn()
                self.dbg("MT", self.MT)
                if self.stop_after == (l, nm):
                    break
            if self.stop_after is not None and self.stop_after[0] == l:
                break
        self.areset()
        if not self.bis & 8:
            self.store_x(seq, t0)

    def load_x(self, seq, t0):
        S, P = self.S, self.P
        STG = [self.aalloc([1024], F32) for _ in range(2)]
        for blk in range(self.NB):
            stg = STG[blk % 2]
            tt = blk // 4
            S.dma("sp", stg[:, :], self.d_x[seq, t0 + blk * 128:t0 + (blk + 1) * 128, :], writes=[stg.res])
            for half in range(2):
                ps = P[(blk * 2 + half) % 4].reshape([128, 4, 128])
                for j in range(4):
                    c = half * 4 + j
                    self.tr(ps[:, j, :], stg[:, c * 128:(c + 1) * 128], self.cF(K_ID), reads=[stg.res, self.CST.res],
                            writes=[ps.res])
                self.cp("act", self.X[:, half * 4:half * 4 + 4, blk * 128:(blk + 1) * 128], ps[:, :, :],
                        reads=[ps.res], writes=[(self.X.res, tt)])
                self.cp("dve", self.XT[:, half * 4:half * 4 + 4, blk * 128:(blk + 1) * 128], ps[:, :, :],
                        reads=[ps.res], writes=[(self.XT.res, tt)])

    def store_x(self, seq, t0):
        S, P = self.S, self.P
        STG = [self.aalloc([1024], F32) for _ in range(2)]
        for blk in range(self.NB):
            stg = STG[blk % 2]
            tt = blk // 4
            for half in range(2):
                ps = P[(blk * 2 + half) % 4].reshape([128, 4, 128])
                for j in range(4):
                    c = half * 4 + j
                    self.tr(ps[:, j, :], self.X[:, c, blk * 128:(blk + 1) * 128], self.cF(K_ID),
                            reads=[(self.X.res, tt), self.CST.res], writes=[ps.res])
                self.cp("act" if half == 0 else "dve", stg[:, half * 512:(half + 1) * 512],
                        ps[:, :, :], reads=[ps.res], writes=[stg.res])
            S.dma("sp", self.d_out[seq, t0 + blk * 128:t0 + (blk + 1) * 128, :], stg[:, :], reads=[stg.res])

    def conformer(self, l):
        T, NT, P, S = self.T, self.NT, self.P, self.S
        w = self.dw[l]
        HPAD = [self.aalloc([32 + T], BF16) for _ in range(2)]
        DG = [self.aalloc([31, 128], BF16) for _ in range(2)]
        CO = self.aalloc([8, T], F32)
        HT = self.aalloc([8, T], BF16)
        SG = [self.aalloc([512], F32) for _ in range(2)]
        wbA = [self.aalloc([8, 128], BF16) for _ in range(2)]
        wbG = [self.aalloc([8, 128], BF16) for _ in range(2)]
        lnt = self.ln_tmp()

        def cload(m):
            S.dma("pool", wbA[m % 2][:, :, :], w["win_fm"][FM_CONF + m], writes=[wbA[m % 2].res])
            S.dma("pool", wbG[m % 2][:, :, :], w["win_fm"][FM_CONF + 8 + m], writes=[wbG[m % 2].res])

        for m in range(8):
            hp, dg = HPAD[m % 2], DG[m % 2]
            self.cp("act", hp[:, 0:32], self.CTAIL[l][:, m, :], reads=[self.CTAIL[l].res], writes=[hp.res])
            self.tt("dve", dg[:, :, :], self.CSB.bc(K_ID, [[0, 31], [1, 128]]),
                    self.COLS[l].bc(C_CDW + m * 31, [[1, 31], [0, 128]]), ALU.mult,
                    reads=[self.CSB.res, self.COLS[l].res], writes=[dg.res])
            wa, wg = wbA[m % 2], wbG[m % 2]
            if m == 0:
                cload(0)
            if m + 1 < 8:
                cload(m + 1)
            for tt in range(NT):
                sl = slice(tt * 512, (tt + 1) * 512)
                a_ps, g_ps, c_ps = P[tt % 2], P[2 + tt % 2], P[4 + tt % 2]
                for k in range(8):
                    self.mm(a_ps[:, :], wa[:, k, :], self.XT[:, k, sl], k == 0, k == 7,
                            reads=[wa.res, (self.XT.res, tt)], writes=[a_ps.res])
                for k in range(8):
                    self.mm(g_ps[:, :], wg[:, k, :], self.XT[:, k, sl], k == 0, k == 7,
                            reads=[wg.res, (self.XT.res, tt)], writes=[g_ps.res])
                sg = SG[tt % 2]
                self.actf(sg[:, :], g_ps[:, :], AF.Sigmoid, reads=[g_ps.res], writes=[sg.res])
                self.tt("dve", hp[:, 32 + tt * 512:32 + (tt + 1) * 512], a_ps[:, :], sg[:, :], ALU.mult,
                        reads=[a_ps.res, sg.res], writes=[hp.res])
                for k in range(31):
                    self.mm(c_ps[:, :], dg[:, k, :], hp[:, 2 + k + tt * 512:2 + k + (tt + 1) * 512], k == 0, k == 30,
                            reads=[dg.res, hp.res], writes=[c_ps.res])
                self.actf(CO[:, m, sl], c_ps[:, :], AF.Identity, reads=[c_ps.res], writes=[(CO.res, tt)],
                          bias=self.col(l, C_CDB + m))
            self.cp("act", self.CTAIL[l][:, m, :], hp[:, T:T + 32], reads=[hp.res], writes=[self.CTAIL[l].res])
        self.dbg("CO", CO)
        for tt in range(NT):
            sl = slice(tt * 512, (tt + 1) * 512)

            def out_fn(m, tn, tt=tt, sl=sl):
                self.actf(HT[:, m, sl], tn[:, :], AF.Silu, reads=[tn.res], writes=[(HT.res, tt)],
                          scale=self.col(l, C_CLG + m), bias=self.col(l, C_CLB + m))

            self.ln_fm(CO, tt, out_fn, lnt)
        self.dbg("HT", HT)
        self.gated_out(l, 1, HT, "wconf", self.mt_first)
        self.mt_first = False

    def outproj_ln1(self, l):
        T, NT, P, S = self.T, self.NT, self.P, self.S
        w = self.dw[l]
        wb = [self.aalloc([8, 128], BF16) for _ in range(3)]
        lnt = self.ln_tmp()
        self.dbg("MT", self.MT)

        def mt_rhs(k, tt):
            return self.MT[:, k, tt * 512:(tt + 1) * 512], (self.MT.res, tt)

        def consumer(i, tt, pss):
            sl = slice(tt * 512, (tt + 1) * 512)
            self.stt(self.X[:, i, sl], self.X[:, i, sl], float(ALPHA), pss[0][:, :], ALU.mult, ALU.add,
                     reads=[(self.X.res, tt), pss[0].res], writes=[(self.X.res, tt)])

        self.proj_fm([(w["wout"], 0, 8, mt_rhs)], 8, consumer, [wb], [[P[0], P[1]]])
        for tt in range(NT):
            sl = slice(tt * 512, (tt + 1) * 512)

            def out_fn(m, tn, tt=tt, sl=sl):
                self.actf(self.XT[:, m, sl], tn[:, :], AF.Identity, reads=[tn.res], writes=[(self.XT.res, tt)],
                          scale=self.col(l, C_L1G + m), bias=self.col(l, C_L1B + m))
                self.actf(self.X[:, m, sl], tn[:, :], AF.Identity, reads=[tn.res], writes=[(self.X.res, tt)],
                          scale=self.COLA[l][:, m:m + 1], bias=self.COLA[l][:, 8 + m:9 + m])

            self.ln_fm(self.X, tt, out_fn, lnt)
        self.dbg("X1T", self.XT)

    def ln2_ple(self, l, seq, t0):
        T, NT, NB, P, S = self.T, self.NT, self.NB, self.P, self.S
        w = self.dw[l]
        lnt = self.ln_tmp()
        self.dbg("XMOE", self.X)
        for tt in range(NT):
            sl = slice(tt * 512, (tt + 1) * 512)

            def out_fn(m, tn, tt=tt, sl=sl):
                self.actf(self.XT[:, m, sl], tn[:, :], AF.Identity, reads=[tn.res], writes=[(self.XT.res, tt)],
                          scale=self.col(l, C_L2G + m), bias=self.col(l, C_L2B + m))
                self.actf(self.X[:, m, sl], tn[:, :], AF.Identity, reads=[tn.res], writes=[(self.X.res, tt)],
                          scale=self.col(l, C_L2G + m), bias=self.col(l, C_L2B + m))

            self.ln_fm(self.X, tt, out_fn, lnt)
        self.dbg("X2T", self.XT)
        PSTG = [self.aalloc([256], F32) for _ in range(2)]
        PT = self.aalloc([2, T], BF16)
        for blk in range(NB):
            stg = PSTG[blk % 2]
            S.dma("sp", stg[:, :], self.d_p[l, seq, t0 + blk * 128:t0 + (blk + 1) * 128, :], writes=[stg.res])
            ps = P[6 + blk % 2].reshape([128, 4, 128])
            for j in range(2):
                self.tr(ps[:, j, :], stg[:, j * 128:(j + 1) * 128], self.cF(K_ID), reads=[stg.res, self.CST.res],
                        writes=[ps.res])
            self.cp("act", PT[:, :, blk * 128:(blk + 1) * 128], ps[:, 0:2, :], reads=[ps.res],
                    writes=[(PT.res, blk // 4)])
        wbA = [self.aalloc([8, 128], BF16) for _ in range(3)]
        wbB = [self.aalloc([8, 128], BF16) for _ in range(3)]
        SG = [self.aalloc([512], F32) for _ in range(2)]
        TM = [self.aalloc([512], F32) for _ in range(2)]
        st = {"n": 0}

        def pt_rhs(k, tt):
            return PT[:, k, tt * 512:(tt + 1) * 512], (PT.res, tt)

        def consumer(i, tt, pss):
            n = st["n"]
            st["n"] += 1
            sl = slice(tt * 512, (tt + 1) * 512)
            sg, tm = SG[n % 2], TM[n % 2]
            self.actf(sg[:, :], pss[0][:, :], AF.Sigmoid, reads=[pss[0].res], writes=[sg.res])
            self.tt("dve", tm[:, :], pss[1][:, :], sg[:, :], ALU.mult, reads=[pss[1].res, sg.res], writes=[tm.res])
            self.tt("dve", self.X[:, i, sl], self.X[:, i, sl], tm[:, :], ALU.add, reads=[tm.res, (self.X.res, tt)],
                    writes=[(self.X.res, tt)])

        self.proj_fm([(w["wpg"], 0, 8, self.xt_rhs), (w["wpp"], 0, 2, pt_rhs)], 8, consumer, [wbA, wbB],
                     [[P[0], P[1]], [P[2], P[3]]])
        if l < self.nlayer - 1:
            for tt in range(NT):
                sl = slice(tt * 512, (tt + 1) * 512)
                for m in range(8):
                    self.cp("act", self.XT[:, m, sl], self.X[:, m, sl], reads=[(self.X.res, tt)],
                            writes=[(self.XT.res, tt)])

    def amark(self):
        return self.aoff

    def arelease(self, mark):
        self.S.fence()
        self.aoff = mark

    def ssd(self, l, first):
        T, NT, NB, P, PB, S = self.T, self.NT, self.NB, self.P, self.PB, self.S
        w = self.dw[l]
        ROWS, ROWX, COLS, CST, CSB = self.ROWS[l], self.ROWX[l], self.COLS[l], self.CST, self.CSB
        YST = self.aalloc([8, T], BF16)
        mark0 = self.amark()
        WZ = self.aalloc([8, 1024], BF16)
        WDT = self.aalloc([8, 16], BF16)
        BFM = self.aalloc([4, T], BF16)
        CFM = self.aalloc([4, T], BF16)
        XST = self.aalloc([NB, 1024], BF16)
        BTK = self.aalloc([NB, 512], BF16)
        DT, DTA, ECS, DTE, CDEC = [self.aalloc([NB, 16], F32) for _ in range(5)]
        mark1 = self.amark()
        XPAD = [self.aalloc([4 + T], BF16) for _ in range(2)]
        DG = [self.aalloc([4, 128], BF16) for _ in range(2)]
        XFM = [self.aalloc([T], BF16) for _ in range(2)]
        wbuf = [self.aalloc([8, 128], BF16) for _ in range(3)]
        S.dma("pool", WZ[:, :, :], w["win_tm"][:, 0:1024].rearrange("(k p) n -> p k n", p=128), writes=[WZ.res])
        S.dma("pool", WDT[:, :, :], w["win_tm"][:, 1024:1040].rearrange("(k p) n -> p k n", p=128), writes=[WDT.res])
        for m in range(16):
            xp, dg = XPAD[m % 2], DG[m % 2]
            if m < 8:
                dst = XFM[m % 2]
                dsl = lambda sl, dst=dst: dst[:, sl]
            elif m < 12:
                dst = BFM
                dsl = lambda sl, g=m - 8: BFM[:, g, sl]
            else:
                dst = CFM
                dsl = lambda sl, g=m - 12: CFM[:, g, sl]
            self.cp("act", xp[:, 0:4], self.STAIL[l][:, m, :], reads=[self.STAIL[l].res], writes=[xp.res])
            self.tt("dve", dg[:, :, :], CSB.bc(K_ID, [[0, 4], [1, 128]]), COLS.bc(C_SCW + m * 4, [[1, 4], [0, 128]]),
                    ALU.mult, reads=[CSB.res, COLS.res], writes=[dg.res])
            wb = wbuf[m % 3]
            if m == 0:
                for mm_ in range(2):
                    S.dma("pool", wbuf[mm_ % 3][:, :, :], w["win_fm"][FM_XBC + mm_], writes=[wbuf[mm_ % 3].res])
            if m + 2 < 16:
                S.dma("pool", wbuf[(m + 2) % 3][:, :, :], w["win_fm"][FM_XBC + m + 2], writes=[wbuf[(m + 2) % 3].res])
            for tt in range(NT):
                sl = slice(tt * 512, (tt + 1) * 512)
                ps, cps = P[tt % 2], P[2 + tt % 2]
                for k in range(8):
                    self.mm(ps[:, :], wb[:, k, :], self.XT[:, k, sl], k == 0, k == 7,
                            reads=[wb.res, (self.XT.res, tt)], writes=[ps.res])
                self.cp("act", xp[:, 4 + tt * 512:4 + (tt + 1) * 512], ps[:, :], reads=[ps.res], writes=[xp.res])
                for k in range(4):
                    self.mm(cps[:, :], dg[:, k, :], xp[:, 1 + k + tt * 512:1 + k + (tt + 1) * 512], k == 0, k == 3,
                            reads=[dg.res, xp.res], writes=[cps.res])
                self.actf(dsl(sl), cps[:, :], AF.Silu, reads=[cps.res], writes=[dst.res],
                          bias=self.col(l, C_SCB + m))
            self.cp("act", self.STAIL[l][:, m, :], xp[:, T:T + 4], reads=[xp.res], writes=[self.STAIL[l].res])
            if m < 12:
                for b0 in range(0, NB, 8):
                    nb = min(8, NB - b0)
                    pb = PB[4 + (m + b0 // 8) % 2].reshape([128, 8, 128])
                    for j in range(nb):
                        blk = b0 + j
                        src = dst[:, blk * 128:(blk + 1) * 128] if m < 8 else BFM[:, m - 8, blk * 128:(blk + 1) * 128]
                        self.tr(pb[:, j, :], src, self.cB(K_ID), reads=[dst.res, CSB.res], writes=[pb.res])
                    if m < 8:
                        self.cp("dve", XST[:, b0:b0 + nb, m * 128:(m + 1) * 128], pb[:, 0:nb, :], reads=[pb.res],
                                writes=[XST.res])
                    else:
                        self.cp("dve", BTK[:, b0:b0 + nb, (m - 8) * 128:(m - 7) * 128], pb[:, 0:nb, :],
                                reads=[pb.res], writes=[BTK.res])
        PD = P[6].reshape([128, 32, 16])
        for blk in range(NB):
            for k in range(8):
                self.mm(PD[:, blk, :], self.XT[:, k, blk * 128:(blk + 1) * 128], WDT[:, k, :], k == 0, k == 7,
                        reads=[(self.XT.res, blk // 4), WDT.res], writes=[PD.res])
        self.tt("dve", DT[:, :, :], PD[:, 0:NB, :], ROWS.bc(R_DTB, [[0, NB], [1, 16]]), ALU.add,
                reads=[PD.res, ROWS.res], writes=[DT.res])
        self.actf(DT[:, :, :], DT[:, :, :], AF.Exp, reads=[DT.res], writes=[DT.res])
        self.actf(DT[:, :, :], DT[:, :, :], AF.Ln, reads=[DT.res], writes=[DT.res], bias=self.cF(K_ONEC, 1))
        self.tt("dve", DTA[:, :, :], DT[:, :, :], ROWX.bc(0, [[0, NB], [1, 16]]), ALU.mult,
                reads=[DT.res, ROWX.res], writes=[DTA.res])
        dta2 = DTA.reshape([128, NB * 16])
        for (cst, dstv, ps) in ((K_TU, ECS, P[7]), (K_TL, DTE, P[6]), (K_ONE, CDEC, P[7])):
            self.mm(ps[:, 0:NB * 16], self.cF(cst), dta2[:, :], True, True, reads=[CST.res, DTA.res], writes=[ps.res])
            self.actf(dstv.reshape([128, NB * 16])[:, :], ps[:, 0:NB * 16], AF.Exp, reads=[ps.res], writes=[dstv.res])
        self.arelease(mark1)
        RS = [self.aalloc([4, 128], F32) for _ in range(2)]
        DEC = [self.aalloc([4, 128], F32) for _ in range(2)]
        MTG = [self.aalloc([4, 128], BF16) for _ in range(2)]
        CBM = self.aalloc([4, 128], F32)
        XDT = [self.aalloc([16, 64], BF16) for _ in range(2)]
        XDD = [self.aalloc([16, 64], BF16) for _ in range(2)]
        XD = [self.aalloc([16, 64], BF16) for _ in range(2)]
        T1 = self.aalloc([1024], F32)
        SZ = self.aalloc([1024], F32)
        YN = self.aalloc([1024], BF16)
        HB = self.aalloc([1024], BF16)
        SS = self.aalloc([4], F32)
        H = self.H[l]
        self.cp("act", HB[:, :], H[:, :], reads=[H.res], writes=[HB.res])
        for c in range(NB):
            csl = slice(c * 128, (c + 1) * 128)
            xdt, xdd, xd = XDT[c % 2], XDD[c % 2], XD[c % 2]
            xs3 = XST.reshape([128, NB, 16, 64])
            self.tt("dve", xdt[:, :, :], xs3[:, c, :, :], DT.bc(c * 16, [[1, 16], [0, 64]]), ALU.mult,
                    reads=[XST.res, DT.res], writes=[xdt.res])
            self.tt("dve", xdd[:, :, :], xdt[:, :, :], DTE.bc(c * 16, [[1, 16], [0, 64]]), ALU.mult,
                    reads=[xdt.res, DTE.res], writes=[xdd.res])
            self.tt("dve", xd[:, :, :], xs3[:, c, :, :], ROWS.bc(R_D, [[1, 16], [0, 64]]), ALU.mult,
                    reads=[XST.res, ROWS.res], writes=[xd.res])
            p0 = P[0].reshape([128, 4, 128])
            for g in range(4):
                self.mm(p0[:, g, :], BFM[:, g, csl], CFM[:, g, csl], True, True, reads=[BFM.res, CFM.res],
                        writes=[p0.res])
            self.tt("dve", CBM[:, :, :], p0[:, :, :], CST.bc(K_TU, [[0, 4], [1, 128]]), ALU.mult,
                    reads=[p0.res, CST.res], writes=[CBM.res])
            xd2 = xd.reshape([128, 1024])
            xdt2 = xdt.reshape([128, 1024])
            for half in range(2):
                self.mm(P[3 + half][:, :], self.cB(K_ID), xd2[:, half * 512:(half + 1) * 512], True, False,
                        reads=[CSB.res, xd.res], writes=[P[3 + half].res])
            for g in range(4):
                rs, dec, mtg = RS[g % 2], DEC[g % 2], MTG[g % 2]
                self.tt("dve", rs[:, :, :], DTA.bc(c * 16 + 4 * g, [[1, 4], [0, 128]]),
                        CST.bc(K_TU, [[0, 4], [1, 128]]), ALU.mult, reads=[DTA.res, CST.res], writes=[rs.res])
                self.mm(P[1][:, :], self.cF(K_TL), rs.reshape([128, 512])[:, :], True, True,
                        reads=[CST.res, rs.res], writes=[P[1].res])
                self.actf(dec.reshape([128, 512])[:, :], P[1][:, :], AF.Exp, reads=[P[1].res], writes=[dec.res])
                self.tt("dve", mtg[:, :, :], dec[:, :, :], CBM.bc(g * 128, [[0, 4], [1, 128]]), ALU.mult,
                        reads=[dec.res, CBM.res], writes=[mtg.res])
                for r in range(4):
                    h = 4 * g + r
                    half = h // 8
                    self.mm(P[3 + half][:, (h % 8) * 64:(h % 8 + 1) * 64], mtg[:, r, :],
                            xdt2[:, h * 64:(h + 1) * 64], False, h % 8 == 7,
                            reads=[mtg.res, xdt.res], writes=[P[3 + half].res])
            for g in range(4):
                ps = P[5 + g // 2]
                self.mm(ps[:, (g % 2) * 256:(g % 2 + 1) * 256], CFM[:, g, csl], HB[:, g * 256:(g + 1) * 256],
                        True, True, reads=[CFM.res, HB.res], writes=[ps.res])
            t13 = T1.reshape([128, 16, 64])
            for half in range(2):
                self.tt("dve", t13[:, half * 8:(half + 1) * 8, :], P[5 + half].reshape([128, 8, 64])[:, :, :],
                        ECS.bc(c * 16 + half * 8, [[1, 8], [0, 64]]), ALU.mult,
                        reads=[P[5 + half].res, ECS.res], writes=[T1.res])
            for half in range(2):
                hs = slice(half * 512, (half + 1) * 512)
                self.tt("dve", T1[:, hs], P[3 + half][:, :], T1[:, hs], ALU.add, reads=[P[3 + half].res, T1.res],
                        writes=[T1.res])
            for half in range(2):
                hs = slice(half * 512, (half + 1) * 512)
                pz = P[7] if half == 0 else P[0]
                for k in range(8):
                    self.mm(pz[:, :], self.XT[:, k, csl], WZ[:, k, hs], k == 0, k == 7,
                            reads=[(self.XT.res, c // 4), WZ.res], writes=[pz.res])
                self.actf(SZ[:, hs], pz[:, :], AF.Silu, reads=[pz.res], writes=[SZ.res])
            self.tt("dve", T1[:, :], T1[:, :], SZ[:, :], ALU.mult, reads=[T1.res, SZ.res], writes=[T1.res])
            self.tt("dve", SZ[:, :], T1[:, :], T1[:, :], ALU.mult, reads=[T1.res], writes=[SZ.res])
            self.S.dve(lambda e: e.tensor_reduce(out=SS[:, :], in_=SZ.reshape([128, 4, 256])[:, :, :], op=ALU.add,
                                                 axis=AX.X), reads=[SZ.res], writes=[SS.res])
            self.ts("dve", SS[:, :], SS[:, :], 1.0 / 256, ALU.mult, reads=[SS.res], writes=[SS.res], s2=float(EPS),
                    op1=ALU.add)
            self.actf(SS[:, :], SS[:, :], AF.Sqrt, reads=[SS.res], writes=[SS.res])
            self.S.dve(lambda e: e.reciprocal(SS[:, :], SS[:, :]), reads=[SS.res], writes=[SS.res])
            self.tt("dve", YN.reshape([128, 4, 256])[:, :, :], T1.reshape([128, 4, 256])[:, :, :],
                    SS.bc(0, [[1, 4], [0, 256]]), ALU.mult, reads=[T1.res, SS.res], writes=[YN.res])
            pb = PB[2].reshape([128, 8, 128])
            for m in range(8):
                self.tr(pb[:, m, :], YN[:, m * 128:(m + 1) * 128], self.cB(K_ID), reads=[YN.res, CSB.res],
                        writes=[pb.res])
            self.tt("dve", YST[:, :, csl], pb[:, :, :], COLS.bc(C_NW, [[1, 8], [0, 128]]), ALU.mult,
                    reads=[pb.res, COLS.res], writes=[(YST.res, c // 4)])
            xdd2 = xdd.reshape([128, 1024])
            for g in range(4):
                ps = P[5 + g // 2]
                self.mm(ps[:, (g % 2) * 256:(g % 2 + 1) * 256], BTK[:, c, g * 128:(g + 1) * 128],
                        xdd2[:, g * 256:(g + 1) * 256], True, True, reads=[BTK.res, xdd.res], writes=[ps.res])
            self.tt("dve", H.reshape([128, 16, 64])[:, :, :], H.reshape([128, 16, 64])[:, :, :],
                    CDEC.bc(c * 16, [[1, 16], [0, 64]]), ALU.mult, reads=[H.res, CDEC.res], writes=[H.res])
            for half in range(2):
                hs = slice(half * 512, (half + 1) * 512)
                self.tt("dve", H[:, hs], P[5 + half][:, :], H[:, hs], ALU.add, reads=[P[5 + half].res, H.res],
                        writes=[H.res])
            self.cp("act", HB[:, :], H[:, :], reads=[H.res], writes=[HB.res])
        self.dbg("YST", YST)
        self.arelease(mark0)
        self.gated_out(l, 0, YST, "wssd", self.mt_first)
        self.mt_first = False

    def attention(self, l, seq, t0, first):
        T, NT, NB, P, PB, S = self.T, self.NT, self.NB, self.P, self.PB, self.S
        w = self.dw[l]
        ROWX, CST, CSB = self.ROWX[l], self.CST, self.CSB
        AT = self.aalloc([8, T], BF16)
        mark0 = self.amark()
        COS = self.aalloc([T], F32)
        SIN = self.aalloc([T], F32)
        QR = self.aalloc([8, T], BF16)
        KD = self.aalloc([4, 128 + T], BF16)
        VA = self.aalloc([1 + NB, 4, 65], BF16)
        AO = self.aalloc([NB, 1024], BF16)
        mark1 = self.amark()
        POSI = self.aalloc([T], I32)
        ANG = self.aalloc([T], F32)
        A2 = self.aalloc([T], F32)
        KI = self.aalloc([T], I32)
        KF = self.aalloc([T], F32)
        S.dma("sp", POSI[:, :], bass.AP(self.d_pos.tensor, seq * SEQ + t0, [[0, 128], [1, T]]), writes=[POSI.res])
        self.cp("dve", ANG[:, :], POSI[:, :], reads=[POSI.res], writes=[ANG.res])
        self.ts("dve", ANG[:, :], ANG[:, :], self.cF(K_INVF, 1), ALU.mult, reads=[ANG.res, CST.res], writes=[ANG.res])
        TWO_PI = 2.0 * math.pi
        C1 = 6.28125
        C2 = TWO_PI - C1
        PI_SAFE = 3.1415925
        for dst, shift in ((SIN, 0.0), (COS, 0.5 * math.pi)):
            self.ts("dve", A2[:, :], ANG[:, :], float(shift), ALU.add, reads=[ANG.res], writes=[A2.res])
            self.ts("dve", KI[:, :], A2[:, :], 1.0 / TWO_PI, ALU.mult, reads=[A2.res], writes=[KI.res])
            self.cp("dve", KF[:, :], KI[:, :], reads=[KI.res], writes=[KF.res])
            self.stt(A2[:, :], KF[:, :], -C1, A2[:, :], ALU.mult, ALU.add, reads=[KF.res, A2.res], writes=[A2.res])
            self.stt(A2[:, :], KF[:, :], -C2, A2[:, :], ALU.mult, ALU.add, reads=[KF.res, A2.res], writes=[A2.res])
            self.ts("dve", A2[:, :], A2[:, :], -PI_SAFE, ALU.max, reads=[A2.res], writes=[A2.res], s2=PI_SAFE,
                    op1=ALU.min)
            self.actf(dst[:, :], A2[:, :], AF.Sin, reads=[A2.res], writes=[dst.res])
        self.arelease(mark1)
        Q32 = [self.aalloc([512], F32) for _ in range(2)]
        TA = [self.aalloc([512], F32) for _ in range(2)]
        TB = [self.aalloc([512], F32) for _ in range(2)]
        KR = [self.aalloc([512], BF16) for _ in range(2)]
        wbuf = [self.aalloc([8, 128], BF16) for _ in range(3)]
        WV = self.aalloc([8, 256], BF16)
        EC = [self.aalloc([512], BF16) for _ in range(2)]
        EP = [self.aalloc([512], BF16) for _ in range(2)]
        DEN = [self.aalloc([4], F32) for _ in range(2)]
        S.dma("pool", WV[:, :, :], w["win_tm"][:, 1040:1296].rearrange("(k p) n -> p k n", p=128), writes=[WV.res])
        st = {"n": 0}

        def rope(i, tt, pss, is_k):
            n = st["n"]
            st["n"] += 1
            sl = slice(tt * 512, (tt + 1) * 512)
            q32, ta, tb, pr = Q32[n % 2], TA[n % 2], TB[n % 2], P[2 + n % 2]
            self.cp("act", q32[:, :], pss[0][:, :], reads=[pss[0].res], writes=[q32.res])
            self.mm(pr[:, :], self.cF(K_ROT), q32[:, :], True, True, reads=[CST.res, q32.res], writes=[pr.res])
            self.tt("dve", ta[:, :], q32[:, :], COS[:, sl], ALU.mult, reads=[q32.res, COS.res], writes=[ta.res])
            self.tt("dve", tb[:, :], pr[:, :], SIN[:, sl], ALU.mult, reads=[pr.res, SIN.res], writes=[tb.res])
            if not is_k:
                self.tt("dve", QR[:, i, sl], ta[:, :], tb[:, :], ALU.add, reads=[ta.res, tb.res],
                        writes=[(QR.res, tt)])
            else:
                kr = KR[n % 2]
                self.tt("dve", kr[:, :], ta[:, :], tb[:, :], ALU.add, reads=[ta.res, tb.res], writes=[kr.res])
                for half in range(2):
                    h = 2 * i + half
                    pd = P[4 + half]
                    self.mm(pd[:, :], self.cB(K_DUP0 + half * 128), kr[:, :], True, True, reads=[CSB.res, kr.res],
                            writes=[pd.res])
                    self.cp("act" if half == 0 else "dve", KD[:, h, 128 + tt * 512:128 + (tt + 1) * 512], pd[:, :],
                            reads=[pd.res], writes=[KD.res])

        self.proj_fm([(w["win_fm"], FM_Q, 8, self.xt_rhs)], 8, lambda i, tt, pss: rope(i, tt, pss, False), [wbuf],
                     [[P[0], P[1]]])
        self.proj_fm([(w["win_fm"], FM_K, 8, self.xt_rhs)], 2, lambda i, tt, pss: rope(i, tt, pss, True), [wbuf],
                     [[P[0], P[1]]])
        self.memset("pool", VA[:, :, :, 64:65], 1.0, [VA.res])
        if not first:
            self.cp("act", KD[:, :, 0:128], self.KCAR[l][:, :, :], reads=[self.KCAR[l].res], writes=[KD.res])
            self.cp("act", VA[:, 0, :, :], self.VCAR[l][:, :, :], reads=[self.VCAR[l].res], writes=[VA.res])
        for blk in range(NB):
            pv = P[6 + blk % 2]
            for k in range(8):
                self.mm(pv[:, 0:256], self.XT[:, k, blk * 128:(blk + 1) * 128], WV[:, k, :], k == 0, k == 7,
                        reads=[(self.XT.res, blk // 4), WV.res], writes=[pv.res])
            self.cp("act", VA[:, 1 + blk, :, 0:64], pv.reshape([128, 8, 64])[:, 0:4, :], reads=[pv.res],
                    writes=[VA.res])
        self.dbg("QR", QR)
        self.dbg("KD", KD)
        n = 0
        for qb in range(NB):
            gfirst = first and qb == 0
            for h in range(4):
                po = P[4 + n % 2]
                ec, ep, den = EC[n % 2], EP[n % 2], DEN[n % 2]
                n += 1
                qsl = slice(qb * 128, (qb + 1) * 128)
                ec3, ep3 = ec.reshape([128, 4, 128]), ep.reshape([128, 4, 128])
                for r in range(4):
                    hq = 4 * h + r
                    ch, hf = hq // 2, hq % 2
                    ps_ = slice(hf * 64, hf * 64 + 64)
                    sc, sp = P[hf], P[2 + hf]
                    cs_ = slice((r // 2) * 128, (r // 2 + 1) * 128)
                    self.mm(sc[:, cs_], KD[ps_, h, 128 + qb * 128:128 + (qb + 1) * 128],
                            QR[ps_, ch, qsl], True, True, reads=[KD.res, (QR.res, qb // 4)], writes=[sc.res])
                    if not gfirst:
                        self.mm(sp[:, cs_], KD[ps_, h, qb * 128:(qb + 1) * 128],
                                QR[ps_, ch, qsl], True, True, reads=[KD.res, (QR.res, qb // 4)], writes=[sp.res])
                for hf in range(2):
                    self.actf(ec3[:, hf::2, :], P[hf].reshape([128, 4, 128])[:, 0:2, :], AF.Exp, reads=[P[hf].res],
                              writes=[ec.res], scale=0.125)
                self.tt("dve", ec3[:, :, :], ec3[:, :, :],
                        CSB.bc(K_TU, [[0, 4], [1, 128]]), ALU.mult, reads=[ec.res, CSB.res], writes=[ec.res])
                if not gfirst:
                    for hf in range(2):
                        self.actf(ep3[:, hf::2, :], P[2 + hf].reshape([128, 4, 128])[:, 0:2, :], AF.Exp,
                                  reads=[P[2 + hf].res], writes=[ep.res], scale=0.125)
                    self.tt("dve", ep3[:, :, :], ep3[:, :, :],
                            CSB.bc(K_TL, [[0, 4], [1, 128]]), ALU.mult, reads=[ep.res, CSB.res], writes=[ep.res])
                po3 = po.reshape([128, 4, 128])
                for r in range(4):
                    if not gfirst:
                        self.mm(po3[:, r, 0:65], ep[:, r * 128:(r + 1) * 128], VA[:, qb, h, :], True, False,
                                reads=[ep.res, VA.res], writes=[po.res])
                    self.mm(po3[:, r, 0:65], ec[:, r * 128:(r + 1) * 128], VA[:, qb + 1, h, :], gfirst, True,
                            reads=[ec.res, VA.res], writes=[po.res])
                self.tt("dve", den[:, :], po3[:, :, 64], ROWX.bc(16 + 4 * h, [[1, 4]]), ALU.add,
                        reads=[po.res, ROWX.res], writes=[den.res])
                self.S.dve(lambda e, den=den: e.reciprocal(den[:, :], den[:, :]), reads=[den.res], writes=[den.res])
                self.tt("dve", AO.reshape([128, NB, 16, 64])[:, qb, 4 * h:4 * h + 4, :], po3[:, :, 0:64],
                        den.bc(0, [[1, 4], [0, 64]]), ALU.mult, reads=[po.res, den.res], writes=[(AO.res, qb)])
        for blk in range(NB):
            pb = PB[6 + blk % 2].reshape([128, 8, 128])
            for m in range(8):
                self.tr(pb[:, m, :], AO[:, blk, m * 128:(m + 1) * 128], self.cB(K_ID), reads=[(AO.res, blk), CSB.res],
                        writes=[pb.res])
            self.cp("act" if blk % 2 else "dve", AT[:, :, blk * 128:(blk + 1) * 128], pb[:, :, :], reads=[pb.res],
                    writes=[(AT.res, blk // 4)])
        self.cp("act", self.KCAR[l][:, :, :], KD[:, :, T:T + 128], reads=[KD.res], writes=[self.KCAR[l].res])
        self.cp("act", self.VCAR[l][:, :, :], VA[:, NB, :, :], reads=[VA.res], writes=[self.VCAR[l].res])
        self.dbg("AT", AT)
        self.arelease(mark0)
        self.gated_out(l, 2, AT, "wattn", self.mt_first)
        self.mt_first = False

    def moe(self, l):
        T, NT, NB, P, S = self.T, self.NT, self.NB, self.P, self.S
        w = self.dw[l]
        ROWS, CST = self.ROWS[l], self.CST
        WR = self.aalloc([8, 36], BF16)
        LOG = self.aalloc([NB, 36], F32)
        WTOK = self.aalloc([NB, 4, 8], F32)
        WT = self.aalloc([T], F32)
        S.dma("pool", WR[:, :, :], w["wr"].rearrange("(k p) n -> p k n", p=128), writes=[WR.res])
        for blk in range(NB):
            pl = P[6 + blk % 2]
            for k in range(8):
                self.mm(pl[:, 0:36], self.XT[:, k, blk * 128:(blk + 1) * 128], WR[:, k, :], k == 0, k == 7,
                        reads=[(self.XT.res, blk // 4), WR.res], writes=[pl.res])
            self.tt("dve", LOG[:, blk, :], pl[:, 0:36], ROWS.bc(R_RB, [[1, 36]]), ALU.add, reads=[pl.res, ROWS.res],
                    writes=[LOG.res])
        mk = self.amark()
        f = lambda shp: self.aalloc(shp, F32)
        GMAX, GS, GE, GMASK = f([NB]), f([NB]), f([NB, 4]), f([NB, 4])
        V1, V2, P1, P2 = f([NB, 4]), f([NB, 4]), f([NB, 4]), f([NB, 4])
        M1, M2, E2 = f([NB, 4, 8]), f([NB, 4, 8]), f([NB, 4, 8])
        GL = lambda: LOG[:, :, 0:4]
        EL = lambda: LOG.bc(4, [[36, NB], [8, 4], [1, 8]])
        red = lambda out, in_, op, rd, wr: self.S.dve(
            lambda e: e.tensor_reduce(out=out, in_=in_, op=op, axis=AX.X), reads=rd, writes=wr)
        red(GMAX[:, :], GL(), ALU.max, [LOG.res], [GMAX.res])
        self.tt("dve", GE[:, :, :], GL(), GMAX.bc(0, [[1, NB], [0, 4]]), ALU.subtract, reads=[LOG.res, GMAX.res],
                writes=[GE.res])
        self.tt("dve", GMASK[:, :, :], GL(), GMAX.bc(0, [[1, NB], [0, 4]]), ALU.is_equal, reads=[LOG.res, GMAX.res],
                writes=[GMASK.res])
        self.actf(GE[:, :, :], GE[:, :, :], AF.Exp, reads=[GE.res], writes=[GE.res])
        red(GS[:, :], GE[:, :, :], ALU.add, [GE.res], [GS.res])
        self.S.dve(lambda e: e.reciprocal(GS[:, :], GS[:, :]), reads=[GS.res], writes=[GS.res])
        self.tt("dve", GMASK[:, :, :], GMASK[:, :, :], GS.bc(0, [[1, NB], [0, 4]]), ALU.mult,
                reads=[GMASK.res, GS.res], writes=[GMASK.res])
        red(V1[:, :, :], EL(), ALU.max, [LOG.res], [V1.res])
        self.tt("dve", M1[:, :, :, :], EL(), V1.bc(0, [[4, NB], [1, 4], [0, 8]]), ALU.is_equal,
                reads=[LOG.res, V1.res], writes=[M1.res])
        self.stt(E2[:, :, :, :], M1[:, :, :, :], -1.0e30, EL(), ALU.mult, ALU.add, reads=[M1.res, LOG.res],
                 writes=[E2.res])
        red(V2[:, :, :], E2[:, :, :, :], ALU.max, [E2.res], [V2.res])
        self.tt("dve", M2[:, :, :, :], E2[:, :, :, :], V2.bc(0, [[4, NB], [1, 4], [0, 8]]), ALU.is_equal,
                reads=[E2.res, V2.res], writes=[M2.res])
        self.tt("dve", P1[:, :, :], V1[:, :, :], V2[:, :, :], ALU.subtract, reads=[V1.res, V2.res], writes=[P1.res])
        self.actf(P1[:, :, :], P1[:, :, :], AF.Sigmoid, reads=[P1.res], writes=[P1.res])
        self.ts("dve", P2[:, :, :], P1[:, :, :], -1.0, ALU.mult, reads=[P1.res], writes=[P2.res], s2=1.0, op1=ALU.add)
        self.tt("dve", P1[:, :, :], P1[:, :, :], GMASK[:, :, :], ALU.mult, reads=[P1.res, GMASK.res], writes=[P1.res])
        self.tt("dve", P2[:, :, :], P2[:, :, :], GMASK[:, :, :], ALU.mult, reads=[P2.res, GMASK.res], writes=[P2.res])
        self.tt("dve", M1[:, :, :, :], M1[:, :, :, :], P1.bc(0, [[4, NB], [1, 4], [0, 8]]), ALU.mult,
                reads=[M1.res, P1.res], writes=[M1.res])
        self.tt("dve", M2[:, :, :, :], M2[:, :, :, :], P2.bc(0, [[4, NB], [1, 4], [0, 8]]), ALU.mult,
                reads=[M2.res, P2.res], writes=[M2.res])
        self.tt("dve", WTOK[:, :, :, :], M1[:, :, :, :], M2[:, :, :, :], ALU.add, reads=[M1.res, M2.res],
                writes=[WTOK.res])
        self.dbg("WTOK", WTOK)
        wt2 = WTOK.reshape([128, NB, 32])
        for b0 in range(0, NB, 4):
            ps = P[7]
            for j in range(4):
                self.tr(ps[0:32, j * 128:(j + 1) * 128], wt2[:, b0 + j, :], self.cF(K_ID), reads=[WTOK.res, CST.res],
                        writes=[ps.res])
            self.cp("act", WT[0:32, b0 * 128:(b0 + 4) * 128], ps[0:32, :], reads=[ps.res], writes=[WT.res])
        self.arelease(mk)
        NWB = 3
        WG = [self.aalloc([4, 8, 128], BF16) for _ in range(NWB)]
        WU = [self.aalloc([4, 8, 128], BF16) for _ in range(NWB)]
        WD = [self.aalloc([4, 1024], BF16) for _ in range(NWB)]
        HT = [self.aalloc([4, 512], BF16) for _ in range(2)]
        SIL = [self.aalloc([512], F32) for _ in range(2)]
        WBC = [self.aalloc([512], F32) for _ in range(2)]
        SEL = [self.aalloc([128], F32) for _ in range(2)]

        def load_expert(e):
            wg, wu, wd = WG[e % NWB], WU[e % NWB], WD[e % NWB]
            S.dma("pool", wg.bc(0, [[1024, 4], [1, 1024]]), w["wg"][e].rearrange("p m k c -> p m (k c)"),
                  writes=[wg.res])
            S.dma("pool", wu.bc(0, [[1024, 4], [1, 1024]]), w["wu"][e].rearrange("p m k c -> p m (k c)"),
                  writes=[wu.res])
            S.dma("pool", wd[:, :, :], w["wd"][e].rearrange("(k p) n -> p k n", p=128), writes=[wd.res])

        PY = [P[4], P[5], P[6]]
        items = [(e, tt) for e in range(self.nexp) for tt in range(NT)]
        st = {"dc": 0}

        def gate_up(n, e, tt):
            wg, wu = WG[e % NWB], WU[e % NWB]
            sl = slice(tt * 512, (tt + 1) * 512)
            ht, wbc = HT[n % 2], WBC[n % 2]
            sel = SEL[e % 2]
            if tt == 0:
                self.cp("act", sel[0:32, :], CST.bc(K_ID + e, [[0, 128]], npart=32), reads=[CST.res],
                        writes=[sel.res])
            self.mm(P[7][:, :], sel[0:32, :], WT[0:32, sl], True, True, reads=[sel.res, WT.res],
                    writes=[P[7].res])
            self.cp("act", wbc[:, :], P[7][:, :], reads=[P[7].res], writes=[wbc.res])
            for m in range(4):
                pg, pu = P[m % 2], P[2 + m % 2]
                sil = SIL[m % 2]
                for k in range(8):
                    self.mm(pg[:, :], wg[:, m, k, :], self.XT[:, k, sl], k == 0, k == 7,
                            reads=[wg.res, (self.XT.res, tt)], writes=[pg.res])
                for k in range(8):
                    self.mm(pu[:, :], wu[:, m, k, :], self.XT[:, k, sl], k == 0, k == 7,
                            reads=[wu.res, (self.XT.res, tt)], writes=[pu.res])
                self.actf(sil[:, :], pg[:, :], AF.Silu, reads=[pg.res], writes=[sil.res])
                self.tt("dve", sil[:, :], pu[:, :], sil[:, :], ALU.mult, reads=[pu.res, sil.res], writes=[sil.res])
                self.tt("dve", ht[:, m, :], sil[:, :], wbc[:, :], ALU.mult, reads=[sil.res, wbc.res],
                        writes=[ht.res])

        def down(n, e, tt):
            wd = WD[e % NWB]
            sl = slice(tt * 512, (tt + 1) * 512)
            ht = HT[n % 2]
            for dc in range(8):
                py = PY[st["dc"] % 3]
                st["dc"] += 1
                for m in range(4):
                    self.mm(py[:, :], wd[:, m, dc * 128:(dc + 1) * 128], ht[:, m, :], m == 0, m == 3,
                            reads=[wd.res, ht.res], writes=[py.res])
                self.tt("dve", self.X[:, dc, sl], py[:, :], self.X[:, dc, sl], ALU.add,
                        reads=[py.res, (self.X.res, tt)], writes=[(self.X.res, tt)])

        for e in range(min(NWB - 1, self.nexp)):
            load_expert(e)
        for n, (e, tt) in enumerate(items):
            gate_up(n, e, tt)
            if n >= 1:
                pe_, ptt = items[n - 1]
                down(n - 1, pe_, ptt)
                if ptt == NT - 1 and pe_ + NWB < self.nexp + 0 and pe_ + NWB - 1 < self.nexp:
                    pass
            if tt == 0 and e + NWB - 1 < self.nexp and e >= 1:
                pass
            if n >= 1 and items[n - 1][1] == NT - 1:
                nxt = items[n - 1][0] + NWB
                if nxt < self.nexp:
                    load_expert(nxt)
            if n == 0 and NWB - 1 < self.nexp:
                load_expert(NWB - 1)
        down(len(items) - 1, *items[-1])


def host_consts():
    c = np.zeros((128, NCF), np.float32)
    k = np.arange(128)[:, None]
    m = np.arange(128)[None, :]
    c[:, K_ID:K_ID + 128] = (k == m)
    c[:, K_TU:K_TU + 128] = (k <= m)
    c[:, K_TL:K_TL + 128] = (k > m)
    c[:, K_ONE:K_ONE + 128] = 1.0
    R = np.zeros((128, 128), np.float32)
    D0 = np.zeros((128, 128), np.float32)
    D1 = np.zeros((128, 128), np.float32)
    for mm_ in range(128):
        b, j = mm_ // 64 * 64, mm_ % 64
        if j < 32:
            R[b + j + 32, mm_] = -1.0
        else:
            R[b + j - 32, mm_] = 1.0
        D0[j, mm_] = 1.0
        D1[64 + j, mm_] = 1.0
    c[:, K_ROT:K_ROT + 128] = R
    c[:, K_DUP0:K_DUP0 + 128] = D0
    c[:, K_DUP1:K_DUP1 + 128] = D1
    invf = (np.float32(10000.0) ** (-np.arange(32, dtype=np.float32) / np.float32(32))).astype(np.float32)
    c[:, K_INVF] = invf[np.arange(128) % 32]
    c[:, K_EPS] = EPS
    c[:, K_ONEC] = 1.0
    return c


def fm_layout(w):
    K, N = w.shape
    return np.ascontiguousarray(w.reshape(K // 128, 128, N // 128, 128).transpose(2, 1, 0, 3))


def colsT(v, n):
    return np.asarray(v, np.float32).reshape(n, 128).T


def prep_weights(inp, l):
    f = lambda n: np.asarray(inp[n][l], np.float32)
    w_in = f("w_in")
    o = {}
    o["win_fm%d" % l] = fm_layout(np.concatenate(
        [w_in[:, 0:3072], w_in[:, 4096:6144], w_in[:, 6160:8208], w_in[:, 8208:9232], w_in[:, 9232:9488]], axis=1))
    o["win_tm%d" % l] = np.ascontiguousarray(np.concatenate(
        [w_in[:, 3072:4096], w_in[:, 6144:6160], w_in[:, 9488:9744]], axis=1))
    for nm, src in (("wssd", "ssd_w_out"), ("wconf", "conf_w_out"), ("wattn", "attn_w_out"), ("wout", "w_out"),
                    ("wpg", "ple_w_gate"), ("wpp", "ple_w_proj")):
        o["%s%d" % (nm, l)] = fm_layout(f(src))
    o["wr%d" % l] = np.ascontiguousarray(np.concatenate([f("moe_w_group"), f("moe_w_expert")], axis=1))
    wg, wu = f("moe_w_gate"), f("moe_w_up")
    o["wg%d" % l] = np.ascontiguousarray(wg.reshape(NEXP, 8, 128, 4, 128).transpose(0, 2, 3, 1, 4))
    o["wu%d" % l] = np.ascontiguousarray(wu.reshape(NEXP, 8, 128, 4, 128).transpose(0, 2, 3, 1, 4))
    o["wd%d" % l] = np.ascontiguousarray(f("moe_w_down"))
    cols = np.zeros((128, NCOLS), np.float32)
    cols[:, C_BGATE:C_BGATE + 24] = colsT(f("b_gate"), 24)
    cols[:, C_SCW:C_SCW + 64] = f("ssd_conv_w").reshape(4, 16, 128).transpose(2, 1, 0).reshape(128, 64)
    cols[:, C_SCB:C_SCB + 16] = colsT(f("ssd_conv_b"), 16)
    cols[:, C_CDW:C_CDW + 248] = f("conf_dw_w").reshape(31, 8, 128).transpose(2, 1, 0).reshape(128, 248)
    for c0, nm in ((C_CDB, "conf_dw_b"), (C_CLG, "conf_ln_g"), (C_CLB, "conf_ln_b"), (C_NW, "ssd_norm_w"),
                   (C_L1G, "ln1_g"), (C_L1B, "ln1_b"), (C_L2G, "ln2_g"), (C_L2B, "ln2_b")):
        cols[:, c0:c0 + 8] = colsT(f(nm), 8)
    o["cols%d" % l] = cols
    rows = np.zeros((1, NROWS), np.float32)
    rows[0, R_DTB:R_DTB + 16] = f("ssd_dt_bias")
    rows[0, R_ALOG:R_ALOG + 16] = f("ssd_a_log")
    rows[0, R_D:R_D + 16] = f("ssd_d")
    rows[0, R_SINK:R_SINK + 16] = f("attn_sinks")
    rows[0, R_RB:R_RB + 4] = f("moe_b_group")
    rows[0, R_RB + 4:R_RB + 36] = f("moe_b_expert")
    o["rows%d" % l] = rows
    return o


def make_in_maps(inp, seq_lists, nlayer=DEPTH):
    shared = {"cst": host_consts()}
    for l in range(nlayer):
        shared.update(prep_weights(inp, l))
    maps = []
    for seqs in seq_lists:
        m = dict(shared)
        m["x"] = np.ascontiguousarray(np.asarray(inp["x"], np.float32)[seqs])
        m["p"] = np.ascontiguousarray(np.asarray(inp["p"], np.float32)[:, seqs])
        m["pos"] = np.ascontiguousarray(np.asarray(inp["positions"], np.int32)[seqs])
        maps.append(m)
    return maps


_NC_CACHE = {}


def kernel(**inputs):
    if "nc" not in _NC_CACHE:
        _NC_CACHE["nc"] = Builder().nc
    nc = _NC_CACHE["nc"]
    seq_lists = [list(range(c * SEQ_PER_CORE, (c + 1) * SEQ_PER_CORE)) for c in range(NCORES)]
    maps = make_in_maps(inputs, seq_lists)
    res = run_bass_kernel_spmd(nc, maps, core_ids=list(range(NCORES)))
    out = np.concatenate([np.asarray(r["out"], np.float32) for r in res.results], axis=0)
    return out
```

```python
import contextlib
import math
import numpy as np
import concourse.bass as bass
import concourse.mybir as mybir
from concourse.bass_utils import run_bass_kernel_spmd

F32 = mybir.dt.float32
BF16 = mybir.dt.bfloat16
I32 = mybir.dt.int32
AF = mybir.ActivationFunctionType
ALU = mybir.AluOpType
AX = mybir.AxisListType

ENGS = ("pe", "act", "dve", "pool", "sp")
NDMASEM = 8

D = 1024
SEQ = 2048
DEPTH = 2
NCORES = 8
SEQ_PER_CORE = 4
PLE = 256
NEXP = 32
FF = 512
ALPHA = (2 * DEPTH) ** 0.25
EPS = 1e-5
OFF_GATE, OFF_Z, OFF_XBC, OFF_DT, OFF_CONF, OFF_Q, OFF_K, OFF_V = 0, 3072, 4096, 6144, 6160, 8208, 9232, 9488
FM_GATE, FM_XBC, FM_CONF, FM_Q, FM_K = 0, 24, 40, 56, 64
NFM = 66
C_BGATE = 0
C_SCW = 24
C_SCB = 88
C_CDW = 104
C_CDB = 352
C_CLG = 360
C_CLB = 368
C_NW = 376
C_L1G = 384
C_L1B = 392
C_L2G = 400
C_L2B = 408
NCOLS = 416
R_DTB, R_ALOG, R_D, R_SINK, R_RB = 0, 16, 32, 48, 64
NROWS = 100
K_ID, K_TU, K_TL, K_ONE, K_ROT, K_DUP0, K_DUP1, K_INVF, K_EPS, K_ONEC = 0, 128, 256, 384, 512, 640, 768, 896, 897, 898
NCF = 900


class Op:
    __slots__ = ("eng", "fn", "dma", "idx", "tick", "sem_i", "sem_v", "deps", "prewait")


class Sched:
    def __init__(self, nc):
        self.nc = nc
        self.ops = []
        self.state = {}
        self.cnt = {e: 0 for e in ENGS}
        self.dcnt = {e: 0 for e in ENGS}
        self.dma_hist = {e: [] for e in ENGS}
        self.last = {e: None for e in ENGS}
        self.pending = {e: set() for e in ENGS}
        self.exclusive = set()

    def fence(self):
        F = set()
        for e in ENGS:
            if self.last[e] is not None:
                F.add(self.last[e])
            for op in self.dma_hist[e][-NDMASEM:]:
                F.add(op)
        for e in ENGS:
            self.pending[e] |= F

    @staticmethod
    def _norm(lst):
        out = []
        for r in lst:
            if isinstance(r, tuple):
                out.append((id(r[0]), r[1]))
            else:
                out.append((id(r), None))
        return out

    def _conf(self, res):
        b, k = res
        d = self.state.get(b)
        if d is None:
            return
        if k is None:
            for st in d.values():
                yield st
        else:
            st = d.get(k)
            if st is not None:
                yield st
            st = d.get(None)
            if st is not None:
                yield st

    def add(self, eng, fn, reads=(), writes=(), dma=False):
        reads = self._norm(reads)
        writes = self._norm(writes)
        if self.exclusive:
            ex = [r for r in reads if r[0] in self.exclusive]
            if ex:
                reads = [r for r in reads if r[0] not in self.exclusive]
                writes = writes + [r for r in ex if r not in writes]
        op = Op()
        op.eng, op.fn, op.dma, op.prewait = eng, fn, dma, None
        op.idx = len(self.ops)
        deps = set()
        for r in reads:
            for st in self._conf(r):
                if st[0] is not None:
                    deps.add(st[0])
        for w in writes:
            for st in self._conf(w):
                if st[0] is not None:
                    deps.add(st[0])
                deps.update(st[1])
        if self.pending[eng]:
            deps |= self.pending[eng]
            self.pending[eng] = set()
        op.deps = deps
        for w in writes:
            b, k = w
            d = self.state.setdefault(b, {})
            if k is None:
                d.clear()
            d[k] = [op, []]
        for r in reads:
            b, k = r
            d = self.state.setdefault(b, {})
            st = d.get(k)
            if st is None:
                st = [None, []]
                for s2 in self._conf(r):
                    if s2[0] is not None and (st[0] is None or s2[0].idx > st[0].idx):
                        st[0] = s2[0]
                d[k] = st
            st[1].append(op)
        if dma:
            i = self.dcnt[eng]
            self.dcnt[eng] += 1
            op.sem_i = i % NDMASEM
            op.sem_v = 16 * (i // NDMASEM + 1)
            hist = self.dma_hist[eng]
            if i >= NDMASEM:
                op.prewait = hist[i - NDMASEM]
            hist.append(op)
            op.tick = None
        else:
            self.cnt[eng] += 1
            op.tick = self.cnt[eng]
            self.last[eng] = op
        self.ops.append(op)
        return op

    def pe(self, fn, reads=(), writes=()):
        return self.add("pe", fn, reads, writes)

    def act(self, fn, reads=(), writes=()):
        return self.add("act", fn, reads, writes)

    def dve(self, fn, reads=(), writes=()):
        return self.add("dve", fn, reads, writes)

    def pool(self, fn, reads=(), writes=()):
        return self.add("pool", fn, reads, writes)

    def dma(self, eng, out, in_, reads=(), writes=()):
        return self.add(eng, lambda e: e.dma_start(out=out, in_=in_), reads, writes, dma=True)

    def emit(self):
        nc = self.nc
        with contextlib.ExitStack() as es:
            esem = {e: es.enter_context(nc.semaphore("s_" + e)) for e in ENGS}
            dsem = {e: [es.enter_context(nc.semaphore("d_%s%d" % (e, i))) for i in range(NDMASEM)]
                    for e in ENGS if self.dcnt[e] > 0}
            block = es.enter_context(nc.Block())
            per = {e: [op for op in self.ops if op.eng == e] for e in ENGS}

            def run(engname, eng):
                waited = {}

                def wait_for(dep):
                    if dep.dma:
                        s = dsem[dep.eng][dep.sem_i]
                        v = dep.sem_v
                    else:
                        if dep.eng == "pe" and engname == "pe":
                            return
                        s = esem[dep.eng]
                        v = dep.tick
                    key = id(s)
                    if waited.get(key, 0) >= v:
                        return
                    waited[key] = v
                    eng.wait_ge(s, v)

                for op in per[engname]:
                    for dep in sorted(op.deps, key=lambda d: d.idx):
                        wait_for(dep)
                    if op.prewait is not None:
                        wait_for(op.prewait)
                    inst = op.fn(eng)
                    if op.dma:
                        inst.then_inc(dsem[engname][op.sem_i], 16)
                    else:
                        inst.then_inc(esem[engname], 1)
                for op in self.dma_hist[engname][-NDMASEM:]:
                    wait_for(op)

            @block.sync
            def _(eng):
                run("sp", eng)

            @block.tensor
            def _(eng):
                run("pe", eng)

            @block.scalar
            def _(eng):
                run("act", eng)

            @block.vector
            def _(eng):
                run("dve", eng)

            @block.gpsimd
            def _(eng):
                run("pool", eng)


def pstride(t):
    return int(np.prod(list(t.shape)[1:]))


class View:
    def __init__(self, h, off, shape, res=None):
        self.h, self.off, self.shape = h, off, list(shape)
        self.dtype = h.dtype
        st, s = [], 1
        for n in reversed(self.shape[1:]):
            st.append(s)
            s *= n
        self.strides = list(reversed(st))
        self.ps = pstride(h)
        self.res = self if res is None else res

    def __getitem__(self, idx):
        if not isinstance(idx, tuple):
            idx = (idx,)
        idx = list(idx) + [slice(None)] * (len(self.shape) - len(idx))
        p = idx[0]
        p0, p1, _ = p.indices(self.shape[0])
        off = self.off
        dims = []
        for i, ix in enumerate(idx[1:]):
            stride, n = self.strides[i], self.shape[i + 1]
            if isinstance(ix, int):
                off += ix * stride
            else:
                a, b, st = ix.indices(n)
                cnt = len(range(a, b, st))
                off += a * stride
                dims.append([stride * st, cnt])
        m = [list(d) for d in dims]
        if not m:
            m = [[1, 1]]
        return bass.AP(self.h, p0 * self.ps + off, [[self.ps, p1 - p0]] + m)

    def bc(self, off, dims, npart=128, p0=0):
        return bass.AP(self.h, p0 * self.ps + self.off + off, [[self.ps, npart]] + [list(d) for d in dims])

    def reshape(self, shape):
        return View(self.h, self.off, shape, res=self.res)


class Builder:
    def __init__(self, nseq=SEQ_PER_CORE, nlayer=DEPTH, T=1024, nseg=None, debug=(), nexp=NEXP, stop_after=None,
                 only=None):
        self.nseq, self.nlayer, self.T = nseq, nlayer, T
        self.NT = T // 512
        self.NB = T // 128
        self.nseg = (SEQ // T) if nseg is None else nseg
        self.debug = set(debug)
        self.nexp = nexp
        self.stop_after = stop_after
        self.only = None if only is None else set(only.split(',')) if isinstance(only, str) else set(only)
        self.dbg_outs = {}
        nc = self.nc = bass.Bass("TRN2", target_bir_lowering=False)
        self.S = Sched(nc)
        self.declare_dram()
        self.alloc()
        self.program()
        self.S.emit()

    def din(self, name, shape, dt=F32):
        return self.nc.dram_tensor(name, list(shape), dt, kind="ExternalInput").ap()

    def pers(self, name, shape, dt):
        h = self.nc.alloc_sbuf_tensor(name, list(shape), dt)
        return View(h, 0, shape)

    def areset(self):
        self.S.fence()
        self.aoff = 0

    def aalloc(self, fshape, dt, npart=128):
        size = {F32: 4, BF16: 2, I32: 4}[dt]
        n = int(np.prod(fshape)) * size
        n = (n + 31) // 32 * 32
        off = self.aoff
        self.aoff += n
        assert self.aoff <= self.arena_bytes, ("arena overflow", self.aoff, self.arena_bytes)
        return View(self.arena[dt], off // size, [npart] + list(fshape))

    def dbg(self, name, view, reads=None):
        if name not in self.debug or name in self.dbg_outs:
            return
        o = self.nc.dram_tensor("dbg_" + name, list(view.shape), view.dtype, kind="ExternalOutput").ap()
        self.dbg_outs[name] = o
        self.S.dma("sp", o, view[:], reads=[view.res] if reads is None else reads)

    def declare_dram(self):
        L = self.nlayer
        self.d_x = self.din("x", [self.nseq, SEQ, D])
        self.d_p = self.din("p", [DEPTH, self.nseq, SEQ, PLE])
        self.d_pos = self.din("pos", [self.nseq, SEQ], I32)
        self.d_cst = self.din("cst", [128, NCF])
        self.d_out = self.nc.dram_tensor("out", [self.nseq, SEQ, D], F32, kind="ExternalOutput").ap()
        self.dw = []
        for l in range(L):
            w = {}
            w["win_fm"] = self.din("win_fm%d" % l, [NFM, 128, 8, 128])
            w["win_tm"] = self.din("win_tm%d" % l, [D, 1296])
            for nm in ("wssd", "wconf", "wattn", "wout", "wpg"):
                w[nm] = self.din("%s%d" % (nm, l), [8, 128, 8, 128])
            w["wpp"] = self.din("wpp%d" % l, [8, 128, 2, 128])
            w["wr"] = self.din("wr%d" % l, [D, 36])
            w["wg"] = self.din("wg%d" % l, [NEXP, 128, 4, 8, 128])
            w["wu"] = self.din("wu%d" % l, [NEXP, 128, 4, 8, 128])
            w["wd"] = self.din("wd%d" % l, [NEXP, FF, D])
            w["cols"] = self.din("cols%d" % l, [128, NCOLS])
            w["rows"] = self.din("rows%d" % l, [1, NROWS])
            self.dw.append(w)

    def alloc(self):
        T, nc, L = self.T, self.nc, self.nlayer
        pers = self.pers
        self.X = pers("X", [128, 8, T], F32)
        self.XT = pers("XT", [128, 8, T], BF16)
        self.MT = pers("MT", [128, 8, T], BF16)
        self.CST = pers("CST", [128, NCF], F32)
        self.CSB = pers("CSB", [128, NCF], BF16)
        self.COLS = [pers("COLS%d" % l, [128, NCOLS], F32) for l in range(L)]
        self.COLA = [pers("COLA%d" % l, [128, 16], F32) for l in range(L)]
        self.ROWS = [pers("ROWS%d" % l, [128, NROWS], F32) for l in range(L)]
        self.ROWX = [pers("ROWX%d" % l, [128, 32], F32) for l in range(L)]
        self.H = [pers("H%d" % l, [128, 1024], F32) for l in range(L)]
        self.CTAIL = [pers("CTAIL%d" % l, [128, 8, 32], BF16) for l in range(L)]
        self.STAIL = [pers("STAIL%d" % l, [128, 16, 4], BF16) for l in range(L)]
        self.KCAR = [pers("KCAR%d" % l, [128, 4, 128], BF16) for l in range(L)]
        self.VCAR = [pers("VCAR%d" % l, [128, 4, 65], BF16) for l in range(L)]
        ph = [nc.alloc_psum_tensor("P%d" % i, [128, 512], F32) for i in range(8)]
        self.P = [View(h, 0, [128, 512]) for h in ph]
        self.PB = [View(h.bitcast(BF16), 0, [128, 1024], res=v) for h, v in zip(ph, self.P)]
        self.S.exclusive = {id(v) for v in self.P}
        rem = nc.sbuf_bytes_remaining
        rem = rem() if callable(rem) else rem
        self.arena_bytes = (int(rem) - 8192) // 64 * 64
        ah = nc.alloc_sbuf_tensor("ARENA", [128, self.arena_bytes // 4], F32)
        self.arena = {F32: ah, BF16: ah.bitcast(BF16), I32: ah.bitcast(I32)}
        self.aoff = 0

    def mm(self, out, lhsT, rhs, start, stop, reads, writes):
        self.S.pe(lambda e: e.matmul(out, lhsT, rhs, start=start, stop=stop), reads, writes)

    def tr(self, out, in_, ident, reads, writes):
        self.S.pe(lambda e: e.transpose(out, in_, ident), reads, writes)

    def actf(self, out, in_, func, reads, writes, scale=1.0, bias=None):
        if bias is None:
            self.S.act(lambda e: e.activation(out=out, in_=in_, func=func, scale=scale), reads, writes)
        else:
            self.S.act(lambda e: e.activation(out=out, in_=in_, func=func, scale=scale, bias=bias), reads, writes)

    def tt(self, eng, out, in0, in1, op, reads, writes):
        self.S.add(eng, lambda e: e.tensor_tensor(out=out, in0=in0, in1=in1, op=op), reads, writes)

    def ts(self, eng, out, in0, s1, op0, reads, writes, s2=None, op1=None):
        if op1 is None:
            self.S.add(eng, lambda e: e.tensor_scalar(out=out, in0=in0, scalar1=s1, scalar2=None, op0=op0), reads, writes)
        else:
            self.S.add(eng, lambda e: e.tensor_scalar(out=out, in0=in0, scalar1=s1, scalar2=s2, op0=op0, op1=op1),
                       reads, writes)

    def stt(self, out, in0, scalar, in1, op0, op1, reads, writes):
        self.S.dve(lambda e: e.scalar_tensor_tensor(out=out, in0=in0, scalar=scalar, in1=in1, op0=op0, op1=op1),
                   reads, writes)

    def cp(self, eng, out, in_, reads, writes):
        if eng == "act":
            self.S.act(lambda e: e.copy(out, in_), reads, writes)
        else:
            self.S.add(eng, lambda e: e.tensor_copy(out, in_), reads, writes)

    def memset(self, eng, ap, val, writes):
        self.S.add(eng, lambda e: e.memset(ap, val), (), writes)

    def cF(self, off, n=128, npart=128, p0=0):
        return self.CST[p0:p0 + npart, off:off + n]

    def cB(self, off, n=128, npart=128, p0=0):
        return self.CSB[p0:p0 + npart, off:off + n]

    def col(self, l, c, npart=128):
        return self.COLS[l][0:npart, c:c + 1]

    def proj_fm(self, srcs, nchunk, consumer, wbufs, psums):
        NT = self.NT
        cnt = 0
        depth = min(len(wb) for wb in wbufs) - 1

        def issue(i):
            for si, (wsrc, c0, KC, rhs_fn) in enumerate(srcs):
                wb = wbufs[si][i % len(wbufs[si])]
                self.S.dma("pool", wb[:, 0:KC, :], wsrc[c0 + i], writes=[wb.res])

        for i in range(min(depth, nchunk)):
            issue(i)
        for i in range(nchunk):
            if i + depth < nchunk:
                issue(i + depth)
            wbs = [wbufs[si][i % len(wbufs[si])] for si in range(len(srcs))]
            for tt in range(NT):
                pss = []
                for si, (wsrc, c0, KC, rhs_fn) in enumerate(srcs):
                    ps = psums[si][cnt % len(psums[si])]
                    for k in range(KC):
                        rap, rres = rhs_fn(k, tt)
                        self.mm(ps[:, :], wbs[si][:, k, :], rap, k == 0, k == KC - 1,
                                reads=[wbs[si].res, rres], writes=[ps.res])
                    pss.append(ps)
                cnt += 1
                consumer(i, tt, pss)

    def xt_rhs(self, k, tt):
        return self.XT[:, k, tt * 512:(tt + 1) * 512], (self.XT.res, tt)

    def gated_out(self, l, branch, YT, wname, first):
        w = self.dw[l]
        wbA = [self.aalloc([8, 128], BF16) for _ in range(3)]
        wbB = [self.aalloc([8, 128], BF16) for _ in range(3)]
        SG = [self.aalloc([512], F32) for _ in range(2)]
        TM = [self.aalloc([512], BF16) for _ in range(2)]
        P = self.P
        st = {"n": 0}

        def yt_rhs(k, tt):
            return YT[:, k, tt * 512:(tt + 1) * 512], (YT.res, tt)

        def consumer(i, tt, pss):
            n = st["n"]
            st["n"] += 1
            sg = SG[n % 2]
            self.actf(sg[:, :], pss[1][:, :], AF.Sigmoid, reads=[pss[1].res], writes=[sg.res],
                      bias=self.col(l, C_BGATE + branch * 8 + i))
            dst = self.MT[:, i, tt * 512:(tt + 1) * 512]
            if first:
                self.tt("dve", dst, pss[0][:, :], sg[:, :], ALU.mult, reads=[pss[0].res, sg.res],
                        writes=[(self.MT.res, tt)])
            else:
                tm = TM[n % 2]
                self.tt("dve", tm[:, :], pss[0][:, :], sg[:, :], ALU.mult, reads=[pss[0].res, sg.res],
                        writes=[tm.res])
                self.tt("dve", dst, dst, tm[:, :], ALU.add, reads=[tm.res, (self.MT.res, tt)],
                        writes=[(self.MT.res, tt)])

        self.proj_fm([(w[wname], 0, 8, yt_rhs), (w["win_fm"], FM_GATE + branch * 8, 8, self.xt_rhs)], 8, consumer,
                     [wbA, wbB], [[P[0], P[1]], [P[2], P[3]]])

    def ln_stats(self, SRC, tt, tmp, banks):
        SQ, MEAN, RSTD, TN = tmp
        B1, B2 = banks
        sl = slice(tt * 512, (tt + 1) * 512)
        for m in range(8):
            self.mm(B1[:, :], self.cF(K_ONE), SRC[:, m, sl], m == 0, m == 7,
                    reads=[self.CST.res, (SRC.res, tt)], writes=[B1.res])
        for m in range(8):
            sq = SQ[m % 2]
            self.actf(sq[:, :], SRC[:, m, sl], AF.Square, reads=[(SRC.res, tt)], writes=[sq.res])
            self.mm(B2[:, :], self.cF(K_ONE), sq[:, :], m == 0, m == 7, reads=[self.CST.res, sq.res],
                    writes=[B2.res])
        self.ts("dve", MEAN[:, :], B1[:, :], 1.0 / 1024, ALU.mult, reads=[B1.res], writes=[MEAN.res])
        self.tt("dve", RSTD[:, :], MEAN[:, :], MEAN[:, :], ALU.mult, reads=[MEAN.res], writes=[RSTD.res])
        self.stt(RSTD[:, :], B2[:, :], 1.0 / 1024, RSTD[:, :], ALU.mult, ALU.subtract, reads=[B2.res, RSTD.res],
                 writes=[RSTD.res])
        self.actf(RSTD[:, :], RSTD[:, :], AF.Sqrt, reads=[RSTD.res], writes=[RSTD.res], bias=self.cF(K_EPS, 1))
        self.S.dve(lambda e: e.reciprocal(RSTD[:, :], RSTD[:, :]), reads=[RSTD.res], writes=[RSTD.res])

    def ln_norm(self, SRC, tt, out_fn, tmp):
        SQ, MEAN, RSTD, TN = tmp
        sl = slice(tt * 512, (tt + 1) * 512)
        for m in range(8):
            tn = TN[m % 2]
            self.tt("dve", tn[:, :], SRC[:, m, sl], MEAN[:, :], ALU.subtract, reads=[(SRC.res, tt), MEAN.res],
                    writes=[tn.res])
            self.tt("dve", tn[:, :], tn[:, :], RSTD[:, :], ALU.mult, reads=[tn.res, RSTD.res], writes=[tn.res])
            out_fn(m, tn)

    def ln_all(self, SRC, make_out_fn):
        P = self.P
        tmps = [self.ln_tmp() for _ in range(self.NT)]
        banks = [(P[6], P[7]), (P[4], P[5])]
        for tt in range(self.NT):
            self.ln_stats(SRC, tt, tmps[tt], banks[tt % 2])
        for tt in range(self.NT):
            self.ln_norm(SRC, tt, make_out_fn(tt), tmps[tt])

    def ln_tmp(self):
        return ([self.aalloc([512], F32) for _ in range(2)], self.aalloc([512], F32), self.aalloc([512], F32),
                [self.aalloc([512], F32) for _ in range(2)])

    def program(self):
        S = self.S
        S.dma("sp", self.CST[:, :], self.d_cst, writes=[self.CST.res])
        self.cp("dve", self.CSB[:, :], self.CST[:, :], reads=[self.CST.res], writes=[self.CSB.res])
        import os
        self.bis = int(os.environ.get("BIS", "0"))
        for l in range(self.nlayer):
            if self.bis & 1:
                break
            S.dma("sp", self.COLS[l][:, :], self.dw[l]["cols"], writes=[self.COLS[l].res])
            S.dma("sp", self.ROWS[l][:, :], bass.AP(self.dw[l]["rows"].tensor, 0, [[0, 128], [1, NROWS]]),
                  writes=[self.ROWS[l].res])
            self.actf(self.ROWX[l][:, 0:16], self.ROWS[l][:, R_ALOG:R_ALOG + 16], AF.Exp, reads=[self.ROWS[l].res],
                      writes=[self.ROWX[l].res])
            self.ts("dve", self.ROWX[l][:, 0:16], self.ROWX[l][:, 0:16], -1.0, ALU.mult, reads=[self.ROWX[l].res],
                    writes=[self.ROWX[l].res])
            self.actf(self.ROWX[l][:, 16:32], self.ROWS[l][:, R_SINK:R_SINK + 16], AF.Exp, reads=[self.ROWS[l].res],
                      writes=[self.ROWX[l].res])
            self.ts("dve", self.COLA[l][:, 0:16], self.COLS[l][:, C_L1G:C_L1G + 16], float(ALPHA), ALU.mult,
                    reads=[self.COLS[l].res], writes=[self.COLA[l].res])
        for seq in range(self.nseq):
            for seg in range(self.nseg):
                self.segment(seq, seg)

    def segment(self, seq, seg):
        T = self.T
        t0 = seg * T
        first = seg == 0
        self.areset()
        if not self.bis & 4:
            self.load_x(seq, t0)
        if first and not self.bis & 2:
            for l in range(self.nlayer):
                self.memset("pool", self.H[l][:, :], 0.0, [self.H[l].res])
                self.memset("pool", self.CTAIL[l][:, :, :], 0.0, [self.CTAIL[l].res])
                self.memset("pool", self.STAIL[l][:, :, :], 0.0, [self.STAIL[l].res])
        for l in range(self.nlayer):
            self.mt_first = True
            stages = [("ssd", lambda: self.ssd(l, first)),
                      ("conf", lambda: self.conformer(l)),
                      ("attn", lambda: self.attention(l, seq, t0, first)),
                      ("ln1", lambda: self.outproj_ln1(l)),
                      ("moe", lambda: self.moe(l)),
                      ("ple", lambda: self.ln2_ple(l, seq, t0))]
            for nm, fn in stages:
                if self.only is not None and nm not in self.only:
                    continue
                self.areset()
                fn()
                self.dbg("MT", self.MT)
                if self.stop_after == (l, nm):
                    break
            if self.stop_after is not None and self.stop_after[0] == l:
                break
        self.areset()
        if not self.bis & 8:
            self.store_x(seq, t0)

    def load_x(self, seq, t0):
        S, P = self.S, self.P
        STG = [self.aalloc([1024], F32) for _ in range(2)]
        for blk in range(self.NB):
            stg = STG[blk % 2]
            tt = blk // 4
            S.dma("sp", stg[:, :], self.d_x[seq, t0 + blk * 128:t0 + (blk + 1) * 128, :], writes=[stg.res])
            for half in range(2):
                ps = P[(blk * 2 + half) % 4].reshape([128, 4, 128])
                for j in range(4):
                    c = half * 4 + j
                    self.tr(ps[:, j, :], stg[:, c * 128:(c + 1) * 128], self.cF(K_ID), reads=[stg.res, self.CST.res],
                            writes=[ps.res])
                self.cp("act", self.X[:, half * 4:half * 4 + 4, blk * 128:(blk + 1) * 128], ps[:, :, :],
                        reads=[ps.res], writes=[(self.X.res, tt)])
                self.cp("dve", self.XT[:, half * 4:half * 4 + 4, blk * 128:(blk + 1) * 128], ps[:, :, :],
                        reads=[ps.res], writes=[(self.XT.res, tt)])

    def store_x(self, seq, t0):
        S, P = self.S, self.P
        STG = [self.aalloc([1024], F32) for _ in range(2)]
        for blk in range(self.NB):
            stg = STG[blk % 2]
            tt = blk // 4
            for half in range(2):
                ps = P[(blk * 2 + half) % 4].reshape([128, 4, 128])
                for j in range(4):
                    c = half * 4 + j
                    self.tr(ps[:, j, :], self.X[:, c, blk * 128:(blk + 1) * 128], self.cF(K_ID),
                            reads=[(self.X.res, tt), self.CST.res], writes=[ps.res])
                self.cp("act" if half == 0 else "dve", stg[:, half * 512:(half + 1) * 512],
                        ps[:, :, :], reads=[ps.res], writes=[stg.res])
            S.dma("sp", self.d_out[seq, t0 + blk * 128:t0 + (blk + 1) * 128, :], stg[:, :], reads=[stg.res])

    def conformer(self, l):
        T, NT, P, S = self.T, self.NT, self.P, self.S
        w = self.dw[l]
        HT = self.aalloc([8, T], BF16)
        mark0 = self.amark()
        HPAD = [self.aalloc([32 + T], BF16) for _ in range(2)]
        DG = [self.aalloc([31, 128], BF16) for _ in range(2)]
        CO = self.aalloc([8, T], F32)
        SG = [self.aalloc([512], F32) for _ in range(2)]
        wbA = [self.aalloc([8, 128], BF16) for _ in range(2)]
        wbG = [self.aalloc([8, 128], BF16) for _ in range(2)]

        def cload(m):
            S.dma("pool", wbA[m % 2][:, :, :], w["win_fm"][FM_CONF + m], writes=[wbA[m % 2].res])
            S.dma("pool", wbG[m % 2][:, :, :], w["win_fm"][FM_CONF + 8 + m], writes=[wbG[m % 2].res])

        def conf_a(m):
            hp, dg = HPAD[m % 2], DG[m % 2]
            self.cp("act", hp[:, 0:32], self.CTAIL[l][:, m, :], reads=[self.CTAIL[l].res], writes=[hp.res])
            self.tt("dve", dg[:, :, :], self.CSB.bc(K_ID, [[0, 31], [1, 128]]),
                    self.COLS[l].bc(C_CDW + m * 31, [[1, 31], [0, 128]]), ALU.mult,
                    reads=[self.CSB.res, self.COLS[l].res], writes=[dg.res])
            wa, wg = wbA[m % 2], wbG[m % 2]
            if m == 0:
                cload(0)
            if m + 1 < 8:
                cload(m + 1)
            for tt in range(NT):
                sl = slice(tt * 512, (tt + 1) * 512)
                a_ps, g_ps = P[tt % 2], P[2 + tt % 2]
                for k in range(8):
                    self.mm(a_ps[:, :], wa[:, k, :], self.XT[:, k, sl], k == 0, k == 7,
                            reads=[wa.res, (self.XT.res, tt)], writes=[a_ps.res])
                for k in range(8):
                    self.mm(g_ps[:, :], wg[:, k, :], self.XT[:, k, sl], k == 0, k == 7,
                            reads=[wg.res, (self.XT.res, tt)], writes=[g_ps.res])
                sg = SG[tt % 2]
                self.actf(sg[:, :], g_ps[:, :], AF.Sigmoid, reads=[g_ps.res], writes=[sg.res])
                self.tt("dve", hp[:, 32 + tt * 512:32 + (tt + 1) * 512], a_ps[:, :], sg[:, :], ALU.mult,
                        reads=[a_ps.res, sg.res], writes=[hp.res])

        def conf_b(m):
            hp, dg = HPAD[m % 2], DG[m % 2]
            for tt in range(NT):
                sl = slice(tt * 512, (tt + 1) * 512)
                c_ps = P[4 + tt % 2]
                for k in range(31):
                    self.mm(c_ps[:, :], dg[:, k, :], hp[:, 2 + k + tt * 512:2 + k + (tt + 1) * 512], k == 0, k == 30,
                            reads=[dg.res, hp.res], writes=[c_ps.res])
                self.actf(CO[:, m, sl], c_ps[:, :], AF.Identity, reads=[c_ps.res], writes=[(CO.res, tt)],
                          bias=self.col(l, C_CDB + m))
            self.cp("act", self.CTAIL[l][:, m, :], hp[:, T:T + 32], reads=[hp.res], writes=[self.CTAIL[l].res])

        conf_a(0)
        for m in range(8):
            if m + 1 < 8:
                conf_a(m + 1)
            conf_b(m)
        self.dbg("CO", CO)
        def mk_conf(tt):
            sl = slice(tt * 512, (tt + 1) * 512)

            def out_fn(m, tn):
                self.actf(HT[:, m, sl], tn[:, :], AF.Silu, reads=[tn.res], writes=[(HT.res, tt)],
                          scale=self.col(l, C_CLG + m), bias=self.col(l, C_CLB + m))
            return out_fn

        self.ln_all(CO, mk_conf)
        self.dbg("HT", HT)
        self.arelease(mark0)
        self.gated_out(l, 1, HT, "wconf", self.mt_first)
        self.mt_first = False

    def outproj_ln1(self, l):
        T, NT, P, S = self.T, self.NT, self.P, self.S
        w = self.dw[l]
        wb = [self.aalloc([8, 128], BF16) for _ in range(3)]
        self.dbg("MT", self.MT)

        def mt_rhs(k, tt):
            return self.MT[:, k, tt * 512:(tt + 1) * 512], (self.MT.res, tt)

        def consumer(i, tt, pss):
            sl = slice(tt * 512, (tt + 1) * 512)
            self.stt(self.X[:, i, sl], self.X[:, i, sl], float(ALPHA), pss[0][:, :], ALU.mult, ALU.add,
                     reads=[(self.X.res, tt), pss[0].res], writes=[(self.X.res, tt)])

        self.proj_fm([(w["wout"], 0, 8, mt_rhs)], 8, consumer, [wb], [[P[0], P[1]]])
        def mk_ln1(tt):
            sl = slice(tt * 512, (tt + 1) * 512)

            def out_fn(m, tn):
                self.actf(self.XT[:, m, sl], tn[:, :], AF.Identity, reads=[tn.res], writes=[(self.XT.res, tt)],
                          scale=self.col(l, C_L1G + m), bias=self.col(l, C_L1B + m))
                self.actf(self.X[:, m, sl], tn[:, :], AF.Identity, reads=[tn.res], writes=[(self.X.res, tt)],
                          scale=self.COLA[l][:, m:m + 1], bias=self.COLA[l][:, 8 + m:9 + m])
            return out_fn

        self.ln_all(self.X, mk_ln1)
        self.dbg("X1T", self.XT)

    def ln2_ple(self, l, seq, t0):
        T, NT, NB, P, S = self.T, self.NT, self.NB, self.P, self.S
        w = self.dw[l]
        self.dbg("XMOE", self.X)
        def mk_ln2(tt):
            sl = slice(tt * 512, (tt + 1) * 512)

            def out_fn(m, tn):
                self.actf(self.XT[:, m, sl], tn[:, :], AF.Identity, reads=[tn.res], writes=[(self.XT.res, tt)],
                          scale=self.col(l, C_L2G + m), bias=self.col(l, C_L2B + m))
                self.actf(self.X[:, m, sl], tn[:, :], AF.Identity, reads=[tn.res], writes=[(self.X.res, tt)],
                          scale=self.col(l, C_L2G + m), bias=self.col(l, C_L2B + m))
            return out_fn

        self.ln_all(self.X, mk_ln2)
        self.dbg("X2T", self.XT)
        PSTG = [self.aalloc([256], F32) for _ in range(2)]
        PT = self.aalloc([2, T], BF16)
        for blk in range(NB):
            stg = PSTG[blk % 2]
            S.dma("sp", stg[:, :], self.d_p[l, seq, t0 + blk * 128:t0 + (blk + 1) * 128, :], writes=[stg.res])
            ps = P[6 + blk % 2].reshape([128, 4, 128])
            for j in range(2):
                self.tr(ps[:, j, :], stg[:, j * 128:(j + 1) * 128], self.cF(K_ID), reads=[stg.res, self.CST.res],
                        writes=[ps.res])
            self.cp("act", PT[:, :, blk * 128:(blk + 1) * 128], ps[:, 0:2, :], reads=[ps.res],
                    writes=[(PT.res, blk // 4)])
        wbA = [self.aalloc([8, 128], BF16) for _ in range(3)]
        wbB = [self.aalloc([8, 128], BF16) for _ in range(3)]
        SG = [self.aalloc([512], F32) for _ in range(2)]
        TM = [self.aalloc([512], F32) for _ in range(2)]
        st = {"n": 0}

        def pt_rhs(k, tt):
            return PT[:, k, tt * 512:(tt + 1) * 512], (PT.res, tt)

        def consumer(i, tt, pss):
            n = st["n"]
            st["n"] += 1
            sl = slice(tt * 512, (tt + 1) * 512)
            sg, tm = SG[n % 2], TM[n % 2]
            self.actf(sg[:, :], pss[0][:, :], AF.Sigmoid, reads=[pss[0].res], writes=[sg.res])
            self.tt("dve", tm[:, :], pss[1][:, :], sg[:, :], ALU.mult, reads=[pss[1].res, sg.res], writes=[tm.res])
            self.tt("dve", self.X[:, i, sl], self.X[:, i, sl], tm[:, :], ALU.add, reads=[tm.res, (self.X.res, tt)],
                    writes=[(self.X.res, tt)])

        self.proj_fm([(w["wpg"], 0, 8, self.xt_rhs), (w["wpp"], 0, 2, pt_rhs)], 8, consumer, [wbA, wbB],
                     [[P[0], P[1]], [P[2], P[3]]])
        if l < self.nlayer - 1:
            for tt in range(NT):
                sl = slice(tt * 512, (tt + 1) * 512)
                for m in range(8):
                    self.cp("act", self.XT[:, m, sl], self.X[:, m, sl], reads=[(self.X.res, tt)],
                            writes=[(self.XT.res, tt)])

    def amark(self):
        return self.aoff

    def arelease(self, mark):
        self.S.fence()
        self.aoff = mark

    def ssd(self, l, first):
        T, NT, NB, P, PB, S = self.T, self.NT, self.NB, self.P, self.PB, self.S
        w = self.dw[l]
        ROWS, ROWX, COLS, CST, CSB = self.ROWS[l], self.ROWX[l], self.COLS[l], self.CST, self.CSB
        YST = self.aalloc([8, T], BF16)
        mark0 = self.amark()
        WZ = self.aalloc([8, 1024], BF16)
        WDT = self.aalloc([8, 16], BF16)
        BFM = self.aalloc([4, T], BF16)
        CFM = self.aalloc([4, T], BF16)
        XST = self.aalloc([NB, 1024], BF16)
        BTK = self.aalloc([NB, 512], BF16)
        DT, DTA, ECS, DTE, CDEC = [self.aalloc([NB, 16], F32) for _ in range(5)]
        mark1 = self.amark()
        XPAD = [self.aalloc([4 + T], BF16) for _ in range(2)]
        DG = [self.aalloc([4, 128], BF16) for _ in range(2)]
        XFM = [self.aalloc([T], BF16) for _ in range(2)]
        wbuf = [self.aalloc([8, 128], BF16) for _ in range(3)]
        S.dma("pool", WZ[:, :, :], w["win_tm"][:, 0:1024].rearrange("(k p) n -> p k n", p=128), writes=[WZ.res])
        S.dma("pool", WDT[:, :, :], w["win_tm"][:, 1024:1040].rearrange("(k p) n -> p k n", p=128), writes=[WDT.res])
        def ssd_a(m):
            xp, dg = XPAD[m % 2], DG[m % 2]
            self.cp("act", xp[:, 0:4], self.STAIL[l][:, m, :], reads=[self.STAIL[l].res], writes=[xp.res])
            self.tt("dve", dg[:, :, :], CSB.bc(K_ID, [[0, 4], [1, 128]]), COLS.bc(C_SCW + m * 4, [[1, 4], [0, 128]]),
                    ALU.mult, reads=[CSB.res, COLS.res], writes=[dg.res])
            wb = wbuf[m % 3]
            if m == 0:
                for mm_ in range(2):
                    S.dma("pool", wbuf[mm_ % 3][:, :, :], w["win_fm"][FM_XBC + mm_], writes=[wbuf[mm_ % 3].res])
            if m + 2 < 16:
                S.dma("pool", wbuf[(m + 2) % 3][:, :, :], w["win_fm"][FM_XBC + m + 2], writes=[wbuf[(m + 2) % 3].res])
            for tt in range(NT):
                sl = slice(tt * 512, (tt + 1) * 512)
                ps = P[tt % 2]
                for k in range(8):
                    self.mm(ps[:, :], wb[:, k, :], self.XT[:, k, sl], k == 0, k == 7,
                            reads=[wb.res, (self.XT.res, tt)], writes=[ps.res])
                self.cp("act", xp[:, 4 + tt * 512:4 + (tt + 1) * 512], ps[:, :], reads=[ps.res], writes=[xp.res])

        def ssd_b(m):
            xp, dg = XPAD[m % 2], DG[m % 2]
            if m < 8:
                dst = XFM[m % 2]
                dsl = lambda sl, dst=dst: dst[:, sl]
            elif m < 12:
                dst = BFM
                dsl = lambda sl, g=m - 8: BFM[:, g, sl]
            else:
                dst = CFM
                dsl = lambda sl, g=m - 12: CFM[:, g, sl]
            for tt in range(NT):
                sl = slice(tt * 512, (tt + 1) * 512)
                cps = P[2 + tt % 2]
                for k in range(4):
                    self.mm(cps[:, :], dg[:, k, :], xp[:, 1 + k + tt * 512:1 + k + (tt + 1) * 512], k == 0, k == 3,
                            reads=[dg.res, xp.res], writes=[cps.res])
                self.actf(dsl(sl), cps[:, :], AF.Silu, reads=[cps.res], writes=[dst.res],
                          bias=self.col(l, C_SCB + m))
            self.cp("act", self.STAIL[l][:, m, :], xp[:, T:T + 4], reads=[xp.res], writes=[self.STAIL[l].res])
            if m < 12:
                for b0 in range(0, NB, 8):
                    nb = min(8, NB - b0)
                    pb = PB[4 + (m + b0 // 8) % 2].reshape([128, 8, 128])
                    for j in range(nb):
                        blk = b0 + j
                        src = dst[:, blk * 128:(blk + 1) * 128] if m < 8 else BFM[:, m - 8, blk * 128:(blk + 1) * 128]
                        self.tr(pb[:, j, :], src, self.cB(K_ID), reads=[dst.res, CSB.res], writes=[pb.res])
                    if m < 8:
                        self.cp("dve", XST[:, b0:b0 + nb, m * 128:(m + 1) * 128], pb[:, 0:nb, :], reads=[pb.res],
                                writes=[XST.res])
                    else:
                        self.cp("dve", BTK[:, b0:b0 + nb, (m - 8) * 128:(m - 7) * 128], pb[:, 0:nb, :],
                                reads=[pb.res], writes=[BTK.res])

        ssd_a(0)
        for m in range(16):
            if m + 1 < 16:
                ssd_a(m + 1)
            ssd_b(m)
        PD = P[6].reshape([128, 32, 16])
        for blk in range(NB):
            for k in range(8):
                self.mm(PD[:, blk, :], self.XT[:, k, blk * 128:(blk + 1) * 128], WDT[:, k, :], k == 0, k == 7,
                        reads=[(self.XT.res, blk // 4), WDT.res], writes=[PD.res])
        self.tt("dve", DT[:, :, :], PD[:, 0:NB, :], ROWS.bc(R_DTB, [[0, NB], [1, 16]]), ALU.add,
                reads=[PD.res, ROWS.res], writes=[DT.res])
        self.actf(DT[:, :, :], DT[:, :, :], AF.Exp, reads=[DT.res], writes=[DT.res])
        self.actf(DT[:, :, :], DT[:, :, :], AF.Ln, reads=[DT.res], writes=[DT.res], bias=self.cF(K_ONEC, 1))
        self.tt("dve", DTA[:, :, :], DT[:, :, :], ROWX.bc(0, [[0, NB], [1, 16]]), ALU.mult,
                reads=[DT.res, ROWX.res], writes=[DTA.res])
        dta2 = DTA.reshape([128, NB * 16])
        for (cst, dstv, ps) in ((K_TU, ECS, P[7]), (K_TL, DTE, P[6]), (K_ONE, CDEC, P[7])):
            self.mm(ps[:, 0:NB * 16], self.cF(cst), dta2[:, :], True, True, reads=[CST.res, DTA.res], writes=[ps.res])
            self.actf(dstv.reshape([128, NB * 16])[:, :], ps[:, 0:NB * 16], AF.Exp, reads=[ps.res], writes=[dstv.res])
        self.arelease(mark1)
        RS = [self.aalloc([4, 128], F32) for _ in range(2)]
        DEC = [self.aalloc([4, 128], F32) for _ in range(2)]
        MTG = [self.aalloc([4, 128], BF16) for _ in range(2)]
        CBM = self.aalloc([4, 128], F32)
        XDT = [self.aalloc([16, 64], BF16) for _ in range(2)]
        XDD = [self.aalloc([16, 64], BF16) for _ in range(2)]
        XD = [self.aalloc([16, 64], BF16) for _ in range(2)]
        T1 = self.aalloc([1024], F32)
        SZ = self.aalloc([1024], F32)
        YN = self.aalloc([1024], BF16)
        HB = self.aalloc([1024], BF16)
        SS = self.aalloc([4], F32)
        H = self.H[l]
        self.cp("act", HB[:, :], H[:, :], reads=[H.res], writes=[HB.res])
        for c in range(NB):
            csl = slice(c * 128, (c + 1) * 128)
            xdt, xdd, xd = XDT[c % 2], XDD[c % 2], XD[c % 2]
            xs3 = XST.reshape([128, NB, 16, 64])
            self.tt("dve", xdt[:, :, :], xs3[:, c, :, :], DT.bc(c * 16, [[1, 16], [0, 64]]), ALU.mult,
                    reads=[XST.res, DT.res], writes=[xdt.res])
            self.tt("dve", xdd[:, :, :], xdt[:, :, :], DTE.bc(c * 16, [[1, 16], [0, 64]]), ALU.mult,
                    reads=[xdt.res, DTE.res], writes=[xdd.res])
            self.tt("dve", xd[:, :, :], xs3[:, c, :, :], ROWS.bc(R_D, [[1, 16], [0, 64]]), ALU.mult,
                    reads=[XST.res, ROWS.res], writes=[xd.res])
            p0 = P[0].reshape([128, 4, 128])
            for g in range(4):
                self.mm(p0[:, g, :], BFM[:, g, csl], CFM[:, g, csl], True, True, reads=[BFM.res, CFM.res],
                        writes=[p0.res])
            self.tt("dve", CBM[:, :, :], p0[:, :, :], CST.bc(K_TU, [[0, 4], [1, 128]]), ALU.mult,
                    reads=[p0.res, CST.res], writes=[CBM.res])
            xd2 = xd.reshape([128, 1024])
            xdt2 = xdt.reshape([128, 1024])
            for half in range(2):
                self.mm(P[3 + half][:, :], self.cB(K_ID), xd2[:, half * 512:(half + 1) * 512], True, False,
                        reads=[CSB.res, xd.res], writes=[P[3 + half].res])
            for g in range(4):
                rs, dec, mtg = RS[g % 2], DEC[g % 2], MTG[g % 2]
                self.tt("dve", rs[:, :, :], DTA.bc(c * 16 + 4 * g, [[1, 4], [0, 128]]),
                        CST.bc(K_TU, [[0, 4], [1, 128]]), ALU.mult, reads=[DTA.res, CST.res], writes=[rs.res])
                self.mm(P[1][:, :], self.cF(K_TL), rs.reshape([128, 512])[:, :], True, True,
                        reads=[CST.res, rs.res], writes=[P[1].res])
                self.actf(dec.reshape([128, 512])[:, :], P[1][:, :], AF.Exp, reads=[P[1].res], writes=[dec.res])
                self.tt("dve", mtg[:, :, :], dec[:, :, :], CBM.bc(g * 128, [[0, 4], [1, 128]]), ALU.mult,
                        reads=[dec.res, CBM.res], writes=[mtg.res])
                for r in range(4):
                    h = 4 * g + r
                    half = h // 8
                    self.mm(P[3 + half][:, (h % 8) * 64:(h % 8 + 1) * 64], mtg[:, r, :],
                            xdt2[:, h * 64:(h + 1) * 64], False, h % 8 == 7,
                            reads=[mtg.res, xdt.res], writes=[P[3 + half].res])
            for g in range(4):
                ps = P[5 + g // 2]
                self.mm(ps[:, (g % 2) * 256:(g % 2 + 1) * 256], CFM[:, g, csl], HB[:, g * 256:(g + 1) * 256],
                        True, True, reads=[CFM.res, HB.res], writes=[ps.res])
            t13 = T1.reshape([128, 16, 64])
            for half in range(2):
                self.tt("dve", t13[:, half * 8:(half + 1) * 8, :], P[5 + half].reshape([128, 8, 64])[:, :, :],
                        ECS.bc(c * 16 + half * 8, [[1, 8], [0, 64]]), ALU.mult,
                        reads=[P[5 + half].res, ECS.res], writes=[T1.res])
            for half in range(2):
                hs = slice(half * 512, (half + 1) * 512)
                self.tt("dve", T1[:, hs], P[3 + half][:, :], T1[:, hs], ALU.add, reads=[P[3 + half].res, T1.res],
                        writes=[T1.res])
            for half in range(2):
                hs = slice(half * 512, (half + 1) * 512)
                pz = P[7] if half == 0 else P[0]
                for k in range(8):
                    self.mm(pz[:, :], self.XT[:, k, csl], WZ[:, k, hs], k == 0, k == 7,
                            reads=[(self.XT.res, c // 4), WZ.res], writes=[pz.res])
                self.actf(SZ[:, hs], pz[:, :], AF.Silu, reads=[pz.res], writes=[SZ.res])
            self.tt("dve", T1[:, :], T1[:, :], SZ[:, :], ALU.mult, reads=[T1.res, SZ.res], writes=[T1.res])
            self.tt("dve", SZ[:, :], T1[:, :], T1[:, :], ALU.mult, reads=[T1.res], writes=[SZ.res])
            self.S.dve(lambda e: e.tensor_reduce(out=SS[:, :], in_=SZ.reshape([128, 4, 256])[:, :, :], op=ALU.add,
                                                 axis=AX.X), reads=[SZ.res], writes=[SS.res])
            self.ts("dve", SS[:, :], SS[:, :], 1.0 / 256, ALU.mult, reads=[SS.res], writes=[SS.res], s2=float(EPS),
                    op1=ALU.add)
            self.actf(SS[:, :], SS[:, :], AF.Sqrt, reads=[SS.res], writes=[SS.res])
            self.S.dve(lambda e: e.reciprocal(SS[:, :], SS[:, :]), reads=[SS.res], writes=[SS.res])
            self.tt("dve", YN.reshape([128, 4, 256])[:, :, :], T1.reshape([128, 4, 256])[:, :, :],
                    SS.bc(0, [[1, 4], [0, 256]]), ALU.mult, reads=[T1.res, SS.res], writes=[YN.res])
            pb = PB[2].reshape([128, 8, 128])
            for m in range(8):
                self.tr(pb[:, m, :], YN[:, m * 128:(m + 1) * 128], self.cB(K_ID), reads=[YN.res, CSB.res],
                        writes=[pb.res])
            self.tt("dve", YST[:, :, csl], pb[:, :, :], COLS.bc(C_NW, [[1, 8], [0, 128]]), ALU.mult,
                    reads=[pb.res, COLS.res], writes=[(YST.res, c // 4)])
            xdd2 = xdd.reshape([128, 1024])
            for g in range(4):
                ps = P[5 + g // 2]
                self.mm(ps[:, (g % 2) * 256:(g % 2 + 1) * 256], BTK[:, c, g * 128:(g + 1) * 128],
                        xdd2[:, g * 256:(g + 1) * 256], True, True, reads=[BTK.res, xdd.res], writes=[ps.res])
            self.tt("dve", H.reshape([128, 16, 64])[:, :, :], H.reshape([128, 16, 64])[:, :, :],
                    CDEC.bc(c * 16, [[1, 16], [0, 64]]), ALU.mult, reads=[H.res, CDEC.res], writes=[H.res])
            for half in range(2):
                hs = slice(half * 512, (half + 1) * 512)
                self.tt("dve", H[:, hs], P[5 + half][:, :], H[:, hs], ALU.add, reads=[P[5 + half].res, H.res],
                        writes=[H.res])
            self.cp("act", HB[:, :], H[:, :], reads=[H.res], writes=[HB.res])
        self.dbg("YST", YST)
        self.arelease(mark0)
        self.gated_out(l, 0, YST, "wssd", self.mt_first)
        self.mt_first = False

    def attention(self, l, seq, t0, first):
        T, NT, NB, P, PB, S = self.T, self.NT, self.NB, self.P, self.PB, self.S
        w = self.dw[l]
        ROWX, CST, CSB = self.ROWX[l], self.CST, self.CSB
        AT = self.aalloc([8, T], BF16)
        mark0 = self.amark()
        COS = self.aalloc([T], F32)
        SIN = self.aalloc([T], F32)
        QR = self.aalloc([8, T], BF16)
        KD = self.aalloc([4, 128 + T], BF16)
        VA = self.aalloc([1 + NB, 4, 65], BF16)
        AO = self.aalloc([NB, 1024], BF16)
        mark1 = self.amark()
        POSI = self.aalloc([T], I32)
        ANG = self.aalloc([T], F32)
        A2 = self.aalloc([T], F32)
        KI = self.aalloc([T], I32)
        KF = self.aalloc([T], F32)
        S.dma("sp", POSI[:, :], bass.AP(self.d_pos.tensor, seq * SEQ + t0, [[0, 128], [1, T]]), writes=[POSI.res])
        self.cp("dve", ANG[:, :], POSI[:, :], reads=[POSI.res], writes=[ANG.res])
        self.ts("dve", ANG[:, :], ANG[:, :], self.cF(K_INVF, 1), ALU.mult, reads=[ANG.res, CST.res], writes=[ANG.res])
        TWO_PI = 2.0 * math.pi
        C1 = 6.28125
        C2 = TWO_PI - C1
        PI_SAFE = 3.1415925
        for dst, shift in ((SIN, 0.0), (COS, 0.5 * math.pi)):
            self.ts("dve", A2[:, :], ANG[:, :], float(shift), ALU.add, reads=[ANG.res], writes=[A2.res])
            self.ts("dve", KI[:, :], A2[:, :], 1.0 / TWO_PI, ALU.mult, reads=[A2.res], writes=[KI.res])
            self.cp("dve", KF[:, :], KI[:, :], reads=[KI.res], writes=[KF.res])
            self.stt(A2[:, :], KF[:, :], -C1, A2[:, :], ALU.mult, ALU.add, reads=[KF.res, A2.res], writes=[A2.res])
            self.stt(A2[:, :], KF[:, :], -C2, A2[:, :], ALU.mult, ALU.add, reads=[KF.res, A2.res], writes=[A2.res])
            self.ts("dve", A2[:, :], A2[:, :], -PI_SAFE, ALU.max, reads=[A2.res], writes=[A2.res], s2=PI_SAFE,
                    op1=ALU.min)
            self.actf(dst[:, :], A2[:, :], AF.Sin, reads=[A2.res], writes=[dst.res])
        self.arelease(mark1)
        Q32 = [self.aalloc([512], F32) for _ in range(2)]
        TA = [self.aalloc([512], F32) for _ in range(2)]
        TB = [self.aalloc([512], F32) for _ in range(2)]
        KR = [self.aalloc([512], BF16) for _ in range(2)]
        wbuf = [self.aalloc([8, 128], BF16) for _ in range(3)]
        WV = self.aalloc([8, 256], BF16)
        EC = [self.aalloc([512], BF16) for _ in range(2)]
        EP = [self.aalloc([512], BF16) for _ in range(2)]
        DEN = [self.aalloc([4], F32) for _ in range(2)]
        S.dma("pool", WV[:, :, :], w["win_tm"][:, 1040:1296].rearrange("(k p) n -> p k n", p=128), writes=[WV.res])
        st = {"n": 0}

        def rope(i, tt, pss, is_k):
            n = st["n"]
            st["n"] += 1
            sl = slice(tt * 512, (tt + 1) * 512)
            q32, ta, tb, pr = Q32[n % 2], TA[n % 2], TB[n % 2], P[2 + n % 2]
            self.cp("act", q32[:, :], pss[0][:, :], reads=[pss[0].res], writes=[q32.res])
            self.mm(pr[:, :], self.cF(K_ROT), q32[:, :], True, True, reads=[CST.res, q32.res], writes=[pr.res])
            self.tt("dve", ta[:, :], q32[:, :], COS[:, sl], ALU.mult, reads=[q32.res, COS.res], writes=[ta.res])
            self.tt("dve", tb[:, :], pr[:, :], SIN[:, sl], ALU.mult, reads=[pr.res, SIN.res], writes=[tb.res])
            if not is_k:
                self.tt("dve", QR[:, i, sl], ta[:, :], tb[:, :], ALU.add, reads=[ta.res, tb.res],
                        writes=[(QR.res, tt)])
            else:
                kr = KR[n % 2]
                self.tt("dve", kr[:, :], ta[:, :], tb[:, :], ALU.add, reads=[ta.res, tb.res], writes=[kr.res])
                for half in range(2):
                    h = 2 * i + half
                    pd = P[4 + half]
                    self.mm(pd[:, :], self.cB(K_DUP0 + half * 128), kr[:, :], True, True, reads=[CSB.res, kr.res],
                            writes=[pd.res])
                    self.cp("act" if half == 0 else "dve", KD[:, h, 128 + tt * 512:128 + (tt + 1) * 512], pd[:, :],
                            reads=[pd.res], writes=[KD.res])

        self.proj_fm([(w["win_fm"], FM_Q, 8, self.xt_rhs)], 8, lambda i, tt, pss: rope(i, tt, pss, False), [wbuf],
                     [[P[0], P[1]]])
        self.proj_fm([(w["win_fm"], FM_K, 8, self.xt_rhs)], 2, lambda i, tt, pss: rope(i, tt, pss, True), [wbuf],
                     [[P[0], P[1]]])
        self.memset("pool", VA[:, :, :, 64:65], 1.0, [VA.res])
        if not first:
            self.cp("act", KD[:, :, 0:128], self.KCAR[l][:, :, :], reads=[self.KCAR[l].res], writes=[KD.res])
            self.cp("act", VA[:, 0, :, :], self.VCAR[l][:, :, :], reads=[self.VCAR[l].res], writes=[VA.res])
        for blk in range(NB):
            pv = P[6 + blk % 2]
            for k in range(8):
                self.mm(pv[:, 0:256], self.XT[:, k, blk * 128:(blk + 1) * 128], WV[:, k, :], k == 0, k == 7,
                        reads=[(self.XT.res, blk // 4), WV.res], writes=[pv.res])
            self.cp("act", VA[:, 1 + blk, :, 0:64], pv.reshape([128, 8, 64])[:, 0:4, :], reads=[pv.res],
                    writes=[VA.res])
        self.dbg("QR", QR)
        self.dbg("KD", KD)
        items = [(qb, h) for qb in range(NB) for h in range(4)]

        def att_a(n, qb, h):
            gfirst = first and qb == 0
            ec, ep = EC[n % 2], EP[n % 2]
            qsl = slice(qb * 128, (qb + 1) * 128)
            ec3, ep3 = ec.reshape([128, 4, 128]), ep.reshape([128, 4, 128])
            for r in range(4):
                hq = 4 * h + r
                ch, hf = hq // 2, hq % 2
                ps_ = slice(hf * 64, hf * 64 + 64)
                sc, sp = P[hf], P[2 + hf]
                cs_ = slice((r // 2) * 128, (r // 2 + 1) * 128)
                self.mm(sc[:, cs_], KD[ps_, h, 128 + qb * 128:128 + (qb + 1) * 128],
                        QR[ps_, ch, qsl], True, True, reads=[KD.res, (QR.res, qb // 4)], writes=[sc.res])
                if not gfirst:
                    self.mm(sp[:, cs_], KD[ps_, h, qb * 128:(qb + 1) * 128],
                            QR[ps_, ch, qsl], True, True, reads=[KD.res, (QR.res, qb // 4)], writes=[sp.res])
            for hf in range(2):
                self.actf(ec3[:, hf::2, :], P[hf].reshape([128, 4, 128])[:, 0:2, :], AF.Exp, reads=[P[hf].res],
                          writes=[ec.res], scale=0.125)
            self.tt("dve", ec3[:, :, :], ec3[:, :, :],
                    CSB.bc(K_TU, [[0, 4], [1, 128]]), ALU.mult, reads=[ec.res, CSB.res], writes=[ec.res])
            if not gfirst:
                for hf in range(2):
                    self.actf(ep3[:, hf::2, :], P[2 + hf].reshape([128, 4, 128])[:, 0:2, :], AF.Exp,
                              reads=[P[2 + hf].res], writes=[ep.res], scale=0.125)
                self.tt("dve", ep3[:, :, :], ep3[:, :, :],
                        CSB.bc(K_TL, [[0, 4], [1, 128]]), ALU.mult, reads=[ep.res, CSB.res], writes=[ep.res])

        def att_b(n, qb, h):
            gfirst = first and qb == 0
            po = P[4 + n % 2]
            ec, ep, den = EC[n % 2], EP[n % 2], DEN[n % 2]
            po3 = po.reshape([128, 4, 128])
            for r in range(4):
                if not gfirst:
                    self.mm(po3[:, r, 0:65], ep[:, r * 128:(r + 1) * 128], VA[:, qb, h, :], True, False,
                            reads=[ep.res, VA.res], writes=[po.res])
                self.mm(po3[:, r, 0:65], ec[:, r * 128:(r + 1) * 128], VA[:, qb + 1, h, :], gfirst, True,
                        reads=[ec.res, VA.res], writes=[po.res])
            self.tt("dve", den[:, :], po3[:, :, 64], ROWX.bc(16 + 4 * h, [[1, 4]]), ALU.add,
                    reads=[po.res, ROWX.res], writes=[den.res])
            self.S.dve(lambda e, den=den: e.reciprocal(den[:, :], den[:, :]), reads=[den.res], writes=[den.res])
            self.tt("dve", AO.reshape([128, NB, 16, 64])[:, qb, 4 * h:4 * h + 4, :], po3[:, :, 0:64],
                    den.bc(0, [[1, 4], [0, 64]]), ALU.mult, reads=[po.res, den.res], writes=[(AO.res, qb)])

        att_a(0, *items[0])
        for n, (qb, h) in enumerate(items):
            if n + 1 < len(items):
                att_a(n + 1, *items[n + 1])
            att_b(n, qb, h)
        for blk in range(NB):
            pb = PB[6 + blk % 2].reshape([128, 8, 128])
            for m in range(8):
                self.tr(pb[:, m, :], AO[:, blk, m * 128:(m + 1) * 128], self.cB(K_ID), reads=[(AO.res, blk), CSB.res],
                        writes=[pb.res])
            self.cp("act" if blk % 2 else "dve", AT[:, :, blk * 128:(blk + 1) * 128], pb[:, :, :], reads=[pb.res],
                    writes=[(AT.res, blk // 4)])
        self.cp("act", self.KCAR[l][:, :, :], KD[:, :, T:T + 128], reads=[KD.res], writes=[self.KCAR[l].res])
        self.cp("act", self.VCAR[l][:, :, :], VA[:, NB, :, :], reads=[VA.res], writes=[self.VCAR[l].res])
        self.dbg("AT", AT)
        self.arelease(mark0)
        self.gated_out(l, 2, AT, "wattn", self.mt_first)
        self.mt_first = False

    def moe(self, l):
        T, NT, NB, P, S = self.T, self.NT, self.NB, self.P, self.S
        w = self.dw[l]
        ROWS, CST = self.ROWS[l], self.CST
        WR = self.aalloc([8, 36], BF16)
        LOG = self.aalloc([NB, 36], F32)
        WTOK = self.aalloc([NB, 4, 8], F32)
        WT = self.aalloc([T], F32)
        S.dma("pool", WR[:, :, :], w["wr"].rearrange("(k p) n -> p k n", p=128), writes=[WR.res])
        for blk in range(NB):
            pl = P[6 + blk % 2]
            for k in range(8):
                self.mm(pl[:, 0:36], self.XT[:, k, blk * 128:(blk + 1) * 128], WR[:, k, :], k == 0, k == 7,
                        reads=[(self.XT.res, blk // 4), WR.res], writes=[pl.res])
            self.tt("dve", LOG[:, blk, :], pl[:, 0:36], ROWS.bc(R_RB, [[1, 36]]), ALU.add, reads=[pl.res, ROWS.res],
                    writes=[LOG.res])
        mk = self.amark()
        f = lambda shp: self.aalloc(shp, F32)
        GMAX, GS, GE, GMASK = f([NB]), f([NB]), f([NB, 4]), f([NB, 4])
        V1, V2, P1, P2 = f([NB, 4]), f([NB, 4]), f([NB, 4]), f([NB, 4])
        M1, M2, E2 = f([NB, 4, 8]), f([NB, 4, 8]), f([NB, 4, 8])
        GL = lambda: LOG[:, :, 0:4]
        EL = lambda: LOG.bc(4, [[36, NB], [8, 4], [1, 8]])
        red = lambda out, in_, op, rd, wr: self.S.dve(
            lambda e: e.tensor_reduce(out=out, in_=in_, op=op, axis=AX.X), reads=rd, writes=wr)
        red(GMAX[:, :], GL(), ALU.max, [LOG.res], [GMAX.res])
        self.tt("dve", GE[:, :, :], GL(), GMAX.bc(0, [[1, NB], [0, 4]]), ALU.subtract, reads=[LOG.res, GMAX.res],
                writes=[GE.res])
        self.tt("dve", GMASK[:, :, :], GL(), GMAX.bc(0, [[1, NB], [0, 4]]), ALU.is_equal, reads=[LOG.res, GMAX.res],
                writes=[GMASK.res])
        self.actf(GE[:, :, :], GE[:, :, :], AF.Exp, reads=[GE.res], writes=[GE.res])
        red(GS[:, :], GE[:, :, :], ALU.add, [GE.res], [GS.res])
        self.S.dve(lambda e: e.reciprocal(GS[:, :], GS[:, :]), reads=[GS.res], writes=[GS.res])
        self.tt("dve", GMASK[:, :, :], GMASK[:, :, :], GS.bc(0, [[1, NB], [0, 4]]), ALU.mult,
                reads=[GMASK.res, GS.res], writes=[GMASK.res])
        red(V1[:, :, :], EL(), ALU.max, [LOG.res], [V1.res])
        self.tt("dve", M1[:, :, :, :], EL(), V1.bc(0, [[4, NB], [1, 4], [0, 8]]), ALU.is_equal,
                reads=[LOG.res, V1.res], writes=[M1.res])
        self.stt(E2[:, :, :, :], M1[:, :, :, :], -1.0e30, EL(), ALU.mult, ALU.add, reads=[M1.res, LOG.res],
                 writes=[E2.res])
        red(V2[:, :, :], E2[:, :, :, :], ALU.max, [E2.res], [V2.res])
        self.tt("dve", M2[:, :, :, :], E2[:, :, :, :], V2.bc(0, [[4, NB], [1, 4], [0, 8]]), ALU.is_equal,
                reads=[E2.res, V2.res], writes=[M2.res])
        self.tt("dve", P1[:, :, :], V1[:, :, :], V2[:, :, :], ALU.subtract, reads=[V1.res, V2.res], writes=[P1.res])
        self.actf(P1[:, :, :], P1[:, :, :], AF.Sigmoid, reads=[P1.res], writes=[P1.res])
        self.ts("dve", P2[:, :, :], P1[:, :, :], -1.0, ALU.mult, reads=[P1.res], writes=[P2.res], s2=1.0, op1=ALU.add)
        self.tt("dve", P1[:, :, :], P1[:, :, :], GMASK[:, :, :], ALU.mult, reads=[P1.res, GMASK.res], writes=[P1.res])
        self.tt("dve", P2[:, :, :], P2[:, :, :], GMASK[:, :, :], ALU.mult, reads=[P2.res, GMASK.res], writes=[P2.res])
        self.tt("dve", M1[:, :, :, :], M1[:, :, :, :], P1.bc(0, [[4, NB], [1, 4], [0, 8]]), ALU.mult,
                reads=[M1.res, P1.res], writes=[M1.res])
        self.tt("dve", M2[:, :, :, :], M2[:, :, :, :], P2.bc(0, [[4, NB], [1, 4], [0, 8]]), ALU.mult,
                reads=[M2.res, P2.res], writes=[M2.res])
        self.tt("dve", WTOK[:, :, :, :], M1[:, :, :, :], M2[:, :, :, :], ALU.add, reads=[M1.res, M2.res],
                writes=[WTOK.res])
        self.dbg("WTOK", WTOK)
        wt2 = WTOK.reshape([128, NB, 32])
        for b0 in range(0, NB, 4):
            ps = P[7]
            for j in range(4):
                self.tr(ps[0:32, j * 128:(j + 1) * 128], wt2[:, b0 + j, :], self.cF(K_ID), reads=[WTOK.res, CST.res],
                        writes=[ps.res])
            self.cp("act", WT[0:32, b0 * 128:(b0 + 4) * 128], ps[0:32, :], reads=[ps.res], writes=[WT.res])
        self.arelease(mk)
        NWB = 3
        WG = [self.aalloc([4, 8, 128], BF16) for _ in range(NWB)]
        WU = [self.aalloc([4, 8, 128], BF16) for _ in range(NWB)]
        WD = [self.aalloc([4, 1024], BF16) for _ in range(NWB)]
        HT = [self.aalloc([4, 512], BF16) for _ in range(2)]
        SIL = [self.aalloc([512], F32) for _ in range(2)]
        WBC = [self.aalloc([512], F32) for _ in range(2)]
        SEL = [self.aalloc([128], F32) for _ in range(2)]

        def load_expert(e):
            wg, wu, wd = WG[e % NWB], WU[e % NWB], WD[e % NWB]
            S.dma("pool", wg.bc(0, [[1024, 4], [1, 1024]]), w["wg"][e].rearrange("p m k c -> p m (k c)"),
                  writes=[wg.res])
            S.dma("pool", wu.bc(0, [[1024, 4], [1, 1024]]), w["wu"][e].rearrange("p m k c -> p m (k c)"),
                  writes=[wu.res])
            S.dma("pool", wd[:, :, :], w["wd"][e].rearrange("(k p) n -> p k n", p=128), writes=[wd.res])

        PY = [P[4], P[5], P[6]]
        items = [(e, tt) for e in range(self.nexp) for tt in range(NT)]
        st = {"dc": 0}

        def gate_up(n, e, tt):
            wg, wu = WG[e % NWB], WU[e % NWB]
            sl = slice(tt * 512, (tt + 1) * 512)
            ht, wbc = HT[n % 2], WBC[n % 2]
            sel = SEL[e % 2]
            if tt == 0:
                self.cp("act", sel[0:32, :], CST.bc(K_ID + e, [[0, 128]], npart=32), reads=[CST.res],
                        writes=[sel.res])
            self.mm(P[7][:, :], sel[0:32, :], WT[0:32, sl], True, True, reads=[sel.res, WT.res],
                    writes=[P[7].res])
            self.cp("act", wbc[:, :], P[7][:, :], reads=[P[7].res], writes=[wbc.res])
            for m in range(4):
                pg, pu = P[m % 2], P[2 + m % 2]
                sil = SIL[m % 2]
                for k in range(8):
                    self.mm(pg[:, :], wg[:, m, k, :], self.XT[:, k, sl], k == 0, k == 7,
                            reads=[wg.res, (self.XT.res, tt)], writes=[pg.res])
                for k in range(8):
                    self.mm(pu[:, :], wu[:, m, k, :], self.XT[:, k, sl], k == 0, k == 7,
                            reads=[wu.res, (self.XT.res, tt)], writes=[pu.res])
                self.actf(sil[:, :], pg[:, :], AF.Silu, reads=[pg.res], writes=[sil.res])
                self.tt("dve", sil[:, :], pu[:, :], sil[:, :], ALU.mult, reads=[pu.res, sil.res], writes=[sil.res])
                self.tt("dve", ht[:, m, :], sil[:, :], wbc[:, :], ALU.mult, reads=[sil.res, wbc.res],
                        writes=[ht.res])

        def down(n, e, tt):
            wd = WD[e % NWB]
            sl = slice(tt * 512, (tt + 1) * 512)
            ht = HT[n % 2]
            for dc in range(8):
                py = PY[st["dc"] % 3]
                st["dc"] += 1
                for m in range(4):
                    self.mm(py[:, :], wd[:, m, dc * 128:(dc + 1) * 128], ht[:, m, :], m == 0, m == 3,
                            reads=[wd.res, ht.res], writes=[py.res])
                self.tt("dve", self.X[:, dc, sl], py[:, :], self.X[:, dc, sl], ALU.add,
                        reads=[py.res, (self.X.res, tt)], writes=[(self.X.res, tt)])

        for e in range(min(NWB - 1, self.nexp)):
            load_expert(e)
        for n, (e, tt) in enumerate(items):
            gate_up(n, e, tt)
            if n >= 1:
                pe_, ptt = items[n - 1]
                down(n - 1, pe_, ptt)
                if ptt == NT - 1 and pe_ + NWB < self.nexp + 0 and pe_ + NWB - 1 < self.nexp:
                    pass
            if tt == 0 and e + NWB - 1 < self.nexp and e >= 1:
                pass
            if n >= 1 and items[n - 1][1] == NT - 1:
                nxt = items[n - 1][0] + NWB
                if nxt < self.nexp:
                    load_expert(nxt)
            if n == 0 and NWB - 1 < self.nexp:
                load_expert(NWB - 1)
        down(len(items) - 1, *items[-1])


def host_consts():
    c = np.zeros((128, NCF), np.float32)
    k = np.arange(128)[:, None]
    m = np.arange(128)[None, :]
    c[:, K_ID:K_ID + 128] = (k == m)
    c[:, K_TU:K_TU + 128] = (k <= m)
    c[:, K_TL:K_TL + 128] = (k > m)
    c[:, K_ONE:K_ONE + 128] = 1.0
    R = np.zeros((128, 128), np.float32)
    D0 = np.zeros((128, 128), np.float32)
    D1 = np.zeros((128, 128), np.float32)
    for mm_ in range(128):
        b, j = mm_ // 64 * 64, mm_ % 64
        if j < 32:
            R[b + j + 32, mm_] = -1.0
        else:
            R[b + j - 32, mm_] = 1.0
        D0[j, mm_] = 1.0
        D1[64 + j, mm_] = 1.0
    c[:, K_ROT:K_ROT + 128] = R
    c[:, K_DUP0:K_DUP0 + 128] = D0
    c[:, K_DUP1:K_DUP1 + 128] = D1
    invf = (np.float32(10000.0) ** (-np.arange(32, dtype=np.float32) / np.float32(32))).astype(np.float32)
    c[:, K_INVF] = invf[np.arange(128) % 32]
    c[:, K_EPS] = EPS
    c[:, K_ONEC] = 1.0
    return c


def fm_layout(w):
    K, N = w.shape
    return np.ascontiguousarray(w.reshape(K // 128, 128, N // 128, 128).transpose(2, 1, 0, 3))


def colsT(v, n):
    return np.asarray(v, np.float32).reshape(n, 128).T


def prep_weights(inp, l):
    f = lambda n: np.asarray(inp[n][l], np.float32)
    w_in = f("w_in")
    o = {}
    o["win_fm%d" % l] = fm_layout(np.concatenate(
        [w_in[:, 0:3072], w_in[:, 4096:6144], w_in[:, 6160:8208], w_in[:, 8208:9232], w_in[:, 9232:9488]], axis=1))
    o["win_tm%d" % l] = np.ascontiguousarray(np.concatenate(
        [w_in[:, 3072:4096], w_in[:, 6144:6160], w_in[:, 9488:9744]], axis=1))
    for nm, src in (("wssd", "ssd_w_out"), ("wconf", "conf_w_out"), ("wattn", "attn_w_out"), ("wout", "w_out"),
                    ("wpg", "ple_w_gate"), ("wpp", "ple_w_proj")):
        o["%s%d" % (nm, l)] = fm_layout(f(src))
    o["wr%d" % l] = np.ascontiguousarray(np.concatenate([f("moe_w_group"), f("moe_w_expert")], axis=1))
    wg, wu = f("moe_w_gate"), f("moe_w_up")
    o["wg%d" % l] = np.ascontiguousarray(wg.reshape(NEXP, 8, 128, 4, 128).transpose(0, 2, 3, 1, 4))
    o["wu%d" % l] = np.ascontiguousarray(wu.reshape(NEXP, 8, 128, 4, 128).transpose(0, 2, 3, 1, 4))
    o["wd%d" % l] = np.ascontiguousarray(f("moe_w_down"))
    cols = np.zeros((128, NCOLS), np.float32)
    cols[:, C_BGATE:C_BGATE + 24] = colsT(f("b_gate"), 24)
    cols[:, C_SCW:C_SCW + 64] = f("ssd_conv_w").reshape(4, 16, 128).transpose(2, 1, 0).reshape(128, 64)
    cols[:, C_SCB:C_SCB + 16] = colsT(f("ssd_conv_b"), 16)
    cols[:, C_CDW:C_CDW + 248] = f("conf_dw_w").reshape(31, 8, 128).transpose(2, 1, 0).reshape(128, 248)
    for c0, nm in ((C_CDB, "conf_dw_b"), (C_CLG, "conf_ln_g"), (C_CLB, "conf_ln_b"), (C_NW, "ssd_norm_w"),
                   (C_L1G, "ln1_g"), (C_L1B, "ln1_b"), (C_L2G, "ln2_g"), (C_L2B, "ln2_b")):
        cols[:, c0:c0 + 8] = colsT(f(nm), 8)
    o["cols%d" % l] = cols
    rows = np.zeros((1, NROWS), np.float32)
    rows[0, R_DTB:R_DTB + 16] = f("ssd_dt_bias")
    rows[0, R_ALOG:R_ALOG + 16] = f("ssd_a_log")
    rows[0, R_D:R_D + 16] = f("ssd_d")
    rows[0, R_SINK:R_SINK + 16] = f("attn_sinks")
    rows[0, R_RB:R_RB + 4] = f("moe_b_group")
    rows[0, R_RB + 4:R_RB + 36] = f("moe_b_expert")
    o["rows%d" % l] = rows
    return o


def make_in_maps(inp, seq_lists, nlayer=DEPTH):
    shared = {"cst": host_consts()}
    for l in range(nlayer):
        shared.update(prep_weights(inp, l))
    maps = []
    for seqs in seq_lists:
        m = dict(shared)
        m["x"] = np.ascontiguousarray(np.asarray(inp["x"], np.float32)[seqs])
        m["p"] = np.ascontiguousarray(np.asarray(inp["p"], np.float32)[:, seqs])
        m["pos"] = np.ascontiguousarray(np.asarray(inp["positions"], np.int32)[seqs])
        maps.append(m)
    return maps


_NC_CACHE = {}


def kernel(**inputs):
    if "nc" not in _NC_CACHE:
        _NC_CACHE["nc"] = Builder().nc
    nc = _NC_CACHE["nc"]
    seq_lists = [list(range(c * SEQ_PER_CORE, (c + 1) * SEQ_PER_CORE)) for c in range(NCORES)]
    maps = make_in_maps(inputs, seq_lists)
    res = run_bass_kernel_spmd(nc, maps, core_ids=list(range(NCORES)))
    out = np.concatenate([np.asarray(r["out"], np.float32) for r in res.results], axis=0)
    return out
```

```python
import contextlib
import math
import numpy as np
import concourse.bass as bass
import concourse.mybir as mybir
from concourse.bass_utils import run_bass_kernel_spmd

F32 = mybir.dt.float32
BF16 = mybir.dt.bfloat16
I32 = mybir.dt.int32
AF = mybir.ActivationFunctionType
ALU = mybir.AluOpType
AX = mybir.AxisListType

ENGS = ("pe", "act", "dve", "pool", "sp")
NDMASEM = 8

D = 1024
SEQ = 2048
DEPTH = 2
NCORES = 8
SEQ_PER_CORE = 4
PLE = 256
NEXP = 32
FF = 512
ALPHA = (2 * DEPTH) ** 0.25
EPS = 1e-5
OFF_GATE, OFF_Z, OFF_XBC, OFF_DT, OFF_CONF, OFF_Q, OFF_K, OFF_V = 0, 3072, 4096, 6144, 6160, 8208, 9232, 9488
FM_GATE, FM_XBC, FM_CONF, FM_Q, FM_K = 0, 24, 40, 56, 64
NFM = 66
C_BGATE = 0
C_SCW = 24
C_SCB = 88
C_CDW = 104
C_CDB = 352
C_CLG = 360
C_CLB = 368
C_NW = 376
C_L1G = 384
C_L1B = 392
C_L2G = 400
C_L2B = 408
NCOLS = 416
R_DTB, R_ALOG, R_D, R_SINK, R_RB = 0, 16, 32, 48, 64
NROWS = 100
K_ID, K_TU, K_TL, K_ONE, K_ROT, K_DUP0, K_DUP1, K_INVF, K_EPS, K_ONEC = 0, 128, 256, 384, 512, 640, 768, 896, 897, 898
NCF = 900


class Op:
    __slots__ = ("eng", "fn", "dma", "idx", "tick", "sem_i", "sem_v", "deps", "prewait")


class Sched:
    def __init__(self, nc):
        self.nc = nc
        self.ops = []
        self.state = {}
        self.cnt = {e: 0 for e in ENGS}
        self.dcnt = {e: 0 for e in ENGS}
        self.dma_hist = {e: [] for e in ENGS}
        self.last = {e: None for e in ENGS}
        self.pending = {e: set() for e in ENGS}
        self.exclusive = set()

    def fence(self):
        F = set()
        for e in ENGS:
            if self.last[e] is not None:
                F.add(self.last[e])
            for op in self.dma_hist[e][-NDMASEM:]:
                F.add(op)
        for e in ENGS:
            self.pending[e] |= F

    @staticmethod
    def _norm(lst):
        out = []
        for r in lst:
            if isinstance(r, tuple):
                out.append((id(r[0]), r[1]))
            else:
                out.append((id(r), None))
        return out

    def _conf(self, res):
        b, k = res
        d = self.state.get(b)
        if d is None:
            return
        if k is None:
            for st in d.values():
                yield st
        else:
            st = d.get(k)
            if st is not None:
                yield st
            st = d.get(None)
            if st is not None:
                yield st

    def add(self, eng, fn, reads=(), writes=(), dma=False):
        reads = self._norm(reads)
        writes = self._norm(writes)
        if self.exclusive:
            ex = [r for r in reads if r[0] in self.exclusive]
            if ex:
                reads = [r for r in reads if r[0] not in self.exclusive]
                writes = writes + [r for r in ex if r not in writes]
        op = Op()
        op.eng, op.fn, op.dma, op.prewait = eng, fn, dma, None
        op.idx = len(self.ops)
        deps = set()
        for r in reads:
            for st in self._conf(r):
                if st[0] is not None:
                    deps.add(st[0])
        for w in writes:
            for st in self._conf(w):
                if st[0] is not None:
                    deps.add(st[0])
                deps.update(st[1])
        if self.pending[eng]:
            deps |= self.pending[eng]
            self.pending[eng] = set()
        op.deps = deps
        for w in writes:
            b, k = w
            d = self.state.setdefault(b, {})
            if k is None:
                d.clear()
            d[k] = [op, []]
        for r in reads:
            b, k = r
            d = self.state.setdefault(b, {})
            st = d.get(k)
            if st is None:
                st = [None, []]
                for s2 in self._conf(r):
                    if s2[0] is not None and (st[0] is None or s2[0].idx > st[0].idx):
                        st[0] = s2[0]
                d[k] = st
            st[1].append(op)
        if dma:
            i = self.dcnt[eng]
            self.dcnt[eng] += 1
            op.sem_i = i % NDMASEM
            op.sem_v = 16 * (i // NDMASEM + 1)
            hist = self.dma_hist[eng]
            if i >= NDMASEM:
                op.prewait = hist[i - NDMASEM]
            hist.append(op)
            op.tick = None
        else:
            self.cnt[eng] += 1
            op.tick = self.cnt[eng]
            self.last[eng] = op
        self.ops.append(op)
        return op

    def pe(self, fn, reads=(), writes=()):
        return self.add("pe", fn, reads, writes)

    def act(self, fn, reads=(), writes=()):
        return self.add("act", fn, reads, writes)

    def dve(self, fn, reads=(), writes=()):
        return self.add("dve", fn, reads, writes)

    def pool(self, fn, reads=(), writes=()):
        return self.add("pool", fn, reads, writes)

    def dma(self, eng, out, in_, reads=(), writes=()):
        return self.add(eng, lambda e: e.dma_start(out=out, in_=in_), reads, writes, dma=True)

    def emit(self):
        nc = self.nc
        with contextlib.ExitStack() as es:
            esem = {e: es.enter_context(nc.semaphore("s_" + e)) for e in ENGS}
            dsem = {e: [es.enter_context(nc.semaphore("d_%s%d" % (e, i))) for i in range(NDMASEM)]
                    for e in ENGS if self.dcnt[e] > 0}
            block = es.enter_context(nc.Block())
            per = {e: [op for op in self.ops if op.eng == e] for e in ENGS}

            def run(engname, eng):
                waited = {}

                def wait_for(dep):
                    if dep.dma:
                        s = dsem[dep.eng][dep.sem_i]
                        v = dep.sem_v
                    else:
                        if dep.eng == "pe" and engname == "pe":
                            return
                        s = esem[dep.eng]
                        v = dep.tick
                    key = id(s)
                    if waited.get(key, 0) >= v:
                        return
                    waited[key] = v
                    eng.wait_ge(s, v)

                for op in per[engname]:
                    for dep in sorted(op.deps, key=lambda d: d.idx):
                        wait_for(dep)
                    if op.prewait is not None:
                        wait_for(op.prewait)
                    inst = op.fn(eng)
                    if op.dma:
                        inst.then_inc(dsem[engname][op.sem_i], 16)
                    else:
                        inst.then_inc(esem[engname], 1)
                for op in self.dma_hist[engname][-NDMASEM:]:
                    wait_for(op)

            @block.sync
            def _(eng):
                run("sp", eng)

            @block.tensor
            def _(eng):
                run("pe", eng)

            @block.scalar
            def _(eng):
                run("act", eng)

            @block.vector
            def _(eng):
                run("dve", eng)

            @block.gpsimd
            def _(eng):
                run("pool", eng)


def pstride(t):
    return int(np.prod(list(t.shape)[1:]))


class View:
    def __init__(self, h, off, shape, res=None):
        self.h, self.off, self.shape = h, off, list(shape)
        self.dtype = h.dtype
        st, s = [], 1
        for n in reversed(self.shape[1:]):
            st.append(s)
            s *= n
        self.strides = list(reversed(st))
        self.ps = pstride(h)
        self.res = self if res is None else res

    def __getitem__(self, idx):
        if not isinstance(idx, tuple):
            idx = (idx,)
        idx = list(idx) + [slice(None)] * (len(self.shape) - len(idx))
        p = idx[0]
        p0, p1, _ = p.indices(self.shape[0])
        off = self.off
        dims = []
        for i, ix in enumerate(idx[1:]):
            stride, n = self.strides[i], self.shape[i + 1]
            if isinstance(ix, int):
                off += ix * stride
            else:
                a, b, st = ix.indices(n)
                cnt = len(range(a, b, st))
                off += a * stride
                dims.append([stride * st, cnt])
        m = [list(d) for d in dims]
        if not m:
            m = [[1, 1]]
        return bass.AP(self.h, p0 * self.ps + off, [[self.ps, p1 - p0]] + m)

    def bc(self, off, dims, npart=128, p0=0):
        return bass.AP(self.h, p0 * self.ps + self.off + off, [[self.ps, npart]] + [list(d) for d in dims])

    def reshape(self, shape):
        return View(self.h, self.off, shape, res=self.res)


class Builder:
    def __init__(self, nseq=SEQ_PER_CORE, nlayer=DEPTH, T=1024, nseg=None, debug=(), nexp=NEXP, stop_after=None,
                 only=None):
        self.nseq, self.nlayer, self.T = nseq, nlayer, T
        self.NT = T // 512
        self.NB = T // 128
        self.nseg = (SEQ // T) if nseg is None else nseg
        self.debug = set(debug)
        self.nexp = nexp
        self.stop_after = stop_after
        self.only = None if only is None else set(only.split(',')) if isinstance(only, str) else set(only)
        self.dbg_outs = {}
        nc = self.nc = bass.Bass("TRN2", target_bir_lowering=False)
        self.S = Sched(nc)
        self.declare_dram()
        self.alloc()
        self.program()
        self.S.emit()

    def din(self, name, shape, dt=F32):
        return self.nc.dram_tensor(name, list(shape), dt, kind="ExternalInput").ap()

    def pers(self, name, shape, dt):
        h = self.nc.alloc_sbuf_tensor(name, list(shape), dt)
        return View(h, 0, shape)

    def areset(self):
        self.S.fence()
        self.aoff = 0

    def aalloc(self, fshape, dt, npart=128):
        size = {F32: 4, BF16: 2, I32: 4}[dt]
        n = int(np.prod(fshape)) * size
        n = (n + 31) // 32 * 32
        off = self.aoff
        self.aoff += n
        assert self.aoff <= self.arena_bytes, ("arena overflow", self.aoff, self.arena_bytes)
        return View(self.arena[dt], off // size, [npart] + list(fshape))

    def dbg(self, name, view, reads=None):
        if name not in self.debug or name in self.dbg_outs:
            return
        o = self.nc.dram_tensor("dbg_" + name, list(view.shape), view.dtype, kind="ExternalOutput").ap()
        self.dbg_outs[name] = o
        self.S.dma("sp", o, view[:], reads=[view.res] if reads is None else reads)

    def declare_dram(self):
        L = self.nlayer
        self.d_x = self.din("x", [self.nseq, SEQ, D])
        self.d_p = self.din("p", [DEPTH, self.nseq, SEQ, PLE])
        self.d_pos = self.din("pos", [self.nseq, SEQ], I32)
        self.d_cst = self.din("cst", [128, NCF])
        self.d_out = self.nc.dram_tensor("out", [self.nseq, SEQ, D], F32, kind="ExternalOutput").ap()
        self.dw = []
        for l in range(L):
            w = {}
            w["win_fm"] = self.din("win_fm%d" % l, [NFM, 128, 8, 128])
            w["win_tm"] = self.din("win_tm%d" % l, [D, 1296])
            for nm in ("wssd", "wconf", "wattn", "wout", "wpg"):
                w[nm] = self.din("%s%d" % (nm, l), [8, 128, 8, 128])
            w["wpp"] = self.din("wpp%d" % l, [8, 128, 2, 128])
            w["wr"] = self.din("wr%d" % l, [D, 36])
            w["wg"] = self.din("wg%d" % l, [NEXP, 128, 4, 8, 128])
            w["wu"] = self.din("wu%d" % l, [NEXP, 128, 4, 8, 128])
            w["wd"] = self.din("wd%d" % l, [NEXP, FF, D])
            w["cols"] = self.din("cols%d" % l, [128, NCOLS])
            w["rows"] = self.din("rows%d" % l, [1, NROWS])
            self.dw.append(w)

    def alloc(self):
        T, nc, L = self.T, self.nc, self.nlayer
        pers = self.pers
        self.X = pers("X", [128, 8, T], F32)
        self.XT = pers("XT", [128, 8, T], BF16)
        self.MT = pers("MT", [128, 8, T], BF16)
        self.CST = pers("CST", [128, NCF], F32)
        self.CSB = pers("CSB", [128, NCF], BF16)
        self.COLS = [pers("COLS%d" % l, [128, NCOLS], F32) for l in range(L)]
        self.COLA = [pers("COLA%d" % l, [128, 16], F32) for l in range(L)]
        self.ROWS = [pers("ROWS%d" % l, [128, NROWS], F32) for l in range(L)]
        self.ROWX = [pers("ROWX%d" % l, [128, 32], F32) for l in range(L)]
        self.H = [pers("H%d" % l, [128, 1024], F32) for l in range(L)]
        self.CTAIL = [pers("CTAIL%d" % l, [128, 8, 32], BF16) for l in range(L)]
        self.STAIL = [pers("STAIL%d" % l, [128, 16, 4], BF16) for l in range(L)]
        self.KCAR = [pers("KCAR%d" % l, [128, 4, 128], BF16) for l in range(L)]
        self.VCAR = [pers("VCAR%d" % l, [128, 4, 65], BF16) for l in range(L)]
        ph = [nc.alloc_psum_tensor("P%d" % i, [128, 512], F32) for i in range(8)]
        self.P = [View(h, 0, [128, 512]) for h in ph]
        self.PB = [View(h.bitcast(BF16), 0, [128, 1024], res=v) for h, v in zip(ph, self.P)]
        self.S.exclusive = {id(v) for v in self.P}
        rem = nc.sbuf_bytes_remaining
        rem = rem() if callable(rem) else rem
        self.arena_bytes = (int(rem) - 8192) // 64 * 64
        ah = nc.alloc_sbuf_tensor("ARENA", [128, self.arena_bytes // 4], F32)
        self.arena = {F32: ah, BF16: ah.bitcast(BF16), I32: ah.bitcast(I32)}
        self.aoff = 0

    def mm(self, out, lhsT, rhs, start, stop, reads, writes):
        self.S.pe(lambda e: e.matmul(out, lhsT, rhs, start=start, stop=stop), reads, writes)

    def tr(self, out, in_, ident, reads, writes):
        self.S.pe(lambda e: e.transpose(out, in_, ident), reads, writes)

    def actf(self, out, in_, func, reads, writes, scale=1.0, bias=None):
        if bias is None:
            self.S.act(lambda e: e.activation(out=out, in_=in_, func=func, scale=scale), reads, writes)
        else:
            self.S.act(lambda e: e.activation(out=out, in_=in_, func=func, scale=scale, bias=bias), reads, writes)

    def tt(self, eng, out, in0, in1, op, reads, writes):
        self.S.add(eng, lambda e: e.tensor_tensor(out=out, in0=in0, in1=in1, op=op), reads, writes)

    def ts(self, eng, out, in0, s1, op0, reads, writes, s2=None, op1=None):
        if op1 is None:
            self.S.add(eng, lambda e: e.tensor_scalar(out=out, in0=in0, scalar1=s1, scalar2=None, op0=op0), reads, writes)
        else:
            self.S.add(eng, lambda e: e.tensor_scalar(out=out, in0=in0, scalar1=s1, scalar2=s2, op0=op0, op1=op1),
                       reads, writes)

    def stt(self, out, in0, scalar, in1, op0, op1, reads, writes):
        self.S.dve(lambda e: e.scalar_tensor_tensor(out=out, in0=in0, scalar=scalar, in1=in1, op0=op0, op1=op1),
                   reads, writes)

    def cp(self, eng, out, in_, reads, writes):
        if eng == "act":
            self.S.act(lambda e: e.copy(out, in_), reads, writes)
        else:
            self.S.add(eng, lambda e: e.tensor_copy(out, in_), reads, writes)

    def memset(self, eng, ap, val, writes):
        self.S.add(eng, lambda e: e.memset(ap, val), (), writes)

    def cF(self, off, n=128, npart=128, p0=0):
        return self.CST[p0:p0 + npart, off:off + n]

    def cB(self, off, n=128, npart=128, p0=0):
        return self.CSB[p0:p0 + npart, off:off + n]

    def col(self, l, c, npart=128):
        return self.COLS[l][0:npart, c:c + 1]

    def proj_fm(self, srcs, nchunk, consumer, wbufs, psums):
        NT = self.NT
        cnt = 0
        depth = min(len(wb) for wb in wbufs) - 1

        def issue(i):
            for si, (wsrc, c0, KC, rhs_fn) in enumerate(srcs):
                wb = wbufs[si][i % len(wbufs[si])]
                self.S.dma("pool", wb[:, 0:KC, :], wsrc[c0 + i], writes=[wb.res])

        for i in range(min(depth, nchunk)):
            issue(i)
        for i in range(nchunk):
            if i + depth < nchunk:
                issue(i + depth)
            wbs = [wbufs[si][i % len(wbufs[si])] for si in range(len(srcs))]
            for tt in range(NT):
                pss = []
                for si, (wsrc, c0, KC, rhs_fn) in enumerate(srcs):
                    ps = psums[si][cnt % len(psums[si])]
                    for k in range(KC):
                        rap, rres = rhs_fn(k, tt)
                        self.mm(ps[:, :], wbs[si][:, k, :], rap, k == 0, k == KC - 1,
                                reads=[wbs[si].res, rres], writes=[ps.res])
                    pss.append(ps)
                cnt += 1
                consumer(i, tt, pss)

    def xt_rhs(self, k, tt):
        return self.XT[:, k, tt * 512:(tt + 1) * 512], (self.XT.res, tt)

    def gated_out(self, l, branch, YT, wname, first):
        w = self.dw[l]
        wbA = [self.aalloc([8, 128], BF16) for _ in range(3)]
        wbB = [self.aalloc([8, 128], BF16) for _ in range(3)]
        SG = [self.aalloc([512], F32) for _ in range(2)]
        TM = [self.aalloc([512], BF16) for _ in range(2)]
        P = self.P
        st = {"n": 0}

        def yt_rhs(k, tt):
            return YT[:, k, tt * 512:(tt + 1) * 512], (YT.res, tt)

        def consumer(i, tt, pss):
            n = st["n"]
            st["n"] += 1
            sg = SG[n % 2]
            self.actf(sg[:, :], pss[1][:, :], AF.Sigmoid, reads=[pss[1].res], writes=[sg.res],
                      bias=self.col(l, C_BGATE + branch * 8 + i))
            dst = self.MT[:, i, tt * 512:(tt + 1) * 512]
            if first:
                self.tt("dve", dst, pss[0][:, :], sg[:, :], ALU.mult, reads=[pss[0].res, sg.res],
                        writes=[(self.MT.res, tt)])
            else:
                tm = TM[n % 2]
                self.tt("dve", tm[:, :], pss[0][:, :], sg[:, :], ALU.mult, reads=[pss[0].res, sg.res],
                        writes=[tm.res])
                self.tt("dve", dst, dst, tm[:, :], ALU.add, reads=[tm.res, (self.MT.res, tt)],
                        writes=[(self.MT.res, tt)])

        self.proj_fm([(w[wname], 0, 8, yt_rhs), (w["win_fm"], FM_GATE + branch * 8, 8, self.xt_rhs)], 8, consumer,
                     [wbA, wbB], [[P[0], P[1]], [P[2], P[3]]])

    def ln_stats(self, SRC, tt, tmp, banks):
        SQ, MEAN, RSTD, TN = tmp
        B1, B2 = banks
        sl = slice(tt * 512, (tt + 1) * 512)
        for m in range(8):
            self.mm(B1[:, :], self.cF(K_ONE), SRC[:, m, sl], m == 0, m == 7,
                    reads=[self.CST.res, (SRC.res, tt)], writes=[B1.res])
        for m in range(8):
            sq = SQ[m % 2]
            self.actf(sq[:, :], SRC[:, m, sl], AF.Square, reads=[(SRC.res, tt)], writes=[sq.res])
            self.mm(B2[:, :], self.cF(K_ONE), sq[:, :], m == 0, m == 7, reads=[self.CST.res, sq.res],
                    writes=[B2.res])
        self.ts("dve", MEAN[:, :], B1[:, :], 1.0 / 1024, ALU.mult, reads=[B1.res], writes=[MEAN.res])
        self.tt("dve", RSTD[:, :], MEAN[:, :], MEAN[:, :], ALU.mult, reads=[MEAN.res], writes=[RSTD.res])
        self.stt(RSTD[:, :], B2[:, :], 1.0 / 1024, RSTD[:, :], ALU.mult, ALU.subtract, reads=[B2.res, RSTD.res],
                 writes=[RSTD.res])
        self.actf(RSTD[:, :], RSTD[:, :], AF.Sqrt, reads=[RSTD.res], writes=[RSTD.res], bias=self.cF(K_EPS, 1))
        self.S.dve(lambda e: e.reciprocal(RSTD[:, :], RSTD[:, :]), reads=[RSTD.res], writes=[RSTD.res])

    def ln_norm(self, SRC, tt, out_fn, tmp):
        SQ, MEAN, RSTD, TN = tmp
        sl = slice(tt * 512, (tt + 1) * 512)
        for m in range(8):
            tn = TN[m % 2]
            self.tt("dve", tn[:, :], SRC[:, m, sl], MEAN[:, :], ALU.subtract, reads=[(SRC.res, tt), MEAN.res],
                    writes=[tn.res])
            self.tt("dve", tn[:, :], tn[:, :], RSTD[:, :], ALU.mult, reads=[tn.res, RSTD.res], writes=[tn.res])
            out_fn(m, tn)

    def ln_all(self, SRC, make_out_fn):
        P = self.P
        tmps = [self.ln_tmp() for _ in range(self.NT)]
        banks = [(P[6], P[7]), (P[4], P[5])]
        for tt in range(self.NT):
            self.ln_stats(SRC, tt, tmps[tt], banks[tt % 2])
        for tt in range(self.NT):
            self.ln_norm(SRC, tt, make_out_fn(tt), tmps[tt])

    def ln_tmp(self):
        return ([self.aalloc([512], F32) for _ in range(2)], self.aalloc([512], F32), self.aalloc([512], F32),
                [self.aalloc([512], F32) for _ in range(2)])

    def program(self):
        S = self.S
        S.dma("sp", self.CST[:, :], self.d_cst, writes=[self.CST.res])
        self.cp("dve", self.CSB[:, :], self.CST[:, :], reads=[self.CST.res], writes=[self.CSB.res])
        for l in range(self.nlayer):
            S.dma("sp", self.COLS[l][:, :], self.dw[l]["cols"], writes=[self.COLS[l].res])
            S.dma("sp", self.ROWS[l][:, :], bass.AP(self.dw[l]["rows"].tensor, 0, [[0, 128], [1, NROWS]]),
                  writes=[self.ROWS[l].res])
            self.actf(self.ROWX[l][:, 0:16], self.ROWS[l][:, R_ALOG:R_ALOG + 16], AF.Exp, reads=[self.ROWS[l].res],
                      writes=[self.ROWX[l].res])
            self.ts("dve", self.ROWX[l][:, 0:16], self.ROWX[l][:, 0:16], -1.0, ALU.mult, reads=[self.ROWX[l].res],
                    writes=[self.ROWX[l].res])
            self.actf(self.ROWX[l][:, 16:32], self.ROWS[l][:, R_SINK:R_SINK + 16], AF.Exp, reads=[self.ROWS[l].res],
                      writes=[self.ROWX[l].res])
            self.ts("dve", self.COLA[l][:, 0:16], self.COLS[l][:, C_L1G:C_L1G + 16], float(ALPHA), ALU.mult,
                    reads=[self.COLS[l].res], writes=[self.COLA[l].res])
        for seq in range(self.nseq):
            for seg in range(self.nseg):
                self.segment(seq, seg)

    def segment(self, seq, seg):
        T = self.T
        t0 = seg * T
        first = seg == 0
        self.areset()
        self.load_x(seq, t0)
        if first:
            for l in range(self.nlayer):
                self.memset("pool", self.H[l][:, :], 0.0, [self.H[l].res])
                self.memset("pool", self.CTAIL[l][:, :, :], 0.0, [self.CTAIL[l].res])
                self.memset("pool", self.STAIL[l][:, :, :], 0.0, [self.STAIL[l].res])
        for l in range(self.nlayer):
            self.mt_first = True
            stages = [("ssd", lambda: self.ssd(l, first)),
                      ("conf", lambda: self.conformer(l)),
                      ("attn", lambda: self.attention(l, seq, t0, first)),
                      ("ln1", lambda: self.outproj_ln1(l)),
                      ("moe", lambda: self.moe(l)),
                      ("ple", lambda: self.ln2_ple(l, seq, t0))]
            for nm, fn in stages:
                if self.only is not None and nm not in self.only:
                    continue
                self.areset()
                fn()
                self.dbg("MT", self.MT)
                if self.stop_after == (l, nm):
                    break
            if self.stop_after is not None and self.stop_after[0] == l:
                break
        self.areset()
        self.store_x(seq, t0)

    def load_x(self, seq, t0):
        S, P = self.S, self.P
        STG = [self.aalloc([1024], F32) for _ in range(2)]
        for blk in range(self.NB):
            stg = STG[blk % 2]
            tt = blk // 4
            S.dma("sp", stg[:, :], self.d_x[seq, t0 + blk * 128:t0 + (blk + 1) * 128, :], writes=[stg.res])
            for half in range(2):
                ps = P[(blk * 2 + half) % 4].reshape([128, 4, 128])
                for j in range(4):
                    c = half * 4 + j
                    self.tr(ps[:, j, :], stg[:, c * 128:(c + 1) * 128], self.cF(K_ID), reads=[stg.res, self.CST.res],
                            writes=[ps.res])
                self.cp("act", self.X[:, half * 4:half * 4 + 4, blk * 128:(blk + 1) * 128], ps[:, :, :],
                        reads=[ps.res], writes=[(self.X.res, tt)])
                self.cp("dve", self.XT[:, half * 4:half * 4 + 4, blk * 128:(blk + 1) * 128], ps[:, :, :],
                        reads=[ps.res], writes=[(self.XT.res, tt)])

    def store_x(self, seq, t0):
        S, P = self.S, self.P
        STG = [self.aalloc([1024], F32) for _ in range(2)]
        for blk in range(self.NB):
            stg = STG[blk % 2]
            tt = blk // 4
            for half in range(2):
                ps = P[(blk * 2 + half) % 4].reshape([128, 4, 128])
                for j in range(4):
                    c = half * 4 + j
                    self.tr(ps[:, j, :], self.X[:, c, blk * 128:(blk + 1) * 128], self.cF(K_ID),
                            reads=[(self.X.res, tt), self.CST.res], writes=[ps.res])
                self.cp("act" if half == 0 else "dve", stg[:, half * 512:(half + 1) * 512],
                        ps[:, :, :], reads=[ps.res], writes=[stg.res])
            S.dma("sp", self.d_out[seq, t0 + blk * 128:t0 + (blk + 1) * 128, :], stg[:, :], reads=[stg.res])

    def conformer(self, l):
        T, NT, P, S = self.T, self.NT, self.P, self.S
        w = self.dw[l]
        HT = self.aalloc([8, T], BF16)
        mark0 = self.amark()
        HPAD = [self.aalloc([32 + T], BF16) for _ in range(2)]
        DG = [self.aalloc([31, 128], BF16) for _ in range(2)]
        CO = self.aalloc([8, T], F32)
        SG = [self.aalloc([512], F32) for _ in range(2)]
        wbA = [self.aalloc([8, 128], BF16) for _ in range(2)]
        wbG = [self.aalloc([8, 128], BF16) for _ in range(2)]

        def cload(m):
            S.dma("pool", wbA[m % 2][:, :, :], w["win_fm"][FM_CONF + m], writes=[wbA[m % 2].res])
            S.dma("pool", wbG[m % 2][:, :, :], w["win_fm"][FM_CONF + 8 + m], writes=[wbG[m % 2].res])

        def conf_a(m):
            hp, dg = HPAD[m % 2], DG[m % 2]
            self.cp("act", hp[:, 0:32], self.CTAIL[l][:, m, :], reads=[self.CTAIL[l].res], writes=[hp.res])
            self.tt("dve", dg[:, :, :], self.CSB.bc(K_ID, [[0, 31], [1, 128]]),
                    self.COLS[l].bc(C_CDW + m * 31, [[1, 31], [0, 128]]), ALU.mult,
                    reads=[self.CSB.res, self.COLS[l].res], writes=[dg.res])
            wa, wg = wbA[m % 2], wbG[m % 2]
            if m == 0:
                cload(0)
            if m + 1 < 8:
                cload(m + 1)
            for tt in range(NT):
                sl = slice(tt * 512, (tt + 1) * 512)
                a_ps, g_ps = P[tt % 2], P[2 + tt % 2]
                for k in range(8):
                    self.mm(a_ps[:, :], wa[:, k, :], self.XT[:, k, sl], k == 0, k == 7,
                            reads=[wa.res, (self.XT.res, tt)], writes=[a_ps.res])
                for k in range(8):
                    self.mm(g_ps[:, :], wg[:, k, :], self.XT[:, k, sl], k == 0, k == 7,
                            reads=[wg.res, (self.XT.res, tt)], writes=[g_ps.res])
                sg = SG[tt % 2]
                self.actf(sg[:, :], g_ps[:, :], AF.Sigmoid, reads=[g_ps.res], writes=[sg.res])
                self.tt("dve", hp[:, 32 + tt * 512:32 + (tt + 1) * 512], a_ps[:, :], sg[:, :], ALU.mult,
                        reads=[a_ps.res, sg.res], writes=[hp.res])

        def conf_b(m):
            hp, dg = HPAD[m % 2], DG[m % 2]
            for tt in range(NT):
                sl = slice(tt * 512, (tt + 1) * 512)
                c_ps = P[4 + tt % 2]
                for k in range(31):
                    self.mm(c_ps[:, :], dg[:, k, :], hp[:, 2 + k + tt * 512:2 + k + (tt + 1) * 512], k == 0, k == 30,
                            reads=[dg.res, hp.res], writes=[c_ps.res])
                self.actf(CO[:, m, sl], c_ps[:, :], AF.Identity, reads=[c_ps.res], writes=[(CO.res, tt)],
                          bias=self.col(l, C_CDB + m))
            self.cp("act", self.CTAIL[l][:, m, :], hp[:, T:T + 32], reads=[hp.res], writes=[self.CTAIL[l].res])

        conf_a(0)
        for m in range(8):
            if m + 1 < 8:
                conf_a(m + 1)
            conf_b(m)
        self.dbg("CO", CO)
        def mk_conf(tt):
            sl = slice(tt * 512, (tt + 1) * 512)

            def out_fn(m, tn):
                self.actf(HT[:, m, sl], tn[:, :], AF.Silu, reads=[tn.res], writes=[(HT.res, tt)],
                          scale=self.col(l, C_CLG + m), bias=self.col(l, C_CLB + m))
            return out_fn

        self.ln_all(CO, mk_conf)
        self.dbg("HT", HT)
        self.arelease(mark0)
        self.gated_out(l, 1, HT, "wconf", self.mt_first)
        self.mt_first = False

    def outproj_ln1(self, l):
        T, NT, P, S = self.T, self.NT, self.P, self.S
        w = self.dw[l]
        wb = [self.aalloc([8, 128], BF16) for _ in range(3)]
        self.dbg("MT", self.MT)

        def mt_rhs(k, tt):
            return self.MT[:, k, tt * 512:(tt + 1) * 512], (self.MT.res, tt)

        def consumer(i, tt, pss):
            sl = slice(tt * 512, (tt + 1) * 512)
            self.stt(self.X[:, i, sl], self.X[:, i, sl], float(ALPHA), pss[0][:, :], ALU.mult, ALU.add,
                     reads=[(self.X.res, tt), pss[0].res], writes=[(self.X.res, tt)])

        self.proj_fm([(w["wout"], 0, 8, mt_rhs)], 8, consumer, [wb], [[P[0], P[1]]])
        def mk_ln1(tt):
            sl = slice(tt * 512, (tt + 1) * 512)

            def out_fn(m, tn):
                self.actf(self.XT[:, m, sl], tn[:, :], AF.Identity, reads=[tn.res], writes=[(self.XT.res, tt)],
                          scale=self.col(l, C_L1G + m), bias=self.col(l, C_L1B + m))
                self.actf(self.X[:, m, sl], tn[:, :], AF.Identity, reads=[tn.res], writes=[(self.X.res, tt)],
                          scale=self.COLA[l][:, m:m + 1], bias=self.COLA[l][:, 8 + m:9 + m])
            return out_fn

        self.ln_all(self.X, mk_ln1)
        self.dbg("X1T", self.XT)

    def ln2_ple(self, l, seq, t0):
        T, NT, NB, P, S = self.T, self.NT, self.NB, self.P, self.S
        w = self.dw[l]
        self.dbg("XMOE", self.X)
        def mk_ln2(tt):
            sl = slice(tt * 512, (tt + 1) * 512)

            def out_fn(m, tn):
                self.actf(self.XT[:, m, sl], tn[:, :], AF.Identity, reads=[tn.res], writes=[(self.XT.res, tt)],
                          scale=self.col(l, C_L2G + m), bias=self.col(l, C_L2B + m))
                self.actf(self.X[:, m, sl], tn[:, :], AF.Identity, reads=[tn.res], writes=[(self.X.res, tt)],
                          scale=self.col(l, C_L2G + m), bias=self.col(l, C_L2B + m))
            return out_fn

        self.ln_all(self.X, mk_ln2)
        self.dbg("X2T", self.XT)
        PSTG = [self.aalloc([256], F32) for _ in range(2)]
        PT = self.aalloc([2, T], BF16)
        for blk in range(NB):
            stg = PSTG[blk % 2]
            S.dma("sp", stg[:, :], self.d_p[l, seq, t0 + blk * 128:t0 + (blk + 1) * 128, :], writes=[stg.res])
            ps = P[6 + blk % 2].reshape([128, 4, 128])
            for j in range(2):
                self.tr(ps[:, j, :], stg[:, j * 128:(j + 1) * 128], self.cF(K_ID), reads=[stg.res, self.CST.res],
                        writes=[ps.res])
            self.cp("act", PT[:, :, blk * 128:(blk + 1) * 128], ps[:, 0:2, :], reads=[ps.res],
                    writes=[(PT.res, blk // 4)])
        wbA = [self.aalloc([8, 128], BF16) for _ in range(3)]
        wbB = [self.aalloc([8, 128], BF16) for _ in range(3)]
        SG = [self.aalloc([512], F32) for _ in range(2)]
        TM = [self.aalloc([512], F32) for _ in range(2)]
        st = {"n": 0}

        def pt_rhs(k, tt):
            return PT[:, k, tt * 512:(tt + 1) * 512], (PT.res, tt)

        def consumer(i, tt, pss):
            n = st["n"]
            st["n"] += 1
            sl = slice(tt * 512, (tt + 1) * 512)
            sg, tm = SG[n % 2], TM[n % 2]
            self.actf(sg[:, :], pss[0][:, :], AF.Sigmoid, reads=[pss[0].res], writes=[sg.res])
            self.tt("dve", tm[:, :], pss[1][:, :], sg[:, :], ALU.mult, reads=[pss[1].res, sg.res], writes=[tm.res])
            self.tt("dve", self.X[:, i, sl], self.X[:, i, sl], tm[:, :], ALU.add, reads=[tm.res, (self.X.res, tt)],
                    writes=[(self.X.res, tt)])

        self.proj_fm([(w["wpg"], 0, 8, self.xt_rhs), (w["wpp"], 0, 2, pt_rhs)], 8, consumer, [wbA, wbB],
                     [[P[0], P[1]], [P[2], P[3]]])
        if l < self.nlayer - 1:
            for tt in range(NT):
                sl = slice(tt * 512, (tt + 1) * 512)
                for m in range(8):
                    self.cp("act", self.XT[:, m, sl], self.X[:, m, sl], reads=[(self.X.res, tt)],
                            writes=[(self.XT.res, tt)])

    def amark(self):
        return self.aoff

    def arelease(self, mark):
        self.S.fence()
        self.aoff = mark

    def ssd(self, l, first):
        T, NT, NB, P, PB, S = self.T, self.NT, self.NB, self.P, self.PB, self.S
        w = self.dw[l]
        ROWS, ROWX, COLS, CST, CSB = self.ROWS[l], self.ROWX[l], self.COLS[l], self.CST, self.CSB
        YST = self.aalloc([8, T], BF16)
        mark0 = self.amark()
        WZ = self.aalloc([8, 1024], BF16)
        WDT = self.aalloc([8, 16], BF16)
        BFM = self.aalloc([4, T], BF16)
        CFM = self.aalloc([4, T], BF16)
        XST = self.aalloc([NB, 1024], BF16)
        BTK = self.aalloc([NB, 512], BF16)
        DT, DTA, ECS, DTE, CDEC = [self.aalloc([NB, 16], F32) for _ in range(5)]
        mark1 = self.amark()
        XPAD = [self.aalloc([4 + T], BF16) for _ in range(2)]
        DG = [self.aalloc([4, 128], BF16) for _ in range(2)]
        XFM = [self.aalloc([T], BF16) for _ in range(2)]
        wbuf = [self.aalloc([8, 128], BF16) for _ in range(3)]
        S.dma("pool", WZ[:, :, :], w["win_tm"][:, 0:1024].rearrange("(k p) n -> p k n", p=128), writes=[WZ.res])
        S.dma("pool", WDT[:, :, :], w["win_tm"][:, 1024:1040].rearrange("(k p) n -> p k n", p=128), writes=[WDT.res])
        def ssd_a(m):
            xp, dg = XPAD[m % 2], DG[m % 2]
            self.cp("act", xp[:, 0:4], self.STAIL[l][:, m, :], reads=[self.STAIL[l].res], writes=[xp.res])
            self.tt("dve", dg[:, :, :], CSB.bc(K_ID, [[0, 4], [1, 128]]), COLS.bc(C_SCW + m * 4, [[1, 4], [0, 128]]),
                    ALU.mult, reads=[CSB.res, COLS.res], writes=[dg.res])
            wb = wbuf[m % 3]
            if m == 0:
                for mm_ in range(2):
                    S.dma("pool", wbuf[mm_ % 3][:, :, :], w["win_fm"][FM_XBC + mm_], writes=[wbuf[mm_ % 3].res])
            if m + 2 < 16:
                S.dma("pool", wbuf[(m + 2) % 3][:, :, :], w["win_fm"][FM_XBC + m + 2], writes=[wbuf[(m + 2) % 3].res])
            for tt in range(NT):
                sl = slice(tt * 512, (tt + 1) * 512)
                ps = P[tt % 2]
                for k in range(8):
                    self.mm(ps[:, :], wb[:, k, :], self.XT[:, k, sl], k == 0, k == 7,
                            reads=[wb.res, (self.XT.res, tt)], writes=[ps.res])
                self.cp("act", xp[:, 4 + tt * 512:4 + (tt + 1) * 512], ps[:, :], reads=[ps.res], writes=[xp.res])

        def ssd_b(m):
            xp, dg = XPAD[m % 2], DG[m % 2]
            if m < 8:
                dst = XFM[m % 2]
                dsl = lambda sl, dst=dst: dst[:, sl]
            elif m < 12:
                dst = BFM
                dsl = lambda sl, g=m - 8: BFM[:, g, sl]
            else:
                dst = CFM
                dsl = lambda sl, g=m - 12: CFM[:, g, sl]
            for tt in range(NT):
                sl = slice(tt * 512, (tt + 1) * 512)
                cps = P[2 + tt % 2]
                for k in range(4):
                    self.mm(cps[:, :], dg[:, k, :], xp[:, 1 + k + tt * 512:1 + k + (tt + 1) * 512], k == 0, k == 3,
                            reads=[dg.res, xp.res], writes=[cps.res])
                self.actf(dsl(sl), cps[:, :], AF.Silu, reads=[cps.res], writes=[dst.res],
                          bias=self.col(l, C_SCB + m))
            self.cp("act", self.STAIL[l][:, m, :], xp[:, T:T + 4], reads=[xp.res], writes=[self.STAIL[l].res])
            if m < 12:
                for b0 in range(0, NB, 8):
                    nb = min(8, NB - b0)
                    pb = PB[4 + (m + b0 // 8) % 2].reshape([128, 8, 128])
                    for j in range(nb):
                        blk = b0 + j
                        src = dst[:, blk * 128:(blk + 1) * 128] if m < 8 else BFM[:, m - 8, blk * 128:(blk + 1) * 128]
                        self.tr(pb[:, j, :], src, self.cB(K_ID), reads=[dst.res, CSB.res], writes=[pb.res])
                    if m < 8:
                        self.cp("dve", XST[:, b0:b0 + nb, m * 128:(m + 1) * 128], pb[:, 0:nb, :], reads=[pb.res],
                                writes=[XST.res])
                    else:
                        self.cp("dve", BTK[:, b0:b0 + nb, (m - 8) * 128:(m - 7) * 128], pb[:, 0:nb, :],
                                reads=[pb.res], writes=[BTK.res])

        ssd_a(0)
        for m in range(16):
            if m + 1 < 16:
                ssd_a(m + 1)
            ssd_b(m)
        PD = P[6].reshape([128, 32, 16])
        for blk in range(NB):
            for k in range(8):
                self.mm(PD[:, blk, :], self.XT[:, k, blk * 128:(blk + 1) * 128], WDT[:, k, :], k == 0, k == 7,
                        reads=[(self.XT.res, blk // 4), WDT.res], writes=[PD.res])
        self.tt("dve", DT[:, :, :], PD[:, 0:NB, :], ROWS.bc(R_DTB, [[0, NB], [1, 16]]), ALU.add,
                reads=[PD.res, ROWS.res], writes=[DT.res])
        self.actf(DT[:, :, :], DT[:, :, :], AF.Exp, reads=[DT.res], writes=[DT.res])
        self.actf(DT[:, :, :], DT[:, :, :], AF.Ln, reads=[DT.res], writes=[DT.res], bias=self.cF(K_ONEC, 1))
        self.tt("dve", DTA[:, :, :], DT[:, :, :], ROWX.bc(0, [[0, NB], [1, 16]]), ALU.mult,
                reads=[DT.res, ROWX.res], writes=[DTA.res])
        dta2 = DTA.reshape([128, NB * 16])
        for (cst, dstv, ps) in ((K_TU, ECS, P[7]), (K_TL, DTE, P[6]), (K_ONE, CDEC, P[7])):
            self.mm(ps[:, 0:NB * 16], self.cF(cst), dta2[:, :], True, True, reads=[CST.res, DTA.res], writes=[ps.res])
            self.actf(dstv.reshape([128, NB * 16])[:, :], ps[:, 0:NB * 16], AF.Exp, reads=[ps.res], writes=[dstv.res])
        self.arelease(mark1)
        RS = [self.aalloc([4, 128], F32) for _ in range(2)]
        DEC = [self.aalloc([4, 128], F32) for _ in range(2)]
        MTG = [self.aalloc([4, 128], BF16) for _ in range(2)]
        CBM = self.aalloc([4, 128], F32)
        XDT = [self.aalloc([16, 64], BF16) for _ in range(2)]
        XDD = [self.aalloc([16, 64], BF16) for _ in range(2)]
        XD = [self.aalloc([16, 64], BF16) for _ in range(2)]
        T1 = self.aalloc([1024], F32)
        SZ = self.aalloc([1024], F32)
        YN = self.aalloc([1024], BF16)
        HB = self.aalloc([1024], BF16)
        SS = self.aalloc([4], F32)
        H = self.H[l]
        self.cp("act", HB[:, :], H[:, :], reads=[H.res], writes=[HB.res])
        for c in range(NB):
            csl = slice(c * 128, (c + 1) * 128)
            xdt, xdd, xd = XDT[c % 2], XDD[c % 2], XD[c % 2]
            xs3 = XST.reshape([128, NB, 16, 64])
            self.tt("dve", xdt[:, :, :], xs3[:, c, :, :], DT.bc(c * 16, [[1, 16], [0, 64]]), ALU.mult,
                    reads=[XST.res, DT.res], writes=[xdt.res])
            self.tt("dve", xdd[:, :, :], xdt[:, :, :], DTE.bc(c * 16, [[1, 16], [0, 64]]), ALU.mult,
                    reads=[xdt.res, DTE.res], writes=[xdd.res])
            self.tt("dve", xd[:, :, :], xs3[:, c, :, :], ROWS.bc(R_D, [[1, 16], [0, 64]]), ALU.mult,
                    reads=[XST.res, ROWS.res], writes=[xd.res])
            p0 = P[0].reshape([128, 4, 128])
            for g in range(4):
                self.mm(p0[:, g, :], BFM[:, g, csl], CFM[:, g, csl], True, True, reads=[BFM.res, CFM.res],
                        writes=[p0.res])
            self.tt("dve", CBM[:, :, :], p0[:, :, :], CST.bc(K_TU, [[0, 4], [1, 128]]), ALU.mult,
                    reads=[p0.res, CST.res], writes=[CBM.res])
            xd2 = xd.reshape([128, 1024])
            xdt2 = xdt.reshape([128, 1024])
            for half in range(2):
                self.mm(P[3 + half][:, :], self.cB(K_ID), xd2[:, half * 512:(half + 1) * 512], True, False,
                        reads=[CSB.res, xd.res], writes=[P[3 + half].res])
            for g in range(4):
                rs, dec, mtg = RS[g % 2], DEC[g % 2], MTG[g % 2]
                self.tt("dve", rs[:, :, :], DTA.bc(c * 16 + 4 * g, [[1, 4], [0, 128]]),
                        CST.bc(K_TU, [[0, 4], [1, 128]]), ALU.mult, reads=[DTA.res, CST.res], writes=[rs.res])
                self.mm(P[1][:, :], self.cF(K_TL), rs.reshape([128, 512])[:, :], True, True,
                        reads=[CST.res, rs.res], writes=[P[1].res])
                self.actf(dec.reshape([128, 512])[:, :], P[1][:, :], AF.Exp, reads=[P[1].res], writes=[dec.res])
                self.tt("dve", mtg[:, :, :], dec[:, :, :], CBM.bc(g * 128, [[0, 4], [1, 128]]), ALU.mult,
                        reads=[dec.res, CBM.res], writes=[mtg.res])
                for r in range(4):
                    h = 4 * g + r
                    half = h // 8
                    self.mm(P[3 + half][:, (h % 8) * 64:(h % 8 + 1) * 64], mtg[:, r, :],
                            xdt2[:, h * 64:(h + 1) * 64], False, h % 8 == 7,
                            reads=[mtg.res, xdt.res], writes=[P[3 + half].res])
            for g in range(4):
                ps = P[5 + g // 2]
                self.mm(ps[:, (g % 2) * 256:(g % 2 + 1) * 256], CFM[:, g, csl], HB[:, g * 256:(g + 1) * 256],
                        True, True, reads=[CFM.res, HB.res], writes=[ps.res])
            t13 = T1.reshape([128, 16, 64])
            for half in range(2):
                self.tt("dve", t13[:, half * 8:(half + 1) * 8, :], P[5 + half].reshape([128, 8, 64])[:, :, :],
                        ECS.bc(c * 16 + half * 8, [[1, 8], [0, 64]]), ALU.mult,
                        reads=[P[5 + half].res, ECS.res], writes=[T1.res])
            for half in range(2):
                hs = slice(half * 512, (half + 1) * 512)
                self.tt("dve", T1[:, hs], P[3 + half][:, :], T1[:, hs], ALU.add, reads=[P[3 + half].res, T1.res],
                        writes=[T1.res])
            for half in range(2):
                hs = slice(half * 512, (half + 1) * 512)
                pz = P[7] if half == 0 else P[0]
                for k in range(8):
                    self.mm(pz[:, :], self.XT[:, k, csl], WZ[:, k, hs], k == 0, k == 7,
                            reads=[(self.XT.res, c // 4), WZ.res], writes=[pz.res])
                self.actf(SZ[:, hs], pz[:, :], AF.Silu, reads=[pz.res], writes=[SZ.res])
            self.tt("dve", T1[:, :], T1[:, :], SZ[:, :], ALU.mult, reads=[T1.res, SZ.res], writes=[T1.res])
            self.tt("dve", SZ[:, :], T1[:, :], T1[:, :], ALU.mult, reads=[T1.res], writes=[SZ.res])
            self.S.dve(lambda e: e.tensor_reduce(out=SS[:, :], in_=SZ.reshape([128, 4, 256])[:, :, :], op=ALU.add,
                                                 axis=AX.X), reads=[SZ.res], writes=[SS.res])
            self.ts("dve", SS[:, :], SS[:, :], 1.0 / 256, ALU.mult, reads=[SS.res], writes=[SS.res], s2=float(EPS),
                    op1=ALU.add)
            self.actf(SS[:, :], SS[:, :], AF.Sqrt, reads=[SS.res], writes=[SS.res])
            self.S.dve(lambda e: e.reciprocal(SS[:, :], SS[:, :]), reads=[SS.res], writes=[SS.res])
            self.tt("dve", YN.reshape([128, 4, 256])[:, :, :], T1.reshape([128, 4, 256])[:, :, :],
                    SS.bc(0, [[1, 4], [0, 256]]), ALU.mult, reads=[T1.res, SS.res], writes=[YN.res])
            pb = PB[2].reshape([128, 8, 128])
            for m in range(8):
                self.tr(pb[:, m, :], YN[:, m * 128:(m + 1) * 128], self.cB(K_ID), reads=[YN.res, CSB.res],
                        writes=[pb.res])
            self.tt("dve", YST[:, :, csl], pb[:, :, :], COLS.bc(C_NW, [[1, 8], [0, 128]]), ALU.mult,
                    reads=[pb.res, COLS.res], writes=[(YST.res, c // 4)])
            xdd2 = xdd.reshape([128, 1024])
            for g in range(4):
                ps = P[5 + g // 2]
                self.mm(ps[:, (g % 2) * 256:(g % 2 + 1) * 256], BTK[:, c, g * 128:(g + 1) * 128],
                        xdd2[:, g * 256:(g + 1) * 256], True, True, reads=[BTK.res, xdd.res], writes=[ps.res])
            self.tt("dve", H.reshape([128, 16, 64])[:, :, :], H.reshape([128, 16, 64])[:, :, :],
                    CDEC.bc(c * 16, [[1, 16], [0, 64]]), ALU.mult, reads=[H.res, CDEC.res], writes=[H.res])
            for half in range(2):
                hs = slice(half * 512, (half + 1) * 512)
                self.tt("dve", H[:, hs], P[5 + half][:, :], H[:, hs], ALU.add, reads=[P[5 + half].res, H.res],
                        writes=[H.res])
            self.cp("act", HB[:, :], H[:, :], reads=[H.res], writes=[HB.res])
        self.dbg("YST", YST)
        self.arelease(mark0)
        self.gated_out(l, 0, YST, "wssd", self.mt_first)
        self.mt_first = False

    def attention(self, l, seq, t0, first):
        T, NT, NB, P, PB, S = self.T, self.NT, self.NB, self.P, self.PB, self.S
        w = self.dw[l]
        ROWX, CST, CSB = self.ROWX[l], self.CST, self.CSB
        AT = self.aalloc([8, T], BF16)
        mark0 = self.amark()
        COS = self.aalloc([T], F32)
        SIN = self.aalloc([T], F32)
        QR = self.aalloc([8, T], BF16)
        KD = self.aalloc([4, 128 + T], BF16)
        VA = self.aalloc([1 + NB, 4, 65], BF16)
        AO = self.aalloc([NB, 1024], BF16)
        mark1 = self.amark()
        POSI = self.aalloc([T], I32)
        ANG = self.aalloc([T], F32)
        A2 = self.aalloc([T], F32)
        KI = self.aalloc([T], I32)
        KF = self.aalloc([T], F32)
        S.dma("sp", POSI[:, :], bass.AP(self.d_pos.tensor, seq * SEQ + t0, [[0, 128], [1, T]]), writes=[POSI.res])
        self.cp("dve", ANG[:, :], POSI[:, :], reads=[POSI.res], writes=[ANG.res])
        self.ts("dve", ANG[:, :], ANG[:, :], self.cF(K_INVF, 1), ALU.mult, reads=[ANG.res, CST.res], writes=[ANG.res])
        TWO_PI = 2.0 * math.pi
        C1 = 6.28125
        C2 = TWO_PI - C1
        PI_SAFE = 3.1415925
        for dst, shift in ((SIN, 0.0), (COS, 0.5 * math.pi)):
            self.ts("dve", A2[:, :], ANG[:, :], float(shift), ALU.add, reads=[ANG.res], writes=[A2.res])
            self.ts("dve", KI[:, :], A2[:, :], 1.0 / TWO_PI, ALU.mult, reads=[A2.res], writes=[KI.res])
            self.cp("dve", KF[:, :], KI[:, :], reads=[KI.res], writes=[KF.res])
            self.stt(A2[:, :], KF[:, :], -C1, A2[:, :], ALU.mult, ALU.add, reads=[KF.res, A2.res], writes=[A2.res])
            self.stt(A2[:, :], KF[:, :], -C2, A2[:, :], ALU.mult, ALU.add, reads=[KF.res, A2.res], writes=[A2.res])
            self.ts("dve", A2[:, :], A2[:, :], -PI_SAFE, ALU.max, reads=[A2.res], writes=[A2.res], s2=PI_SAFE,
                    op1=ALU.min)
            self.actf(dst[:, :], A2[:, :], AF.Sin, reads=[A2.res], writes=[dst.res])
        self.arelease(mark1)
        Q32 = [self.aalloc([512], F32) for _ in range(2)]
        TA = [self.aalloc([512], F32) for _ in range(2)]
        TB = [self.aalloc([512], F32) for _ in range(2)]
        KR = [self.aalloc([512], BF16) for _ in range(2)]
        wbuf = [self.aalloc([8, 128], BF16) for _ in range(3)]
        WV = self.aalloc([8, 256], BF16)
        EC = [self.aalloc([512], BF16) for _ in range(2)]
        EP = [self.aalloc([512], BF16) for _ in range(2)]
        DEN = [self.aalloc([4], F32) for _ in range(2)]
        S.dma("pool", WV[:, :, :], w["win_tm"][:, 1040:1296].rearrange("(k p) n -> p k n", p=128), writes=[WV.res])
        st = {"n": 0}

        def rope(i, tt, pss, is_k):
            n = st["n"]
            st["n"] += 1
            sl = slice(tt * 512, (tt + 1) * 512)
            q32, ta, tb, pr = Q32[n % 2], TA[n % 2], TB[n % 2], P[2 + n % 2]
            self.cp("act", q32[:, :], pss[0][:, :], reads=[pss[0].res], writes=[q32.res])
            self.mm(pr[:, :], self.cF(K_ROT), q32[:, :], True, True, reads=[CST.res, q32.res], writes=[pr.res])
            self.tt("dve", ta[:, :], q32[:, :], COS[:, sl], ALU.mult, reads=[q32.res, COS.res], writes=[ta.res])
            self.tt("dve", tb[:, :], pr[:, :], SIN[:, sl], ALU.mult, reads=[pr.res, SIN.res], writes=[tb.res])
            if not is_k:
                self.tt("dve", QR[:, i, sl], ta[:, :], tb[:, :], ALU.add, reads=[ta.res, tb.res],
                        writes=[(QR.res, tt)])
            else:
                kr = KR[n % 2]
                self.tt("dve", kr[:, :], ta[:, :], tb[:, :], ALU.add, reads=[ta.res, tb.res], writes=[kr.res])
                for half in range(2):
                    h = 2 * i + half
                    pd = P[4 + half]
                    self.mm(pd[:, :], self.cB(K_DUP0 + half * 128), kr[:, :], True, True, reads=[CSB.res, kr.res],
                            writes=[pd.res])
                    self.cp("act" if half == 0 else "dve", KD[:, h, 128 + tt * 512:128 + (tt + 1) * 512], pd[:, :],
                            reads=[pd.res], writes=[KD.res])

        self.proj_fm([(w["win_fm"], FM_Q, 8, self.xt_rhs)], 8, lambda i, tt, pss: rope(i, tt, pss, False), [wbuf],
                     [[P[0], P[1]]])
        self.proj_fm([(w["win_fm"], FM_K, 8, self.xt_rhs)], 2, lambda i, tt, pss: rope(i, tt, pss, True), [wbuf],
                     [[P[0], P[1]]])
        self.memset("pool", VA[:, :, :, 64:65], 1.0, [VA.res])
        if not first:
            self.cp("act", KD[:, :, 0:128], self.KCAR[l][:, :, :], reads=[self.KCAR[l].res], writes=[KD.res])
            self.cp("act", VA[:, 0, :, :], self.VCAR[l][:, :, :], reads=[self.VCAR[l].res], writes=[VA.res])
        for blk in range(NB):
            pv = P[6 + blk % 2]
            for k in range(8):
                self.mm(pv[:, 0:256], self.XT[:, k, blk * 128:(blk + 1) * 128], WV[:, k, :], k == 0, k == 7,
                        reads=[(self.XT.res, blk // 4), WV.res], writes=[pv.res])
            self.cp("act", VA[:, 1 + blk, :, 0:64], pv.reshape([128, 8, 64])[:, 0:4, :], reads=[pv.res],
                    writes=[VA.res])
        self.dbg("QR", QR)
        self.dbg("KD", KD)
        items = [(qb, h) for qb in range(NB) for h in range(4)]

        def att_a(n, qb, h):
            gfirst = first and qb == 0
            ec, ep = EC[n % 2], EP[n % 2]
            qsl = slice(qb * 128, (qb + 1) * 128)
            ec3, ep3 = ec.reshape([128, 4, 128]), ep.reshape([128, 4, 128])
            for r in range(4):
                hq = 4 * h + r
                ch, hf = hq // 2, hq % 2
                ps_ = slice(hf * 64, hf * 64 + 64)
                sc, sp = P[hf], P[2 + hf]
                cs_ = slice((r // 2) * 128, (r // 2 + 1) * 128)
                self.mm(sc[:, cs_], KD[ps_, h, 128 + qb * 128:128 + (qb + 1) * 128],
                        QR[ps_, ch, qsl], True, True, reads=[KD.res, (QR.res, qb // 4)], writes=[sc.res])
                if not gfirst:
                    self.mm(sp[:, cs_], KD[ps_, h, qb * 128:(qb + 1) * 128],
                            QR[ps_, ch, qsl], True, True, reads=[KD.res, (QR.res, qb // 4)], writes=[sp.res])
            for hf in range(2):
                self.actf(ec3[:, hf::2, :], P[hf].reshape([128, 4, 128])[:, 0:2, :], AF.Exp, reads=[P[hf].res],
                          writes=[ec.res], scale=0.125)
            self.tt("dve", ec3[:, :, :], ec3[:, :, :],
                    CSB.bc(K_TU, [[0, 4], [1, 128]]), ALU.mult, reads=[ec.res, CSB.res], writes=[ec.res])
            if not gfirst:
                for hf in range(2):
                    self.actf(ep3[:, hf::2, :], P[2 + hf].reshape([128, 4, 128])[:, 0:2, :], AF.Exp,
                              reads=[P[2 + hf].res], writes=[ep.res], scale=0.125)
                self.tt("dve", ep3[:, :, :], ep3[:, :, :],
                        CSB.bc(K_TL, [[0, 4], [1, 128]]), ALU.mult, reads=[ep.res, CSB.res], writes=[ep.res])

        def att_b(n, qb, h):
            gfirst = first and qb == 0
            po = P[4 + n % 2]
            ec, ep, den = EC[n % 2], EP[n % 2], DEN[n % 2]
            po3 = po.reshape([128, 4, 128])
            for r in range(4):
                if not gfirst:
                    self.mm(po3[:, r, 0:65], ep[:, r * 128:(r + 1) * 128], VA[:, qb, h, :], True, False,
                            reads=[ep.res, VA.res], writes=[po.res])
                self.mm(po3[:, r, 0:65], ec[:, r * 128:(r + 1) * 128], VA[:, qb + 1, h, :], gfirst, True,
                        reads=[ec.res, VA.res], writes=[po.res])
            self.tt("dve", den[:, :], po3[:, :, 64], ROWX.bc(16 + 4 * h, [[1, 4]]), ALU.add,
                    reads=[po.res, ROWX.res], writes=[den.res])
            self.S.dve(lambda e, den=den: e.reciprocal(den[:, :], den[:, :]), reads=[den.res], writes=[den.res])
            self.tt("dve", AO.reshape([128, NB, 16, 64])[:, qb, 4 * h:4 * h + 4, :], po3[:, :, 0:64],
                    den.bc(0, [[1, 4], [0, 64]]), ALU.mult, reads=[po.res, den.res], writes=[(AO.res, qb)])

        att_a(0, *items[0])
        for n, (qb, h) in enumerate(items):
            if n + 1 < len(items):
                att_a(n + 1, *items[n + 1])
            att_b(n, qb, h)
        for blk in range(NB):
            pb = PB[6 + blk % 2].reshape([128, 8, 128])
            for m in range(8):
                self.tr(pb[:, m, :], AO[:, blk, m * 128:(m + 1) * 128], self.cB(K_ID), reads=[(AO.res, blk), CSB.res],
                        writes=[pb.res])
            self.cp("act" if blk % 2 else "dve", AT[:, :, blk * 128:(blk + 1) * 128], pb[:, :, :], reads=[pb.res],
                    writes=[(AT.res, blk // 4)])
        self.cp("act", self.KCAR[l][:, :, :], KD[:, :, T:T + 128], reads=[KD.res], writes=[self.KCAR[l].res])
        self.cp("act", self.VCAR[l][:, :, :], VA[:, NB, :, :], reads=[VA.res], writes=[self.VCAR[l].res])
        self.dbg("AT", AT)
        self.arelease(mark0)
        self.gated_out(l, 2, AT, "wattn", self.mt_first)
        self.mt_first = False

    def moe(self, l):
        T, NT, NB, P, S = self.T, self.NT, self.NB, self.P, self.S
        w = self.dw[l]
        ROWS, CST = self.ROWS[l], self.CST
        WR = self.aalloc([8, 36], BF16)
        LOG = self.aalloc([NB, 36], F32)
        WTOK = self.aalloc([NB, 4, 8], F32)
        WT = self.aalloc([T], F32)
        S.dma("pool", WR[:, :, :], w["wr"].rearrange("(k p) n -> p k n", p=128), writes=[WR.res])
        for blk in range(NB):
            pl = P[6 + blk % 2]
            for k in range(8):
                self.mm(pl[:, 0:36], self.XT[:, k, blk * 128:(blk + 1) * 128], WR[:, k, :], k == 0, k == 7,
                        reads=[(self.XT.res, blk // 4), WR.res], writes=[pl.res])
            self.tt("dve", LOG[:, blk, :], pl[:, 0:36], ROWS.bc(R_RB, [[1, 36]]), ALU.add, reads=[pl.res, ROWS.res],
                    writes=[LOG.res])
        mk = self.amark()
        f = lambda shp: self.aalloc(shp, F32)
        GMAX, GS, GE, GMASK = f([NB]), f([NB]), f([NB, 4]), f([NB, 4])
        V1, V2, P1, P2 = f([NB, 4]), f([NB, 4]), f([NB, 4]), f([NB, 4])
        M1, M2, E2 = f([NB, 4, 8]), f([NB, 4, 8]), f([NB, 4, 8])
        GL = lambda: LOG[:, :, 0:4]
        EL = lambda: LOG.bc(4, [[36, NB], [8, 4], [1, 8]])
        red = lambda out, in_, op, rd, wr: self.S.dve(
            lambda e: e.tensor_reduce(out=out, in_=in_, op=op, axis=AX.X), reads=rd, writes=wr)
        red(GMAX[:, :], GL(), ALU.max, [LOG.res], [GMAX.res])
        self.tt("dve", GE[:, :, :], GL(), GMAX.bc(0, [[1, NB], [0, 4]]), ALU.subtract, reads=[LOG.res, GMAX.res],
                writes=[GE.res])
        self.tt("dve", GMASK[:, :, :], GL(), GMAX.bc(0, [[1, NB], [0, 4]]), ALU.is_equal, reads=[LOG.res, GMAX.res],
                writes=[GMASK.res])
        self.actf(GE[:, :, :], GE[:, :, :], AF.Exp, reads=[GE.res], writes=[GE.res])
        red(GS[:, :], GE[:, :, :], ALU.add, [GE.res], [GS.res])
        self.S.dve(lambda e: e.reciprocal(GS[:, :], GS[:, :]), reads=[GS.res], writes=[GS.res])
        self.tt("dve", GMASK[:, :, :], GMASK[:, :, :], GS.bc(0, [[1, NB], [0, 4]]), ALU.mult,
                reads=[GMASK.res, GS.res], writes=[GMASK.res])
        red(V1[:, :, :], EL(), ALU.max, [LOG.res], [V1.res])
        self.tt("dve", M1[:, :, :, :], EL(), V1.bc(0, [[4, NB], [1, 4], [0, 8]]), ALU.is_equal,
                reads=[LOG.res, V1.res], writes=[M1.res])
        self.stt(E2[:, :, :, :], M1[:, :, :, :], -1.0e30, EL(), ALU.mult, ALU.add, reads=[M1.res, LOG.res],
                 writes=[E2.res])
        red(V2[:, :, :], E2[:, :, :, :], ALU.max, [E2.res], [V2.res])
        self.tt("dve", M2[:, :, :, :], E2[:, :, :, :], V2.bc(0, [[4, NB], [1, 4], [0, 8]]), ALU.is_equal,
                reads=[E2.res, V2.res], writes=[M2.res])
        self.tt("dve", P1[:, :, :], V1[:, :, :], V2[:, :, :], ALU.subtract, reads=[V1.res, V2.res], writes=[P1.res])
        self.actf(P1[:, :, :], P1[:, :, :], AF.Sigmoid, reads=[P1.res], writes=[P1.res])
        self.ts("dve", P2[:, :, :], P1[:, :, :], -1.0, ALU.mult, reads=[P1.res], writes=[P2.res], s2=1.0, op1=ALU.add)
        self.tt("dve", P1[:, :, :], P1[:, :, :], GMASK[:, :, :], ALU.mult, reads=[P1.res, GMASK.res], writes=[P1.res])
        self.tt("dve", P2[:, :, :], P2[:, :, :], GMASK[:, :, :], ALU.mult, reads=[P2.res, GMASK.res], writes=[P2.res])
        self.tt("dve", M1[:, :, :, :], M1[:, :, :, :], P1.bc(0, [[4, NB], [1, 4], [0, 8]]), ALU.mult,
                reads=[M1.res, P1.res], writes=[M1.res])
        self.tt("dve", M2[:, :, :, :], M2[:, :, :, :], P2.bc(0, [[4, NB], [1, 4], [0, 8]]), ALU.mult,
                reads=[M2.res, P2.res], writes=[M2.res])
        self.tt("dve", WTOK[:, :, :, :], M1[:, :, :, :], M2[:, :, :, :], ALU.add, reads=[M1.res, M2.res],
                writes=[WTOK.res])
        self.dbg("WTOK", WTOK)
        wt2 = WTOK.reshape([128, NB, 32])
        for b0 in range(0, NB, 4):
            ps = P[7]
            for j in range(4):
                self.tr(ps[0:32, j * 128:(j + 1) * 128], wt2[:, b0 + j, :], self.cF(K_ID), reads=[WTOK.res, CST.res],
                        writes=[ps.res])
            self.cp("act", WT[0:32, b0 * 128:(b0 + 4) * 128], ps[0:32, :], reads=[ps.res], writes=[WT.res])
        self.arelease(mk)
        NWB = 3
        WG = [self.aalloc([4, 8, 128], BF16) for _ in range(NWB)]
        WU = [self.aalloc([4, 8, 128], BF16) for _ in range(NWB)]
        WD = [self.aalloc([4, 1024], BF16) for _ in range(NWB)]
        HT = [self.aalloc([4, 512], BF16) for _ in range(2)]
        SIL = [self.aalloc([512], F32) for _ in range(2)]
        WBC = [self.aalloc([512], F32) for _ in range(2)]
        SEL = [self.aalloc([128], F32) for _ in range(2)]

        def load_expert(e):
            wg, wu, wd = WG[e % NWB], WU[e % NWB], WD[e % NWB]
            S.dma("pool", wg.bc(0, [[1024, 4], [1, 1024]]), w["wg"][e].rearrange("p m k c -> p m (k c)"),
                  writes=[wg.res])
            S.dma("pool", wu.bc(0, [[1024, 4], [1, 1024]]), w["wu"][e].rearrange("p m k c -> p m (k c)"),
                  writes=[wu.res])
            S.dma("pool", wd[:, :, :], w["wd"][e].rearrange("(k p) n -> p k n", p=128), writes=[wd.res])

        PY = [P[4], P[5], P[6]]
        items = [(e, tt) for e in range(self.nexp) for tt in range(NT)]
        st = {"dc": 0}

        def gate_up(n, e, tt):
            wg, wu = WG[e % NWB], WU[e % NWB]
            sl = slice(tt * 512, (tt + 1) * 512)
            ht, wbc = HT[n % 2], WBC[n % 2]
            sel = SEL[e % 2]
            if tt == 0:
                self.cp("act", sel[0:32, :], CST.bc(K_ID + e, [[0, 128]], npart=32), reads=[CST.res],
                        writes=[sel.res])
            self.mm(P[7][:, :], sel[0:32, :], WT[0:32, sl], True, True, reads=[sel.res, WT.res],
                    writes=[P[7].res])
            self.cp("act", wbc[:, :], P[7][:, :], reads=[P[7].res], writes=[wbc.res])
            for m in range(4):
                pg, pu = P[m % 2], P[2 + m % 2]
                sil = SIL[m % 2]
                for k in range(8):
                    self.mm(pg[:, :], wg[:, m, k, :], self.XT[:, k, sl], k == 0, k == 7,
                            reads=[wg.res, (self.XT.res, tt)], writes=[pg.res])
                for k in range(8):
                    self.mm(pu[:, :], wu[:, m, k, :], self.XT[:, k, sl], k == 0, k == 7,
                            reads=[wu.res, (self.XT.res, tt)], writes=[pu.res])
                self.actf(sil[:, :], pg[:, :], AF.Silu, reads=[pg.res], writes=[sil.res])
                self.tt("dve", sil[:, :], pu[:, :], sil[:, :], ALU.mult, reads=[pu.res, sil.res], writes=[sil.res])
                self.tt("dve", ht[:, m, :], sil[:, :], wbc[:, :], ALU.mult, reads=[sil.res, wbc.res],
                        writes=[ht.res])

        def down(n, e, tt):
            wd = WD[e % NWB]
            sl = slice(tt * 512, (tt + 1) * 512)
            ht = HT[n % 2]
            for dc in range(8):
                py = PY[st["dc"] % 3]
                st["dc"] += 1
                for m in range(4):
                    self.mm(py[:, :], wd[:, m, dc * 128:(dc + 1) * 128], ht[:, m, :], m == 0, m == 3,
                            reads=[wd.res, ht.res], writes=[py.res])
                self.tt("dve", self.X[:, dc, sl], py[:, :], self.X[:, dc, sl], ALU.add,
                        reads=[py.res, (self.X.res, tt)], writes=[(self.X.res, tt)])

        for e in range(min(NWB - 1, self.nexp)):
            load_expert(e)
        for n, (e, tt) in enumerate(items):
            gate_up(n, e, tt)
            if n >= 1:
                pe_, ptt = items[n - 1]
                down(n - 1, pe_, ptt)
                if ptt == NT - 1 and pe_ + NWB < self.nexp + 0 and pe_ + NWB - 1 < self.nexp:
                    pass
            if tt == 0 and e + NWB - 1 < self.nexp and e >= 1:
                pass
            if n >= 1 and items[n - 1][1] == NT - 1:
                nxt = items[n - 1][0] + NWB
                if nxt < self.nexp:
                    load_expert(nxt)
            if n == 0 and NWB - 1 < self.nexp:
                load_expert(NWB - 1)
        down(len(items) - 1, *items[-1])


def host_consts():
    c = np.zeros((128, NCF), np.float32)
    k = np.arange(128)[:, None]
    m = np.arange(128)[None, :]
    c[:, K_ID:K_ID + 128] = (k == m)
    c[:, K_TU:K_TU + 128] = (k <= m)
    c[:, K_TL:K_TL + 128] = (k > m)
    c[:, K_ONE:K_ONE + 128] = 1.0
    R = np.zeros((128, 128), np.float32)
    D0 = np.zeros((128, 128), np.float32)
    D1 = np.zeros((128, 128), np.float32)
    for mm_ in range(128):
        b, j = mm_ // 64 * 64, mm_ % 64
        if j < 32:
            R[b + j + 32, mm_] = -1.0
        else:
            R[b + j - 32, mm_] = 1.0
        D0[j, mm_] = 1.0
        D1[64 + j, mm_] = 1.0
    c[:, K_ROT:K_ROT + 128] = R
    c[:, K_DUP0:K_DUP0 + 128] = D0
    c[:, K_DUP1:K_DUP1 + 128] = D1
    invf = (np.float32(10000.0) ** (-np.arange(32, dtype=np.float32) / np.float32(32))).astype(np.float32)
    c[:, K_INVF] = invf[np.arange(128) % 32]
    c[:, K_EPS] = EPS
    c[:, K_ONEC] = 1.0
    return c


def fm_layout(w):
    K, N = w.shape
    return np.ascontiguousarray(w.reshape(K // 128, 128, N // 128, 128).transpose(2, 1, 0, 3))


def colsT(v, n):
    return np.asarray(v, np.float32).reshape(n, 128).T


def prep_weights(inp, l):
    f = lambda n: np.asarray(inp[n][l], np.float32)
    w_in = f("w_in")
    o = {}
    o["win_fm%d" % l] = fm_layout(np.concatenate(
        [w_in[:, 0:3072], w_in[:, 4096:6144], w_in[:, 6160:8208], w_in[:, 8208:9232], w_in[:, 9232:9488]], axis=1))
    o["win_tm%d" % l] = np.ascontiguousarray(np.concatenate(
        [w_in[:, 3072:4096], w_in[:, 6144:6160], w_in[:, 9488:9744]], axis=1))
    for nm, src in (("wssd", "ssd_w_out"), ("wconf", "conf_w_out"), ("wattn", "attn_w_out"), ("wout", "w_out"),
                    ("wpg", "ple_w_gate"), ("wpp", "ple_w_proj")):
        o["%s%d" % (nm, l)] = fm_layout(f(src))
    o["wr%d" % l] = np.ascontiguousarray(np.concatenate([f("moe_w_group"), f("moe_w_expert")], axis=1))
    wg, wu = f("moe_w_gate"), f("moe_w_up")
    o["wg%d" % l] = np.ascontiguousarray(wg.reshape(NEXP, 8, 128, 4, 128).transpose(0, 2, 3, 1, 4))
    o["wu%d" % l] = np.ascontiguousarray(wu.reshape(NEXP, 8, 128, 4, 128).transpose(0, 2, 3, 1, 4))
    o["wd%d" % l] = np.ascontiguousarray(f("moe_w_down"))
    cols = np.zeros((128, NCOLS), np.float32)
    cols[:, C_BGATE:C_BGATE + 24] = colsT(f("b_gate"), 24)
    cols[:, C_SCW:C_SCW + 64] = f("ssd_conv_w").reshape(4, 16, 128).transpose(2, 1, 0).reshape(128, 64)
    cols[:, C_SCB:C_SCB + 16] = colsT(f("ssd_conv_b"), 16)
    cols[:, C_CDW:C_CDW + 248] = f("conf_dw_w").reshape(31, 8, 128).transpose(2, 1, 0).reshape(128, 248)
    for c0, nm in ((C_CDB, "conf_dw_b"), (C_CLG, "conf_ln_g"), (C_CLB, "conf_ln_b"), (C_NW, "ssd_norm_w"),
                   (C_L1G, "ln1_g"), (C_L1B, "ln1_b"), (C_L2G, "ln2_g"), (C_L2B, "ln2_b")):
        cols[:, c0:c0 + 8] = colsT(f(nm), 8)
    o["cols%d" % l] = cols
    rows = np.zeros((1, NROWS), np.float32)
    rows[0, R_DTB:R_DTB + 16] = f("ssd_dt_bias")
    rows[0, R_ALOG:R_ALOG + 16] = f("ssd_a_log")
    rows[0, R_D:R_D + 16] = f("ssd_d")
    rows[0, R_SINK:R_SINK + 16] = f("attn_sinks")
    rows[0, R_RB:R_RB + 4] = f("moe_b_group")
    rows[0, R_RB + 4:R_RB + 36] = f("moe_b_expert")
    o["rows%d" % l] = rows
    return o


def make_in_maps(inp, seq_lists, nlayer=DEPTH):
    shared = {"cst": host_consts()}
    for l in range(nlayer):
        shared.update(prep_weights(inp, l))
    maps = []
    for seqs in seq_lists:
        m = dict(shared)
        m["x"] = np.ascontiguousarray(np.asarray(inp["x"], np.float32)[seqs])
        m["p"] = np.ascontiguousarray(np.asarray(inp["p"], np.float32)[:, seqs])
        m["pos"] = np.ascontiguousarray(np.asarray(inp["positions"], np.int32)[seqs])
        maps.append(m)
    return maps


_NC_CACHE = {}


def kernel(**inputs):
    if "nc" not in _NC_CACHE:
        _NC_CACHE["nc"] = Builder().nc
    nc = _NC_CACHE["nc"]
    seq_lists = [list(range(c * SEQ_PER_CORE, (c + 1) * SEQ_PER_CORE)) for c in range(NCORES)]
    maps = make_in_maps(inputs, seq_lists)
    res = run_bass_kernel_spmd(nc, maps, core_ids=list(range(NCORES)))
    out = np.concatenate([np.asarray(r["out"], np.float32) for r in res.results], axis=0)
    return out
```

```python
import contextlib
import math
import numpy as np
import concourse.bass as bass
import concourse.mybir as mybir
from concourse.bass_utils import run_bass_kernel_spmd

F32 = mybir.dt.float32
BF16 = mybir.dt.bfloat16
I32 = mybir.dt.int32
AF = mybir.ActivationFunctionType
ALU = mybir.AluOpType
AX = mybir.AxisListType

ENGS = ("pe", "act", "dve", "pool", "sp")
NDMASEM = 8

D = 1024
SEQ = 2048
DEPTH = 2
NCORES = 8
SEQ_PER_CORE = 4
PLE = 256
NEXP = 32
FF = 512
ALPHA = (2 * DEPTH) ** 0.25
EPS = 1e-5
OFF_GATE, OFF_Z, OFF_XBC, OFF_DT, OFF_CONF, OFF_Q, OFF_K, OFF_V = 0, 3072, 4096, 6144, 6160, 8208, 9232, 9488
FM_GATE, FM_XBC, FM_CONF, FM_Q, FM_K = 0, 24, 40, 56, 64
NFM = 66
C_BGATE = 0
C_SCW = 24
C_SCB = 88
C_CDW = 104
C_CDB = 352
C_CLG = 360
C_CLB = 368
C_NW = 376
C_L1G = 384
C_L1B = 392
C_L2G = 400
C_L2B = 408
NCOLS = 416
R_DTB, R_ALOG, R_D, R_SINK, R_RB = 0, 16, 32, 48, 64
NROWS = 100
K_ID, K_TU, K_TL, K_ONE, K_ROT, K_DUP0, K_DUP1, K_INVF, K_EPS, K_ONEC = 0, 128, 256, 384, 512, 640, 768, 896, 897, 898
NCF = 900


class Op:
    __slots__ = ("eng", "fn", "dma", "idx", "tick", "sem_i", "sem_v", "deps", "prewait")


class Sched:
    def __init__(self, nc):
        self.nc = nc
        self.ops = []
        self.state = {}
        self.cnt = {e: 0 for e in ENGS}
        self.dcnt = {e: 0 for e in ENGS}
        self.dma_hist = {e: [] for e in ENGS}
        self.last = {e: None for e in ENGS}
        self.pending = {e: set() for e in ENGS}
        self.exclusive = set()

    def fence(self):
        F = set()
        for e in ENGS:
            if self.last[e] is not None:
                F.add(self.last[e])
            for op in self.dma_hist[e][-NDMASEM:]:
                F.add(op)
        for e in ENGS:
            self.pending[e] |= F

    @staticmethod
    def _norm(lst):
        out = []
        for r in lst:
            if isinstance(r, tuple):
                out.append((id(r[0]), r[1]))
            else:
                out.append((id(r), None))
        return out

    def _conf(self, res):
        b, k = res
        d = self.state.get(b)
        if d is None:
            return
        if k is None:
            for st in d.values():
                yield st
        else:
            st = d.get(k)
            if st is not None:
                yield st
            st = d.get(None)
            if st is not None:
                yield st

    def add(self, eng, fn, reads=(), writes=(), dma=False):
        reads = self._norm(reads)
        writes = self._norm(writes)
        if self.exclusive:
            ex = [r for r in reads if r[0] in self.exclusive]
            if ex:
                reads = [r for r in reads if r[0] not in self.exclusive]
                writes = writes + [r for r in ex if r not in writes]
        op = Op()
        op.eng, op.fn, op.dma, op.prewait = eng, fn, dma, None
        op.idx = len(self.ops)
        deps = set()
        for r in reads:
            for st in self._conf(r):
                if st[0] is not None:
                    deps.add(st[0])
        for w in writes:
            for st in self._conf(w):
                if st[0] is not None:
                    deps.add(st[0])
                deps.update(st[1])
        if self.pending[eng]:
            deps |= self.pending[eng]
            self.pending[eng] = set()
        op.deps = deps
        for w in writes:
            b, k = w
            d = self.state.setdefault(b, {})
            if k is None:
                d.clear()
            d[k] = [op, []]
        for r in reads:
            b, k = r
            d = self.state.setdefault(b, {})
            st = d.get(k)
            if st is None:
                st = [None, []]
                for s2 in self._conf(r):
                    if s2[0] is not None and (st[0] is None or s2[0].idx > st[0].idx):
                        st[0] = s2[0]
                d[k] = st
            st[1].append(op)
        if dma:
            i = self.dcnt[eng]
            self.dcnt[eng] += 1
            op.sem_i = i % NDMASEM
            op.sem_v = 16 * (i // NDMASEM + 1)
            hist = self.dma_hist[eng]
            if i >= NDMASEM:
                op.prewait = hist[i - NDMASEM]
            hist.append(op)
            op.tick = None
        else:
            self.cnt[eng] += 1
            op.tick = self.cnt[eng]
            self.last[eng] = op
        self.ops.append(op)
        return op

    def pe(self, fn, reads=(), writes=()):
        return self.add("pe", fn, reads, writes)

    def act(self, fn, reads=(), writes=()):
        return self.add("act", fn, reads, writes)

    def dve(self, fn, reads=(), writes=()):
        return self.add("dve", fn, reads, writes)

    def pool(self, fn, reads=(), writes=()):
        return self.add("pool", fn, reads, writes)

    def dma(self, eng, out, in_, reads=(), writes=()):
        return self.add(eng, lambda e: e.dma_start(out=out, in_=in_), reads, writes, dma=True)

    def emit(self):
        nc = self.nc
        with contextlib.ExitStack() as es:
            esem = {e: es.enter_context(nc.semaphore("s_" + e)) for e in ENGS}
            dsem = {e: [es.enter_context(nc.semaphore("d_%s%d" % (e, i))) for i in range(NDMASEM)]
                    for e in ENGS if self.dcnt[e] > 0}
            block = es.enter_context(nc.Block())
            per = {e: [op for op in self.ops if op.eng == e] for e in ENGS}

            def run(engname, eng):
                waited = {}

                def wait_for(dep):
                    if dep.dma:
                        s = dsem[dep.eng][dep.sem_i]
                        v = dep.sem_v
                    else:
                        if dep.eng == "pe" and engname == "pe":
                            return
                        s = esem[dep.eng]
                        v = dep.tick
                    key = id(s)
                    if waited.get(key, 0) >= v:
                        return
                    waited[key] = v
                    eng.wait_ge(s, v)

                for op in per[engname]:
                    for dep in sorted(op.deps, key=lambda d: d.idx):
                        wait_for(dep)
                    if op.prewait is not None:
                        wait_for(op.prewait)
                    inst = op.fn(eng)
                    if op.dma:
                        inst.then_inc(dsem[engname][op.sem_i], 16)
                    else:
                        inst.then_inc(esem[engname], 1)
                for op in self.dma_hist[engname][-NDMASEM:]:
                    wait_for(op)

            @block.sync
            def _(eng):
                run("sp", eng)

            @block.tensor
            def _(eng):
                run("pe", eng)

            @block.scalar
            def _(eng):
                run("act", eng)

            @block.vector
            def _(eng):
                run("dve", eng)

            @block.gpsimd
            def _(eng):
                run("pool", eng)


def pstride(t):
    return int(np.prod(list(t.shape)[1:]))


class View:
    def __init__(self, h, off, shape, res=None):
        self.h, self.off, self.shape = h, off, list(shape)
        self.dtype = h.dtype
        st, s = [], 1
        for n in reversed(self.shape[1:]):
            st.append(s)
            s *= n
        self.strides = list(reversed(st))
        self.ps = pstride(h)
        self.res = self if res is None else res

    def __getitem__(self, idx):
        if not isinstance(idx, tuple):
            idx = (idx,)
        idx = list(idx) + [slice(None)] * (len(self.shape) - len(idx))
        p = idx[0]
        p0, p1, _ = p.indices(self.shape[0])
        off = self.off
        dims = []
        for i, ix in enumerate(idx[1:]):
            stride, n = self.strides[i], self.shape[i + 1]
            if isinstance(ix, int):
                off += ix * stride
            else:
                a, b, st = ix.indices(n)
                cnt = len(range(a, b, st))
                off += a * stride
                dims.append([stride * st, cnt])
        m = [list(d) for d in dims]
        if not m:
            m = [[1, 1]]
        return bass.AP(self.h, p0 * self.ps + off, [[self.ps, p1 - p0]] + m)

    def bc(self, off, dims, npart=128, p0=0):
        return bass.AP(self.h, p0 * self.ps + self.off + off, [[self.ps, npart]] + [list(d) for d in dims])

    def reshape(self, shape):
        return View(self.h, self.off, shape, res=self.res)


class Builder:
    def __init__(self, nseq=SEQ_PER_CORE, nlayer=DEPTH, T=1024, nseg=None, debug=(), nexp=NEXP, stop_after=None,
                 only=None):
        self.nseq, self.nlayer, self.T = nseq, nlayer, T
        self.NT = T // 512
        self.NB = T // 128
        self.nseg = (SEQ // T) if nseg is None else nseg
        self.debug = set(debug)
        self.nexp = nexp
        self.stop_after = stop_after
        self.only = None if only is None else set(only.split(',')) if isinstance(only, str) else set(only)
        self.dbg_outs = {}
        nc = self.nc = bass.Bass("TRN2", target_bir_lowering=False)
        self.S = Sched(nc)
        self.declare_dram()
        self.alloc()
        self.program()
        self.S.emit()

    def din(self, name, shape, dt=F32):
        return self.nc.dram_tensor(name, list(shape), dt, kind="ExternalInput").ap()

    def pers(self, name, shape, dt):
        h = self.nc.alloc_sbuf_tensor(name, list(shape), dt)
        return View(h, 0, shape)

    def areset(self):
        self.S.fence()
        self.aoff = 0

    def aalloc(self, fshape, dt, npart=128):
        size = {F32: 4, BF16: 2, I32: 4}[dt]
        n = int(np.prod(fshape)) * size
        n = (n + 31) // 32 * 32
        off = self.aoff
        self.aoff += n
        assert self.aoff <= self.arena_bytes, ("arena overflow", self.aoff, self.arena_bytes)
        return View(self.arena[dt], off // size, [npart] + list(fshape))

    def dbg(self, name, view, reads=None):
        if name not in self.debug or name in self.dbg_outs:
            return
        o = self.nc.dram_tensor("dbg_" + name, list(view.shape), view.dtype, kind="ExternalOutput").ap()
        self.dbg_outs[name] = o
        self.S.dma("sp", o, view[:], reads=[view.res] if reads is None else reads)

    def declare_dram(self):
        L = self.nlayer
        self.d_x = self.din("x", [self.nseq, SEQ, D])
        self.d_p = self.din("p", [DEPTH, self.nseq, SEQ, PLE])
        self.d_pos = self.din("pos", [self.nseq, SEQ], I32)
        self.d_cst = self.din("cst", [128, NCF])
        self.d_out = self.nc.dram_tensor("out", [self.nseq, SEQ, D], F32, kind="ExternalOutput").ap()
        self.dw = []
        for l in range(L):
            w = {}
            w["win_fm"] = self.din("win_fm%d" % l, [NFM, 128, 8, 128])
            w["win_tm"] = self.din("win_tm%d" % l, [D, 1296])
            for nm in ("wssd", "wconf", "wattn", "wout", "wpg"):
                w[nm] = self.din("%s%d" % (nm, l), [8, 128, 8, 128])
            w["wpp"] = self.din("wpp%d" % l, [8, 128, 2, 128])
            w["wr"] = self.din("wr%d" % l, [D, 36])
            w["wg"] = self.din("wg%d" % l, [NEXP, 128, 4, 8, 128])
            w["wu"] = self.din("wu%d" % l, [NEXP, 128, 4, 8, 128])
            w["wd"] = self.din("wd%d" % l, [NEXP, FF, D])
            w["cols"] = self.din("cols%d" % l, [128, NCOLS])
            w["rows"] = self.din("rows%d" % l, [1, NROWS])
            self.dw.append(w)

    def alloc(self):
        T, nc, L = self.T, self.nc, self.nlayer
        pers = self.pers
        self.X = pers("X", [128, 8, T], F32)
        self.XT = pers("XT", [128, 8, T], BF16)
        self.MT = pers("MT", [128, 8, T], BF16)
        self.CST = pers("CST", [128, NCF], F32)
        self.CSB = pers("CSB", [128, NCF], BF16)
        self.COLS = [pers("COLS%d" % l, [128, NCOLS], F32) for l in range(L)]
        self.COLA = [pers("COLA%d" % l, [128, 16], F32) for l in range(L)]
        self.ROWS = [pers("ROWS%d" % l, [128, NROWS], F32) for l in range(L)]
        self.ROWX = [pers("ROWX%d" % l, [128, 32], F32) for l in range(L)]
        self.H = [pers("H%d" % l, [128, 1024], F32) for l in range(L)]
        self.CTAIL = [pers("CTAIL%d" % l, [128, 8, 32], BF16) for l in range(L)]
        self.STAIL = [pers("STAIL%d" % l, [128, 16, 4], BF16) for l in range(L)]
        self.KCAR = [pers("KCAR%d" % l, [128, 4, 128], BF16) for l in range(L)]
        self.VCAR = [pers("VCAR%d" % l, [128, 4, 65], BF16) for l in range(L)]
        ph = [nc.alloc_psum_tensor("P%d" % i, [128, 512], F32) for i in range(8)]
        self.P = [View(h, 0, [128, 512]) for h in ph]
        self.PB = [View(h.bitcast(BF16), 0, [128, 1024], res=v) for h, v in zip(ph, self.P)]
        self.S.exclusive = {id(v) for v in self.P}
        rem = nc.sbuf_bytes_remaining
        rem = rem() if callable(rem) else rem
        self.arena_bytes = (int(rem) - 8192) // 64 * 64
        ah = nc.alloc_sbuf_tensor("ARENA", [128, self.arena_bytes // 4], F32)
        self.arena = {F32: ah, BF16: ah.bitcast(BF16), I32: ah.bitcast(I32)}
        self.aoff = 0

    def mm(self, out, lhsT, rhs, start, stop, reads, writes):
        self.S.pe(lambda e: e.matmul(out, lhsT, rhs, start=start, stop=stop), reads, writes)

    def tr(self, out, in_, ident, reads, writes):
        self.S.pe(lambda e: e.transpose(out, in_, ident), reads, writes)

    def actf(self, out, in_, func, reads, writes, scale=1.0, bias=None):
        if bias is None:
            self.S.act(lambda e: e.activation(out=out, in_=in_, func=func, scale=scale), reads, writes)
        else:
            self.S.act(lambda e: e.activation(out=out, in_=in_, func=func, scale=scale, bias=bias), reads, writes)

    def tt(self, eng, out, in0, in1, op, reads, writes):
        self.S.add(eng, lambda e: e.tensor_tensor(out=out, in0=in0, in1=in1, op=op), reads, writes)

    def ts(self, eng, out, in0, s1, op0, reads, writes, s2=None, op1=None):
        if op1 is None:
            self.S.add(eng, lambda e: e.tensor_scalar(out=out, in0=in0, scalar1=s1, scalar2=None, op0=op0), reads, writes)
        else:
            self.S.add(eng, lambda e: e.tensor_scalar(out=out, in0=in0, scalar1=s1, scalar2=s2, op0=op0, op1=op1),
                       reads, writes)

    def stt(self, out, in0, scalar, in1, op0, op1, reads, writes):
        self.S.dve(lambda e: e.scalar_tensor_tensor(out=out, in0=in0, scalar=scalar, in1=in1, op0=op0, op1=op1),
                   reads, writes)

    def cp(self, eng, out, in_, reads, writes):
        if eng == "act":
            self.S.act(lambda e: e.copy(out, in_), reads, writes)
        else:
            self.S.add(eng, lambda e: e.tensor_copy(out, in_), reads, writes)

    def memset(self, eng, ap, val, writes):
        self.S.add(eng, lambda e: e.memset(ap, val), (), writes)

    def cF(self, off, n=128, npart=128, p0=0):
        return self.CST[p0:p0 + npart, off:off + n]

    def cB(self, off, n=128, npart=128, p0=0):
        return self.CSB[p0:p0 + npart, off:off + n]

    def col(self, l, c, npart=128):
        return self.COLS[l][0:npart, c:c + 1]

    def proj_fm(self, srcs, nchunk, consumer, wbufs, psums):
        NT = self.NT
        cnt = 0
        depth = min(len(wb) for wb in wbufs) - 1

        def issue(i):
            for si, (wsrc, c0, KC, rhs_fn) in enumerate(srcs):
                wb = wbufs[si][i % len(wbufs[si])]
                self.S.dma("pool", wb[:, 0:KC, :], wsrc[c0 + i], writes=[wb.res])

        for i in range(min(depth, nchunk)):
            issue(i)
        for i in range(nchunk):
            if i + depth < nchunk:
                issue(i + depth)
            wbs = [wbufs[si][i % len(wbufs[si])] for si in range(len(srcs))]
            for tt in range(NT):
                pss = []
                for si, (wsrc, c0, KC, rhs_fn) in enumerate(srcs):
                    ps = psums[si][cnt % len(psums[si])]
                    for k in range(KC):
                        rap, rres = rhs_fn(k, tt)
                        self.mm(ps[:, :], wbs[si][:, k, :], rap, k == 0, k == KC - 1,
                                reads=[wbs[si].res, rres], writes=[ps.res])
                    pss.append(ps)
                cnt += 1
                consumer(i, tt, pss)

    def xt_rhs(self, k, tt):
        return self.XT[:, k, tt * 512:(tt + 1) * 512], (self.XT.res, tt)

    def gated_out(self, l, branch, YT, wname, first):
        w = self.dw[l]
        wbA = [self.aalloc([8, 128], BF16) for _ in range(3)]
        wbB = [self.aalloc([8, 128], BF16) for _ in range(3)]
        SG = [self.aalloc([512], F32) for _ in range(2)]
        TM = [self.aalloc([512], BF16) for _ in range(2)]
        P = self.P
        st = {"n": 0}

        def yt_rhs(k, tt):
            return YT[:, k, tt * 512:(tt + 1) * 512], (YT.res, tt)

        def consumer(i, tt, pss):
            n = st["n"]
            st["n"] += 1
            sg = SG[n % 2]
            self.actf(sg[:, :], pss[1][:, :], AF.Sigmoid, reads=[pss[1].res], writes=[sg.res],
                      bias=self.col(l, C_BGATE + branch * 8 + i))
            dst = self.MT[:, i, tt * 512:(tt + 1) * 512]
            if first:
                self.tt("dve", dst, pss[0][:, :], sg[:, :], ALU.mult, reads=[pss[0].res, sg.res],
                        writes=[(self.MT.res, tt)])
            else:
                tm = TM[n % 2]
                self.tt("dve", tm[:, :], pss[0][:, :], sg[:, :], ALU.mult, reads=[pss[0].res, sg.res],
                        writes=[tm.res])
                self.tt("dve", dst, dst, tm[:, :], ALU.add, reads=[tm.res, (self.MT.res, tt)],
                        writes=[(self.MT.res, tt)])

        self.proj_fm([(w[wname], 0, 8, yt_rhs), (w["win_fm"], FM_GATE + branch * 8, 8, self.xt_rhs)], 8, consumer,
                     [wbA, wbB], [[P[0], P[1]], [P[2], P[3]]])

    def ln_stats(self, SRC, tt, tmp, banks):
        SQ, MEAN, RSTD, TN = tmp
        B1, B2 = banks
        sl = slice(tt * 512, (tt + 1) * 512)
        for m in range(8):
            self.mm(B1[:, :], self.cF(K_ONE), SRC[:, m, sl], m == 0, m == 7,
                    reads=[self.CST.res, (SRC.res, tt)], writes=[B1.res])
        for m in range(8):
            sq = SQ[m % 2]
            self.actf(sq[:, :], SRC[:, m, sl], AF.Square, reads=[(SRC.res, tt)], writes=[sq.res])
            self.mm(B2[:, :], self.cF(K_ONE), sq[:, :], m == 0, m == 7, reads=[self.CST.res, sq.res],
                    writes=[B2.res])
        self.ts("dve", MEAN[:, :], B1[:, :], 1.0 / 1024, ALU.mult, reads=[B1.res], writes=[MEAN.res])
        self.tt("dve", RSTD[:, :], MEAN[:, :], MEAN[:, :], ALU.mult, reads=[MEAN.res], writes=[RSTD.res])
        self.stt(RSTD[:, :], B2[:, :], 1.0 / 1024, RSTD[:, :], ALU.mult, ALU.subtract, reads=[B2.res, RSTD.res],
                 writes=[RSTD.res])
        self.actf(RSTD[:, :], RSTD[:, :], AF.Sqrt, reads=[RSTD.res], writes=[RSTD.res], bias=self.cF(K_EPS, 1))
        self.S.dve(lambda e: e.reciprocal(RSTD[:, :], RSTD[:, :]), reads=[RSTD.res], writes=[RSTD.res])

    def ln_norm(self, SRC, tt, out_fn, tmp):
        SQ, MEAN, RSTD, TN = tmp
        sl = slice(tt * 512, (tt + 1) * 512)
        for m in range(8):
            tn = TN[m % 2]
            self.tt("dve", tn[:, :], SRC[:, m, sl], MEAN[:, :], ALU.subtract, reads=[(SRC.res, tt), MEAN.res],
                    writes=[tn.res])
            self.tt("dve", tn[:, :], tn[:, :], RSTD[:, :], ALU.mult, reads=[tn.res, RSTD.res], writes=[tn.res])
            out_fn(m, tn)

    def ln_all(self, SRC, make_out_fn):
        P = self.P
        tmps = [self.ln_tmp() for _ in range(self.NT)]
        banks = [(P[6], P[7]), (P[4], P[5])]
        for tt in range(self.NT):
            self.ln_stats(SRC, tt, tmps[tt], banks[tt % 2])
        for tt in range(self.NT):
            self.ln_norm(SRC, tt, make_out_fn(tt), tmps[tt])

    def ln_tmp(self):
        return ([self.aalloc([512], F32) for _ in range(2)], self.aalloc([512], F32), self.aalloc([512], F32),
                [self.aalloc([512], F32) for _ in range(2)])

    def program(self):
        S = self.S
        S.dma("sp", self.CST[:, :], self.d_cst, writes=[self.CST.res])
        self.cp("dve", self.CSB[:, :], self.CST[:, :], reads=[self.CST.res], writes=[self.CSB.res])
        for l in range(self.nlayer):
            S.dma("sp", self.COLS[l][:, :], self.dw[l]["cols"], writes=[self.COLS[l].res])
            S.dma("sp", self.ROWS[l][:, :], bass.AP(self.dw[l]["rows"].tensor, 0, [[0, 128], [1, NROWS]]),
                  writes=[self.ROWS[l].res])
            self.actf(self.ROWX[l][:, 0:16], self.ROWS[l][:, R_ALOG:R_ALOG + 16], AF.Exp, reads=[self.ROWS[l].res],
                      writes=[self.ROWX[l].res])
            self.ts("dve", self.ROWX[l][:, 0:16], self.ROWX[l][:, 0:16], -1.0, ALU.mult, reads=[self.ROWX[l].res],
                    writes=[self.ROWX[l].res])
            self.actf(self.ROWX[l][:, 16:32], self.ROWS[l][:, R_SINK:R_SINK + 16], AF.Exp, reads=[self.ROWS[l].res],
                      writes=[self.ROWX[l].res])
            self.ts("dve", self.COLA[l][:, 0:16], self.COLS[l][:, C_L1G:C_L1G + 16], float(ALPHA), ALU.mult,
                    reads=[self.COLS[l].res], writes=[self.COLA[l].res])
        for seq in range(self.nseq):
            for seg in range(self.nseg):
                self.segment(seq, seg)

    def segment(self, seq, seg):
        T = self.T
        t0 = seg * T
        first = seg == 0
        self.areset()
        self.load_x(seq, t0)
        if first:
            for l in range(self.nlayer):
                self.memset("pool", self.H[l][:, :], 0.0, [self.H[l].res])
                self.memset("pool", self.CTAIL[l][:, :, :], 0.0, [self.CTAIL[l].res])
                self.memset("pool", self.STAIL[l][:, :, :], 0.0, [self.STAIL[l].res])
        for l in range(self.nlayer):
            self.mt_first = True
            stages = [("ssd", lambda: self.ssd(l, first)),
                      ("conf", lambda: self.conformer(l)),
                      ("attn", lambda: self.attention(l, seq, t0, first)),
                      ("ln1", lambda: self.outproj_ln1(l)),
                      ("moe", lambda: self.moe(l)),
                      ("ple", lambda: self.ln2_ple(l, seq, t0))]
            for nm, fn in stages:
                if self.only is not None and nm not in self.only:
                    continue
                self.areset()
                fn()
                self.dbg("MT", self.MT)
                if self.stop_after == (l, nm):
                    break
            if self.stop_after is not None and self.stop_after[0] == l:
                break
        self.areset()
        self.store_x(seq, t0)

    def load_x(self, seq, t0):
        S, P = self.S, self.P
        STG = [self.aalloc([1024], F32) for _ in range(2)]
        for blk in range(self.NB):
            stg = STG[blk % 2]
            tt = blk // 4
            S.dma("sp", stg[:, :], self.d_x[seq, t0 + blk * 128:t0 + (blk + 1) * 128, :], writes=[stg.res])
            for half in range(2):
                ps = P[(blk * 2 + half) % 4].reshape([128, 4, 128])
                for j in range(4):
                    c = half * 4 + j
                    self.tr(ps[:, j, :], stg[:, c * 128:(c + 1) * 128], self.cF(K_ID), reads=[stg.res, self.CST.res],
                            writes=[ps.res])
                self.cp("act", self.X[:, half * 4:half * 4 + 4, blk * 128:(blk + 1) * 128], ps[:, :, :],
                        reads=[ps.res], writes=[(self.X.res, tt)])
                self.cp("dve", self.XT[:, half * 4:half * 4 + 4, blk * 128:(blk + 1) * 128], ps[:, :, :],
                        reads=[ps.res], writes=[(self.XT.res, tt)])

    def store_x(self, seq, t0):
        S, P = self.S, self.P
        STG = [self.aalloc([1024], F32) for _ in range(2)]
        for blk in range(self.NB):
            stg = STG[blk % 2]
            tt = blk // 4
            for half in range(2):
                ps = P[(blk * 2 + half) % 4].reshape([128, 4, 128])
                for j in range(4):
                    c = half * 4 + j
                    self.tr(ps[:, j, :], self.X[:, c, blk * 128:(blk + 1) * 128], self.cF(K_ID),
                            reads=[(self.X.res, tt), self.CST.res], writes=[ps.res])
                self.cp("act" if half == 0 else "dve", stg[:, half * 512:(half + 1) * 512],
                        ps[:, :, :], reads=[ps.res], writes=[stg.res])
            S.dma("sp", self.d_out[seq, t0 + blk * 128:t0 + (blk + 1) * 128, :], stg[:, :], reads=[stg.res])

    def conformer(self, l):
        T, NT, P, S = self.T, self.NT, self.P, self.S
        w = self.dw[l]
        HT = self.aalloc([8, T], BF16)
        mark0 = self.amark()
        HPAD = [self.aalloc([32 + T], BF16) for _ in range(2)]
        DG = [self.aalloc([31, 128], BF16) for _ in range(2)]
        CO = self.aalloc([8, T], F32)
        SG = [self.aalloc([512], F32) for _ in range(2)]
        wbA = [self.aalloc([8, 128], BF16) for _ in range(2)]
        wbG = [self.aalloc([8, 128], BF16) for _ in range(2)]

        def cload(m):
            S.dma("pool", wbA[m % 2][:, :, :], w["win_fm"][FM_CONF + m], writes=[wbA[m % 2].res])
            S.dma("pool", wbG[m % 2][:, :, :], w["win_fm"][FM_CONF + 8 + m], writes=[wbG[m % 2].res])

        def conf_a(m):
            hp, dg = HPAD[m % 2], DG[m % 2]
            self.cp("act", hp[:, 0:32], self.CTAIL[l][:, m, :], reads=[self.CTAIL[l].res], writes=[hp.res])
            self.tt("dve", dg[:, :, :], self.CSB.bc(K_ID, [[0, 31], [1, 128]]),
                    self.COLS[l].bc(C_CDW + m * 31, [[1, 31], [0, 128]]), ALU.mult,
                    reads=[self.CSB.res, self.COLS[l].res], writes=[dg.res])
            wa, wg = wbA[m % 2], wbG[m % 2]
            if m == 0:
                cload(0)
            if m + 1 < 8:
                cload(m + 1)
            for tt in range(NT):
                sl = slice(tt * 512, (tt + 1) * 512)
                a_ps, g_ps = P[tt % 2], P[2 + tt % 2]
                for k in range(8):
                    self.mm(a_ps[:, :], wa[:, k, :], self.XT[:, k, sl], k == 0, k == 7,
                            reads=[wa.res, (self.XT.res, tt)], writes=[a_ps.res])
                for k in range(8):
                    self.mm(g_ps[:, :], wg[:, k, :], self.XT[:, k, sl], k == 0, k == 7,
                            reads=[wg.res, (self.XT.res, tt)], writes=[g_ps.res])
                sg = SG[tt % 2]
                self.actf(sg[:, :], g_ps[:, :], AF.Sigmoid, reads=[g_ps.res], writes=[sg.res])
                self.tt("dve", hp[:, 32 + tt * 512:32 + (tt + 1) * 512], a_ps[:, :], sg[:, :], ALU.mult,
                        reads=[a_ps.res, sg.res], writes=[hp.res])

        def conf_b(m):
            hp, dg = HPAD[m % 2], DG[m % 2]
            for tt in range(NT):
                sl = slice(tt * 512, (tt + 1) * 512)
                c_ps = P[4 + tt % 2]
                for k in range(31):
                    self.mm(c_ps[:, :], dg[:, k, :], hp[:, 2 + k + tt * 512:2 + k + (tt + 1) * 512], k == 0, k == 30,
                            reads=[dg.res, hp.res], writes=[c_ps.res])
                self.actf(CO[:, m, sl], c_ps[:, :], AF.Identity, reads=[c_ps.res], writes=[(CO.res, tt)],
                          bias=self.col(l, C_CDB + m))
            self.cp("act", self.CTAIL[l][:, m, :], hp[:, T:T + 32], reads=[hp.res], writes=[self.CTAIL[l].res])

        conf_a(0)
        for m in range(8):
            if m + 1 < 8:
                conf_a(m + 1)
            conf_b(m)
        self.dbg("CO", CO)
        def mk_conf(tt):
            sl = slice(tt * 512, (tt + 1) * 512)

            def out_fn(m, tn):
                self.actf(HT[:, m, sl], tn[:, :], AF.Silu, reads=[tn.res], writes=[(HT.res, tt)],
                          scale=self.col(l, C_CLG + m), bias=self.col(l, C_CLB + m))
            return out_fn

        self.ln_all(CO, mk_conf)
        self.dbg("HT", HT)
        self.arelease(mark0)
        self.gated_out(l, 1, HT, "wconf", self.mt_first)
        self.mt_first = False

    def outproj_ln1(self, l):
        T, NT, P, S = self.T, self.NT, self.P, self.S
        w = self.dw[l]
        wb = [self.aalloc([8, 128], BF16) for _ in range(3)]
        self.dbg("MT", self.MT)

        def mt_rhs(k, tt):
            return self.MT[:, k, tt * 512:(tt + 1) * 512], (self.MT.res, tt)

        def consumer(i, tt, pss):
            sl = slice(tt * 512, (tt + 1) * 512)
            self.stt(self.X[:, i, sl], self.X[:, i, sl], float(ALPHA), pss[0][:, :], ALU.mult, ALU.add,
                     reads=[(self.X.res, tt), pss[0].res], writes=[(self.X.res, tt)])

        self.proj_fm([(w["wout"], 0, 8, mt_rhs)], 8, consumer, [wb], [[P[0], P[1]]])
        def mk_ln1(tt):
            sl = slice(tt * 512, (tt + 1) * 512)

            def out_fn(m, tn):
                self.actf(self.XT[:, m, sl], tn[:, :], AF.Identity, reads=[tn.res], writes=[(self.XT.res, tt)],
                          scale=self.col(l, C_L1G + m), bias=self.col(l, C_L1B + m))
                self.actf(self.X[:, m, sl], tn[:, :], AF.Identity, reads=[tn.res], writes=[(self.X.res, tt)],
                          scale=self.COLA[l][:, m:m + 1], bias=self.COLA[l][:, 8 + m:9 + m])
            return out_fn

        self.ln_all(self.X, mk_ln1)
        self.dbg("X1T", self.XT)

    def ln2_ple(self, l, seq, t0):
        T, NT, NB, P, S = self.T, self.NT, self.NB, self.P, self.S
        w = self.dw[l]
        self.dbg("XMOE", self.X)
        def mk_ln2(tt):
            sl = slice(tt * 512, (tt + 1) * 512)

            def out_fn(m, tn):
                self.actf(self.XT[:, m, sl], tn[:, :], AF.Identity, reads=[tn.res], writes=[(self.XT.res, tt)],
                          scale=self.col(l, C_L2G + m), bias=self.col(l, C_L2B + m))
                self.actf(self.X[:, m, sl], tn[:, :], AF.Identity, reads=[tn.res], writes=[(self.X.res, tt)],
                          scale=self.col(l, C_L2G + m), bias=self.col(l, C_L2B + m))
            return out_fn

        self.ln_all(self.X, mk_ln2)
        self.dbg("X2T", self.XT)
        PSTG = [self.aalloc([256], F32) for _ in range(2)]
        PT = self.aalloc([2, T], BF16)
        for blk in range(NB):
            stg = PSTG[blk % 2]
            S.dma("sp", stg[:, :], self.d_p[l, seq, t0 + blk * 128:t0 + (blk + 1) * 128, :], writes=[stg.res])
            ps = P[6 + blk % 2].reshape([128, 4, 128])
            for j in range(2):
                self.tr(ps[:, j, :], stg[:, j * 128:(j + 1) * 128], self.cF(K_ID), reads=[stg.res, self.CST.res],
                        writes=[ps.res])
            self.cp("act", PT[:, :, blk * 128:(blk + 1) * 128], ps[:, 0:2, :], reads=[ps.res],
                    writes=[(PT.res, blk // 4)])
        wbA = [self.aalloc([8, 128], BF16) for _ in range(3)]
        wbB = [self.aalloc([8, 128], BF16) for _ in range(3)]
        SG = [self.aalloc([512], F32) for _ in range(2)]
        TM = [self.aalloc([512], F32) for _ in range(2)]
        st = {"n": 0}

        def pt_rhs(k, tt):
            return PT[:, k, tt * 512:(tt + 1) * 512], (PT.res, tt)

        def consumer(i, tt, pss):
            n = st["n"]
            st["n"] += 1
            sl = slice(tt * 512, (tt + 1) * 512)
            sg, tm = SG[n % 2], TM[n % 2]
            self.actf(sg[:, :], pss[0][:, :], AF.Sigmoid, reads=[pss[0].res], writes=[sg.res])
            self.tt("dve", tm[:, :], pss[1][:, :], sg[:, :], ALU.mult, reads=[pss[1].res, sg.res], writes=[tm.res])
            self.tt("dve", self.X[:, i, sl], self.X[:, i, sl], tm[:, :], ALU.add, reads=[tm.res, (self.X.res, tt)],
                    writes=[(self.X.res, tt)])

        self.proj_fm([(w["wpg"], 0, 8, self.xt_rhs), (w["wpp"], 0, 2, pt_rhs)], 8, consumer, [wbA, wbB],
                     [[P[0], P[1]], [P[2], P[3]]])
        if l < self.nlayer - 1:
            for tt in range(NT):
                sl = slice(tt * 512, (tt + 1) * 512)
                for m in range(8):
                    self.cp("act", self.XT[:, m, sl], self.X[:, m, sl], reads=[(self.X.res, tt)],
                            writes=[(self.XT.res, tt)])

    def amark(self):
        return self.aoff

    def arelease(self, mark):
        self.S.fence()
        self.aoff = mark

    def ssd(self, l, first):
        T, NT, NB, P, PB, S = self.T, self.NT, self.NB, self.P, self.PB, self.S
        w = self.dw[l]
        ROWS, ROWX, COLS, CST, CSB = self.ROWS[l], self.ROWX[l], self.COLS[l], self.CST, self.CSB
        YST = self.aalloc([8, T], BF16)
        mark0 = self.amark()
        WZ = self.aalloc([8, 1024], BF16)
        WDT = self.aalloc([8, 16], BF16)
        BFM = self.aalloc([4, T], BF16)
        CFM = self.aalloc([4, T], BF16)
        XST = self.aalloc([NB, 1024], BF16)
        BTK = self.aalloc([NB, 512], BF16)
        DT, DTA, ECS, DTE, CDEC = [self.aalloc([NB, 16], F32) for _ in range(5)]
        mark1 = self.amark()
        XPAD = [self.aalloc([4 + T], BF16) for _ in range(2)]
        DG = [self.aalloc([4, 128], BF16) for _ in range(2)]
        XFM = [self.aalloc([T], BF16) for _ in range(2)]
        wbuf = [self.aalloc([8, 128], BF16) for _ in range(3)]
        S.dma("pool", WZ[:, :, :], w["win_tm"][:, 0:1024].rearrange("(k p) n -> p k n", p=128), writes=[WZ.res])
        S.dma("pool", WDT[:, :, :], w["win_tm"][:, 1024:1040].rearrange("(k p) n -> p k n", p=128), writes=[WDT.res])
        def ssd_a(m):
            xp, dg = XPAD[m % 2], DG[m % 2]
            self.cp("act", xp[:, 0:4], self.STAIL[l][:, m, :], reads=[self.STAIL[l].res], writes=[xp.res])
            self.tt("dve", dg[:, :, :], CSB.bc(K_ID, [[0, 4], [1, 128]]), COLS.bc(C_SCW + m * 4, [[1, 4], [0, 128]]),
                    ALU.mult, reads=[CSB.res, COLS.res], writes=[dg.res])
            wb = wbuf[m % 3]
            if m == 0:
                for mm_ in range(2):
                    S.dma("pool", wbuf[mm_ % 3][:, :, :], w["win_fm"][FM_XBC + mm_], writes=[wbuf[mm_ % 3].res])
            if m + 2 < 16:
                S.dma("pool", wbuf[(m + 2) % 3][:, :, :], w["win_fm"][FM_XBC + m + 2], writes=[wbuf[(m + 2) % 3].res])
            for tt in range(NT):
                sl = slice(tt * 512, (tt + 1) * 512)
                ps = P[tt % 2]
                for k in range(8):
                    self.mm(ps[:, :], wb[:, k, :], self.XT[:, k, sl], k == 0, k == 7,
                            reads=[wb.res, (self.XT.res, tt)], writes=[ps.res])
                self.cp("act", xp[:, 4 + tt * 512:4 + (tt + 1) * 512], ps[:, :], reads=[ps.res], writes=[xp.res])

        def ssd_b(m):
            xp, dg = XPAD[m % 2], DG[m % 2]
            if m < 8:
                dst = XFM[m % 2]
                dsl = lambda sl, dst=dst: dst[:, sl]
            elif m < 12:
                dst = BFM
                dsl = lambda sl, g=m - 8: BFM[:, g, sl]
            else:
                dst = CFM
                dsl = lambda sl, g=m - 12: CFM[:, g, sl]
            for tt in range(NT):
                sl = slice(tt * 512, (tt + 1) * 512)
                cps = P[2 + tt % 2]
                for k in range(4):
                    self.mm(cps[:, :], dg[:, k, :], xp[:, 1 + k + tt * 512:1 + k + (tt + 1) * 512], k == 0, k == 3,
                            reads=[dg.res, xp.res], writes=[cps.res])
                self.actf(dsl(sl), cps[:, :], AF.Silu, reads=[cps.res], writes=[dst.res],
                          bias=self.col(l, C_SCB + m))
            self.cp("act", self.STAIL[l][:, m, :], xp[:, T:T + 4], reads=[xp.res], writes=[self.STAIL[l].res])
            if m < 12:
                for b0 in range(0, NB, 8):
                    nb = min(8, NB - b0)
                    pb = PB[4 + (m + b0 // 8) % 2].reshape([128, 8, 128])
                    for j in range(nb):
                        blk = b0 + j
                        src = dst[:, blk * 128:(blk + 1) * 128] if m < 8 else BFM[:, m - 8, blk * 128:(blk + 1) * 128]
                        self.tr(pb[:, j, :], src, self.cB(K_ID), reads=[dst.res, CSB.res], writes=[pb.res])
                    if m < 8:
                        self.cp("dve", XST[:, b0:b0 + nb, m * 128:(m + 1) * 128], pb[:, 0:nb, :], reads=[pb.res],
                                writes=[XST.res])
                    else:
                        self.cp("dve", BTK[:, b0:b0 + nb, (m - 8) * 128:(m - 7) * 128], pb[:, 0:nb, :],
                                reads=[pb.res], writes=[BTK.res])

        ssd_a(0)
        for m in range(16):
            if m + 1 < 16:
                ssd_a(m + 1)
            ssd_b(m)
        PD = P[6].reshape([128, 32, 16])
        for blk in range(NB):
            for k in range(8):
                self.mm(PD[:, blk, :], self.XT[:, k, blk * 128:(blk + 1) * 128], WDT[:, k, :], k == 0, k == 7,
                        reads=[(self.XT.res, blk // 4), WDT.res], writes=[PD.res])
        self.tt("dve", DT[:, :, :], PD[:, 0:NB, :], ROWS.bc(R_DTB, [[0, NB], [1, 16]]), ALU.add,
                reads=[PD.res, ROWS.res], writes=[DT.res])
        self.actf(DT[:, :, :], DT[:, :, :], AF.Exp, reads=[DT.res], writes=[DT.res])
        self.actf(DT[:, :, :], DT[:, :, :], AF.Ln, reads=[DT.res], writes=[DT.res], bias=self.cF(K_ONEC, 1))
        self.tt("dve", DTA[:, :, :], DT[:, :, :], ROWX.bc(0, [[0, NB], [1, 16]]), ALU.mult,
                reads=[DT.res, ROWX.res], writes=[DTA.res])
        dta2 = DTA.reshape([128, NB * 16])
        for (cst, dstv, ps) in ((K_TU, ECS, P[7]), (K_TL, DTE, P[6]), (K_ONE, CDEC, P[7])):
            self.mm(ps[:, 0:NB * 16], self.cF(cst), dta2[:, :], True, True, reads=[CST.res, DTA.res], writes=[ps.res])
            self.actf(dstv.reshape([128, NB * 16])[:, :], ps[:, 0:NB * 16], AF.Exp, reads=[ps.res], writes=[dstv.res])
        self.arelease(mark1)
        RS = [self.aalloc([4, 128], F32) for _ in range(2)]
        DEC = [self.aalloc([4, 128], F32) for _ in range(2)]
        MTG = [self.aalloc([4, 128], BF16) for _ in range(2)]
        CBM = self.aalloc([4, 128], F32)
        XDT = [self.aalloc([16, 64], BF16) for _ in range(2)]
        XDD = [self.aalloc([16, 64], BF16) for _ in range(2)]
        XD = [self.aalloc([16, 64], BF16) for _ in range(2)]
        T1 = self.aalloc([1024], F32)
        SZ = self.aalloc([1024], F32)
        YN = self.aalloc([1024], BF16)
        HB = self.aalloc([1024], BF16)
        SS = self.aalloc([4], F32)
        H = self.H[l]
        self.cp("act", HB[:, :], H[:, :], reads=[H.res], writes=[HB.res])
        for c in range(NB):
            csl = slice(c * 128, (c + 1) * 128)
            xdt, xdd, xd = XDT[c % 2], XDD[c % 2], XD[c % 2]
            xs3 = XST.reshape([128, NB, 16, 64])
            self.tt("dve", xdt[:, :, :], xs3[:, c, :, :], DT.bc(c * 16, [[1, 16], [0, 64]]), ALU.mult,
                    reads=[XST.res, DT.res], writes=[xdt.res])
            self.tt("dve", xdd[:, :, :], xdt[:, :, :], DTE.bc(c * 16, [[1, 16], [0, 64]]), ALU.mult,
                    reads=[xdt.res, DTE.res], writes=[xdd.res])
            self.tt("dve", xd[:, :, :], xs3[:, c, :, :], ROWS.bc(R_D, [[1, 16], [0, 64]]), ALU.mult,
                    reads=[XST.res, ROWS.res], writes=[xd.res])
            p0 = P[0].reshape([128, 4, 128])
            for g in range(4):
                self.mm(p0[:, g, :], BFM[:, g, csl], CFM[:, g, csl], True, True, reads=[BFM.res, CFM.res],
                        writes=[p0.res])
            self.tt("dve", CBM[:, :, :], p0[:, :, :], CST.bc(K_TU, [[0, 4], [1, 128]]), ALU.mult,
                    reads=[p0.res, CST.res], writes=[CBM.res])
            xd2 = xd.reshape([128, 1024])
            xdt2 = xdt.reshape([128, 1024])
            for half in range(2):
                self.mm(P[3 + half][:, :], self.cB(K_ID), xd2[:, half * 512:(half + 1) * 512], True, False,
                        reads=[CSB.res, xd.res], writes=[P[3 + half].res])
            for g in range(4):
                rs, dec, mtg = RS[g % 2], DEC[g % 2], MTG[g % 2]
                self.tt("dve", rs[:, :, :], DTA.bc(c * 16 + 4 * g, [[1, 4], [0, 128]]),
                        CST.bc(K_TU, [[0, 4], [1, 128]]), ALU.mult, reads=[DTA.res, CST.res], writes=[rs.res])
                self.mm(P[1][:, :], self.cF(K_TL), rs.reshape([128, 512])[:, :], True, True,
                        reads=[CST.res, rs.res], writes=[P[1].res])
                self.actf(dec.reshape([128, 512])[:, :], P[1][:, :], AF.Exp, reads=[P[1].res], writes=[dec.res])
                self.tt("dve", mtg[:, :, :], dec[:, :, :], CBM.bc(g * 128, [[0, 4], [1, 128]]), ALU.mult,
                        reads=[dec.res, CBM.res], writes=[mtg.res])
                for r in range(4):
                    h = 4 * g + r
                    half = h // 8
                    self.mm(P[3 + half][:, (h % 8) * 64:(h % 8 + 1) * 64], mtg[:, r, :],
                            xdt2[:, h * 64:(h + 1) * 64], False, h % 8 == 7,
                            reads=[mtg.res, xdt.res], writes=[P[3 + half].res])
            for g in range(4):
                ps = P[5 + g // 2]
                self.mm(ps[:, (g % 2) * 256:(g % 2 + 1) * 256], CFM[:, g, csl], HB[:, g * 256:(g + 1) * 256],
                        True, True, reads=[CFM.res, HB.res], writes=[ps.res])
            t13 = T1.reshape([128, 16, 64])
            for half in range(2):
                self.tt("dve", t13[:, half * 8:(half + 1) * 8, :], P[5 + half].reshape([128, 8, 64])[:, :, :],
                        ECS.bc(c * 16 + half * 8, [[1, 8], [0, 64]]), ALU.mult,
                        reads=[P[5 + half].res, ECS.res], writes=[T1.res])
            for half in range(2):
                hs = slice(half * 512, (half + 1) * 512)
                self.tt("dve", T1[:, hs], P[3 + half][:, :], T1[:, hs], ALU.add, reads=[P[3 + half].res, T1.res],
                        writes=[T1.res])
            for half in range(2):
                hs = slice(half * 512, (half + 1) * 512)
                pz = P[7] if half == 0 else P[0]
                for k in range(8):
                    self.mm(pz[:, :], self.XT[:, k, csl], WZ[:, k, hs], k == 0, k == 7,
                            reads=[(self.XT.res, c // 4), WZ.res], writes=[pz.res])
                self.actf(SZ[:, hs], pz[:, :], AF.Silu, reads=[pz.res], writes=[SZ.res])
            self.tt("dve", T1[:, :], T1[:, :], SZ[:, :], ALU.mult, reads=[T1.res, SZ.res], writes=[T1.res])
            self.tt("dve", SZ[:, :], T1[:, :], T1[:, :], ALU.mult, reads=[T1.res], writes=[SZ.res])
            self.S.dve(lambda e: e.tensor_reduce(out=SS[:, :], in_=SZ.reshape([128, 4, 256])[:, :, :], op=ALU.add,
                                                 axis=AX.X), reads=[SZ.res], writes=[SS.res])
            self.ts("dve", SS[:, :], SS[:, :], 1.0 / 256, ALU.mult, reads=[SS.res], writes=[SS.res], s2=float(EPS),
                    op1=ALU.add)
            self.actf(SS[:, :], SS[:, :], AF.Sqrt, reads=[SS.res], writes=[SS.res])
            self.S.dve(lambda e: e.reciprocal(SS[:, :], SS[:, :]), reads=[SS.res], writes=[SS.res])
            self.tt("dve", YN.reshape([128, 4, 256])[:, :, :], T1.reshape([128, 4, 256])[:, :, :],
                    SS.bc(0, [[1, 4], [0, 256]]), ALU.mult, reads=[T1.res, SS.res], writes=[YN.res])
            pb = PB[2].reshape([128, 8, 128])
            for m in range(8):
                self.tr(pb[:, m, :], YN[:, m * 128:(m + 1) * 128], self.cB(K_ID), reads=[YN.res, CSB.res],
                        writes=[pb.res])
            self.tt("dve", YST[:, :, csl], pb[:, :, :], COLS.bc(C_NW, [[1, 8], [0, 128]]), ALU.mult,
                    reads=[pb.res, COLS.res], writes=[(YST.res, c // 4)])
            xdd2 = xdd.reshape([128, 1024])
            for g in range(4):
                ps = P[5 + g // 2]
                self.mm(ps[:, (g % 2) * 256:(g % 2 + 1) * 256], BTK[:, c, g * 128:(g + 1) * 128],
                        xdd2[:, g * 256:(g + 1) * 256], True, True, reads=[BTK.res, xdd.res], writes=[ps.res])
            self.tt("dve", H.reshape([128, 16, 64])[:, :, :], H.reshape([128, 16, 64])[:, :, :],
                    CDEC.bc(c * 16, [[1, 16], [0, 64]]), ALU.mult, reads=[H.res, CDEC.res], writes=[H.res])
            for half in range(2):
                hs = slice(half * 512, (half + 1) * 512)
                self.tt("dve", H[:, hs], P[5 + half][:, :], H[:, hs], ALU.add, reads=[P[5 + half].res, H.res],
                        writes=[H.res])
            self.cp("act", HB[:, :], H[:, :], reads=[H.res], writes=[HB.res])
        self.dbg("YST", YST)
        self.arelease(mark0)
        self.gated_out(l, 0, YST, "wssd", self.mt_first)
        self.mt_first = False

    def attention(self, l, seq, t0, first):
        T, NT, NB, P, PB, S = self.T, self.NT, self.NB, self.P, self.PB, self.S
        w = self.dw[l]
        ROWX, CST, CSB = self.ROWX[l], self.CST, self.CSB
        AT = self.aalloc([8, T], BF16)
        mark0 = self.amark()
        COS = self.aalloc([T], F32)
        SIN = self.aalloc([T], F32)
        QR = self.aalloc([8, T], BF16)
        KD = self.aalloc([4, 128 + T], BF16)
        VA = self.aalloc([1 + NB, 4, 65], BF16)
        AO = self.aalloc([NB, 1024], BF16)
        mark1 = self.amark()
        POSI = self.aalloc([T], I32)
        ANG = self.aalloc([T], F32)
        A2 = self.aalloc([T], F32)
        KI = self.aalloc([T], I32)
        KF = self.aalloc([T], F32)
        S.dma("sp", POSI[:, :], bass.AP(self.d_pos.tensor, seq * SEQ + t0, [[0, 128], [1, T]]), writes=[POSI.res])
        self.cp("dve", ANG[:, :], POSI[:, :], reads=[POSI.res], writes=[ANG.res])
        self.ts("dve", ANG[:, :], ANG[:, :], self.cF(K_INVF, 1), ALU.mult, reads=[ANG.res, CST.res], writes=[ANG.res])
        TWO_PI = 2.0 * math.pi
        C1 = 6.28125
        C2 = TWO_PI - C1
        PI_SAFE = 3.1415925
        for dst, shift in ((SIN, 0.0), (COS, 0.5 * math.pi)):
            self.ts("dve", A2[:, :], ANG[:, :], float(shift), ALU.add, reads=[ANG.res], writes=[A2.res])
            self.ts("dve", KI[:, :], A2[:, :], 1.0 / TWO_PI, ALU.mult, reads=[A2.res], writes=[KI.res])
            self.cp("dve", KF[:, :], KI[:, :], reads=[KI.res], writes=[KF.res])
            self.stt(A2[:, :], KF[:, :], -C1, A2[:, :], ALU.mult, ALU.add, reads=[KF.res, A2.res], writes=[A2.res])
            self.stt(A2[:, :], KF[:, :], -C2, A2[:, :], ALU.mult, ALU.add, reads=[KF.res, A2.res], writes=[A2.res])
            self.ts("dve", A2[:, :], A2[:, :], -PI_SAFE, ALU.max, reads=[A2.res], writes=[A2.res], s2=PI_SAFE,
                    op1=ALU.min)
            self.actf(dst[:, :], A2[:, :], AF.Sin, reads=[A2.res], writes=[dst.res])
        self.arelease(mark1)
        Q32 = [self.aalloc([512], F32) for _ in range(2)]
        TA = [self.aalloc([512], F32) for _ in range(2)]
        TB = [self.aalloc([512], F32) for _ in range(2)]
        KR = [self.aalloc([512], BF16) for _ in range(2)]
        wbuf = [self.aalloc([8, 128], BF16) for _ in range(3)]
        WV = self.aalloc([8, 256], BF16)
        EC = [self.aalloc([512], BF16) for _ in range(2)]
        EP = [self.aalloc([512], BF16) for _ in range(2)]
        DEN = [self.aalloc([4], F32) for _ in range(2)]
        S.dma("pool", WV[:, :, :], w["win_tm"][:, 1040:1296].rearrange("(k p) n -> p k n", p=128), writes=[WV.res])
        st = {"n": 0}

        def rope(i, tt, pss, is_k):
            n = st["n"]
            st["n"] += 1
            sl = slice(tt * 512, (tt + 1) * 512)
            q32, ta, tb, pr = Q32[n % 2], TA[n % 2], TB[n % 2], P[2 + n % 2]
            self.cp("act", q32[:, :], pss[0][:, :], reads=[pss[0].res], writes=[q32.res])
            self.mm(pr[:, :], self.cF(K_ROT), q32[:, :], True, True, reads=[CST.res, q32.res], writes=[pr.res])
            self.tt("dve", ta[:, :], q32[:, :], COS[:, sl], ALU.mult, reads=[q32.res, COS.res], writes=[ta.res])
            self.tt("dve", tb[:, :], pr[:, :], SIN[:, sl], ALU.mult, reads=[pr.res, SIN.res], writes=[tb.res])
            if not is_k:
                self.tt("dve", QR[:, i, sl], ta[:, :], tb[:, :], ALU.add, reads=[ta.res, tb.res],
                        writes=[(QR.res, tt)])
            else:
                kr = KR[n % 2]
                self.tt("dve", kr[:, :], ta[:, :], tb[:, :], ALU.add, reads=[ta.res, tb.res], writes=[kr.res])
                for half in range(2):
                    h = 2 * i + half
                    pd = P[4 + half]
                    self.mm(pd[:, :], self.cB(K_DUP0 + half * 128), kr[:, :], True, True, reads=[CSB.res, kr.res],
                            writes=[pd.res])
                    self.cp("act" if half == 0 else "dve", KD[:, h, 128 + tt * 512:128 + (tt + 1) * 512], pd[:, :],
                            reads=[pd.res], writes=[KD.res])

        self.proj_fm([(w["win_fm"], FM_Q, 8, self.xt_rhs)], 8, lambda i, tt, pss: rope(i, tt, pss, False), [wbuf],
                     [[P[0], P[1]]])
        self.proj_fm([(w["win_fm"], FM_K, 8, self.xt_rhs)], 2, lambda i, tt, pss: rope(i, tt, pss, True), [wbuf],
                     [[P[0], P[1]]])
        self.memset("pool", VA[:, :, :, 64:65], 1.0, [VA.res])
        if not first:
            self.cp("act", KD[:, :, 0:128], self.KCAR[l][:, :, :], reads=[self.KCAR[l].res], writes=[KD.res])
            self.cp("act", VA[:, 0, :, :], self.VCAR[l][:, :, :], reads=[self.VCAR[l].res], writes=[VA.res])
        for blk in range(NB):
            pv = P[6 + blk % 2]
            for k in range(8):
                self.mm(pv[:, 0:256], self.XT[:, k, blk * 128:(blk + 1) * 128], WV[:, k, :], k == 0, k == 7,
                        reads=[(self.XT.res, blk // 4), WV.res], writes=[pv.res])
            self.cp("act", VA[:, 1 + blk, :, 0:64], pv.reshape([128, 8, 64])[:, 0:4, :], reads=[pv.res],
                    writes=[VA.res])
        self.dbg("QR", QR)
        self.dbg("KD", KD)
        items = [(qb, h) for qb in range(NB) for h in range(4)]

        def att_a(n, qb, h):
            gfirst = first and qb == 0
            ec, ep = EC[n % 2], EP[n % 2]
            qsl = slice(qb * 128, (qb + 1) * 128)
            ec3, ep3 = ec.reshape([128, 4, 128]), ep.reshape([128, 4, 128])
            for r in range(4):
                hq = 4 * h + r
                ch, hf = hq // 2, hq % 2
                ps_ = slice(hf * 64, hf * 64 + 64)
                sc, sp = P[hf], P[2 + hf]
                cs_ = slice((r // 2) * 128, (r // 2 + 1) * 128)
                self.mm(sc[:, cs_], KD[ps_, h, 128 + qb * 128:128 + (qb + 1) * 128],
                        QR[ps_, ch, qsl], True, True, reads=[KD.res, (QR.res, qb // 4)], writes=[sc.res])
                if not gfirst:
                    self.mm(sp[:, cs_], KD[ps_, h, qb * 128:(qb + 1) * 128],
                            QR[ps_, ch, qsl], True, True, reads=[KD.res, (QR.res, qb // 4)], writes=[sp.res])
            for hf in range(2):
                self.actf(ec3[:, hf::2, :], P[hf].reshape([128, 4, 128])[:, 0:2, :], AF.Exp, reads=[P[hf].res],
                          writes=[ec.res], scale=0.125)
            self.tt("dve", ec3[:, :, :], ec3[:, :, :],
                    CSB.bc(K_TU, [[0, 4], [1, 128]]), ALU.mult, reads=[ec.res, CSB.res], writes=[ec.res])
            if not gfirst:
                for hf in range(2):
                    self.actf(ep3[:, hf::2, :], P[2 + hf].reshape([128, 4, 128])[:, 0:2, :], AF.Exp,
                              reads=[P[2 + hf].res], writes=[ep.res], scale=0.125)
                self.tt("dve", ep3[:, :, :], ep3[:, :, :],
                        CSB.bc(K_TL, [[0, 4], [1, 128]]), ALU.mult, reads=[ep.res, CSB.res], writes=[ep.res])

        def att_b(n, qb, h):
            gfirst = first and qb == 0
            po = P[4 + n % 2]
            ec, ep, den = EC[n % 2], EP[n % 2], DEN[n % 2]
            po3 = po.reshape([128, 4, 128])
            for r in range(4):
                if not gfirst:
                    self.mm(po3[:, r, 0:65], ep[:, r * 128:(r + 1) * 128], VA[:, qb, h, :], True, False,
                            reads=[ep.res, VA.res], writes=[po.res])
                self.mm(po3[:, r, 0:65], ec[:, r * 128:(r + 1) * 128], VA[:, qb + 1, h, :], gfirst, True,
                        reads=[ec.res, VA.res], writes=[po.res])
            self.tt("dve", den[:, :], po3[:, :, 64], ROWX.bc(16 + 4 * h, [[1, 4]]), ALU.add,
                    reads=[po.res, ROWX.res], writes=[den.res])
            self.S.dve(lambda e, den=den: e.reciprocal(den[:, :], den[:, :]), reads=[den.res], writes=[den.res])
            self.tt("dve", AO.reshape([128, NB, 16, 64])[:, qb, 4 * h:4 * h + 4, :], po3[:, :, 0:64],
                    den.bc(0, [[1, 4], [0, 64]]), ALU.mult, reads=[po.res, den.res], writes=[(AO.res, qb)])

        att_a(0, *items[0])
        for n, (qb, h) in enumerate(items):
            if n + 1 < len(items):
                att_a(n + 1, *items[n + 1])
            att_b(n, qb, h)
        for blk in range(NB):
            pb = PB[6 + blk % 2].reshape([128, 8, 128])
            for m in range(8):
                self.tr(pb[:, m, :], AO[:, blk, m * 128:(m + 1) * 128], self.cB(K_ID), reads=[(AO.res, blk), CSB.res],
                        writes=[pb.res])
            self.cp("act" if blk % 2 else "dve", AT[:, :, blk * 128:(blk + 1) * 128], pb[:, :, :], reads=[pb.res],
                    writes=[(AT.res, blk // 4)])
        self.cp("act", self.KCAR[l][:, :, :], KD[:, :, T:T + 128], reads=[KD.res], writes=[self.KCAR[l].res])
        self.cp("act", self.VCAR[l][:, :, :], VA[:, NB, :, :], reads=[VA.res], writes=[self.VCAR[l].res])
        self.dbg("AT", AT)
        self.arelease(mark0)
        self.gated_out(l, 2, AT, "wattn", self.mt_first)
        self.mt_first = False

    def moe(self, l):
        T, NT, NB, P, S = self.T, self.NT, self.NB, self.P, self.S
        w = self.dw[l]
        ROWS, CST = self.ROWS[l], self.CST
        WR = self.aalloc([8, 36], BF16)
        LOG = self.aalloc([NB, 36], F32)
        WTOK = self.aalloc([NB, 4, 8], F32)
        WT = self.aalloc([T], BF16)
        S.dma("pool", WR[:, :, :], w["wr"].rearrange("(k p) n -> p k n", p=128), writes=[WR.res])
        for blk in range(NB):
            pl = P[6 + blk % 2]
            for k in range(8):
                self.mm(pl[:, 0:36], self.XT[:, k, blk * 128:(blk + 1) * 128], WR[:, k, :], k == 0, k == 7,
                        reads=[(self.XT.res, blk // 4), WR.res], writes=[pl.res])
            self.tt("dve", LOG[:, blk, :], pl[:, 0:36], ROWS.bc(R_RB, [[1, 36]]), ALU.add, reads=[pl.res, ROWS.res],
                    writes=[LOG.res])
        mk = self.amark()
        f = lambda shp: self.aalloc(shp, F32)
        GMAX, GS, GE, GMASK = f([NB]), f([NB]), f([NB, 4]), f([NB, 4])
        V1, V2, P1, P2 = f([NB, 4]), f([NB, 4]), f([NB, 4]), f([NB, 4])
        M1, M2, E2 = f([NB, 4, 8]), f([NB, 4, 8]), f([NB, 4, 8])
        GL = lambda: LOG[:, :, 0:4]
        EL = lambda: LOG.bc(4, [[36, NB], [8, 4], [1, 8]])
        red = lambda out, in_, op, rd, wr: self.S.dve(
            lambda e: e.tensor_reduce(out=out, in_=in_, op=op, axis=AX.X), reads=rd, writes=wr)
        red(GMAX[:, :], GL(), ALU.max, [LOG.res], [GMAX.res])
        self.tt("dve", GE[:, :, :], GL(), GMAX.bc(0, [[1, NB], [0, 4]]), ALU.subtract, reads=[LOG.res, GMAX.res],
                writes=[GE.res])
        self.tt("dve", GMASK[:, :, :], GL(), GMAX.bc(0, [[1, NB], [0, 4]]), ALU.is_equal, reads=[LOG.res, GMAX.res],
                writes=[GMASK.res])
        self.actf(GE[:, :, :], GE[:, :, :], AF.Exp, reads=[GE.res], writes=[GE.res])
        red(GS[:, :], GE[:, :, :], ALU.add, [GE.res], [GS.res])
        self.S.dve(lambda e: e.reciprocal(GS[:, :], GS[:, :]), reads=[GS.res], writes=[GS.res])
        self.tt("dve", GMASK[:, :, :], GMASK[:, :, :], GS.bc(0, [[1, NB], [0, 4]]), ALU.mult,
                reads=[GMASK.res, GS.res], writes=[GMASK.res])
        red(V1[:, :, :], EL(), ALU.max, [LOG.res], [V1.res])
        self.tt("dve", M1[:, :, :, :], EL(), V1.bc(0, [[4, NB], [1, 4], [0, 8]]), ALU.is_equal,
                reads=[LOG.res, V1.res], writes=[M1.res])
        self.stt(E2[:, :, :, :], M1[:, :, :, :], -1.0e30, EL(), ALU.mult, ALU.add, reads=[M1.res, LOG.res],
                 writes=[E2.res])
        red(V2[:, :, :], E2[:, :, :, :], ALU.max, [E2.res], [V2.res])
        self.tt("dve", M2[:, :, :, :], E2[:, :, :, :], V2.bc(0, [[4, NB], [1, 4], [0, 8]]), ALU.is_equal,
                reads=[E2.res, V2.res], writes=[M2.res])
        self.tt("dve", P1[:, :, :], V1[:, :, :], V2[:, :, :], ALU.subtract, reads=[V1.res, V2.res], writes=[P1.res])
        self.actf(P1[:, :, :], P1[:, :, :], AF.Sigmoid, reads=[P1.res], writes=[P1.res])
        self.ts("dve", P2[:, :, :], P1[:, :, :], -1.0, ALU.mult, reads=[P1.res], writes=[P2.res], s2=1.0, op1=ALU.add)
        self.tt("dve", P1[:, :, :], P1[:, :, :], GMASK[:, :, :], ALU.mult, reads=[P1.res, GMASK.res], writes=[P1.res])
        self.tt("dve", P2[:, :, :], P2[:, :, :], GMASK[:, :, :], ALU.mult, reads=[P2.res, GMASK.res], writes=[P2.res])
        self.tt("dve", M1[:, :, :, :], M1[:, :, :, :], P1.bc(0, [[4, NB], [1, 4], [0, 8]]), ALU.mult,
                reads=[M1.res, P1.res], writes=[M1.res])
        self.tt("dve", M2[:, :, :, :], M2[:, :, :, :], P2.bc(0, [[4, NB], [1, 4], [0, 8]]), ALU.mult,
                reads=[M2.res, P2.res], writes=[M2.res])
        self.tt("dve", WTOK[:, :, :, :], M1[:, :, :, :], M2[:, :, :, :], ALU.add, reads=[M1.res, M2.res],
                writes=[WTOK.res])
        self.dbg("WTOK", WTOK)
        wt2 = WTOK.reshape([128, NB, 32])
        for b0 in range(0, NB, 4):
            ps = P[7]
            for j in range(4):
                self.tr(ps[0:32, j * 128:(j + 1) * 128], wt2[:, b0 + j, :], self.cF(K_ID), reads=[WTOK.res, CST.res],
                        writes=[ps.res])
            self.cp("act", WT[0:32, b0 * 128:(b0 + 4) * 128], ps[0:32, :], reads=[ps.res], writes=[WT.res])
        self.arelease(mk)
        NWB = 3
        WG = [self.aalloc([4, 8, 128], BF16) for _ in range(NWB)]
        WU = [self.aalloc([4, 8, 128], BF16) for _ in range(NWB)]
        WD = [self.aalloc([4, 1024], BF16) for _ in range(NWB)]
        HT = [self.aalloc([4, 512], BF16) for _ in range(2)]
        SIL = [self.aalloc([512], F32) for _ in range(2)]
        WBC = [self.aalloc([512], F32) for _ in range(2)]
        SEL = [self.aalloc([128], BF16) for _ in range(2)]

        def load_expert(e):
            wg, wu, wd = WG[e % NWB], WU[e % NWB], WD[e % NWB]
            S.dma("pool", wg.bc(0, [[1024, 4], [1, 1024]]), w["wg"][e].rearrange("p m k c -> p m (k c)"),
                  writes=[wg.res])
            S.dma("pool", wu.bc(0, [[1024, 4], [1, 1024]]), w["wu"][e].rearrange("p m k c -> p m (k c)"),
                  writes=[wu.res])
            S.dma("pool", wd[:, :, :], w["wd"][e].rearrange("(k p) n -> p k n", p=128), writes=[wd.res])

        PY = [P[4], P[5], P[6]]
        items = [(e, tt) for e in range(self.nexp) for tt in range(NT)]
        st = {"dc": 0}

        def gate_up(n, e, tt):
            wg, wu = WG[e % NWB], WU[e % NWB]
            sl = slice(tt * 512, (tt + 1) * 512)
            ht, wbc = HT[n % 2], WBC[n % 2]
            sel = SEL[e % 2]
            if tt == 0:
                self.cp("act", sel[0:32, :], self.CSB.bc(K_ID + e, [[0, 128]], npart=32), reads=[self.CSB.res],
                        writes=[sel.res])
            self.mm(P[7][:, :], sel[0:32, :], WT[0:32, sl], True, True, reads=[sel.res, WT.res],
                    writes=[P[7].res])
            self.cp("act", wbc[:, :], P[7][:, :], reads=[P[7].res], writes=[wbc.res])
            for m in range(4):
                pg, pu = P[m % 2], P[2 + m % 2]
                sil = SIL[m % 2]
                for k in range(8):
                    self.mm(pg[:, :], wg[:, m, k, :], self.XT[:, k, sl], k == 0, k == 7,
                            reads=[wg.res, (self.XT.res, tt)], writes=[pg.res])
                for k in range(8):
                    self.mm(pu[:, :], wu[:, m, k, :], self.XT[:, k, sl], k == 0, k == 7,
                            reads=[wu.res, (self.XT.res, tt)], writes=[pu.res])
                self.actf(sil[:, :], pg[:, :], AF.Silu, reads=[pg.res], writes=[sil.res])
                self.tt("dve", sil[:, :], pu[:, :], sil[:, :], ALU.mult, reads=[pu.res, sil.res], writes=[sil.res])
                self.tt("dve", ht[:, m, :], sil[:, :], wbc[:, :], ALU.mult, reads=[sil.res, wbc.res],
                        writes=[ht.res])

        def down(n, e, tt):
            wd = WD[e % NWB]
            sl = slice(tt * 512, (tt + 1) * 512)
            ht = HT[n % 2]
            for dc in range(8):
                py = PY[st["dc"] % 3]
                st["dc"] += 1
                for m in range(4):
                    self.mm(py[:, :], wd[:, m, dc * 128:(dc + 1) * 128], ht[:, m, :], m == 0, m == 3,
                            reads=[wd.res, ht.res], writes=[py.res])
                self.tt("dve", self.X[:, dc, sl], py[:, :], self.X[:, dc, sl], ALU.add,
                        reads=[py.res, (self.X.res, tt)], writes=[(self.X.res, tt)])

        for e in range(min(NWB - 1, self.nexp)):
            load_expert(e)
        for n, (e, tt) in enumerate(items):
            gate_up(n, e, tt)
            if n >= 1:
                pe_, ptt = items[n - 1]
                down(n - 1, pe_, ptt)
                if ptt == NT - 1 and pe_ + NWB < self.nexp + 0 and pe_ + NWB - 1 < self.nexp:
                    pass
            if tt == 0 and e + NWB - 1 < self.nexp and e >= 1:
                pass
            if n >= 1 and items[n - 1][1] == NT - 1:
                nxt = items[n - 1][0] + NWB
                if nxt < self.nexp:
                    load_expert(nxt)
            if n == 0 and NWB - 1 < self.nexp:
                load_expert(NWB - 1)
        down(len(items) - 1, *items[-1])


def host_consts():
    c = np.zeros((128, NCF), np.float32)
    k = np.arange(128)[:, None]
    m = np.arange(128)[None, :]
    c[:, K_ID:K_ID + 128] = (k == m)
    c[:, K_TU:K_TU + 128] = (k <= m)
    c[:, K_TL:K_TL + 128] = (k > m)
    c[:, K_ONE:K_ONE + 128] = 1.0
    R = np.zeros((128, 128), np.float32)
    D0 = np.zeros((128, 128), np.float32)
    D1 = np.zeros((128, 128), np.float32)
    for mm_ in range(128):
        b, j = mm_ // 64 * 64, mm_ % 64
        if j < 32:
            R[b + j + 32, mm_] = -1.0
        else:
            R[b + j - 32, mm_] = 1.0
        D0[j, mm_] = 1.0
        D1[64 + j, mm_] = 1.0
    c[:, K_ROT:K_ROT + 128] = R
    c[:, K_DUP0:K_DUP0 + 128] = D0
    c[:, K_DUP1:K_DUP1 + 128] = D1
    invf = (np.float32(10000.0) ** (-np.arange(32, dtype=np.float32) / np.float32(32))).astype(np.float32)
    c[:, K_INVF] = invf[np.arange(128) % 32]
    c[:, K_EPS] = EPS
    c[:, K_ONEC] = 1.0
    return c


def fm_layout(w):
    K, N = w.shape
    return np.ascontiguousarray(w.reshape(K // 128, 128, N // 128, 128).transpose(2, 1, 0, 3))


def colsT(v, n):
    return np.asarray(v, np.float32).reshape(n, 128).T


def prep_weights(inp, l):
    f = lambda n: np.asarray(inp[n][l], np.float32)
    w_in = f("w_in")
    o = {}
    o["win_fm%d" % l] = fm_layout(np.concatenate(
        [w_in[:, 0:3072], w_in[:, 4096:6144], w_in[:, 6160:8208], w_in[:, 8208:9232], w_in[:, 9232:9488]], axis=1))
    o["win_tm%d" % l] = np.ascontiguousarray(np.concatenate(
        [w_in[:, 3072:4096], w_in[:, 6144:6160], w_in[:, 9488:9744]], axis=1))
    for nm, src in (("wssd", "ssd_w_out"), ("wconf", "conf_w_out"), ("wattn", "attn_w_out"), ("wout", "w_out"),
                    ("wpg", "ple_w_gate"), ("wpp", "ple_w_proj")):
        o["%s%d" % (nm, l)] = fm_layout(f(src))
    o["wr%d" % l] = np.ascontiguousarray(np.concatenate([f("moe_w_group"), f("moe_w_expert")], axis=1))
    wg, wu = f("moe_w_gate"), f("moe_w_up")
    o["wg%d" % l] = np.ascontiguousarray(wg.reshape(NEXP, 8, 128, 4, 128).transpose(0, 2, 3, 1, 4))
    o["wu%d" % l] = np.ascontiguousarray(wu.reshape(NEXP, 8, 128, 4, 128).transpose(0, 2, 3, 1, 4))
    o["wd%d" % l] = np.ascontiguousarray(f("moe_w_down"))
    cols = np.zeros((128, NCOLS), np.float32)
    cols[:, C_BGATE:C_BGATE + 24] = colsT(f("b_gate"), 24)
    cols[:, C_SCW:C_SCW + 64] = f("ssd_conv_w").reshape(4, 16, 128).transpose(2, 1, 0).reshape(128, 64)
    cols[:, C_SCB:C_SCB + 16] = colsT(f("ssd_conv_b"), 16)
    cols[:, C_CDW:C_CDW + 248] = f("conf_dw_w").reshape(31, 8, 128).transpose(2, 1, 0).reshape(128, 248)
    for c0, nm in ((C_CDB, "conf_dw_b"), (C_CLG, "conf_ln_g"), (C_CLB, "conf_ln_b"), (C_NW, "ssd_norm_w"),
                   (C_L1G, "ln1_g"), (C_L1B, "ln1_b"), (C_L2G, "ln2_g"), (C_L2B, "ln2_b")):
        cols[:, c0:c0 + 8] = colsT(f(nm), 8)
    o["cols%d" % l] = cols
    rows = np.zeros((1, NROWS), np.float32)
    rows[0, R_DTB:R_DTB + 16] = f("ssd_dt_bias")
    rows[0, R_ALOG:R_ALOG + 16] = f("ssd_a_log")
    rows[0, R_D:R_D + 16] = f("ssd_d")
    rows[0, R_SINK:R_SINK + 16] = f("attn_sinks")
    rows[0, R_RB:R_RB + 4] = f("moe_b_group")
    rows[0, R_RB + 4:R_RB + 36] = f("moe_b_expert")
    o["rows%d" % l] = rows
    return o


def make_in_maps(inp, seq_lists, nlayer=DEPTH):
    shared = {"cst": host_consts()}
    for l in range(nlayer):
        shared.update(prep_weights(inp, l))
    maps = []
    for seqs in seq_lists:
        m = dict(shared)
        m["x"] = np.ascontiguousarray(np.asarray(inp["x"], np.float32)[seqs])
        m["p"] = np.ascontiguousarray(np.asarray(inp["p"], np.float32)[:, seqs])
        m["pos"] = np.ascontiguousarray(np.asarray(inp["positions"], np.int32)[seqs])
        maps.append(m)
    return maps


_NC_CACHE = {}


def kernel(**inputs):
    if "nc" not in _NC_CACHE:
        _NC_CACHE["nc"] = Builder().nc
    nc = _NC_CACHE["nc"]
    seq_lists = [list(range(c * SEQ_PER_CORE, (c + 1) * SEQ_PER_CORE)) for c in range(NCORES)]
    maps = make_in_maps(inputs, seq_lists)
    res = run_bass_kernel_spmd(nc, maps, core_ids=list(range(NCORES)))
    out = np.concatenate([np.asarray(r["out"], np.float32) for r in res.results], axis=0)
    return out
```
